# Optimizing a Trainium2 kernel written in Bass

```python
import jax
import jax.numpy as jnp
from jax import lax

D_MODEL = 1024
BATCH = 4
SEQ = 8192
DEPTH = 2

GRID_W = 64
CTX_LEN = 256
HEAD_DIM = 64
A_Q_HEADS = 8
A_KV_HEADS = 2
A_GROUP = A_Q_HEADS // A_KV_HEADS
A_WINDOW = 128
A_BLOCK = 128
B_HEADS = 4
NA_ROWS = 8
NA_COLS = 16
C_CHANNELS = 256
C_CONV_WIDTH = 31
A_Q_W = A_Q_HEADS * HEAD_DIM
A_KV_W = A_KV_HEADS * HEAD_DIM
B_W = B_HEADS * HEAD_DIM
MIX_WIDTH = A_Q_W + B_W + C_CHANNELS
IN_WIDTH = A_Q_W + 2 * A_KV_W + 3 * B_W + 2 * C_CHANNELS
OFF_AQ = 0
OFF_AK = OFF_AQ + A_Q_W
OFF_AV = OFF_AK + A_KV_W
OFF_BQ = OFF_AV + A_KV_W
OFF_BK = OFF_BQ + B_W
OFF_BV = OFF_BK + B_W
OFF_C = OFF_BV + B_W
N_EXPERTS = 16
EXPERT_FF = 1024
EC_CAPACITY = 2
ROPE_BASE = 10000.0
LN_EPS = 1e-6
N_MOD = 6
MOD_INIT = 0.5
DEEPNORM_ALPHA = (2 * DEPTH) ** 0.25
DEEPNORM_BETA = (8 * DEPTH) ** -0.25
NEG_INF = -1e30

kernel_name = 'hybrid_diffusion_parallel_heads_ec_moe'


def layer_norm(x, g=None, b=None):
    xf = x.astype(jnp.float32)
    mu = jnp.mean(xf, axis=-1, keepdims=True)
    var = jnp.mean(jnp.square(xf - mu), axis=-1, keepdims=True)
    y = (xf - mu) * lax.rsqrt(var + LN_EPS)
    if g is not None:
        y = y * g.astype(jnp.float32) + b.astype(jnp.float32)
    return y.astype(x.dtype)


def modulate(x, shift, scale):
    return layer_norm(x) * (1 + scale) + shift


def adaln(cond, w_mod_l, b_mod_l, n_chunks):
    m = jax.nn.silu(cond) @ w_mod_l[:, :n_chunks * D_MODEL] + b_mod_l[:n_chunks * D_MODEL]
    return jnp.split(m, n_chunks, axis=-1)


def split_heads(t, n_heads):
    return t.reshape(t.shape[:-1] + (n_heads, HEAD_DIM))


def axial_rope(n_tokens, dtype):
    t = jnp.arange(n_tokens, dtype=jnp.int32)
    row = (t // GRID_W).astype(jnp.float32)[:, None]
    col = (t % GRID_W).astype(jnp.float32)[:, None]
    n_freq = HEAD_DIM // 4
    inv_freq = ROPE_BASE ** (-jnp.arange(n_freq, dtype=jnp.float32) / n_freq)
    ang_r = row * inv_freq
    ang_c = col * inv_freq
    return (jnp.cos(ang_r)[:, None, :].astype(dtype), jnp.sin(ang_r)[:, None, :].astype(dtype),
            jnp.cos(ang_c)[:, None, :].astype(dtype), jnp.sin(ang_c)[:, None, :].astype(dtype))


def rope_half(x, cos, sin):
    x1, x2 = jnp.split(x, 2, axis=-1)
    return jnp.concatenate([x1 * cos - x2 * sin, x2 * cos + x1 * sin], axis=-1)


def apply_axial_rope(x, cos_r, sin_r, cos_c, sin_c):
    xr, xc = jnp.split(x, 2, axis=-1)
    return jnp.concatenate([rope_half(xr, cos_r, sin_r), rope_half(xc, cos_c, sin_c)], axis=-1)


def window_gqa_latent(q, k, v, kc, vc, sink):
    bsz, n_lat = q.shape[:2]
    n_blk = n_lat // A_BLOCK
    span = A_BLOCK + 2 * A_WINDOW
    pad = ((0, 0), (A_WINDOW, A_WINDOW), (0, 0), (0, 0))
    kp = jnp.pad(k, pad)
    vp = jnp.pad(v, pad)
    scale = HEAD_DIM ** -0.5
    sink_l = sink.reshape(1, A_KV_HEADS, A_GROUP, 1, 1).astype(jnp.float32)
    q_off = jnp.arange(A_BLOCK)
    k_off = jnp.arange(span) - A_WINDOW
    band = jnp.abs(k_off[None, :] - q_off[:, None]) <= A_WINDOW

    def block(i):
        start = i * A_BLOCK
        qb = lax.dynamic_slice_in_dim(q, start, A_BLOCK, axis=1)
        kb = lax.dynamic_slice_in_dim(kp, start, span, axis=1)
        vb = lax.dynamic_slice_in_dim(vp, start, span, axis=1)
        kpos = start + k_off
        valid = band & ((kpos >= 0) & (kpos < n_lat))[None, :]
        s_win = jnp.einsum('bqhgd,bkhd->bhgqk', qb, kb).astype(jnp.float32) * scale
        s_win = jnp.where(valid, s_win, NEG_INF)
        s_ctx = jnp.einsum('bqhgd,bkhd->bhgqk', qb, kc).astype(jnp.float32) * scale
        s_sink = jnp.broadcast_to(sink_l, s_win.shape[:-1] + (1,))
        p = jax.nn.softmax(jnp.concatenate([s_win, s_ctx, s_sink], axis=-1), axis=-1).astype(q.dtype)
        return (jnp.einsum('bhgqk,bkhd->bqhgd', p[..., :span], vb)
                + jnp.einsum('bhgqk,bkhd->bqhgd', p[..., span:span + kc.shape[1]], vc))

    out = lax.map(block, jnp.arange(n_blk))
    return jnp.moveaxis(out, 0, 1).reshape(bsz, n_lat, A_Q_W)


def context_gqa(qc, kc, vc, sink):
    s = jnp.einsum('bqhgd,bkhd->bhgqk', qc, kc).astype(jnp.float32) * HEAD_DIM ** -0.5
    s_sink = jnp.broadcast_to(sink.reshape(1, A_KV_HEADS, A_GROUP, 1, 1).astype(jnp.float32), s.shape[:-1] + (1,))
    p = jax.nn.softmax(jnp.concatenate([s, s_sink], axis=-1), axis=-1).astype(qc.dtype)
    o = jnp.einsum('bhgqk,bkhd->bqhgd', p[..., :-1], vc)
    return o.reshape(qc.shape[0], qc.shape[1], A_Q_W)


def neighbourhood_latent(q, k, v, kc, vc, rel_bias):
    bsz, n_lat = q.shape[:2]
    rows = n_lat // GRID_W
    kr_n = min(NA_ROWS, rows)
    n_keys = kr_n * GRID_W
    scale = HEAD_DIM ** -0.5
    qg = q.reshape(bsz, rows, GRID_W, B_HEADS, HEAD_DIM)
    kg = k.reshape(bsz, rows, GRID_W, B_HEADS, HEAD_DIM)
    vg = v.reshape(bsz, rows, GRID_W, B_HEADS, HEAD_DIM)
    cols = jnp.arange(GRID_W)
    c_start = jnp.clip(cols - NA_COLS // 2, 0, GRID_W - NA_COLS)
    col_mask = (cols[None, :] >= c_start[:, None]) & (cols[None, :] < c_start[:, None] + NA_COLS)
    mask = jnp.broadcast_to(col_mask[:, None, :], (GRID_W, kr_n, GRID_W)).reshape(GRID_W, n_keys)
    dc = jnp.clip(cols[None, :] - cols[:, None], -(NA_COLS - 1), NA_COLS - 1) + NA_COLS - 1

    def row(r):
        r_start = jnp.clip(r - kr_n // 2, 0, rows - kr_n)
        qr = lax.dynamic_index_in_dim(qg, r, axis=1, keepdims=False)
        kr = lax.dynamic_slice_in_dim(kg, r_start, kr_n, axis=1).reshape(bsz, n_keys, B_HEADS, HEAD_DIM)
        vr = lax.dynamic_slice_in_dim(vg, r_start, kr_n, axis=1).reshape(bsz, n_keys, B_HEADS, HEAD_DIM)
        dr = r_start + jnp.arange(kr_n) - r + NA_ROWS - 1
        bias = rel_bias[:, dr[None, :, None], dc[:, None, :]].reshape(B_HEADS, GRID_W, n_keys)
        s_nb = jnp.einsum('bqhd,bkhd->bhqk', qr, kr).astype(jnp.float32) * scale + bias.astype(jnp.float32)
        s_nb = jnp.where(mask, s_nb, NEG_INF)
        s_ctx = jnp.einsum('bqhd,bkhd->bhqk', qr, kc).astype(jnp.float32) * scale
        p = jax.nn.softmax(jnp.concatenate([s_nb, s_ctx], axis=-1), axis=-1).astype(q.dtype)
        return (jnp.einsum('bhqk,bkhd->bqhd', p[..., :n_keys], vr)
                + jnp.einsum('bhqk,bkhd->bqhd', p[..., n_keys:], vc))

    out = lax.map(row, jnp.arange(rows))
    return jnp.moveaxis(out, 0, 1).reshape(bsz, n_lat, B_W)


def context_mha(qc, kc, vc):
    s = jnp.einsum('bqhd,bkhd->bhqk', qc, kc).astype(jnp.float32) * HEAD_DIM ** -0.5
    p = jax.nn.softmax(s, axis=-1).astype(qc.dtype)
    o = jnp.einsum('bhqk,bkhd->bqhd', p, vc)
    return o.reshape(qc.shape[0], qc.shape[1], B_W)


def conformer_conv(u, conv_w, conv_b, ln_g, ln_b):
    a, gate = jnp.split(u, 2, axis=-1)
    h = a * jax.nn.sigmoid(gate)
    half = C_CONV_WIDTH // 2
    h = lax.conv_general_dilated(h, conv_w[:, None, :], window_strides=(1,), padding=((half, half),),
                                 dimension_numbers=('NWC', 'WIO', 'NWC'),
                                 feature_group_count=C_CHANNELS) + conv_b
    return jax.nn.silu(layer_norm(h, ln_g, ln_b))


def ec_moe(h, w_router, w_gate, w_up, w_down):
    bsz, n_tok, d = h.shape
    cap = EC_CAPACITY * n_tok // N_EXPERTS
    aff = jax.nn.softmax(jnp.einsum('bnd,de->bne', h, w_router).astype(jnp.float32), axis=-1)
    g, idx = lax.top_k(jnp.swapaxes(aff, 1, 2), cap)
    xs = jax.vmap(lambda hb, ib: hb[ib])(h, idx)
    hid = jax.nn.silu(jnp.einsum('becd,edf->becf', xs, w_gate)) * jnp.einsum('becd,edf->becf', xs, w_up)
    ye = jnp.einsum('becf,efd->becd', hid, w_down) * g[..., None].astype(h.dtype)
    return jax.vmap(lambda yb, ib: jnp.zeros((n_tok, d), h.dtype).at[ib.reshape(-1)].add(yb.reshape(-1, d)))(ye, idx)


def context_kv(hc, w_in_l):
    ka, va = jnp.split(hc @ w_in_l[:, OFF_AK:OFF_BQ], 2, axis=-1)
    kb, vb = jnp.split(hc @ w_in_l[:, OFF_BK:OFF_C], 2, axis=-1)
    return (split_heads(ka, A_KV_HEADS), split_heads(va, A_KV_HEADS),
            split_heads(kb, B_HEADS), split_heads(vb, B_HEADS))


def setup_inputs(seed: int = 0) -> dict:
    key = jax.random.key(seed)
    ks = jax.random.split(key, 22)
    L, D = DEPTH, D_MODEL

    def nrm(k, shape, s):
        return s * jax.random.normal(k, shape, jnp.float32)

    return {
        'x': nrm(ks[0], (BATCH, SEQ, D), 1.0),
        'c': nrm(ks[1], (BATCH, D), 1.0),
        'ctx': nrm(ks[2], (BATCH, CTX_LEN, D), 1.0),
        'c_ctx': nrm(ks[3], (D,), 1.0),
        'w_mod': nrm(ks[4], (L, D, N_MOD * D), MOD_INIT * D ** -0.5),
        'b_mod': nrm(ks[5], (L, N_MOD * D), 0.02),
        'w_in': nrm(ks[6], (L, D, IN_WIDTH), D ** -0.5),
        'a_sink': nrm(ks[7], (L, A_Q_HEADS), 0.5),
        'nat_bias': nrm(ks[8], (L, B_HEADS, 2 * NA_ROWS - 1, 2 * NA_COLS - 1), 0.1),
        'conv_w': nrm(ks[9], (L, C_CONV_WIDTH, C_CHANNELS), C_CONV_WIDTH ** -0.5),
        'conv_b': nrm(ks[10], (L, C_CHANNELS), 0.02),
        'conv_ln_g': 1.0 + nrm(ks[11], (L, C_CHANNELS), 0.05),
        'conv_ln_b': nrm(ks[12], (L, C_CHANNELS), 0.02),
        'w_out': nrm(ks[13], (L, MIX_WIDTH, D), DEEPNORM_BETA * MIX_WIDTH ** -0.5),
        'ln1_g': 1.0 + nrm(ks[14], (L, D), 0.05),
        'ln1_b': nrm(ks[15], (L, D), 0.02),
        'w_router': nrm(ks[16], (L, D, N_EXPERTS), D ** -0.5),
        'w_gate': nrm(ks[17], (L, N_EXPERTS, D, EXPERT_FF), D ** -0.5),
        'w_up': nrm(ks[18], (L, N_EXPERTS, D, EXPERT_FF), D ** -0.5),
        'w_down': nrm(ks[19], (L, N_EXPERTS, EXPERT_FF, D), DEEPNORM_BETA * EXPERT_FF ** -0.5),
        'ln2_g': 1.0 + nrm(ks[20], (L, D), 0.05),
        'ln2_b': nrm(ks[21], (L, D), 0.02),
    }


def reference(x, c, ctx, c_ctx, w_mod, b_mod, w_in, a_sink, nat_bias, conv_w, conv_b, conv_ln_g, conv_ln_b,
              w_out, ln1_g, ln1_b, w_router, w_gate, w_up, w_down, ln2_g, ln2_b):
    bsz, n_lat, _ = x.shape
    n_ctx = ctx.shape[1]
    rope = axial_rope(n_lat, x.dtype)
    for l in range(DEPTH):
        last = l == DEPTH - 1
        w_in_l = w_in[l]
        sh1, sc1, g1, sh2, sc2, g2 = [m[:, None, :] for m in adaln(c, w_mod[l], b_mod[l], N_MOD)]

        ctx_mod = adaln(c_ctx, w_mod[l], b_mod[l], 2 if last else N_MOD)
        hc = modulate(ctx, ctx_mod[0], ctx_mod[1])
        kc_a, vc_a, kc_b, vc_b = context_kv(hc, w_in_l)
        if not last:
            qc_a = split_heads(hc @ w_in_l[:, OFF_AQ:OFF_AK], A_Q_HEADS).reshape(bsz, n_ctx, A_KV_HEADS, A_GROUP, HEAD_DIM)
            qc_b = split_heads(hc @ w_in_l[:, OFF_BQ:OFF_BK], B_HEADS)
            oc = jnp.concatenate([
                context_gqa(qc_a, kc_a, vc_a, a_sink[l]),
                context_mha(qc_b, kc_b, vc_b),
                conformer_conv(hc @ w_in_l[:, OFF_C:], conv_w[l], conv_b[l], conv_ln_g[l], conv_ln_b[l]),
            ], axis=-1) @ w_out[l]
            ctx_new = layer_norm(DEEPNORM_ALPHA * ctx + ctx_mod[2] * oc, ln1_g[l], ln1_b[l])
            hc2 = modulate(ctx_new, ctx_mod[3], ctx_mod[4])
            ctx_new = layer_norm(DEEPNORM_ALPHA * ctx_new + ctx_mod[5] * ec_moe(hc2, w_router[l], w_gate[l], w_up[l], w_down[l]),
                                 ln2_g[l], ln2_b[l])

        h = modulate(x, sh1, sc1)
        u = h @ w_in_l
        q_a = apply_axial_rope(split_heads(u[..., OFF_AQ:OFF_AK], A_Q_HEADS), *rope)
        k_a = apply_axial_rope(split_heads(u[..., OFF_AK:OFF_AV], A_KV_HEADS), *rope)
        v_a = split_heads(u[..., OFF_AV:OFF_BQ], A_KV_HEADS)
        q_b = split_heads(u[..., OFF_BQ:OFF_BK], B_HEADS)
        k_b = split_heads(u[..., OFF_BK:OFF_BV], B_HEADS)
        v_b = split_heads(u[..., OFF_BV:OFF_C], B_HEADS)
        o_a = window_gqa_latent(q_a.reshape(bsz, n_lat, A_KV_HEADS, A_GROUP, HEAD_DIM), k_a, v_a, kc_a, vc_a, a_sink[l])
        o_b = neighbourhood_latent(q_b, k_b, v_b, kc_b, vc_b, nat_bias[l])
        o_c = conformer_conv(u[..., OFF_C:], conv_w[l], conv_b[l], conv_ln_g[l], conv_ln_b[l])
        o = jnp.concatenate([o_a, o_b, o_c], axis=-1) @ w_out[l]
        x = layer_norm(DEEPNORM_ALPHA * x + g1 * o, ln1_g[l], ln1_b[l])
        h2 = modulate(x, sh2, sc2)
        x = layer_norm(DEEPNORM_ALPHA * x + g2 * ec_moe(h2, w_router[l], w_gate[l], w_up[l], w_down[l]),
                       ln2_g[l], ln2_b[l])
        if not last:
            ctx = ctx_new
    return x
```

```python
import os
import numpy as np
from contextlib import ExitStack
import concourse.bass as bass
import concourse.mybir as mybir
from concourse.bass_utils import run_bass_kernel_spmd

F32 = mybir.dt.float32
BF16 = mybir.dt.bfloat16
I32 = mybir.dt.int32
AF = mybir.ActivationFunctionType
ALU = mybir.AluOpType
AX = mybir.AxisListType

D = 1024
L = 2
NCTX = 2
NE = 16
EPS = 1e-6
ALPHA = float((2 * L) ** 0.25)
SCALE = 0.125
NEG = -30000.0
BIG = 1.0e6
QA, QB, KA, VA, KB, VB, CH, AW = 0, 1024, 1536, 1664, 1794, 2050, 2310, 2566
QW = KA
KVW = CH - KA
RW = 1024 + 2 + 32
NCHB = 21


class Sched:
    EPOCH = 16000

    def __init__(self, nc, es, ndma=16):
        self.nc, self.es = nc, es
        self.eng = dict(pe=nc.tensor, act=nc.scalar, dve=nc.vector, pool=nc.gpsimd, sp=nc.sync)
        self.sems = {k: [] for k in self.eng}
        self.cnt = {k: 0 for k in self.eng}
        self.dpool = {'sp': list(range(0, ndma)), 'pool': list(range(ndma, ndma + 8)), 'act': list(range(ndma + 8, ndma + 12))}
        ntot = ndma + 12
        self.dsem = [es.enter_context(nc.semaphore(f"dq{i}")) for i in range(ntot)]
        self.dval = [0] * ntot
        self.dnext = {'sp': 0, 'pool': 0, 'act': 0}
        self.waited = {k: {} for k in self.eng}
        self.lastw = {}
        self.readers = {}
        self.same_engine_sync = True
        self.nwaits = 0

    def _sem(self, e, seq):
        i = (seq - 1) // self.EPOCH
        while len(self.sems[e]) <= i:
            self.sems[e].append(self.es.enter_context(self.nc.semaphore(f"s_{e}{len(self.sems[e])}")))
        return self.sems[e][i], (seq - 1) % self.EPOCH + 1

    def _wait(self, e, tok):
        kind, src, val = tok
        if kind == 'e':
            if src == e and (e == 'pe' or not self.same_engine_sync):
                return
            assert val <= self.cnt[src], f"dependency on unsignalled op {tok}"
            key = ('e', src)
        else:
            key = ('d', src)
        if self.waited[e].get(key, 0) >= val:
            return
        self.waited[e][key] = val
        if kind == 'e':
            sem, v = self._sem(src, val)
        else:
            sem, v = self.dsem[src], val
        self.eng[e].wait_ge(sem, v)
        self.nwaits += 1

    def _deps(self, r, w):
        deps = []
        for k in r:
            if k in self.lastw:
                deps.append(self.lastw[k])
            if isinstance(k, tuple) and k[0] == 'pb':
                deps.extend(self.readers.get(k, {}).values())
        for k in w:
            if k in self.lastw:
                deps.append(self.lastw[k])
            deps.extend(self.readers.get(k, {}).values())
        return deps

    def _record(self, tok, r, w):
        for k in r:
            d = self.readers.setdefault(k, {})
            key = (tok[0], tok[1])
            if key not in d or d[key][2] < tok[2]:
                d[key] = tok
        for k in w:
            self.lastw[k] = tok
            self.readers[k] = {}

    def op(self, e, fn, r=(), w=(), signal=True, extra=()):
        for d in self._deps(r, w):
            self._wait(e, d)
        for d in extra:
            self._wait(e, d)
        inst = fn(self.eng[e])
        if signal:
            self.cnt[e] += 1
            seq = self.cnt[e]
            sem, _ = self._sem(e, seq)
            inst.then_inc(sem, 1)
        else:
            seq = self.cnt[e] + 1
        tok = ('e', e, seq)
        self._record(tok, r, w)
        return tok

    def dma(self, q, fn, r=(), w=(), extra=()):
        pl = self.dpool[q]
        slot = pl[self.dnext[q] % len(pl)]
        self.dnext[q] += 1
        if self.dval[slot]:
            self._wait(q, ('d', slot, self.dval[slot]))
        for d in self._deps(r, w):
            self._wait(q, d)
        for d in extra:
            self._wait(q, d)
        inst = fn(self.eng[q])
        inst.then_inc(self.dsem[slot], 16)
        self.dval[slot] += 16
        tok = ('d', slot, self.dval[slot])
        self._record(tok, r, w)
        return tok

    def barrier(self):
        toks = [('e', f, self.cnt[f]) for f in self.eng if self.cnt[f] > 0]
        toks += [('d', s, v) for s, v in enumerate(self.dval) if v > 0]
        for e in self.eng:
            for t in toks:
                self._wait(e, t)


def build_program(NLAT=64, dbg=False, nlayers=L, stop_after=None, skip_mods=False):
    NT = NLAT + NCTX
    NTOK = NT * 128
    CAPL = 16 * NLAT
    CAPC = 32

    nc = bass.Bass("TRN2", target_bir_lowering=False)
    dk = "ExternalOutput" if dbg else "Internal"

    def din(name, shape, dt=F32):
        return nc.dram_tensor(name, list(shape), dt, kind="ExternalInput").ap()

    xlat = din("xlat", [NLAT * 128, D])
    xctx = din("xctx", [NCTX * 128, D])
    cvec = din("cvec", [128, 16])
    if not skip_mods:
        w_mod = din("w_mod", [L, D, 6 * D])
        b_mod = din("b_mod", [L, 6 * D])
    w_in = din("w_in", [L, D, 2048])
    a_sink = din("a_sink", [L, 8])
    natT = din("natT", [L, 128, NCHB * 512])
    conv_w = din("conv_w", [L, 128, 62])
    conv_b = din("conv_b", [L, 128, 2])
    conv_ln_g = din("conv_ln_g", [L, 256])
    conv_ln_b = din("conv_ln_b", [L, 256])
    w_out = din("w_out", [L, D, D])
    ln1_g = din("ln1_g", [L, D])
    ln1_b = din("ln1_b", [L, D])
    w_router = din("w_router", [L, D, NE])
    need_moe = stop_after is None or stop_after.startswith(('M', 'F'))
    if need_moe:
        w_gate = din("w_gate", [L, NE, D, D])
        w_up = din("w_up", [L, NE, D, D])
        w_down = din("w_down", [L, NE, D, D])
    ln2_g = din("ln2_g", [L, D])
    ln2_b = din("ln2_b", [L, D])
    rope = din("rope", [NLAT, 128, 128])
    consts = din("consts", [128, 6 * 128])
    out = nc.dram_tensor("out", [NLAT * 128, D], F32, kind="ExternalOutput").ap()

    modbc = nc.dram_tensor("modbc", [L, 2, 128, 6 * D], F32, kind=dk).ap()
    scrA = nc.dram_tensor("scrA", [NT, 128, AW], BF16, kind=dk).ap()
    x1s = nc.dram_tensor("x1s", [NTOK, D], F32, kind=dk).ap()
    h2s = nc.dram_tensor("h2s", [NTOK, RW], BF16, kind=dk).ap()
    xs_d = nc.dram_tensor("xs_d", [NTOK, D], F32, kind=dk).ap()
    macc = nc.dram_tensor("macc", [NTOK, D], F32, kind=dk).ap()
    CAPT0 = CAPL + CAPC
    Xs = [nc.dram_tensor(f"Xs{e}", [CAPT0, RW], BF16, kind=dk).ap() for e in range(NE)]
    otok_d = nc.dram_tensor("otok_d", [NTOK, D], BF16, kind=dk).ap() if dbg else None

    es = ExitStack()
    S = Sched(nc, es)
    es.enter_context(nc.allow_non_contiguous_dma(reason="small strided parameter loads"))
    es.enter_context(nc.allow_low_precision(reason="0/1 mask counts <= 128 are exact in bf16"))

    uid = [0]

    def sb(stack, name, shape, dt):
        uid[0] += 1
        return stack.enter_context(nc.sbuf_tensor(f"{name}_{uid[0]}", list(shape), dt))

    banks = [es.enter_context(nc.psum_tensor(f"pb{i}", [128, 512], F32)) for i in range(8)]
    bank_rr = [0]

    def nbank():
        i = bank_rr[0]
        bank_rr[0] = (i + 1) % 8
        return banks[i], ('pb', i)

    cst = sb(es, "cst", [128, 6 * 128], F32)
    identb = sb(es, "identb", [128, 128], BF16)
    triub = sb(es, "triub", [128, 128], BF16)
    masklr = sb(es, "masklr", [128, 2, 128], BF16)
    onesb = sb(es, "onesb", [128, 128], BF16)
    aff_all = sb(es, "aff_all", [128, NT, NE], F32)
    stat = sb(es, "stat", [128, 12], F32)
    mv = sb(es, "mv", [128, 2], F32)
    sd = sb(es, "sd", [128, 1], F32)
    rstd = sb(es, "rstd", [128, 1], F32)
    nmr = sb(es, "nmr", [128, 1], F32)
    identf = cst[:, 0:128]
    iotap = cst[:, 5 * 128:5 * 128 + 1]
    pm0 = cst[:, 5 * 128 + 1:5 * 128 + 2]
    pm1 = cst[:, 5 * 128 + 2:5 * 128 + 3]

    S.dma('sp', lambda q: q.dma_start(out=cst[:, :], in_=consts), w=['cst'])
    S.op('dve', lambda e: e.tensor_copy(out=identb[:, :], in_=cst[:, 0:128]), r=['cst'], w=['identb'])
    S.op('dve', lambda e: e.tensor_copy(out=triub[:, :], in_=cst[:, 128:256]), r=['cst'], w=['triub'])
    S.op('dve', lambda e: e.tensor_copy(out=masklr[:, :, :].rearrange("p a b -> p (a b)"), in_=cst[:, 256:512]), r=['cst'], w=['masklr'])
    S.op('dve', lambda e: e.tensor_copy(out=onesb[:, :], in_=cst[:, 512:640]), r=['cst'], w=['onesb'])

    def ln_stats(src_ap, src_key, width=1024):
        nch = (width + 511) // 512
        for c in range(nch):
            S.op('dve', lambda e, c=c: e.bn_stats(out=stat[:, 6 * c:6 * c + 6], in_=src_ap[:, c * 512:min(width, (c + 1) * 512)]),
                 r=[src_key], w=['stat'])
        S.op('dve', lambda e: e.bn_aggr(out=mv[:, :], in_=stat[:, 0:6 * nch]), r=['stat'], w=['mv'])
        S.op('dve', lambda e: e.tensor_scalar(out=sd[:, :], in0=mv[:, 1:2], scalar1=EPS, scalar2=None, op0=ALU.add), r=['mv'], w=['sd'])
        S.op('act', lambda e: e.activation(out=sd[:, :], in_=sd[:, :], func=AF.Sqrt), r=['sd'], w=['sd'])
        S.op('dve', lambda e: e.reciprocal(out=rstd[:, :], in_=sd[:, :]), r=['sd'], w=['rstd'])
        S.op('dve', lambda e: e.tensor_scalar(out=nmr[:, :], in0=mv[:, 0:1], scalar1=rstd[:, 0:1], scalar2=-1.0, op0=ALU.mult, op1=ALU.mult),
             r=['mv', 'rstd'], w=['nmr'])

    def ln_apply(dst_ap, dst_key, src_ap, src_key):
        S.op('act', lambda e: e.activation(out=dst_ap, in_=src_ap, func=AF.Identity, scale=rstd[:, 0:1], bias=nmr[:, 0:1]),
             r=[src_key, 'rstd', 'nmr'], w=[dst_key])

    def transposes_to(dst_ap, dst_key, src_fn, n, rows=128, src_key=None, evac='act'):
        bk, bkey = nbank()
        bv = bk[:, :].bitcast(BF16)
        for k in range(n):
            S.op('pe', lambda e, k=k: e.transpose(out=bv[:, k * 128:k * 128 + rows], in_=src_fn(k), identity=identb[:rows, :rows]),
                 r=[src_key, 'identb'], w=[bkey], signal=(k == n - 1))
        if rows == 128:
            src = bv[:, 0:n * 128]
        else:
            src = bv[:, 0:n * 128].rearrange("p (k s) -> p k s", s=128)[:, :, 0:rows]
        if evac == 'act':
            S.op('act', lambda e: e.copy(out=dst_ap, in_=src), r=[bkey], w=[dst_key])
        else:
            S.op('dve', lambda e: e.tensor_copy(out=dst_ap, in_=src), r=[bkey], w=[dst_key])

    with ExitStack() as ph:
        cT = sb(ph, "cT", [128, 8, 2], F32)
        cS = sb(ph, "cS", [128, 8, 2], F32)
        lbc = [sb(ph, f"lbc{s}", [128, 8, 128], BF16) for s in range(2)]
        wm = [sb(ph, f"wm{i}", [128, 8, 512], BF16) for i in range(2)]
        bm = [sb(ph, f"bm{i}", [128, 512], F32) for i in range(2)]
        mo = [sb(ph, f"mo{i}", [128, 512], F32) for i in range(4)]
        S.dma('sp', lambda q: q.dma_start(out=cT[:, :, :], in_=cvec.rearrange("p (k s) -> p k s", s=2)), w=['cT'])
        S.op('act', lambda e: e.activation(out=cS[:, :, :], in_=cT[:, :, :], func=AF.Silu), r=['cT'], w=['cS'])
        for s in range(2):
            S.op('dve', lambda e, s=s: e.tensor_copy(out=lbc[s][:, :, :], in_=cS[:, :, s:s + 1].to_broadcast([128, 8, 128])),
                 r=['cS'], w=[('lbc', s)])
        it = 0
        for l in range(nlayers if not skip_mods else 0):
            for ch in range(12):
                n0 = ch * 512
                slot = it % 2
                S.dma('pool', lambda q, slot=slot, l=l, n0=n0: q.dma_start(
                    out=wm[slot][:, :, :], in_=w_mod[l, :, n0:n0 + 512].rearrange("(ko ki) n -> ki ko n", ki=128)), w=[('wm', slot)])
                S.dma('sp', lambda q, slot=slot, l=l, n0=n0: q.dma_start(
                    out=bm[slot][:, :], in_=b_mod[l:l + 1, n0:n0 + 512].to_broadcast([128, 512])), w=[('bm', slot)])
                addc = 1.0 if (ch // 2) in (1, 4) else 0.0
                for s in range(2):
                    bk, bkey = nbank()
                    for k in range(8):
                        S.op('pe', lambda e, k=k, s=s, slot=slot, bk=bk: e.matmul(bk[:, :], lhsT=lbc[s][:, k, :], rhs=wm[slot][:, k, :],
                                                                               start=(k == 0), stop=(k == 7)),
                             r=[('lbc', s), ('wm', slot)], w=[bkey], signal=(k == 7))
                    ms = (it * 2 + s) % 4
                    S.op('dve', lambda e, bk=bk, ms=ms, slot=slot: e.scalar_tensor_tensor(
                        out=mo[ms][:, :], in0=bk[:, :], scalar=addc, in1=bm[slot][:, :], op0=ALU.add, op1=ALU.add),
                         r=[bkey, ('bm', slot)], w=[('mo', ms)])
                    S.dma('sp', lambda q, ms=ms, l=l, s=s, n0=n0: q.dma_start(out=modbc[l, s, :, n0:n0 + 512], in_=mo[ms][:, :]),
                          r=[('mo', ms)], w=[('modbc', l, s)])
                it += 1
        S.barrier()
    if stop_after == 'mods':
        return _finish(nc, S, es)

    for l in range(nlayers):
        last = (l == L - 1)
        tiles_b = list(range(NT)) if not last else list(range(NCTX, NT))
        CAPT = CAPL + (0 if last else CAPC)

        def xsrc(ti):
            if l == 0:
                return xctx[ti * 128:(ti + 1) * 128, :] if ti < NCTX else xlat[(ti - NCTX) * 128:(ti - NCTX + 1) * 128, :]
            return xs_d[ti * 128:(ti + 1) * 128, :]

        with ExitStack() as ph:
            w_in_sb = sb(ph, "w_in_sb", [128, 8, 2048], BF16)
            modA = [[sb(ph, f"modA{s}{j}", [128, D], F32) for j in range(2)] for s in range(2)]
            xin = [sb(ph, f"xinA{i}", [128, D], F32) for i in range(2)]
            ropet = [sb(ph, f"ropet{i}", [128, 128], F32) for i in range(2)]
            f32a = sb(ph, "f32a", [128, D], F32)
            hb = sb(ph, "hb", [128, D], BF16)
            hT = sb(ph, "hT", [128, 8, 128], BF16)
            ropeA = sb(ph, "ropeA", [128, 512], F32)
            ropeB = sb(ph, "ropeB", [128, 512], F32)
            qperm = sb(ph, "qperm", [128, 512], BF16)
            kst = sb(ph, "kst", [128, 640], BF16)
            sg = sb(ph, "sg", [128, 256], F32)
            chtok = sb(ph, "chtok", [128, 256], BF16)
            aout = [sb(ph, f"aout{i}", [128, AW], BF16) for i in range(2)]
            for i in range(2):
                S.op('pool', lambda e, i=i: e.memset(aout[i][:, VA:VA + 130], 1.0), w=[('aout', i)])
                S.op('pool', lambda e, i=i: e.memset(aout[i][:, VB:VB + 260], 1.0), w=[('aout', i)])
            for hf in range(2):
                S.dma('pool', lambda q, hf=hf: q.dma_start(out=w_in_sb[:, :, hf * 1024:(hf + 1) * 1024],
                                                        in_=w_in[l, :, hf * 1024:(hf + 1) * 1024].rearrange("(ko ki) n -> ki ko n", ki=128)),
                      w=['w_in_sb'])
            for s in range(2):
                for j in range(2):
                    S.dma('sp', lambda q, s=s, j=j: q.dma_start(out=modA[s][j][:, :], in_=modbc[l, s, :, j * D:(j + 1) * D]),
                          r=[('modbc', l, s)], w=[('modA', s, j)])

            for ti in range(NT):
                s = 0 if ti < NCTX else 1
                lat = ti >= NCTX
                xs_, xk = xin[ti % 2], ('xinA', ti % 2)
                ao, aok = aout[ti % 2], ('aout', ti % 2)
                rt, rk = ropet[ti % 2], ('ropet', ti % 2)
                S.dma('sp', lambda q, ti=ti, xs_=xs_: q.dma_start(out=xs_[:, :], in_=xsrc(ti)), r=[('xs_d',)] if l > 0 else [], w=[xk])
                if lat:
                    S.dma('sp', lambda q, ti=ti, rt=rt: q.dma_start(out=rt[:, :], in_=rope[ti - NCTX, :, :]), w=[rk])
                ln_stats(xs_, xk)
                ln_apply(f32a[:, :], 'f32a', xs_[:, :], xk)
                S.op('dve', lambda e, s=s: e.tensor_tensor(out=f32a[:, :], in0=f32a[:, :], in1=modA[s][1][:, :], op=ALU.mult),
                     r=['f32a', ('modA', s, 1)], w=['f32a'])
                S.op('pool', lambda e, s=s: e.tensor_tensor(out=hb[:, :], in0=f32a[:, :], in1=modA[s][0][:, :], op=ALU.add),
                     r=['f32a', ('modA', s, 0)], w=['hb'])
                transposes_to(hT[:, :, :].rearrange("p k t -> p (k t)"), 'hT', lambda k: hb[:, k * 128:(k + 1) * 128], 8, src_key='hb')
                ub = []
                for n in range(4):
                    bk, bkey = nbank()
                    for k in range(8):
                        S.op('pe', lambda e, k=k, n=n, bk=bk: e.matmul(bk[:, :], lhsT=hT[:, k, :], rhs=w_in_sb[:, k, n * 512:(n + 1) * 512],
                                                                     start=(k == 0), stop=(k == 7)),
                             r=['hT', 'w_in_sb'], w=[bkey], signal=(k == 7))
                    ub.append((bk, bkey))
                b0, k0 = ub[0]
                qp_v = qperm[:, :].rearrange("p (g r d) -> p r g d", g=4, r=2, d=64)
                if lat:
                    S.op('dve', lambda e, b0=b0, rt=rt: e.tensor_tensor(
                        out=ropeA[:, :].rearrange("p (h d) -> p h d", d=64), in0=b0[:, :].rearrange("p (h d) -> p h d", d=64),
                        in1=rt[:, 0:64].unsqueeze(1).to_broadcast([128, 8, 64]), op=ALU.mult), r=[k0, rk], w=['ropeA'])
                    for half in range(2):
                        S.op('dve', lambda e, b0=b0, rt=rt, half=half: e.tensor_tensor(
                            out=ropeB[:, :].rearrange("p (h r f d) -> p h r f d", h=8, r=2, f=2, d=16)[:, :, :, half, :],
                            in0=b0[:, :].rearrange("p (h r f d) -> p h r f d", h=8, r=2, f=2, d=16)[:, :, :, 1 - half, :],
                            in1=rt[:, 64:128].rearrange("p (r f d) -> p r f d", r=2, f=2, d=16)[:, :, half, :].unsqueeze(1).to_broadcast([128, 8, 2, 16]),
                            op=ALU.mult), r=[k0, rk], w=['ropeB'])
                    S.op('pool', lambda e: e.tensor_tensor(out=qp_v, in0=ropeA[:, :].rearrange("p (r g d) -> p r g d", r=2, g=4, d=64),
                                                          in1=ropeB[:, :].rearrange("p (r g d) -> p r g d", r=2, g=4, d=64), op=ALU.add),
                         r=['ropeA', 'ropeB'], w=['qperm'])
                else:
                    S.op('act', lambda e, b0=b0: e.copy(out=qp_v, in_=b0[:, :].rearrange("p (r g d) -> p r g d", r=2, g=4, d=64)),
                         r=[k0], w=['qperm'])
                bk, bkey = nbank()
                bv = bk[:, :].bitcast(BF16)
                for k in range(4):
                    S.op('pe', lambda e, k=k, bv=bv: e.transpose(out=bv[:, k * 128:(k + 1) * 128], in_=qperm[:, k * 128:(k + 1) * 128], identity=identb[:, :]),
                         r=['qperm', 'identb'], w=[bkey], signal=(k == 3))
                if os.environ.get('EVAC', 'mask') == 'mask':
                    S.op('act', lambda e, bv=bv, ao=ao: e.activation(out=ao[:, QA:QA + 512], in_=bv[:, 0:512], func=AF.Copy, scale=pm0), r=[bkey, 'cst'], w=[aok])
                    S.op('dve', lambda e, bv=bv, ao=ao: e.tensor_scalar(out=ao[:, QA + 512:QA + 1024], in0=bv[:, 0:512], scalar1=pm1, scalar2=None, op0=ALU.mult),
                         r=[bkey, 'cst'], w=[aok])
                else:
                    S.op('act', lambda e, bv=bv, ao=ao: e.copy(out=ao[:, QA:QA + 512], in_=bv[:, 0:512]), r=[bkey], w=[aok])
                    S.op('dve', lambda e, bv=bv, ao=ao: e.tensor_copy(out=ao[:, QA + 512:QA + 1024], in_=bv[:, 0:512]), r=[bkey], w=[aok])
                b1, k1 = ub[1]
                if lat:
                    S.op('dve', lambda e, b1=b1, rt=rt: e.tensor_tensor(
                        out=ropeA[:, 0:128].rearrange("p (h d) -> p h d", d=64), in0=b1[:, 0:128].rearrange("p (h d) -> p h d", d=64),
                        in1=rt[:, 0:64].unsqueeze(1).to_broadcast([128, 2, 64]), op=ALU.mult), r=[k1, rk], w=['ropeA'])
                    for half in range(2):
                        S.op('dve', lambda e, b1=b1, rt=rt, half=half: e.tensor_tensor(
                            out=ropeB[:, 0:128].rearrange("p (h r f d) -> p h r f d", h=2, r=2, f=2, d=16)[:, :, :, half, :],
                            in0=b1[:, 0:128].rearrange("p (h r f d) -> p h r f d", h=2, r=2, f=2, d=16)[:, :, :, 1 - half, :],
                            in1=rt[:, 64:128].rearrange("p (r f d) -> p r f d", r=2, f=2, d=16)[:, :, half, :].unsqueeze(1).to_broadcast([128, 2, 2, 16]),
                            op=ALU.mult), r=[k1, rk], w=['ropeB'])
                    S.op('pool', lambda e: e.tensor_tensor(out=kst[:, 0:128], in0=ropeA[:, 0:128], in1=ropeB[:, 0:128], op=ALU.add),
                         r=['ropeA', 'ropeB'], w=['kst'])
                else:
                    S.op('act', lambda e, b1=b1: e.copy(out=kst[:, 0:128], in_=b1[:, 0:128]), r=[k1], w=['kst'])
                S.op('act', lambda e, b1=b1, ao=ao: e.copy(out=ao[:, VA:VA + 130].rearrange("p (h d) -> p h d", d=65)[:, :, 0:64],
                                                         in_=b1[:, 128:256].rearrange("p (h d) -> p h d", d=64)), r=[k1], w=[aok])
                S.op('act', lambda e, b1=b1: e.copy(out=kst[:, 128:384], in_=b1[:, 256:512]), r=[k1], w=['kst'])
                b2, k2 = ub[2]
                S.op('dve', lambda e, b2=b2: e.tensor_copy(out=kst[:, 384:640], in_=b2[:, 0:256]), r=[k2], w=['kst'])
                S.op('act', lambda e, b2=b2, ao=ao: e.copy(out=ao[:, VB:VB + 260].rearrange("p (h d) -> p h d", d=65)[:, :, 0:64],
                                                         in_=b2[:, 256:512].rearrange("p (h d) -> p h d", d=64)), r=[k2], w=[aok])
                order = [1, 2, 0, 3, 4]
                bk, bkey = nbank()
                bv = bk[:, :].bitcast(BF16)
                for j, blk in enumerate(order):
                    S.op('pe', lambda e, j=j, blk=blk, bv=bv: e.transpose(out=bv[:, j * 128:(j + 1) * 128], in_=kst[:, blk * 128:(blk + 1) * 128],
                                                                         identity=identb[:, :]), r=['kst', 'identb'], w=[bkey], signal=(j == 4))
                if os.environ.get('EVAC', 'mask') == 'mask':
                    S.op('act', lambda e, bv=bv, ao=ao: e.activation(out=ao[:, QB:QB + 256], in_=bv[:, 0:256], func=AF.Copy, scale=pm0), r=[bkey, 'cst'], w=[aok])
                    S.op('dve', lambda e, bv=bv, ao=ao: e.tensor_scalar(out=ao[:, QB + 256:QB + 512], in0=bv[:, 0:256], scalar1=pm1, scalar2=None, op0=ALU.mult),
                         r=[bkey, 'cst'], w=[aok])
                else:
                    S.op('act', lambda e, bv=bv, ao=ao: e.copy(out=ao[:, QB:QB + 256], in_=bv[:, 0:256]), r=[bkey], w=[aok])
                    S.op('dve', lambda e, bv=bv, ao=ao: e.tensor_copy(out=ao[:, QB + 256:QB + 512], in_=bv[:, 0:256]), r=[bkey], w=[aok])
                S.op('dve', lambda e, bv=bv, ao=ao: e.tensor_copy(out=ao[:, KA:KA + 128], in_=bv[:, 256:384]), r=[bkey], w=[aok])
                S.op('dve', lambda e, bv=bv, ao=ao: e.tensor_copy(out=ao[:, KB:KB + 256], in_=bv[:, 384:640]), r=[bkey], w=[aok])
                b3, k3 = ub[3]
                S.op('act', lambda e, b3=b3: e.activation(out=sg[:, :], in_=b3[:, 256:512], func=AF.Sigmoid), r=[k3], w=['sg'])
                S.op('dve', lambda e, b3=b3: e.tensor_tensor(out=chtok[:, :], in0=b3[:, 0:256], in1=sg[:, :], op=ALU.mult), r=[k3, 'sg'], w=['chtok'])
                transposes_to(ao[:, CH:CH + 256], aok, lambda k: chtok[:, k * 128:(k + 1) * 128], 2, src_key='chtok', evac='dve')
                S.dma('sp', lambda q, ti=ti, ao=ao: q.dma_start(out=scrA[ti, :, 0:QW], in_=ao[:, 0:QW]), r=[aok], w=[('scrAq', ti)])
                S.dma('sp', lambda q, ti=ti, ao=ao: q.dma_start(out=scrA[ti, :, QW:AW], in_=ao[:, QW:AW]), r=[aok], w=[('scrA', ti)])
            S.barrier()
        if stop_after == f'A{l}':
            return _finish(nc, S, es)

        with ExitStack() as ph:
            w_out_sb = sb(ph, "w_out_sb", [128, 8, D], BF16)
            w_r_sb = sb(ph, "w_r_sb", [128, 8, NE], BF16)
            nat_sb = sb(ph, "nat_sb", [128, NCHB, 512], BF16)
            cwT = sb(ph, "cwT", [128, 2, 31], F32)
            cdiag = sb(ph, "cdiag", [128, 2, 31, 128], BF16)
            convb = sb(ph, "convb", [128, 2], F32)
            clng = sb(ph, "clng", [128, 256], F32)
            clnb = sb(ph, "clnb", [128, 256], F32)
            modB = [[sb(ph, f"modB{s}{j}", [128, D], F32) for j in range(3)] for s in range(2)]
            ln1g = sb(ph, "ln1g", [128, D], F32)
            ln1b = sb(ph, "ln1b", [128, D], F32)
            esink = sb(ph, "esink", [128, 8], F32)
            NQR, NKR, NCW = 3, 8, 4
            qring = [sb(ph, f"qring{i}", [128, QW], BF16) for i in range(NQR)]
            kvring = [sb(ph, f"kvring{i}", [128, KVW], BF16) for i in range(NKR)]
            kvctx = [sb(ph, f"kvctx{i}", [128, KVW], BF16) for i in range(NCTX)]
            chwin = [sb(ph, f"chwin{i}", [128, 2, 160], BF16) for i in range(NCW)]
            xin = [sb(ph, f"xinB{i}", [128, D], F32) for i in range(2)]
            pT = sb(ph, "pT", [128, 7, 512], BF16)
            btmp = [sb(ph, f"btmp{i}", [128, 512], F32) for i in range(2)]
            otok = sb(ph, "otok", [128, D], BF16)
            oT = sb(ph, "oT", [128, 8, 128], BF16)
            cvT = sb(ph, "cvT", [128, 2, 128], F32)
            cvn = sb(ph, "cvn", [128, 256], F32)
            f32b = sb(ph, "f32b", [128, D], F32)
            f32c = sb(ph, "f32c", [128, D], F32)
            h2row = [sb(ph, f"h2row{i}", [128, RW], BF16) for i in range(2)]
            h2T = sb(ph, "h2T", [128, 8, 128], BF16)
            den = sb(ph, "den", [128, 4], F32)
            rden = sb(ph, "rden", [128, 4], F32)
            ex = sb(ph, "ex", [128, NE], F32)
            ssum = sb(ph, "ssum", [128, 1], F32)

            for hf in range(1):
                S.dma('pool', lambda q: q.dma_start(out=w_out_sb[:, :, :], in_=w_out[l, :, :].rearrange("(ko ki) n -> ki ko n", ki=128)), w=['w_out_sb'])
            S.dma('pool', lambda q: q.dma_start(out=w_r_sb[:, :, :], in_=w_router[l, :, :].rearrange("(ko ki) n -> ki ko n", ki=128)), w=['w_r_sb'])
            for c3 in range(0, NCHB, 3):
                c4 = min(NCHB, c3 + 3)
                S.dma('pool', lambda q, c3=c3, c4=c4: q.dma_start(out=nat_sb[:, c3:c4, :], in_=natT[l, :, c3 * 512:c4 * 512].rearrange("p (a b) -> p a b", b=512)),
                      w=['nat_sb'])
            S.dma('sp', lambda q: q.dma_start(out=cwT[:, :, :], in_=conv_w[l, :, :].rearrange("c (cc j) -> c cc j", cc=2)), w=['cwT'])
            S.dma('sp', lambda q: q.dma_start(out=convb[:, :], in_=conv_b[l, :, :]), w=['convb'])
            S.dma('sp', lambda q: q.dma_start(out=clng[:, :], in_=conv_ln_g[l:l + 1, :].to_broadcast([128, 256])), w=['clng'])
            S.dma('sp', lambda q: q.dma_start(out=clnb[:, :], in_=conv_ln_b[l:l + 1, :].to_broadcast([128, 256])), w=['clnb'])
            S.dma('sp', lambda q: q.dma_start(out=ln1g[:, :], in_=ln1_g[l:l + 1, :].to_broadcast([128, D])), w=['ln1g'])
            S.dma('sp', lambda q: q.dma_start(out=ln1b[:, :], in_=ln1_b[l:l + 1, :].to_broadcast([128, D])), w=['ln1b'])
            S.dma('sp', lambda q: q.dma_start(out=esink[:, :], in_=a_sink[l:l + 1, :].to_broadcast([128, 8])), w=['esink'])
            S.op('act', lambda e: e.activation(out=esink[:, :], in_=esink[:, :], func=AF.Exp), r=['esink'], w=['esink'])
            for s in range(2):
                for j, chn in enumerate((2, 4, 3)):
                    S.dma('sp', lambda q, s=s, j=j, chn=chn: q.dma_start(out=modB[s][j][:, :], in_=modbc[l, s, :, chn * D:(chn + 1) * D]),
                          r=[('modbc', l, s)], w=[('modB', s, j)])
            for cc in range(2):
                for j in range(31):
                    S.op('pool', lambda e, cc=cc, j=j: e.tensor_scalar(out=cdiag[:, cc, j, :], in0=cst[:, 0:128], scalar1=cwT[:, cc, j:j + 1],
                                                                      scalar2=None, op0=ALU.mult), r=['cst', 'cwT'], w=['cdiag'])
            for c in range(NCTX):
                S.dma('sp', lambda q, c=c: q.dma_start(out=kvctx[c][:, :], in_=scrA[c, :, KA:CH]), r=[('scrA', c)], w=[('kvctx', c)])

            loaded = set()

            def load_tile(t):
                if t in loaded or t < 0 or t >= NT:
                    return
                loaded.add(t)
                if t >= NCTX:
                    S.dma('sp', lambda q: q.dma_start(out=kvring[t % NKR][:, :], in_=scrA[t, :, KA:CH]), r=[('scrA', t)], w=[('kvring', t % NKR)])

            def load_own(t):
                S.dma('sp', lambda q: q.dma_start(out=qring[t % NQR][:, :], in_=scrA[t, :, 0:QW]), r=[('scrAq', t)], w=[('qring', t % NQR)])
                cw, cwk = chwin[t % NCW], ('chwin', t % NCW)
                first = t in (0, NCTX)
                lastt = t in (NCTX - 1, NT - 1)
                S.dma('sp', lambda q: q.dma_start(out=cw[:, :, 16:144], in_=scrA[t, :, CH:CH + 256].rearrange("p (c n) -> p c n", c=2)),
                      r=[('scrA', t)], w=[cwk])
                if first:
                    S.op('pool', lambda e: e.memset(cw[:, :, 0:16], 0.0), w=[cwk])
                else:
                    S.dma('sp', lambda q: q.dma_start(out=cw[:, :, 0:16], in_=scrA[t - 1, :, CH:CH + 256].rearrange("p (c n) -> p c n", c=2)[:, :, 112:128]),
                          r=[('scrA', t - 1)], w=[cwk])
                if lastt:
                    S.op('pool', lambda e: e.memset(cw[:, :, 144:160], 0.0), w=[cwk])
                else:
                    S.dma('sp', lambda q: q.dma_start(out=cw[:, :, 144:160], in_=scrA[t + 1, :, CH:CH + 256].rearrange("p (c n) -> p c n", c=2)[:, :, 0:16]),
                          r=[('scrA', t + 1)], w=[cwk])
                S.dma('sp', lambda q: q.dma_start(out=xin[t % 2][:, :], in_=xsrc(t)), r=[('xs_d',)] if l > 0 else [], w=[('xinB', t % 2)])

            def kvbuf(t):
                if t < NCTX:
                    return kvctx[t], ('kvctx', t)
                return kvring[t % NKR], ('kvring', t % NKR)

            for ti in tiles_b:
                s = 0 if ti < NCTX else 1
                lat = ti >= NCTX
                i = ti - NCTX
                for t2 in range(ti - 2, ti + 4):
                    if lat and t2 >= NCTX:
                        load_tile(t2)
                load_own(ti)
                qr, qk = qring[ti % NQR], ('qring', ti % NQR)
                cw, cwk = chwin[ti % NCW], ('chwin', ti % NCW)
                xs_, xk = xin[ti % 2], ('xinB', ti % 2)
                hr, hk = h2row[ti % 2], ('h2row', ti % 2)

                if stop_after == f'B{l}:setup':
                    break
                if lat:
                    chunksA = []
                    if i - 1 >= 0:
                        chunksA.append((ti - 1, 0))
                    chunksA.append((ti, None))
                    if i + 1 < NLAT:
                        chunksA.append((ti + 1, 1))
                    chunksA += [(0, None), (1, None)]
                else:
                    chunksA = [(0, None), (1, None)]
                for grp in range(2):
                    p0 = 64 * grp
                    for ci, (ct, mk) in enumerate(chunksA):
                        kb_, kk = kvbuf(ct)
                        bk, bkey = nbank()
                        S.op('pe', lambda e, bk=bk, kb_=kb_: e.matmul(bk[:, :], lhsT=kb_[:, 0:128], rhs=qr[:, QA + grp * 512:QA + (grp + 1) * 512],
                                                                     start=True, stop=True), r=[kk, qk], w=[bkey])
                        S.op('act', lambda e, bk=bk, ci=ci: e.activation(out=pT[:, ci, :], in_=bk[:, :], func=AF.Exp, scale=SCALE),
                             r=[bkey], w=[('pT', ci)])
                        if mk is not None:
                            S.op('pool', lambda e, ci=ci, mk=mk: e.tensor_tensor(
                                out=pT[:, ci, :].rearrange("p (g t) -> p g t", g=4), in0=pT[:, ci, :].rearrange("p (g t) -> p g t", g=4),
                                in1=masklr[:, mk, :].unsqueeze(1).to_broadcast([128, 4, 128]), op=ALU.mult), r=[('pT', ci), 'masklr'], w=[('pT', ci)])
                    ob, obk = nbank()
                    nchk = len(chunksA)
                    for g in range(4):
                        for ci, (ct, mk) in enumerate(chunksA):
                            kb_, kk = kvbuf(ct)
                            S.op('pe', lambda e, g=g, ci=ci, kb_=kb_, ob=ob: e.matmul(
                                ob[:, g * 65:(g + 1) * 65], lhsT=pT[:, ci, g * 128:(g + 1) * 128],
                                rhs=kb_[:, VA - KA + grp * 65:VA - KA + (grp + 1) * 65], start=(ci == 0), stop=(ci == nchk - 1)),
                                 r=[('pT', ci), kk], w=[obk], signal=(g == 3 and ci == nchk - 1))
                    obv = ob[:, 0:260].rearrange("p (g d) -> p g d", d=65)
                    S.op('dve', lambda e, obv=obv: e.tensor_tensor(out=den[:, :], in0=obv[:, :, 64], in1=esink[:, grp * 4:(grp + 1) * 4], op=ALU.add),
                         r=[obk, 'esink'], w=['den'])
                    S.op('dve', lambda e: e.reciprocal(out=rden[:, :], in_=den[:, :]), r=['den'], w=['rden'])
                    S.op('dve', lambda e, obv=obv: e.tensor_tensor(
                        out=otok[:, grp * 256:(grp + 1) * 256].rearrange("p (g d) -> p g d", d=64), in0=obv[:, :, 0:64],
                        in1=rden[:, :].unsqueeze(2).to_broadcast([128, 4, 64]), op=ALU.mult), r=[obk, 'rden'], w=['otok'])

                if stop_after == f'B{l}:attA':
                    break
                if lat:
                    if NLAT >= 5 and 2 <= i <= NLAT - 3:
                        chunksB = [(ti + d - 2, d) for d in range(5)]
                    elif i == 0:
                        chunksB = [(NCTX + j, 5 + j) for j in range(4)]
                    elif i == 1:
                        chunksB = [(NCTX + j, 9 + j) for j in range(4)]
                    elif i == NLAT - 2:
                        chunksB = [(NCTX + NLAT - 4 + j, 13 + j) for j in range(4)]
                    else:
                        chunksB = [(NCTX + NLAT - 4 + j, 17 + j) for j in range(4)]
                    chunksB += [(0, None), (1, None)]
                else:
                    chunksB = [(0, None), (1, None)]
                for ci, (ct, ent) in enumerate(chunksB):
                    kb_, kk = kvbuf(ct)
                    bk, bkey = nbank()
                    for h in range(4):
                        p, m = h // 2, h % 2
                        S.op('pe', lambda e, bk=bk, kb_=kb_, h=h, p=p, m=m: e.matmul(
                            bk[:, h * 128:(h + 1) * 128], lhsT=kb_[:, KB - KA + p * 128:KB - KA + (p + 1) * 128],
                            rhs=qr[:, QB + m * 256 + p * 128:QB + m * 256 + (p + 1) * 128], start=True, stop=True),
                             r=[kk, qk], w=[bkey], signal=(h == 3))
                    if ent is not None:
                        bt, btk = btmp[ci % 2], ('btmp', ci % 2)
                        S.op('dve', lambda e, bk=bk, bt=bt, ent=ent: e.scalar_tensor_tensor(
                            out=bt[:, :], in0=bk[:, :], scalar=SCALE, in1=nat_sb[:, ent, :], op0=ALU.mult, op1=ALU.add),
                             r=[bkey, 'nat_sb'], w=[btk])
                        S.op('act', lambda e, bt=bt, ci=ci: e.activation(out=pT[:, ci, :], in_=bt[:, :], func=AF.Exp), r=[btk], w=[('pT', ci)])
                    else:
                        S.op('act', lambda e, bk=bk, ci=ci: e.activation(out=pT[:, ci, :], in_=bk[:, :], func=AF.Exp, scale=SCALE),
                             r=[bkey], w=[('pT', ci)])
                ob, obk = nbank()
                nchk = len(chunksB)
                for h in range(4):
                    for ci, (ct, ent) in enumerate(chunksB):
                        kb_, kk = kvbuf(ct)
                        S.op('pe', lambda e, h=h, ci=ci, kb_=kb_, ob=ob: e.matmul(
                            ob[:, h * 65:(h + 1) * 65], lhsT=pT[:, ci, h * 128:(h + 1) * 128],
                            rhs=kb_[:, VB - KA + h * 65:VB - KA + (h + 1) * 65], start=(ci == 0), stop=(ci == nchk - 1)),
                             r=[('pT', ci), kk], w=[obk], signal=(h == 3 and ci == nchk - 1))
                obv = ob[:, 0:260].rearrange("p (g d) -> p g d", d=65)
                S.op('dve', lambda e, obv=obv: e.reciprocal(out=rden[:, :], in_=obv[:, :, 64]), r=[obk], w=['rden'])
                S.op('dve', lambda e, obv=obv: e.tensor_tensor(
                    out=otok[:, 512:768].rearrange("p (g d) -> p g d", d=64), in0=obv[:, :, 0:64],
                    in1=rden[:, :].unsqueeze(2).to_broadcast([128, 4, 64]), op=ALU.mult), r=[obk, 'rden'], w=['otok'])

                if stop_after == f'B{l}:attB':
                    break
                bk, bkey = nbank()
                for cc in range(2):
                    for j in range(31):
                        S.op('pe', lambda e, bk=bk, cc=cc, j=j: e.matmul(bk[:, cc * 128:(cc + 1) * 128], lhsT=cdiag[:, cc, j, :],
                                                                        rhs=cw[:, cc, 1 + j:1 + j + 128], start=(j == 0), stop=(j == 30)),
                             r=['cdiag', cwk], w=[bkey], signal=(cc == 1 and j == 30))
                for cc in range(2):
                    S.op('act', lambda e, bk=bk, cc=cc: e.activation(out=cvT[:, cc, :], in_=bk[:, cc * 128:(cc + 1) * 128], func=AF.Identity,
                                                                    bias=convb[:, cc:cc + 1], scale=1.0), r=[bkey, 'convb'], w=['cvT'])
                bk2, bkey2 = nbank()
                for cc in range(2):
                    S.op('pe', lambda e, bk2=bk2, cc=cc: e.transpose(out=bk2[:, cc * 128:(cc + 1) * 128], in_=cvT[:, cc, :], identity=identf),
                         r=['cvT', 'cst'], w=[bkey2], signal=(cc == 1))
                ln_stats(bk2, bkey2, width=256)
                ln_apply(cvn[:, :], 'cvn', bk2[:, 0:256], bkey2)
                S.op('dve', lambda e: e.tensor_tensor(out=cvn[:, :], in0=cvn[:, :], in1=clng[:, :], op=ALU.mult), r=['cvn', 'clng'], w=['cvn'])
                S.op('pool', lambda e: e.tensor_tensor(out=cvn[:, :], in0=cvn[:, :], in1=clnb[:, :], op=ALU.add), r=['cvn', 'clnb'], w=['cvn'])
                S.op('act', lambda e: e.activation(out=otok[:, 768:1024], in_=cvn[:, :], func=AF.Silu), r=['cvn'], w=['otok'])

                if dbg:
                    S.dma('sp', lambda q: q.dma_start(out=otok_d[ti * 128:(ti + 1) * 128, :], in_=otok[:, :]), r=['otok'], w=[('dbg_otok', ti)])

                if stop_after == f'B{l}:conv':
                    break
                transposes_to(oT[:, :, :].rearrange("p k t -> p (k t)"), 'oT', lambda k: otok[:, k * 128:(k + 1) * 128], 8, src_key='otok')
                for n in range(2):
                    bk, bkey = nbank()
                    for k in range(8):
                        S.op('pe', lambda e, bk=bk, k=k, n=n: e.matmul(bk[:, :], lhsT=oT[:, k, :], rhs=w_out_sb[:, k, n * 512:(n + 1) * 512],
                                                                     start=(k == 0), stop=(k == 7)), r=['oT', 'w_out_sb'], w=[bkey], signal=(k == 7))
                    S.op('dve', lambda e, bk=bk, n=n: e.tensor_tensor(out=f32b[:, n * 512:(n + 1) * 512], in0=bk[:, :],
                                                                     in1=modB[s][0][:, n * 512:(n + 1) * 512], op=ALU.mult),
                         r=[bkey, ('modB', s, 0)], w=['f32b'])
                S.op('dve', lambda e: e.scalar_tensor_tensor(out=f32b[:, :], in0=xs_[:, :], scalar=ALPHA, in1=f32b[:, :], op0=ALU.mult, op1=ALU.add),
                     r=[xk, 'f32b'], w=['f32b'])
                ln_stats(f32b, 'f32b')
                ln_apply(f32c[:, :], 'f32c', f32b[:, :], 'f32b')
                S.op('dve', lambda e: e.tensor_tensor(out=f32c[:, :], in0=f32c[:, :], in1=ln1g[:, :], op=ALU.mult), r=['f32c', 'ln1g'], w=['f32c'])
                S.op('pool', lambda e: e.tensor_tensor(out=f32c[:, :], in0=f32c[:, :], in1=ln1b[:, :], op=ALU.add), r=['f32c', 'ln1b'], w=['f32c'])
                S.dma('sp', lambda q: q.dma_start(out=x1s[ti * 128:(ti + 1) * 128, :], in_=f32c[:, :]), r=['f32c'], w=[('x1s', ti)])
                if stop_after == f'B{l}:proj':
                    break
                ln_stats(f32c, 'f32c')
                ln_apply(f32b[:, :], 'f32b', f32c[:, :], 'f32c')
                S.op('dve', lambda e: e.tensor_tensor(out=f32b[:, :], in0=f32b[:, :], in1=modB[s][1][:, :], op=ALU.mult),
                     r=['f32b', ('modB', s, 1)], w=['f32b'])
                S.op('pool', lambda e: e.tensor_tensor(out=hr[:, 0:1024], in0=f32b[:, :], in1=modB[s][2][:, :], op=ALU.add),
                     r=['f32b', ('modB', s, 2)], w=[hk])
                transposes_to(h2T[:, :, :].rearrange("p k t -> p (k t)"), 'h2T', lambda k: hr[:, k * 128:(k + 1) * 128], 8, src_key=hk)
                bk, bkey = nbank()
                for k in range(8):
                    S.op('pe', lambda e, bk=bk, k=k: e.matmul(bk[:, 0:NE], lhsT=h2T[:, k, :], rhs=w_r_sb[:, k, :], start=(k == 0), stop=(k == 7)),
                         r=['h2T', 'w_r_sb'], w=[bkey], signal=(k == 7))
                S.op('act', lambda e, bk=bk: e.activation(out=ex[:, :], in_=bk[:, 0:NE], func=AF.Exp, accum_out=ssum[:, 0:1]), r=[bkey], w=['ex', 'ssum'])
                S.op('dve', lambda e: e.reciprocal(out=ssum[:, :], in_=ssum[:, :]), r=['ssum'], w=['ssum'])
                S.op('dve', lambda e: e.tensor_scalar(out=aff_all[:, ti, :], in0=ex[:, :], scalar1=ssum[:, 0:1], scalar2=None, op0=ALU.mult),
                     r=['ex', 'ssum'], w=[('aff', ti)])
                S.op('dve', lambda e: e.tensor_copy(out=hr[:, 1026:1042], in_=aff_all[:, ti, :]), r=[('aff', ti)], w=[hk])
                S.op('dve', lambda e: e.tensor_tensor(out=hr[:, 1042:1058], in0=aff_all[:, ti, :], in1=hr[:, 1026:1042], op=ALU.subtract),
                     r=[('aff', ti), hk], w=[hk])
                S.op('dve', lambda e: e.tensor_scalar(out=hr[:, 1024:1025], in0=iotap, scalar1=0.0, scalar2=float(ti), op0=ALU.mult, op1=ALU.add),
                     r=['cst'], w=[hk])
                S.op('dve', lambda e: e.tensor_copy(out=hr[:, 1025:1026], in_=iotap), r=['cst'], w=[hk])
                S.dma('sp', lambda q: q.dma_start(out=h2s[ti * 128:(ti + 1) * 128, :], in_=hr[:, :]), r=[hk], w=[('h2s', ti)])
            S.barrier()
        if stop_after is not None and stop_after.startswith(f'B{l}'):
            return _finish(nc, S, es)

        NTB = len(tiles_b)
        t0b = tiles_b[0]
        with ExitStack() as ph:
            cmpb = sb(ph, "cmpb", [128, NT, NE], BF16)
            lo = sb(ph, "lo", [128, 32], F32)
            hi = sb(ph, "hi", [128, 32], F32)
            mid = sb(ph, "mid", [128, 32], F32)
            kvec = sb(ph, "kvec", [128, 32], F32)
            cntp = sb(ph, "cntp", [128, 32], BF16)
            ge = sb(ph, "ge", [128, 32], F32)
            gm = sb(ph, "gm", [128, 32], F32)
            posf = sb(ph, "posf", [128, NT, NE], F32)
            tot = sb(ph, "tot", [128, NT, NE], F32)
            base = sb(ph, "base", [128, NT + 1, NE], F32)
            sel = sb(ph, "sel", [128, NT, NE], F32)
            posi = sb(ph, "posi", [128, NT, NE], I32)
            affk = [('aff', t) for t in range(NT)]
            S.op('dve', lambda e: e.memset(lo[:, :], 0.0), w=['lo'])
            S.op('dve', lambda e: e.memset(hi[:, :], 1.0), w=['hi'])
            S.op('dve', lambda e: e.memset(kvec[:, 0:16], float(CAPL)), w=['kvec'])
            S.op('dve', lambda e: e.memset(kvec[:, 16:32], float(CAPC)), w=['kvec'])
            S.op('dve', lambda e: e.memset(cntp[:, :], 0.0), w=['cntp'])
            if last:
                S.op('dve', lambda e: e.memset(cmpb[:, 0:NCTX, :], 0.0), w=['cmpb'])
            lat_aff = aff_all[:, NCTX:NT, :]
            ctx_aff = aff_all[:, 0:NCTX, :]

            def compare(thr):
                S.op('dve', lambda e: e.tensor_tensor(out=cmpb[:, NCTX:NT, :], in0=lat_aff, in1=thr[:, 0:16].unsqueeze(1).to_broadcast([128, NLAT, NE]),
                                                      op=ALU.is_ge), r=affk + ['thr'], w=['cmpb'])
                if not last:
                    S.op('dve', lambda e: e.tensor_tensor(out=cmpb[:, 0:NCTX, :], in0=ctx_aff, in1=thr[:, 16:32].unsqueeze(1).to_broadcast([128, NCTX, NE]),
                                                          op=ALU.is_ge), r=affk + ['thr'], w=['cmpb'])

            for itn in range(30):
                S.op('dve', lambda e: e.tensor_tensor(out=mid[:, :], in0=lo[:, :], in1=hi[:, :], op=ALU.add), r=['lo', 'hi'], w=['thr'])
                S.op('dve', lambda e: e.tensor_scalar(out=mid[:, :], in0=mid[:, :], scalar1=0.5, scalar2=None, op0=ALU.mult), r=['thr'], w=['thr'])
                compare(mid)
                S.op('dve', lambda e: e.tensor_reduce(out=cntp[:, 0:16], in_=cmpb[:, NCTX:NT, :].rearrange("p t e -> p e t"), axis=AX.X, op=ALU.add),
                     r=['cmpb'], w=['cntp'])
                if not last:
                    S.op('dve', lambda e: e.tensor_reduce(out=cntp[:, 16:32], in_=cmpb[:, 0:NCTX, :].rearrange("p t e -> p e t"), axis=AX.X, op=ALU.add),
                         r=['cmpb'], w=['cntp'])
                bk, bkey = nbank()
                S.op('pe', lambda e, bk=bk: e.matmul(bk[:, 0:32], lhsT=onesb[:, :], rhs=cntp[:, :], start=True, stop=True), r=['onesb', 'cntp'], w=[bkey])
                S.op('dve', lambda e, bk=bk: e.tensor_tensor(out=ge[:, :], in0=bk[:, 0:32], in1=kvec[:, :], op=ALU.is_ge), r=[bkey, 'kvec'], w=['ge'])
                S.op('dve', lambda e: e.tensor_tensor(out=gm[:, :], in0=ge[:, :], in1=mid[:, :], op=ALU.mult), r=['ge', 'thr'], w=['gm'])
                S.op('dve', lambda e: e.tensor_tensor(out=lo[:, :], in0=lo[:, :], in1=gm[:, :], op=ALU.max), r=['lo', 'gm'], w=['lo'])
                S.op('dve', lambda e: e.scalar_tensor_tensor(out=gm[:, :], in0=ge[:, :], scalar=2.0, in1=mid[:, :], op0=ALU.mult, op1=ALU.add),
                     r=['ge', 'thr', 'gm'], w=['gm'])
                S.op('dve', lambda e: e.tensor_tensor(out=hi[:, :], in0=hi[:, :], in1=gm[:, :], op=ALU.min), r=['hi', 'gm'], w=['hi'])
            S.op('dve', lambda e: e.tensor_copy(out=mid[:, :], in_=lo[:, :]), r=['lo'], w=['thr'])
            compare(mid)
            cflat = cmpb[:, :, :].rearrange("p t e -> p (t e)")
            pflat = posf[:, :, :].rearrange("p t e -> p (t e)")
            tflat = tot[:, :, :].rearrange("p t e -> p (t e)")
            ncol = NT * NE
            for c0 in range(0, ncol, 512):
                cs = min(512, ncol - c0)
                bk, bkey = nbank()
                S.op('pe', lambda e, bk=bk, c0=c0, cs=cs: e.matmul(bk[:, 0:cs], lhsT=triub[:, :], rhs=cflat[:, c0:c0 + cs], start=True, stop=True),
                     r=['triub', 'cmpb'], w=[bkey])
                S.op('act', lambda e, bk=bk, c0=c0, cs=cs: e.copy(out=pflat[:, c0:c0 + cs], in_=bk[:, 0:cs]), r=[bkey], w=['posf'])
                bk, bkey = nbank()
                S.op('pe', lambda e, bk=bk, c0=c0, cs=cs: e.matmul(bk[:, 0:cs], lhsT=onesb[:, :], rhs=cflat[:, c0:c0 + cs], start=True, stop=True),
                     r=['onesb', 'cmpb'], w=[bkey])
                S.op('act', lambda e, bk=bk, c0=c0, cs=cs: e.copy(out=tflat[:, c0:c0 + cs], in_=bk[:, 0:cs]), r=[bkey], w=['tot'])
            S.op('dve', lambda e: e.memset(base[:, 0, :], float(CAPL)), w=['base'])
            S.op('dve', lambda e: e.memset(base[:, NCTX, :], 0.0), w=['base'])
            for t in range(NT):
                if t == NCTX - 1:
                    continue
                S.op('dve', lambda e, t=t: e.tensor_tensor(out=base[:, t + 1, :], in0=base[:, t, :], in1=tot[:, t, :], op=ALU.add),
                     r=['base', 'tot'], w=['base'])
            S.op('dve', lambda e: e.tensor_tensor(out=posf[:, :, :], in0=posf[:, :, :], in1=base[:, 0:NT, :], op=ALU.add), r=['posf', 'base'], w=['posf'])
            S.op('dve', lambda e: e.scalar_tensor_tensor(out=sel[:, NCTX:NT, :], in0=posf[:, NCTX:NT, :], scalar=float(CAPL), in1=cmpb[:, NCTX:NT, :],
                                                         op0=ALU.is_lt, op1=ALU.mult), r=['posf', 'cmpb'], w=['sel'])
            S.op('dve', lambda e: e.scalar_tensor_tensor(out=sel[:, 0:NCTX, :], in0=posf[:, 0:NCTX, :], scalar=float(CAPL + CAPC), in1=cmpb[:, 0:NCTX, :],
                                                         op0=ALU.is_lt, op1=ALU.mult), r=['posf', 'cmpb'], w=['sel'])
            S.op('dve', lambda e: e.scalar_tensor_tensor(out=posf[:, :, :], in0=posf[:, :, :], scalar=-BIG, in1=sel[:, :, :], op0=ALU.add, op1=ALU.mult),
                 r=['posf', 'sel'], w=['posf'])
            S.op('dve', lambda e: e.tensor_scalar(out=posi[:, :, :], in0=posf[:, :, :], scalar1=BIG, scalar2=None, op0=ALU.add), r=['posf'], w=['posi'])

            h2ld = [sb(ph, f"h2ld{i}", [128, RW], BF16) for i in range(3)]
            breg = nc.gpsimd.to_reg(CAPT - 1)
            for n, ti in enumerate(tiles_b):
                hl, hlk = h2ld[n % 3], ('h2ld', n % 3)
                S.dma('sp', lambda q, ti=ti, hl=hl: q.dma_start(out=hl[:, :], in_=h2s[ti * 128:(ti + 1) * 128, :]), r=[('h2s', ti)], w=[hlk])
                for ex_ in range(NE):
                    S.dma('pool', lambda q, ti=ti, ex_=ex_, hl=hl: q.indirect_dma_start(
                        out=Xs[ex_][:, :], out_offset=bass.IndirectOffsetOnAxis(ap=posi[:, ti, ex_:ex_ + 1], axis=0),
                        in_=hl[:, :], in_offset=None, bounds_check=breg, oob_is_err=False), r=[hlk, 'posi'], w=[('Xs', ex_)])
            S.barrier()
        if stop_after == f'R{l}':
            return _finish(nc, S, es)

        NST = (CAPT + 127) // 128
        CAPP = NST * 128
        NSL = 3 if CAPP % 3 == 0 and CAPP // 3 <= 512 else (CAPP + 511) // 512
        SLW = CAPP // NSL
        assert SLW * NSL == CAPP and SLW <= 512
        with ExitStack() as ph:
            wg = [sb(ph, f"wg{i}", [128, 8, D], BF16) for i in range(2)]
            wu = [sb(ph, f"wu{i}", [128, 8, D], BF16) for i in range(2)]
            wd = [sb(ph, f"wd{i}", [128, 8, D], BF16) for i in range(2)]
            xsb = sb(ph, "xsb", [128, NST, RW], BF16)
            XT = sb(ph, "XT", [128, 8, NST * 128], BF16)
            hidT = sb(ph, "hidT", [128, 8, NST * 128], BF16)
            sgt = [sb(ph, f"sgt{i}", [128, 512], F32) for i in range(2)]
            ysb = [sb(ph, f"ysb{i}", [128, D], F32) for i in range(3)]
            gcol = sb(ph, "gcol", [128, NST], F32)
            idxi = sb(ph, "idxi", [128, NST], I32)
            S.op('pool', lambda e: e.memset(xsb[:, NST - 1, :], 0.0), w=['xsb'])
            zt = ysb[0]
            S.op('pool', lambda e: e.memset(zt[:, :], 0.0), w=[('ysb', 0)])
            ztoks = []
            for ti in tiles_b:
                ztoks.append(S.dma('sp', lambda q, ti=ti: q.dma_start(out=macc[ti * 128:(ti + 1) * 128, :], in_=zt[:, :]),
                                   r=[('ysb', 0), ('macc_rd', ti)], w=[('macc_z', ti)]))
            prev_sc = list(ztoks)

            def load_w(ex_):
                sl = ex_ % 2
                for wsb, wdr, nm in ((wg, w_gate, 'wg'), (wu, w_up, 'wu'), (wd, w_down, 'wd')):
                    S.dma('pool', lambda q, wsb=wsb, wdr=wdr: q.dma_start(
                        out=wsb[sl][:, :, :], in_=wdr[l, ex_, :, :].rearrange("(ko ki) n -> ki ko n", ki=128)), w=[(nm, sl)])

            load_w(0)
            ycount = 0
            for ex_ in range(NE):
                sl = ex_ % 2
                if ex_ + 1 < NE:
                    load_w(ex_ + 1)
                nfull = CAPT // 128
                rem = CAPT - nfull * 128
                S.dma('sp', lambda q, ex_=ex_: q.dma_start(out=xsb[:, 0:nfull, :], in_=Xs[ex_][0:nfull * 128, :].rearrange("(s p) w -> p s w", p=128)),
                      r=[('Xs', ex_)], w=['xsb'])
                if rem:
                    S.dma('sp', lambda q, ex_=ex_: q.dma_start(out=xsb[0:rem, nfull, :], in_=Xs[ex_][nfull * 128:CAPT, :]), r=[('Xs', ex_)], w=['xsb'])
                S.op('dve', lambda e, ex_=ex_: e.tensor_tensor(out=gcol[:, 0:nfull], in0=xsb[:, 0:nfull, 1026 + ex_], in1=xsb[:, 0:nfull, 1042 + ex_], op=ALU.add),
                     r=['xsb'], w=['gcol'])
                S.op('dve', lambda e: e.scalar_tensor_tensor(out=idxi[:, 0:nfull], in0=xsb[:, 0:nfull, 1024], scalar=128.0, in1=xsb[:, 0:nfull, 1025],
                                                             op0=ALU.mult, op1=ALU.add), r=['xsb'], w=['idxi'])
                if rem:
                    S.op('dve', lambda e, ex_=ex_: e.tensor_tensor(out=gcol[0:rem, nfull:nfull + 1], in0=xsb[0:rem, nfull, 1026 + ex_:1027 + ex_],
                                                                  in1=xsb[0:rem, nfull, 1042 + ex_:1043 + ex_], op=ALU.add), r=['xsb'], w=['gcol'])
                    S.op('dve', lambda e: e.scalar_tensor_tensor(out=idxi[0:rem, nfull:nfull + 1], in0=xsb[0:rem, nfull, 1024:1025], scalar=128.0,
                                                                 in1=xsb[0:rem, nfull, 1025:1026], op0=ALU.mult, op1=ALU.add), r=['xsb'], w=['idxi'])
                for st in range(NST):
                    rows = 128 if st < nfull else rem
                    transposes_to(XT[:, :, st * 128:(st + 1) * 128], 'XT', lambda k, st=st: xsb[:, st, k * 128:(k + 1) * 128], 8,
                                  src_key='xsb', evac=('act' if st % 2 == 0 else 'dve'))
                for fc in range(8):
                    for sn in range(NSL):
                        n0 = sn * SLW
                        bg, bgk = nbank()
                        for k in range(8):
                            S.op('pe', lambda e, bg=bg, k=k, fc=fc, n0=n0: e.matmul(bg[:, 0:SLW], lhsT=wg[sl][:, k, fc * 128:(fc + 1) * 128],
                                                                                   rhs=XT[:, k, n0:n0 + SLW], start=(k == 0), stop=(k == 7)),
                                 r=[('wg', sl), 'XT'], w=[bgk], signal=(k == 7))
                        bu, buk = nbank()
                        for k in range(8):
                            S.op('pe', lambda e, bu=bu, k=k, fc=fc, n0=n0: e.matmul(bu[:, 0:SLW], lhsT=wu[sl][:, k, fc * 128:(fc + 1) * 128],
                                                                                   rhs=XT[:, k, n0:n0 + SLW], start=(k == 0), stop=(k == 7)),
                                 r=[('wu', sl), 'XT'], w=[buk], signal=(k == 7))
                        sgi = (fc * NSL + sn) % 2
                        S.op('act', lambda e, bg=bg, sgi=sgi: e.activation(out=sgt[sgi][:, 0:SLW], in_=bg[:, 0:SLW], func=AF.Silu), r=[bgk], w=[('sgt', sgi)])
                        S.op('dve', lambda e, bu=bu, sgi=sgi, fc=fc, n0=n0: e.tensor_tensor(out=hidT[:, fc, n0:n0 + SLW], in0=bu[:, 0:SLW], in1=sgt[sgi][:, 0:SLW],
                                                                                           op=ALU.mult), r=[buk, ('sgt', sgi)], w=['hidT'])
                cur_sc = []
                for st in range(NST):
                    rows = 128 if st < nfull else rem
                    yi = ycount % 3
                    ycount += 1
                    for half in range(2):
                        by, byk = nbank()
                        for fc in range(8):
                            S.op('pe', lambda e, by=by, fc=fc, st=st, rows=rows, half=half: e.matmul(
                                by[:, :], lhsT=hidT[:, fc, st * 128:(st + 1) * 128], rhs=wd[sl][:, fc, half * 512:(half + 1) * 512],
                                start=(fc == 0), stop=(fc == 7)), r=['hidT', ('wd', sl)], w=[byk], signal=(fc == 7))
                        if half == 0:
                            S.op('act', lambda e, by=by, yi=yi, st=st, rows=rows: e.activation(out=ysb[yi][0:rows, 0:512], in_=by[0:rows, :], func=AF.Copy,
                                                                                              scale=gcol[0:rows, st:st + 1]), r=[byk, 'gcol'], w=[('ysb', yi)])
                        else:
                            S.op('dve', lambda e, by=by, yi=yi, st=st, rows=rows: e.tensor_scalar(out=ysb[yi][0:rows, 512:1024], in0=by[0:rows, :],
                                                                                                 scalar1=gcol[0:rows, st:st + 1], scalar2=None, op0=ALU.mult),
                                 r=[byk, 'gcol'], w=[('ysb', yi)])
                    cur_sc.append(S.dma('pool', lambda q, yi=yi, st=st, rows=rows: q.indirect_dma_start(
                        out=macc[:, :], out_offset=bass.IndirectOffsetOnAxis(ap=idxi[0:rows, st:st + 1], axis=0),
                        in_=ysb[yi][0:rows, :], in_offset=None, compute_op=ALU.add), r=[('ysb', yi), 'idxi'], w=[('macc_sc', ex_, st)], extra=prev_sc))
                prev_sc = cur_sc
            S.barrier()
        if stop_after == f'M{l}':
            return _finish(nc, S, es)

        with ExitStack() as ph:
            g2 = [sb(ph, f"g2_{s}", [128, D], F32) for s in range(2)]
            ln2g = sb(ph, "ln2g", [128, D], F32)
            ln2b = sb(ph, "ln2b", [128, D], F32)
            xa = [sb(ph, f"xa{i}", [128, D], F32) for i in range(2)]
            xm = [sb(ph, f"xm{i}", [128, D], F32) for i in range(2)]
            xo = [sb(ph, f"xo{i}", [128, D], F32) for i in range(2)]
            for s in range(2):
                S.dma('sp', lambda q, s=s: q.dma_start(out=g2[s][:, :], in_=modbc[l, s, :, 5 * D:6 * D]), r=[('modbc', l, s)], w=[('g2', s)])
            S.dma('sp', lambda q: q.dma_start(out=ln2g[:, :], in_=ln2_g[l:l + 1, :].to_broadcast([128, D])), w=['ln2g'])
            S.dma('sp', lambda q: q.dma_start(out=ln2b[:, :], in_=ln2_b[l:l + 1, :].to_broadcast([128, D])), w=['ln2b'])
            out_toks = []
            for n, ti in enumerate(tiles_b):
                s = 0 if ti < NCTX else 1
                a, ak = xa[n % 2], ('xa', n % 2)
                m, mk = xm[n % 2], ('xm', n % 2)
                o, ok = xo[n % 2], ('xo', n % 2)
                S.dma('sp', lambda q, ti=ti, a=a: q.dma_start(out=a[:, :], in_=x1s[ti * 128:(ti + 1) * 128, :]), r=[('x1s', ti)], w=[ak])
                S.dma('sp', lambda q, ti=ti, m=m: q.dma_start(out=m[:, :], in_=macc[ti * 128:(ti + 1) * 128, :]), w=[mk, ('macc_rd', ti)], extra=prev_sc)
                S.op('dve', lambda e, m=m, s=s: e.tensor_tensor(out=m[:, :], in0=m[:, :], in1=g2[s][:, :], op=ALU.mult), r=[mk, ('g2', s)], w=[mk])
                S.op('dve', lambda e, m=m, a=a: e.scalar_tensor_tensor(out=m[:, :], in0=a[:, :], scalar=ALPHA, in1=m[:, :], op0=ALU.mult, op1=ALU.add),
                     r=[ak, mk], w=[mk])
                ln_stats(m, mk)
                ln_apply(o[:, :], ok, m[:, :], mk)
                S.op('dve', lambda e, o=o: e.tensor_tensor(out=o[:, :], in0=o[:, :], in1=ln2g[:, :], op=ALU.mult), r=[ok, 'ln2g'], w=[ok])
                S.op('pool', lambda e, o=o: e.tensor_tensor(out=o[:, :], in0=o[:, :], in1=ln2b[:, :], op=ALU.add), r=[ok, 'ln2b'], w=[ok])
                if last:
                    i = ti - NCTX
                    out_toks.append(S.dma('sp', lambda q, i=i, o=o: q.dma_start(out=out[i * 128:(i + 1) * 128, :], in_=o[:, :]), r=[ok], w=[('out', i)]))
                else:
                    S.dma('sp', lambda q, ti=ti, o=o: q.dma_start(out=xs_d[ti * 128:(ti + 1) * 128, :], in_=o[:, :]), r=[ok], w=[('xs_d',)])
            S.barrier()
        if stop_after == f'F{l}':
            return _finish(nc, S, es)
    return _finish(nc, S, es)


def _finish(nc, S, es):
    S.barrier()
    es.close()
    return nc


def _consts():
    c = np.zeros((128, 6 * 128), np.float32)
    p = np.arange(128)
    c[:, 0:128] = np.eye(128, dtype=np.float32)
    c[:, 128:256] = (p[:, None] < p[None, :]).astype(np.float32)
    c[:, 256:384] = (p[:, None] >= p[None, :]).astype(np.float32)
    c[:, 384:512] = (p[:, None] <= p[None, :]).astype(np.float32)
    c[:, 512:640] = 1.0
    c[:, 640] = p.astype(np.float32)
    c[:, 641] = (p < 64).astype(np.float32)
    c[:, 642] = (p >= 64).astype(np.float32)
    return c


def _rope_table(NLAT):
    t = np.arange(NLAT * 128, dtype=np.int32)
    row = (t // 64).astype(np.float32)[:, None]
    col = (t % 64).astype(np.float32)[:, None]
    inv = (np.float32(10000.0) ** (-np.arange(16, dtype=np.float32) / np.float32(16))).astype(np.float32)
    ar = (row * inv).astype(np.float32)
    ac = (col * inv).astype(np.float32)
    cr, sr, cc, sc = np.cos(ar), np.sin(ar), np.cos(ac), np.sin(ac)
    tab = np.concatenate([cr, cr, cc, cc, -sr, sr, -sc, sc], axis=1).astype(np.float32)
    return np.ascontiguousarray(tab.reshape(NLAT, 128, 128))


def _nat_entries(NLAT):
    ents = [(2, d) for d in range(5)] if NLAT >= 5 else [(0, 0)] * 5
    ents = [(2, 2 + d - 2) for d in range(5)] if NLAT >= 5 else ents
    ents += [(0, j) for j in range(4)] + [(1, j) for j in range(4)]
    ents += [(NLAT - 2, NLAT - 4 + j) for j in range(4)] + [(NLAT - 1, NLAT - 4 + j) for j in range(4)]
    return ents


def _nat_table(nat_bias, NLAT):
    rows = NLAT * 2
    ents = _nat_entries(NLAT)
    kk = np.arange(128)
    Lh = nat_bias.shape[0]
    out = np.empty((Lh, 128, NCHB, 4, 128), np.float32)
    for n, (i, j) in enumerate(ents):
        kr = 2 * j + kk // 64
        kc = kk % 64
        qr = 2 * i + kk // 64
        qc = kk % 64
        rs = np.clip(qr - 4, 0, rows - 8)
        cs = np.clip(qc - 8, 0, 48)
        valid = ((kr[:, None] >= rs[None, :]) & (kr[:, None] < rs[None, :] + 8) &
                 (kc[:, None] >= cs[None, :]) & (kc[:, None] < cs[None, :] + 16))
        dr = np.clip(kr[:, None] - qr[None, :] + 7, 0, 14)
        dc = np.clip(kc[:, None] - qc[None, :], -15, 15) + 15
        g = nat_bias[:, :, dr, dc]
        g = np.where(valid[None, None], g, np.float32(NEG))
        out[:, :, n, :, :] = np.transpose(g, (0, 2, 1, 3))
    return np.ascontiguousarray(out.reshape(Lh, 128, NCHB * 512))


def make_in_maps(inputs, NLAT, samples, moe=True):
    names = ['w_mod', 'b_mod', 'w_in', 'a_sink', 'conv_w', 'conv_b', 'conv_ln_g', 'conv_ln_b', 'w_out', 'ln1_g', 'ln1_b',
             'w_router', 'ln2_g', 'ln2_b'] + (['w_gate', 'w_up', 'w_down'] if moe else [])
    shared = {k: np.ascontiguousarray(np.asarray(inputs[k], np.float32)) for k in names}
    cw = shared['conv_w']
    shared['conv_w'] = np.ascontiguousarray(cw.reshape(L, 31, 2, 128).transpose(0, 3, 2, 1).reshape(L, 128, 62))
    shared['conv_b'] = np.ascontiguousarray(shared['conv_b'].reshape(L, 2, 128).transpose(0, 2, 1))
    shared['natT'] = _nat_table(np.asarray(inputs['nat_bias'], np.float32), NLAT)
    shared['rope'] = _rope_table(NLAT)
    shared['consts'] = _consts()
    maps = []
    for b in samples:
        m = dict(shared)
        m['xlat'] = np.ascontiguousarray(np.asarray(inputs['x'][b, :NLAT * 128], np.float32))
        m['xctx'] = np.ascontiguousarray(np.asarray(inputs['ctx'][b], np.float32))
        cv = np.stack([np.asarray(inputs['c_ctx'], np.float32), np.asarray(inputs['c'][b], np.float32)])
        m['cvec'] = np.ascontiguousarray(cv.reshape(2, 8, 128).transpose(2, 1, 0).reshape(128, 16))
        maps.append(m)
    return maps


def kernel(**inputs):
    NLAT = 64
    nc = build_program(NLAT)
    maps = make_in_maps(inputs, NLAT, [0, 1, 2, 3, 0, 1, 2, 3])
    res = run_bass_kernel_spmd(nc, maps, core_ids=list(range(8)))
    return np.stack([np.asarray(res.results[b]["out"], np.float32).reshape(NLAT * 128, D) for b in range(4)])
```

```python
import os
import numpy as np
from contextlib import ExitStack
import concourse.bass as bass
import concourse.mybir as mybir
from concourse.bass_utils import run_bass_kernel_spmd

F32 = mybir.dt.float32
BF16 = mybir.dt.bfloat16
I32 = mybir.dt.int32
AF = mybir.ActivationFunctionType
ALU = mybir.AluOpType
AX = mybir.AxisListType

D = 1024
L = 2
NCTX = 2
NE = 16
EPS = 1e-6
ALPHA = float((2 * L) ** 0.25)
SCALE = 0.125
NEG = -30000.0
BIG = 1.0e6
QA, QB, KA, VA, KB, VB, CH, AW = 0, 1024, 1536, 1664, 1794, 2050, 2310, 2566
QW = KA
KVW = CH - KA
RW = 1024 + 2 + 32
NCHB = 21


class Sched:
    EPOCH = 16000

    def __init__(self, nc, es, ndma=16):
        self.nc, self.es = nc, es
        self.eng = dict(pe=nc.tensor, act=nc.scalar, dve=nc.vector, pool=nc.gpsimd, sp=nc.sync)
        self.sems = {k: [] for k in self.eng}
        self.cnt = {k: 0 for k in self.eng}
        self.dpool = {'sp': list(range(0, ndma)), 'pool': list(range(ndma, ndma + 8)), 'act': list(range(ndma + 8, ndma + 12))}
        ntot = ndma + 12
        self.dsem = [es.enter_context(nc.semaphore(f"dq{i}")) for i in range(ntot)]
        self.dval = [0] * ntot
        self.dnext = {'sp': 0, 'pool': 0, 'act': 0}
        self.waited = {k: {} for k in self.eng}
        self.lastw = {}
        self.readers = {}
        self.same_engine_sync = True
        self.nwaits = 0

    def _sem(self, e, seq):
        i = (seq - 1) // self.EPOCH
        while len(self.sems[e]) <= i:
            self.sems[e].append(self.es.enter_context(self.nc.semaphore(f"s_{e}{len(self.sems[e])}")))
        return self.sems[e][i], (seq - 1) % self.EPOCH + 1

    def _wait(self, e, tok):
        kind, src, val = tok
        if kind == 'e':
            if src == e and (e == 'pe' or not self.same_engine_sync):
                return
            assert val <= self.cnt[src], f"dependency on unsignalled op {tok}"
            key = ('e', src)
        else:
            key = ('d', src)
        if self.waited[e].get(key, 0) >= val:
            return
        self.waited[e][key] = val
        if kind == 'e':
            sem, v = self._sem(src, val)
        else:
            sem, v = self.dsem[src], val
        self.eng[e].wait_ge(sem, v)
        self.nwaits += 1

    def _deps(self, r, w):
        deps = []
        for k in r:
            if k in self.lastw:
                deps.append(self.lastw[k])
            if isinstance(k, tuple) and k[0] == 'pb':
                deps.extend(self.readers.get(k, {}).values())
        for k in w:
            if k in self.lastw:
                deps.append(self.lastw[k])
            deps.extend(self.readers.get(k, {}).values())
        return deps

    def _record(self, tok, r, w):
        for k in r:
            d = self.readers.setdefault(k, {})
            key = (tok[0], tok[1])
            if key not in d or d[key][2] < tok[2]:
                d[key] = tok
        for k in w:
            self.lastw[k] = tok
            self.readers[k] = {}

    def op(self, e, fn, r=(), w=(), signal=True, extra=()):
        for d in self._deps(r, w):
            self._wait(e, d)
        for d in extra:
            self._wait(e, d)
        inst = fn(self.eng[e])
        if signal:
            self.cnt[e] += 1
            seq = self.cnt[e]
            sem, _ = self._sem(e, seq)
            inst.then_inc(sem, 1)
        else:
            seq = self.cnt[e] + 1
        tok = ('e', e, seq)
        self._record(tok, r, w)
        return tok

    def dma(self, q, fn, r=(), w=(), extra=()):
        pl = self.dpool[q]
        slot = pl[self.dnext[q] % len(pl)]
        self.dnext[q] += 1
        if self.dval[slot]:
            self._wait(q, ('d', slot, self.dval[slot]))
        for d in self._deps(r, w):
            self._wait(q, d)
        for d in extra:
            self._wait(q, d)
        inst = fn(self.eng[q])
        inst.then_inc(self.dsem[slot], 16)
        self.dval[slot] += 16
        tok = ('d', slot, self.dval[slot])
        self._record(tok, r, w)
        return tok

    def barrier(self):
        toks = [('e', f, self.cnt[f]) for f in self.eng if self.cnt[f] > 0]
        toks += [('d', s, v) for s, v in enumerate(self.dval) if v > 0]
        for e in self.eng:
            for t in toks:
                self._wait(e, t)


def build_program(NLAT=64, dbg=False, nlayers=L, stop_after=None, skip_mods=False):
    NT = NLAT + NCTX
    NTOK = NT * 128
    CAPL = 16 * NLAT
    CAPC = 32

    nc = bass.Bass("TRN2", target_bir_lowering=False)
    dk = "ExternalOutput" if dbg else "Internal"

    def din(name, shape, dt=F32):
        return nc.dram_tensor(name, list(shape), dt, kind="ExternalInput").ap()

    xlat = din("xlat", [NLAT * 128, D])
    xctx = din("xctx", [NCTX * 128, D])
    cvec = din("cvec", [128, 16])
    if not skip_mods:
        w_mod = din("w_mod", [L, D, 6 * D])
        b_mod = din("b_mod", [L, 6 * D])
    w_in = din("w_in", [L, D, 2048])
    a_sink = din("a_sink", [L, 8])
    natT = din("natT", [L, 128, NCHB * 512])
    conv_w = din("conv_w", [L, 128, 62])
    conv_b = din("conv_b", [L, 128, 2])
    conv_ln_g = din("conv_ln_g", [L, 256])
    conv_ln_b = din("conv_ln_b", [L, 256])
    w_out = din("w_out", [L, D, D])
    ln1_g = din("ln1_g", [L, D])
    ln1_b = din("ln1_b", [L, D])
    w_router = din("w_router", [L, D, NE])
    need_moe = stop_after is None or stop_after.startswith(('M', 'F'))
    if need_moe:
        w_gate = din("w_gate", [L, NE, D, D])
        w_up = din("w_up", [L, NE, D, D])
        w_down = din("w_down", [L, NE, D, D])
    ln2_g = din("ln2_g", [L, D])
    ln2_b = din("ln2_b", [L, D])
    rope = din("rope", [NLAT, 128, 128])
    consts = din("consts", [128, 6 * 128])
    out = nc.dram_tensor("out", [NLAT * 128, D], F32, kind="ExternalOutput").ap()

    modbc = nc.dram_tensor("modbc", [L, 2, 128, 6 * D], F32, kind=dk).ap()
    scrA = nc.dram_tensor("scrA", [NT, 128, AW], BF16, kind=dk).ap()
    x1s = nc.dram_tensor("x1s", [NTOK, D], F32, kind=dk).ap()
    h2s = nc.dram_tensor("h2s", [NTOK, RW], BF16, kind=dk).ap()
    xs_d = nc.dram_tensor("xs_d", [NTOK, D], F32, kind=dk).ap()
    macc = nc.dram_tensor("macc", [NTOK, D], F32, kind=dk).ap()
    CAPT0 = CAPL + CAPC
    Xs = [nc.dram_tensor(f"Xs{e}", [CAPT0, RW], BF16, kind=dk).ap() for e in range(NE)]
    otok_d = nc.dram_tensor("otok_d", [NTOK, D], BF16, kind=dk).ap() if dbg else None

    es = ExitStack()
    S = Sched(nc, es)
    es.enter_context(nc.allow_non_contiguous_dma(reason="small strided parameter loads"))
    es.enter_context(nc.allow_low_precision(reason="0/1 mask counts <= 128 are exact in bf16"))

    uid = [0]

    def sb(stack, name, shape, dt):
        uid[0] += 1
        return stack.enter_context(nc.sbuf_tensor(f"{name}_{uid[0]}", list(shape), dt))

    banks = [es.enter_context(nc.psum_tensor(f"pb{i}", [128, 512], F32)) for i in range(8)]
    bank_rr = [0]

    def nbank():
        i = bank_rr[0]
        bank_rr[0] = (i + 1) % 8
        return banks[i], ('pb', i)

    cst = sb(es, "cst", [128, 6 * 128], F32)
    identb = sb(es, "identb", [128, 128], BF16)
    triub = sb(es, "triub", [128, 128], BF16)
    masklr = sb(es, "masklr", [128, 2, 128], BF16)
    onesb = sb(es, "onesb", [128, 128], BF16)
    aff_all = sb(es, "aff_all", [128, NT, NE], F32)
    NLN = 4
    stat_l = [sb(es, "stat", [128, 12], F32) for _ in range(NLN)]
    mv_l = [sb(es, "mv", [128, 2], F32) for _ in range(NLN)]
    sd_l = [sb(es, "sd", [128, 1], F32) for _ in range(NLN)]
    rstd_l = [sb(es, "rstd", [128, 1], F32) for _ in range(NLN)]
    nmr_l = [sb(es, "nmr", [128, 1], F32) for _ in range(NLN)]
    lncur = [0]
    identf = cst[:, 0:128]
    iotap = cst[:, 5 * 128:5 * 128 + 1]
    pm0 = cst[:, 5 * 128 + 1:5 * 128 + 2]
    pm1 = cst[:, 5 * 128 + 2:5 * 128 + 3]

    S.dma('sp', lambda q: q.dma_start(out=cst[:, :], in_=consts), w=['cst'])
    S.op('dve', lambda e: e.tensor_copy(out=identb[:, :], in_=cst[:, 0:128]), r=['cst'], w=['identb'])
    S.op('dve', lambda e: e.tensor_copy(out=triub[:, :], in_=cst[:, 128:256]), r=['cst'], w=['triub'])
    S.op('dve', lambda e: e.tensor_copy(out=masklr[:, :, :].rearrange("p a b -> p (a b)"), in_=cst[:, 256:512]), r=['cst'], w=['masklr'])
    S.op('dve', lambda e: e.tensor_copy(out=onesb[:, :], in_=cst[:, 512:640]), r=['cst'], w=['onesb'])

    def ln_stats(src_ap, src_key, width=1024):
        nch = (width + 511) // 512
        lncur[0] = (lncur[0] + 1) % NLN
        j = lncur[0]
        stat, mv, sd, rstd, nmr = stat_l[j], mv_l[j], sd_l[j], rstd_l[j], nmr_l[j]
        kst_, kmv, ksd, krs, knm = ('stat', j), ('mv', j), ('sd', j), ('rstd', j), ('nmr', j)
        for c in range(nch):
            S.op('dve', lambda e, c=c: e.bn_stats(out=stat[:, 6 * c:6 * c + 6], in_=src_ap[:, c * 512:min(width, (c + 1) * 512)]),
                 r=[src_key], w=[kst_])
        S.op('dve', lambda e: e.bn_aggr(out=mv[:, :], in_=stat[:, 0:6 * nch]), r=[kst_], w=[kmv])
        S.op('dve', lambda e: e.tensor_scalar(out=sd[:, :], in0=mv[:, 1:2], scalar1=EPS, scalar2=None, op0=ALU.add), r=[kmv], w=[ksd])
        S.op('act', lambda e: e.activation(out=sd[:, :], in_=sd[:, :], func=AF.Sqrt), r=[ksd], w=[ksd])
        S.op('dve', lambda e: e.reciprocal(out=rstd[:, :], in_=sd[:, :]), r=[ksd], w=[krs])
        S.op('dve', lambda e: e.tensor_scalar(out=nmr[:, :], in0=mv[:, 0:1], scalar1=rstd[:, 0:1], scalar2=-1.0, op0=ALU.mult, op1=ALU.mult),
             r=[kmv, krs], w=[knm])

    def ln_apply(dst_ap, dst_key, src_ap, src_key):
        j = lncur[0]
        S.op('act', lambda e: e.activation(out=dst_ap, in_=src_ap, func=AF.Identity, scale=rstd_l[j][:, 0:1], bias=nmr_l[j][:, 0:1]),
             r=[src_key, ('rstd', j), ('nmr', j)], w=[dst_key])

    def transposes_to(dst_ap, dst_key, src_fn, n, rows=128, src_key=None, evac='act'):
        bk, bkey = nbank()
        bv = bk[:, :].bitcast(BF16)
        for k in range(n):
            S.op('pe', lambda e, k=k: e.transpose(out=bv[:, k * 128:k * 128 + rows], in_=src_fn(k), identity=identb[:rows, :rows]),
                 r=[src_key, 'identb'], w=[bkey], signal=(k == n - 1))
        if rows == 128:
            src = bv[:, 0:n * 128]
        else:
            src = bv[:, 0:n * 128].rearrange("p (k s) -> p k s", s=128)[:, :, 0:rows]
        if evac == 'act':
            S.op('act', lambda e: e.copy(out=dst_ap, in_=src), r=[bkey], w=[dst_key])
        else:
            S.op('dve', lambda e: e.tensor_copy(out=dst_ap, in_=src), r=[bkey], w=[dst_key])

    with ExitStack() as ph:
        cT = sb(ph, "cT", [128, 8, 2], F32)
        cS = sb(ph, "cS", [128, 8, 2], F32)
        lbc = [sb(ph, f"lbc{s}", [128, 8, 128], BF16) for s in range(2)]
        wm = [sb(ph, f"wm{i}", [128, 8, 512], BF16) for i in range(2)]
        bm = [sb(ph, f"bm{i}", [128, 512], F32) for i in range(2)]
        mo = [sb(ph, f"mo{i}", [128, 512], F32) for i in range(4)]
        S.dma('sp', lambda q: q.dma_start(out=cT[:, :, :], in_=cvec.rearrange("p (k s) -> p k s", s=2)), w=['cT'])
        S.op('act', lambda e: e.activation(out=cS[:, :, :], in_=cT[:, :, :], func=AF.Silu), r=['cT'], w=['cS'])
        for s in range(2):
            S.op('dve', lambda e, s=s: e.tensor_copy(out=lbc[s][:, :, :], in_=cS[:, :, s:s + 1].to_broadcast([128, 8, 128])),
                 r=['cS'], w=[('lbc', s)])
        it = 0
        for l in range(nlayers if not skip_mods else 0):
            for ch in range(12):
                n0 = ch * 512
                slot = it % 2
                S.dma('pool', lambda q, slot=slot, l=l, n0=n0: q.dma_start(
                    out=wm[slot][:, :, :], in_=w_mod[l, :, n0:n0 + 512].rearrange("(ko ki) n -> ki ko n", ki=128)), w=[('wm', slot)])
                S.dma('sp', lambda q, slot=slot, l=l, n0=n0: q.dma_start(
                    out=bm[slot][:, :], in_=b_mod[l:l + 1, n0:n0 + 512].to_broadcast([128, 512])), w=[('bm', slot)])
                addc = 1.0 if (ch // 2) in (1, 4) else 0.0
                for s in range(2):
                    bk, bkey = nbank()
                    for k in range(8):
                        S.op('pe', lambda e, k=k, s=s, slot=slot, bk=bk: e.matmul(bk[:, :], lhsT=lbc[s][:, k, :], rhs=wm[slot][:, k, :],
                                                                               start=(k == 0), stop=(k == 7)),
                             r=[('lbc', s), ('wm', slot)], w=[bkey], signal=(k == 7))
                    ms = (it * 2 + s) % 4
                    S.op('dve', lambda e, bk=bk, ms=ms, slot=slot: e.scalar_tensor_tensor(
                        out=mo[ms][:, :], in0=bk[:, :], scalar=addc, in1=bm[slot][:, :], op0=ALU.add, op1=ALU.add),
                         r=[bkey, ('bm', slot)], w=[('mo', ms)])
                    S.dma('sp', lambda q, ms=ms, l=l, s=s, n0=n0: q.dma_start(out=modbc[l, s, :, n0:n0 + 512], in_=mo[ms][:, :]),
                          r=[('mo', ms)], w=[('modbc', l, s)])
                it += 1
        S.barrier()
    if stop_after == 'mods':
        return _finish(nc, S, es)

    for l in range(nlayers):
        last = (l == L - 1)
        tiles_b = list(range(NT)) if not last else list(range(NCTX, NT))
        CAPT = CAPL + (0 if last else CAPC)

        def xsrc(ti):
            if l == 0:
                return xctx[ti * 128:(ti + 1) * 128, :] if ti < NCTX else xlat[(ti - NCTX) * 128:(ti - NCTX + 1) * 128, :]
            return xs_d[ti * 128:(ti + 1) * 128, :]

        with ExitStack() as ph:
            w_in_sb = sb(ph, "w_in_sb", [128, 8, 2048], BF16)
            modA = [[sb(ph, f"modA{s}{j}", [128, D], F32) for j in range(2)] for s in range(2)]
            xin = [sb(ph, f"xinA{i}", [128, D], F32) for i in range(2)]
            ropet = [sb(ph, f"ropet{i}", [128, 128], F32) for i in range(2)]
            f32a_l = [sb(ph, "f32a", [128, D], F32) for _ in range(2)]
            hb_l = [sb(ph, "hb", [128, D], BF16) for _ in range(2)]
            hT_l = [sb(ph, "hT", [128, 8, 128], BF16) for _ in range(2)]
            ropeA_l = [sb(ph, "ropeA", [128, 512], F32) for _ in range(2)]
            ropeB_l = [sb(ph, "ropeB", [128, 512], F32) for _ in range(2)]
            qperm_l = [sb(ph, "qperm", [128, 512], BF16) for _ in range(2)]
            kst_l = [sb(ph, "kst", [128, 640], BF16) for _ in range(2)]
            sg_l = [sb(ph, "sg", [128, 256], F32) for _ in range(2)]
            chtok_l = [sb(ph, "chtok", [128, 256], BF16) for _ in range(2)]
            aout = [sb(ph, f"aout{i}", [128, AW], BF16) for i in range(2)]
            for i in range(2):
                S.op('pool', lambda e, i=i: e.memset(aout[i][:, VA:VA + 130], 1.0), w=[('aout', i)])
                S.op('pool', lambda e, i=i: e.memset(aout[i][:, VB:VB + 260], 1.0), w=[('aout', i)])
            for hf in range(2):
                S.dma('pool', lambda q, hf=hf: q.dma_start(out=w_in_sb[:, :, hf * 1024:(hf + 1) * 1024],
                                                        in_=w_in[l, :, hf * 1024:(hf + 1) * 1024].rearrange("(ko ki) n -> ki ko n", ki=128)),
                      w=['w_in_sb'])
            for s in range(2):
                for j in range(2):
                    S.dma('sp', lambda q, s=s, j=j: q.dma_start(out=modA[s][j][:, :], in_=modbc[l, s, :, j * D:(j + 1) * D]),
                          r=[('modbc', l, s)], w=[('modA', s, j)])

            for ti in range(NT):
                s = 0 if ti < NCTX else 1
                lat = ti >= NCTX
                xs_, xk = xin[ti % 2], ('xinA', ti % 2)
                f32a = f32a_l[ti % 2]
                k_f32a = ('f32a', ti % 2)
                hb = hb_l[ti % 2]
                k_hb = ('hb', ti % 2)
                hT = hT_l[ti % 2]
                k_hT = ('hT', ti % 2)
                ropeA = ropeA_l[ti % 2]
                k_ropeA = ('ropeA', ti % 2)
                ropeB = ropeB_l[ti % 2]
                k_ropeB = ('ropeB', ti % 2)
                qperm = qperm_l[ti % 2]
                k_qperm = ('qperm', ti % 2)
                kst = kst_l[ti % 2]
                k_kst = ('kst', ti % 2)
                sg = sg_l[ti % 2]
                k_sg = ('sg', ti % 2)
                chtok = chtok_l[ti % 2]
                k_chtok = ('chtok', ti % 2)
                ao, aok = aout[ti % 2], ('aout', ti % 2)
                rt, rk = ropet[ti % 2], ('ropet', ti % 2)
                S.dma('sp', lambda q, ti=ti, xs_=xs_: q.dma_start(out=xs_[:, :], in_=xsrc(ti)), r=[('xs_d',)] if l > 0 else [], w=[xk])
                if lat:
                    S.dma('sp', lambda q, ti=ti, rt=rt: q.dma_start(out=rt[:, :], in_=rope[ti - NCTX, :, :]), w=[rk])
                ln_stats(xs_, xk)
                ln_apply(f32a[:, :], k_f32a, xs_[:, :], xk)
                S.op('dve', lambda e, s=s: e.tensor_tensor(out=f32a[:, :], in0=f32a[:, :], in1=modA[s][1][:, :], op=ALU.mult),
                     r=[k_f32a, ('modA', s, 1)], w=[k_f32a])
                S.op('pool', lambda e, s=s: e.tensor_tensor(out=hb[:, :], in0=f32a[:, :], in1=modA[s][0][:, :], op=ALU.add),
                     r=[k_f32a, ('modA', s, 0)], w=[k_hb])
                transposes_to(hT[:, :, :].rearrange("p k t -> p (k t)"), k_hT, lambda k: hb[:, k * 128:(k + 1) * 128], 8, src_key=k_hb)
                ub = []
                for n in range(4):
                    bk, bkey = nbank()
                    for k in range(8):
                        S.op('pe', lambda e, k=k, n=n, bk=bk: e.matmul(bk[:, :], lhsT=hT[:, k, :], rhs=w_in_sb[:, k, n * 512:(n + 1) * 512],
                                                                     start=(k == 0), stop=(k == 7)),
                             r=[k_hT, 'w_in_sb'], w=[bkey], signal=(k == 7))
                    ub.append((bk, bkey))
                b0, k0 = ub[0]
                qp_v = qperm[:, :].rearrange("p (g r d) -> p r g d", g=4, r=2, d=64)
                if lat:
                    S.op('dve', lambda e, b0=b0, rt=rt: e.tensor_tensor(
                        out=ropeA[:, :].rearrange("p (h d) -> p h d", d=64), in0=b0[:, :].rearrange("p (h d) -> p h d", d=64),
                        in1=rt[:, 0:64].unsqueeze(1).to_broadcast([128, 8, 64]), op=ALU.mult), r=[k0, rk], w=[k_ropeA])
                    for half in range(2):
                        S.op('dve', lambda e, b0=b0, rt=rt, half=half: e.tensor_tensor(
                            out=ropeB[:, :].rearrange("p (h r f d) -> p h r f d", h=8, r=2, f=2, d=16)[:, :, :, half, :],
                            in0=b0[:, :].rearrange("p (h r f d) -> p h r f d", h=8, r=2, f=2, d=16)[:, :, :, 1 - half, :],
                            in1=rt[:, 64:128].rearrange("p (r f d) -> p r f d", r=2, f=2, d=16)[:, :, half, :].unsqueeze(1).to_broadcast([128, 8, 2, 16]),
                            op=ALU.mult), r=[k0, rk], w=[k_ropeB])
                    S.op('pool', lambda e: e.tensor_tensor(out=qp_v, in0=ropeA[:, :].rearrange("p (r g d) -> p r g d", r=2, g=4, d=64),
                                                          in1=ropeB[:, :].rearrange("p (r g d) -> p r g d", r=2, g=4, d=64), op=ALU.add),
                         r=[k_ropeA, k_ropeB], w=[k_qperm])
                else:
                    S.op('act', lambda e, b0=b0: e.copy(out=qp_v, in_=b0[:, :].rearrange("p (r g d) -> p r g d", r=2, g=4, d=64)),
                         r=[k0], w=[k_qperm])
                bk, bkey = nbank()
                bv = bk[:, :].bitcast(BF16)
                for k in range(4):
                    S.op('pe', lambda e, k=k, bv=bv: e.transpose(out=bv[:, k * 128:(k + 1) * 128], in_=qperm[:, k * 128:(k + 1) * 128], identity=identb[:, :]),
                         r=[k_qperm, 'identb'], w=[bkey], signal=(k == 3))
                if os.environ.get('EVAC', 'mask') == 'mask':
                    S.op('act', lambda e, bv=bv, ao=ao: e.activation(out=ao[:, QA:QA + 512], in_=bv[:, 0:512], func=AF.Copy, scale=pm0), r=[bkey, 'cst'], w=[aok])
                    S.op('dve', lambda e, bv=bv, ao=ao: e.tensor_scalar(out=ao[:, QA + 512:QA + 1024], in0=bv[:, 0:512], scalar1=pm1, scalar2=None, op0=ALU.mult),
                         r=[bkey, 'cst'], w=[aok])
                else:
                    S.op('act', lambda e, bv=bv, ao=ao: e.copy(out=ao[:, QA:QA + 512], in_=bv[:, 0:512]), r=[bkey], w=[aok])
                    S.op('dve', lambda e, bv=bv, ao=ao: e.tensor_copy(out=ao[:, QA + 512:QA + 1024], in_=bv[:, 0:512]), r=[bkey], w=[aok])
                b1, k1 = ub[1]
                if lat:
                    S.op('dve', lambda e, b1=b1, rt=rt: e.tensor_tensor(
                        out=ropeA[:, 0:128].rearrange("p (h d) -> p h d", d=64), in0=b1[:, 0:128].rearrange("p (h d) -> p h d", d=64),
                        in1=rt[:, 0:64].unsqueeze(1).to_broadcast([128, 2, 64]), op=ALU.mult), r=[k1, rk], w=[k_ropeA])
                    for half in range(2):
                        S.op('dve', lambda e, b1=b1, rt=rt, half=half: e.tensor_tensor(
                            out=ropeB[:, 0:128].rearrange("p (h r f d) -> p h r f d", h=2, r=2, f=2, d=16)[:, :, :, half, :],
                            in0=b1[:, 0:128].rearrange("p (h r f d) -> p h r f d", h=2, r=2, f=2, d=16)[:, :, :, 1 - half, :],
                            in1=rt[:, 64:128].rearrange("p (r f d) -> p r f d", r=2, f=2, d=16)[:, :, half, :].unsqueeze(1).to_broadcast([128, 2, 2, 16]),
                            op=ALU.mult), r=[k1, rk], w=[k_ropeB])
                    S.op('pool', lambda e: e.tensor_tensor(out=kst[:, 0:128], in0=ropeA[:, 0:128], in1=ropeB[:, 0:128], op=ALU.add),
                         r=[k_ropeA, k_ropeB], w=[k_kst])
                else:
                    S.op('act', lambda e, b1=b1: e.copy(out=kst[:, 0:128], in_=b1[:, 0:128]), r=[k1], w=[k_kst])
                S.op('act', lambda e, b1=b1, ao=ao: e.copy(out=ao[:, VA:VA + 130].rearrange("p (h d) -> p h d", d=65)[:, :, 0:64],
                                                         in_=b1[:, 128:256].rearrange("p (h d) -> p h d", d=64)), r=[k1], w=[aok])
                S.op('act', lambda e, b1=b1: e.copy(out=kst[:, 128:384], in_=b1[:, 256:512]), r=[k1], w=[k_kst])
                b2, k2 = ub[2]
                S.op('dve', lambda e, b2=b2: e.tensor_copy(out=kst[:, 384:640], in_=b2[:, 0:256]), r=[k2], w=[k_kst])
                S.op('act', lambda e, b2=b2, ao=ao: e.copy(out=ao[:, VB:VB + 260].rearrange("p (h d) -> p h d", d=65)[:, :, 0:64],
                                                         in_=b2[:, 256:512].rearrange("p (h d) -> p h d", d=64)), r=[k2], w=[aok])
                order = [1, 2, 0, 3, 4]
                bk, bkey = nbank()
                bv = bk[:, :].bitcast(BF16)
                for j, blk in enumerate(order):
                    S.op('pe', lambda e, j=j, blk=blk, bv=bv: e.transpose(out=bv[:, j * 128:(j + 1) * 128], in_=kst[:, blk * 128:(blk + 1) * 128],
                                                                         identity=identb[:, :]), r=[k_kst, 'identb'], w=[bkey], signal=(j == 4))
                if os.environ.get('EVAC', 'mask') == 'mask':
                    S.op('act', lambda e, bv=bv, ao=ao: e.activation(out=ao[:, QB:QB + 256], in_=bv[:, 0:256], func=AF.Copy, scale=pm0), r=[bkey, 'cst'], w=[aok])
                    S.op('dve', lambda e, bv=bv, ao=ao: e.tensor_scalar(out=ao[:, QB + 256:QB + 512], in0=bv[:, 0:256], scalar1=pm1, scalar2=None, op0=ALU.mult),
                         r=[bkey, 'cst'], w=[aok])
                else:
                    S.op('act', lambda e, bv=bv, ao=ao: e.copy(out=ao[:, QB:QB + 256], in_=bv[:, 0:256]), r=[bkey], w=[aok])
                    S.op('dve', lambda e, bv=bv, ao=ao: e.tensor_copy(out=ao[:, QB + 256:QB + 512], in_=bv[:, 0:256]), r=[bkey], w=[aok])
                S.op('dve', lambda e, bv=bv, ao=ao: e.tensor_copy(out=ao[:, KA:KA + 128], in_=bv[:, 256:384]), r=[bkey], w=[aok])
                S.op('dve', lambda e, bv=bv, ao=ao: e.tensor_copy(out=ao[:, KB:KB + 256], in_=bv[:, 384:640]), r=[bkey], w=[aok])
                b3, k3 = ub[3]
                S.op('act', lambda e, b3=b3: e.activation(out=sg[:, :], in_=b3[:, 256:512], func=AF.Sigmoid), r=[k3], w=[k_sg])
                S.op('dve', lambda e, b3=b3: e.tensor_tensor(out=chtok[:, :], in0=b3[:, 0:256], in1=sg[:, :], op=ALU.mult), r=[k3, k_sg], w=[k_chtok])
                transposes_to(ao[:, CH:CH + 256], aok, lambda k: chtok[:, k * 128:(k + 1) * 128], 2, src_key=k_chtok, evac='dve')
                S.dma('sp', lambda q, ti=ti, ao=ao: q.dma_start(out=scrA[ti, :, 0:QW], in_=ao[:, 0:QW]), r=[aok], w=[('scrAq', ti)])
                S.dma('sp', lambda q, ti=ti, ao=ao: q.dma_start(out=scrA[ti, :, QW:AW], in_=ao[:, QW:AW]), r=[aok], w=[('scrA', ti)])
            S.barrier()
        if stop_after == f'A{l}':
            return _finish(nc, S, es)

        with ExitStack() as ph:
            w_out_sb = sb(ph, "w_out_sb", [128, 8, D], BF16)
            w_r_sb = sb(ph, "w_r_sb", [128, 8, NE], BF16)
            nat_sb = sb(ph, "nat_sb", [128, NCHB, 512], BF16)
            cwT = sb(ph, "cwT", [128, 2, 31], F32)
            cdiag = sb(ph, "cdiag", [128, 2, 31, 128], BF16)
            convb = sb(ph, "convb", [128, 2], F32)
            clng = sb(ph, "clng", [128, 256], F32)
            clnb = sb(ph, "clnb", [128, 256], F32)
            modB = [[sb(ph, f"modB{s}{j}", [128, D], F32) for j in range(3)] for s in range(2)]
            ln1g = sb(ph, "ln1g", [128, D], F32)
            ln1b = sb(ph, "ln1b", [128, D], F32)
            esink = sb(ph, "esink", [128, 8], F32)
            NQR, NKR, NCW = 3, 8, 4
            qring = [sb(ph, f"qring{i}", [128, QW], BF16) for i in range(NQR)]
            kvring = [sb(ph, f"kvring{i}", [128, KVW], BF16) for i in range(NKR)]
            kvctx = [sb(ph, f"kvctx{i}", [128, KVW], BF16) for i in range(NCTX)]
            chwin = [sb(ph, f"chwin{i}", [128, 2, 160], BF16) for i in range(NCW)]
            xin = [sb(ph, f"xinB{i}", [128, D], F32) for i in range(2)]
            pT = sb(ph, "pT", [128, 7, 512], BF16)
            btmp = [sb(ph, f"btmp{i}", [128, 512], F32) for i in range(2)]
            otok_l = [sb(ph, "otok", [128, D], BF16) for _ in range(2)]
            oT_l = [sb(ph, "oT", [128, 8, 128], BF16) for _ in range(2)]
            cvT_l = [sb(ph, "cvT", [128, 2, 128], F32) for _ in range(2)]
            cvn_l = [sb(ph, "cvn", [128, 256], F32) for _ in range(2)]
            f32b_l = [sb(ph, "f32b", [128, D], F32) for _ in range(2)]
            f32c_l = [sb(ph, "f32c", [128, D], F32) for _ in range(2)]
            h2row = [sb(ph, f"h2row{i}", [128, RW], BF16) for i in range(2)]
            h2T_l = [sb(ph, "h2T", [128, 8, 128], BF16) for _ in range(2)]
            den_l = [sb(ph, "den", [128, 4], F32) for _ in range(2)]
            rden_l = [sb(ph, "rden", [128, 4], F32) for _ in range(2)]
            ex_l = [sb(ph, "ex", [128, NE], F32) for _ in range(2)]
            ssum_l = [sb(ph, "ssum", [128, 1], F32) for _ in range(2)]

            for hf in range(1):
                S.dma('pool', lambda q: q.dma_start(out=w_out_sb[:, :, :], in_=w_out[l, :, :].rearrange("(ko ki) n -> ki ko n", ki=128)), w=['w_out_sb'])
            S.dma('pool', lambda q: q.dma_start(out=w_r_sb[:, :, :], in_=w_router[l, :, :].rearrange("(ko ki) n -> ki ko n", ki=128)), w=['w_r_sb'])
            for c3 in range(0, NCHB, 3):
                c4 = min(NCHB, c3 + 3)
                S.dma('pool', lambda q, c3=c3, c4=c4: q.dma_start(out=nat_sb[:, c3:c4, :], in_=natT[l, :, c3 * 512:c4 * 512].rearrange("p (a b) -> p a b", b=512)),
                      w=['nat_sb'])
            S.dma('sp', lambda q: q.dma_start(out=cwT[:, :, :], in_=conv_w[l, :, :].rearrange("c (cc j) -> c cc j", cc=2)), w=['cwT'])
            S.dma('sp', lambda q: q.dma_start(out=convb[:, :], in_=conv_b[l, :, :]), w=['convb'])
            S.dma('sp', lambda q: q.dma_start(out=clng[:, :], in_=conv_ln_g[l:l + 1, :].to_broadcast([128, 256])), w=['clng'])
            S.dma('sp', lambda q: q.dma_start(out=clnb[:, :], in_=conv_ln_b[l:l + 1, :].to_broadcast([128, 256])), w=['clnb'])
            S.dma('sp', lambda q: q.dma_start(out=ln1g[:, :], in_=ln1_g[l:l + 1, :].to_broadcast([128, D])), w=['ln1g'])
            S.dma('sp', lambda q: q.dma_start(out=ln1b[:, :], in_=ln1_b[l:l + 1, :].to_broadcast([128, D])), w=['ln1b'])
            S.dma('sp', lambda q: q.dma_start(out=esink[:, :], in_=a_sink[l:l + 1, :].to_broadcast([128, 8])), w=['esink'])
            S.op('act', lambda e: e.activation(out=esink[:, :], in_=esink[:, :], func=AF.Exp), r=['esink'], w=['esink'])
            for s in range(2):
                for j, chn in enumerate((2, 4, 3)):
                    S.dma('sp', lambda q, s=s, j=j, chn=chn: q.dma_start(out=modB[s][j][:, :], in_=modbc[l, s, :, chn * D:(chn + 1) * D]),
                          r=[('modbc', l, s)], w=[('modB', s, j)])
            for cc in range(2):
                for j in range(31):
                    S.op('pool', lambda e, cc=cc, j=j: e.tensor_scalar(out=cdiag[:, cc, j, :], in0=cst[:, 0:128], scalar1=cwT[:, cc, j:j + 1],
                                                                      scalar2=None, op0=ALU.mult), r=['cst', 'cwT'], w=['cdiag'])
            for c in range(NCTX):
                S.dma('sp', lambda q, c=c: q.dma_start(out=kvctx[c][:, :], in_=scrA[c, :, KA:CH]), r=[('scrA', c)], w=[('kvctx', c)])

            loaded = set()

            def load_tile(t):
                if t in loaded or t < 0 or t >= NT:
                    return
                loaded.add(t)
                if t >= NCTX:
                    S.dma('sp', lambda q: q.dma_start(out=kvring[t % NKR][:, :], in_=scrA[t, :, KA:CH]), r=[('scrA', t)], w=[('kvring', t % NKR)])

            def load_own(t):
                S.dma('sp', lambda q: q.dma_start(out=qring[t % NQR][:, :], in_=scrA[t, :, 0:QW]), r=[('scrAq', t)], w=[('qring', t % NQR)])
                cw, cwk = chwin[t % NCW], ('chwin', t % NCW)
                first = t in (0, NCTX)
                lastt = t in (NCTX - 1, NT - 1)
                S.dma('sp', lambda q: q.dma_start(out=cw[:, :, 16:144], in_=scrA[t, :, CH:CH + 256].rearrange("p (c n) -> p c n", c=2)),
                      r=[('scrA', t)], w=[cwk])
                if first:
                    S.op('pool', lambda e: e.memset(cw[:, :, 0:16], 0.0), w=[cwk])
                else:
                    S.dma('sp', lambda q: q.dma_start(out=cw[:, :, 0:16], in_=scrA[t - 1, :, CH:CH + 256].rearrange("p (c n) -> p c n", c=2)[:, :, 112:128]),
                          r=[('scrA', t - 1)], w=[cwk])
                if lastt:
                    S.op('pool', lambda e: e.memset(cw[:, :, 144:160], 0.0), w=[cwk])
                else:
                    S.dma('sp', lambda q: q.dma_start(out=cw[:, :, 144:160], in_=scrA[t + 1, :, CH:CH + 256].rearrange("p (c n) -> p c n", c=2)[:, :, 0:16]),
                          r=[('scrA', t + 1)], w=[cwk])
                S.dma('sp', lambda q: q.dma_start(out=xin[t % 2][:, :], in_=xsrc(t)), r=[('xs_d',)] if l > 0 else [], w=[('xinB', t % 2)])

            def kvbuf(t):
                if t < NCTX:
                    return kvctx[t], ('kvctx', t)
                return kvring[t % NKR], ('kvring', t % NKR)

            for ti in tiles_b:
                s = 0 if ti < NCTX else 1
                lat = ti >= NCTX
                i = ti - NCTX
                otok = otok_l[ti % 2]
                k_otok = ('otok', ti % 2)
                oT = oT_l[ti % 2]
                k_oT = ('oT', ti % 2)
                cvT = cvT_l[ti % 2]
                k_cvT = ('cvT', ti % 2)
                cvn = cvn_l[ti % 2]
                k_cvn = ('cvn', ti % 2)
                f32b = f32b_l[ti % 2]
                k_f32b = ('f32b', ti % 2)
                f32c = f32c_l[ti % 2]
                k_f32c = ('f32c', ti % 2)
                h2T = h2T_l[ti % 2]
                k_h2T = ('h2T', ti % 2)
                den = den_l[ti % 2]
                k_den = ('den', ti % 2)
                rden = rden_l[ti % 2]
                k_rden = ('rden', ti % 2)
                ex = ex_l[ti % 2]
                k_ex = ('ex', ti % 2)
                ssum = ssum_l[ti % 2]
                k_ssum = ('ssum', ti % 2)
                for t2 in range(ti - 2, ti + 4):
                    if lat and t2 >= NCTX:
                        load_tile(t2)
                load_own(ti)
                qr, qk = qring[ti % NQR], ('qring', ti % NQR)
                cw, cwk = chwin[ti % NCW], ('chwin', ti % NCW)
                xs_, xk = xin[ti % 2], ('xinB', ti % 2)
                hr, hk = h2row[ti % 2], ('h2row', ti % 2)

                if stop_after == f'B{l}:setup':
                    break
                if lat:
                    chunksA = []
                    if i - 1 >= 0:
                        chunksA.append((ti - 1, 0))
                    chunksA.append((ti, None))
                    if i + 1 < NLAT:
                        chunksA.append((ti + 1, 1))
                    chunksA += [(0, None), (1, None)]
                else:
                    chunksA = [(0, None), (1, None)]
                for grp in range(2):
                    p0 = 64 * grp
                    for ci, (ct, mk) in enumerate(chunksA):
                        kb_, kk = kvbuf(ct)
                        bk, bkey = nbank()
                        S.op('pe', lambda e, bk=bk, kb_=kb_: e.matmul(bk[:, :], lhsT=kb_[:, 0:128], rhs=qr[:, QA + grp * 512:QA + (grp + 1) * 512],
                                                                     start=True, stop=True), r=[kk, qk], w=[bkey])
                        S.op('act', lambda e, bk=bk, ci=ci: e.activation(out=pT[:, ci, :], in_=bk[:, :], func=AF.Exp, scale=SCALE),
                             r=[bkey], w=[('pT', ci)])
                        if mk is not None:
                            S.op('pool', lambda e, ci=ci, mk=mk: e.tensor_tensor(
                                out=pT[:, ci, :].rearrange("p (g t) -> p g t", g=4), in0=pT[:, ci, :].rearrange("p (g t) -> p g t", g=4),
                                in1=masklr[:, mk, :].unsqueeze(1).to_broadcast([128, 4, 128]), op=ALU.mult), r=[('pT', ci), 'masklr'], w=[('pT', ci)])
                    ob, obk = nbank()
                    nchk = len(chunksA)
                    for g in range(4):
                        for ci, (ct, mk) in enumerate(chunksA):
                            kb_, kk = kvbuf(ct)
                            S.op('pe', lambda e, g=g, ci=ci, kb_=kb_, ob=ob: e.matmul(
                                ob[:, g * 65:(g + 1) * 65], lhsT=pT[:, ci, g * 128:(g + 1) * 128],
                                rhs=kb_[:, VA - KA + grp * 65:VA - KA + (grp + 1) * 65], start=(ci == 0), stop=(ci == nchk - 1)),
                                 r=[('pT', ci), kk], w=[obk], signal=(g == 3 and ci == nchk - 1))
                    obv = ob[:, 0:260].rearrange("p (g d) -> p g d", d=65)
                    S.op('dve', lambda e, obv=obv: e.tensor_tensor(out=den[:, :], in0=obv[:, :, 64], in1=esink[:, grp * 4:(grp + 1) * 4], op=ALU.add),
                         r=[obk, 'esink'], w=[k_den])
                    S.op('dve', lambda e: e.reciprocal(out=rden[:, :], in_=den[:, :]), r=[k_den], w=[k_rden])
                    S.op('dve', lambda e, obv=obv: e.tensor_tensor(
                        out=otok[:, grp * 256:(grp + 1) * 256].rearrange("p (g d) -> p g d", d=64), in0=obv[:, :, 0:64],
                        in1=rden[:, :].unsqueeze(2).to_broadcast([128, 4, 64]), op=ALU.mult), r=[obk, k_rden], w=[k_otok])

                if stop_after == f'B{l}:attA':
                    break
                if lat:
                    if NLAT >= 5 and 2 <= i <= NLAT - 3:
                        chunksB = [(ti + d - 2, d) for d in range(5)]
                    elif i == 0:
                        chunksB = [(NCTX + j, 5 + j) for j in range(4)]
                    elif i == 1:
                        chunksB = [(NCTX + j, 9 + j) for j in range(4)]
                    elif i == NLAT - 2:
                        chunksB = [(NCTX + NLAT - 4 + j, 13 + j) for j in range(4)]
                    else:
                        chunksB = [(NCTX + NLAT - 4 + j, 17 + j) for j in range(4)]
                    chunksB += [(0, None), (1, None)]
                else:
                    chunksB = [(0, None), (1, None)]
                for ci, (ct, ent) in enumerate(chunksB):
                    kb_, kk = kvbuf(ct)
                    bk, bkey = nbank()
                    for h in range(4):
                        p, m = h // 2, h % 2
                        S.op('pe', lambda e, bk=bk, kb_=kb_, h=h, p=p, m=m: e.matmul(
                            bk[:, h * 128:(h + 1) * 128], lhsT=kb_[:, KB - KA + p * 128:KB - KA + (p + 1) * 128],
                            rhs=qr[:, QB + m * 256 + p * 128:QB + m * 256 + (p + 1) * 128], start=True, stop=True),
                             r=[kk, qk], w=[bkey], signal=(h == 3))
                    if ent is not None:
                        bt, btk = btmp[ci % 2], ('btmp', ci % 2)
                        S.op('dve', lambda e, bk=bk, bt=bt, ent=ent: e.scalar_tensor_tensor(
                            out=bt[:, :], in0=bk[:, :], scalar=SCALE, in1=nat_sb[:, ent, :], op0=ALU.mult, op1=ALU.add),
                             r=[bkey, 'nat_sb'], w=[btk])
                        S.op('act', lambda e, bt=bt, ci=ci: e.activation(out=pT[:, ci, :], in_=bt[:, :], func=AF.Exp), r=[btk], w=[('pT', ci)])
                    else:
                        S.op('act', lambda e, bk=bk, ci=ci: e.activation(out=pT[:, ci, :], in_=bk[:, :], func=AF.Exp, scale=SCALE),
                             r=[bkey], w=[('pT', ci)])
                ob, obk = nbank()
                nchk = len(chunksB)
                for h in range(4):
                    for ci, (ct, ent) in enumerate(chunksB):
                        kb_, kk = kvbuf(ct)
                        S.op('pe', lambda e, h=h, ci=ci, kb_=kb_, ob=ob: e.matmul(
                            ob[:, h * 65:(h + 1) * 65], lhsT=pT[:, ci, h * 128:(h + 1) * 128],
                            rhs=kb_[:, VB - KA + h * 65:VB - KA + (h + 1) * 65], start=(ci == 0), stop=(ci == nchk - 1)),
                             r=[('pT', ci), kk], w=[obk], signal=(h == 3 and ci == nchk - 1))
                obv = ob[:, 0:260].rearrange("p (g d) -> p g d", d=65)
                S.op('dve', lambda e, obv=obv: e.reciprocal(out=rden[:, :], in_=obv[:, :, 64]), r=[obk], w=[k_rden])
                S.op('dve', lambda e, obv=obv: e.tensor_tensor(
                    out=otok[:, 512:768].rearrange("p (g d) -> p g d", d=64), in0=obv[:, :, 0:64],
                    in1=rden[:, :].unsqueeze(2).to_broadcast([128, 4, 64]), op=ALU.mult), r=[obk, k_rden], w=[k_otok])

                if stop_after == f'B{l}:attB':
                    break
                bk, bkey = nbank()
                for cc in range(2):
                    for j in range(31):
                        S.op('pe', lambda e, bk=bk, cc=cc, j=j: e.matmul(bk[:, cc * 128:(cc + 1) * 128], lhsT=cdiag[:, cc, j, :],
                                                                        rhs=cw[:, cc, 1 + j:1 + j + 128], start=(j == 0), stop=(j == 30)),
                             r=['cdiag', cwk], w=[bkey], signal=(cc == 1 and j == 30))
                for cc in range(2):
                    S.op('act', lambda e, bk=bk, cc=cc: e.activation(out=cvT[:, cc, :], in_=bk[:, cc * 128:(cc + 1) * 128], func=AF.Identity,
                                                                    bias=convb[:, cc:cc + 1], scale=1.0), r=[bkey, 'convb'], w=[k_cvT])
                bk2, bkey2 = nbank()
                for cc in range(2):
                    S.op('pe', lambda e, bk2=bk2, cc=cc: e.transpose(out=bk2[:, cc * 128:(cc + 1) * 128], in_=cvT[:, cc, :], identity=identf),
                         r=[k_cvT, 'cst'], w=[bkey2], signal=(cc == 1))
                ln_stats(bk2, bkey2, width=256)
                ln_apply(cvn[:, :], k_cvn, bk2[:, 0:256], bkey2)
                S.op('dve', lambda e: e.tensor_tensor(out=cvn[:, :], in0=cvn[:, :], in1=clng[:, :], op=ALU.mult), r=[k_cvn, 'clng'], w=[k_cvn])
                S.op('pool', lambda e: e.tensor_tensor(out=cvn[:, :], in0=cvn[:, :], in1=clnb[:, :], op=ALU.add), r=[k_cvn, 'clnb'], w=[k_cvn])
                S.op('act', lambda e: e.activation(out=otok[:, 768:1024], in_=cvn[:, :], func=AF.Silu), r=[k_cvn], w=[k_otok])

                if dbg:
                    S.dma('sp', lambda q: q.dma_start(out=otok_d[ti * 128:(ti + 1) * 128, :], in_=otok[:, :]), r=[k_otok], w=[('dbg_otok', ti)])

                if stop_after == f'B{l}:conv':
                    break
                transposes_to(oT[:, :, :].rearrange("p k t -> p (k t)"), k_oT, lambda k: otok[:, k * 128:(k + 1) * 128], 8, src_key=k_otok)
                for n in range(2):
                    bk, bkey = nbank()
                    for k in range(8):
                        S.op('pe', lambda e, bk=bk, k=k, n=n: e.matmul(bk[:, :], lhsT=oT[:, k, :], rhs=w_out_sb[:, k, n * 512:(n + 1) * 512],
                                                                     start=(k == 0), stop=(k == 7)), r=[k_oT, 'w_out_sb'], w=[bkey], signal=(k == 7))
                    S.op('dve', lambda e, bk=bk, n=n: e.tensor_tensor(out=f32b[:, n * 512:(n + 1) * 512], in0=bk[:, :],
                                                                     in1=modB[s][0][:, n * 512:(n + 1) * 512], op=ALU.mult),
                         r=[bkey, ('modB', s, 0)], w=[k_f32b])
                S.op('dve', lambda e: e.scalar_tensor_tensor(out=f32b[:, :], in0=xs_[:, :], scalar=ALPHA, in1=f32b[:, :], op0=ALU.mult, op1=ALU.add),
                     r=[xk, k_f32b], w=[k_f32b])
                ln_stats(f32b, k_f32b)
                ln_apply(f32c[:, :], k_f32c, f32b[:, :], k_f32b)
                S.op('dve', lambda e: e.tensor_tensor(out=f32c[:, :], in0=f32c[:, :], in1=ln1g[:, :], op=ALU.mult), r=[k_f32c, 'ln1g'], w=[k_f32c])
                S.op('pool', lambda e: e.tensor_tensor(out=f32c[:, :], in0=f32c[:, :], in1=ln1b[:, :], op=ALU.add), r=[k_f32c, 'ln1b'], w=[k_f32c])
                S.dma('sp', lambda q: q.dma_start(out=x1s[ti * 128:(ti + 1) * 128, :], in_=f32c[:, :]), r=[k_f32c], w=[('x1s', ti)])
                if stop_after == f'B{l}:proj':
                    break
                ln_stats(f32c, k_f32c)
                ln_apply(f32b[:, :], k_f32b, f32c[:, :], k_f32c)
                S.op('dve', lambda e: e.tensor_tensor(out=f32b[:, :], in0=f32b[:, :], in1=modB[s][1][:, :], op=ALU.mult),
                     r=[k_f32b, ('modB', s, 1)], w=[k_f32b])
                S.op('pool', lambda e: e.tensor_tensor(out=hr[:, 0:1024], in0=f32b[:, :], in1=modB[s][2][:, :], op=ALU.add),
                     r=[k_f32b, ('modB', s, 2)], w=[hk])
                transposes_to(h2T[:, :, :].rearrange("p k t -> p (k t)"), k_h2T, lambda k: hr[:, k * 128:(k + 1) * 128], 8, src_key=hk)
                bk, bkey = nbank()
                for k in range(8):
                    S.op('pe', lambda e, bk=bk, k=k: e.matmul(bk[:, 0:NE], lhsT=h2T[:, k, :], rhs=w_r_sb[:, k, :], start=(k == 0), stop=(k == 7)),
                         r=[k_h2T, 'w_r_sb'], w=[bkey], signal=(k == 7))
                S.op('act', lambda e, bk=bk: e.activation(out=ex[:, :], in_=bk[:, 0:NE], func=AF.Exp, accum_out=ssum[:, 0:1]), r=[bkey], w=[k_ex, k_ssum])
                S.op('dve', lambda e: e.reciprocal(out=ssum[:, :], in_=ssum[:, :]), r=[k_ssum], w=[k_ssum])
                S.op('dve', lambda e: e.tensor_scalar(out=aff_all[:, ti, :], in0=ex[:, :], scalar1=ssum[:, 0:1], scalar2=None, op0=ALU.mult),
                     r=[k_ex, k_ssum], w=[('aff', ti)])
                S.op('dve', lambda e: e.tensor_copy(out=hr[:, 1026:1042], in_=aff_all[:, ti, :]), r=[('aff', ti)], w=[hk])
                S.op('dve', lambda e: e.tensor_tensor(out=hr[:, 1042:1058], in0=aff_all[:, ti, :], in1=hr[:, 1026:1042], op=ALU.subtract),
                     r=[('aff', ti), hk], w=[hk])
                S.op('dve', lambda e: e.tensor_scalar(out=hr[:, 1024:1025], in0=iotap, scalar1=0.0, scalar2=float(ti), op0=ALU.mult, op1=ALU.add),
                     r=['cst'], w=[hk])
                S.op('dve', lambda e: e.tensor_copy(out=hr[:, 1025:1026], in_=iotap), r=['cst'], w=[hk])
                S.dma('sp', lambda q: q.dma_start(out=h2s[ti * 128:(ti + 1) * 128, :], in_=hr[:, :]), r=[hk], w=[('h2s', ti)])
            S.barrier()
        if stop_after is not None and stop_after.startswith(f'B{l}'):
            return _finish(nc, S, es)

        NTB = len(tiles_b)
        t0b = tiles_b[0]
        with ExitStack() as ph:
            cmpb = sb(ph, "cmpb", [128, NT, NE], BF16)
            lo = sb(ph, "lo", [128, 32], F32)
            hi = sb(ph, "hi", [128, 32], F32)
            mid = sb(ph, "mid", [128, 32], F32)
            kvec = sb(ph, "kvec", [128, 32], F32)
            cntp = sb(ph, "cntp", [128, 32], BF16)
            ge = sb(ph, "ge", [128, 32], F32)
            gm = sb(ph, "gm", [128, 32], F32)
            posf = sb(ph, "posf", [128, NT, NE], F32)
            tot = sb(ph, "tot", [128, NT, NE], F32)
            base = sb(ph, "base", [128, NT + 1, NE], F32)
            sel = sb(ph, "sel", [128, NT, NE], F32)
            posi = sb(ph, "posi", [128, NT, NE], I32)
            affk = [('aff', t) for t in range(NT)]
            S.op('dve', lambda e: e.memset(lo[:, :], 0.0), w=['lo'])
            S.op('dve', lambda e: e.memset(hi[:, :], 1.0), w=['hi'])
            S.op('dve', lambda e: e.memset(kvec[:, 0:16], float(CAPL)), w=['kvec'])
            S.op('dve', lambda e: e.memset(kvec[:, 16:32], float(CAPC)), w=['kvec'])
            S.op('dve', lambda e: e.memset(cntp[:, :], 0.0), w=['cntp'])
            if last:
                S.op('dve', lambda e: e.memset(cmpb[:, 0:NCTX, :], 0.0), w=['cmpb'])
            lat_aff = aff_all[:, NCTX:NT, :]
            ctx_aff = aff_all[:, 0:NCTX, :]

            def compare(thr):
                S.op('dve', lambda e: e.tensor_tensor(out=cmpb[:, NCTX:NT, :], in0=lat_aff, in1=thr[:, 0:16].unsqueeze(1).to_broadcast([128, NLAT, NE]),
                                                      op=ALU.is_ge), r=affk + ['thr'], w=['cmpb'])
                if not last:
                    S.op('dve', lambda e: e.tensor_tensor(out=cmpb[:, 0:NCTX, :], in0=ctx_aff, in1=thr[:, 16:32].unsqueeze(1).to_broadcast([128, NCTX, NE]),
                                                          op=ALU.is_ge), r=affk + ['thr'], w=['cmpb'])

            for itn in range(30):
                S.op('dve', lambda e: e.tensor_tensor(out=mid[:, :], in0=lo[:, :], in1=hi[:, :], op=ALU.add), r=['lo', 'hi'], w=['thr'])
                S.op('dve', lambda e: e.tensor_scalar(out=mid[:, :], in0=mid[:, :], scalar1=0.5, scalar2=None, op0=ALU.mult), r=['thr'], w=['thr'])
                compare(mid)
                S.op('dve', lambda e: e.tensor_reduce(out=cntp[:, 0:16], in_=cmpb[:, NCTX:NT, :].rearrange("p t e -> p e t"), axis=AX.X, op=ALU.add),
                     r=['cmpb'], w=['cntp'])
                if not last:
                    S.op('dve', lambda e: e.tensor_reduce(out=cntp[:, 16:32], in_=cmpb[:, 0:NCTX, :].rearrange("p t e -> p e t"), axis=AX.X, op=ALU.add),
                         r=['cmpb'], w=['cntp'])
                bk, bkey = nbank()
                S.op('pe', lambda e, bk=bk: e.matmul(bk[:, 0:32], lhsT=onesb[:, :], rhs=cntp[:, :], start=True, stop=True), r=['onesb', 'cntp'], w=[bkey])
                S.op('dve', lambda e, bk=bk: e.tensor_tensor(out=ge[:, :], in0=bk[:, 0:32], in1=kvec[:, :], op=ALU.is_ge), r=[bkey, 'kvec'], w=['ge'])
                S.op('dve', lambda e: e.tensor_tensor(out=gm[:, :], in0=ge[:, :], in1=mid[:, :], op=ALU.mult), r=['ge', 'thr'], w=['gm'])
                S.op('dve', lambda e: e.tensor_tensor(out=lo[:, :], in0=lo[:, :], in1=gm[:, :], op=ALU.max), r=['lo', 'gm'], w=['lo'])
                S.op('dve', lambda e: e.scalar_tensor_tensor(out=gm[:, :], in0=ge[:, :], scalar=2.0, in1=mid[:, :], op0=ALU.mult, op1=ALU.add),
                     r=['ge', 'thr', 'gm'], w=['gm'])
                S.op('dve', lambda e: e.tensor_tensor(out=hi[:, :], in0=hi[:, :], in1=gm[:, :], op=ALU.min), r=['hi', 'gm'], w=['hi'])
            S.op('dve', lambda e: e.tensor_copy(out=mid[:, :], in_=lo[:, :]), r=['lo'], w=['thr'])
            compare(mid)
            cflat = cmpb[:, :, :].rearrange("p t e -> p (t e)")
            pflat = posf[:, :, :].rearrange("p t e -> p (t e)")
            tflat = tot[:, :, :].rearrange("p t e -> p (t e)")
            ncol = NT * NE
            for c0 in range(0, ncol, 512):
                cs = min(512, ncol - c0)
                bk, bkey = nbank()
                S.op('pe', lambda e, bk=bk, c0=c0, cs=cs: e.matmul(bk[:, 0:cs], lhsT=triub[:, :], rhs=cflat[:, c0:c0 + cs], start=True, stop=True),
                     r=['triub', 'cmpb'], w=[bkey])
                S.op('act', lambda e, bk=bk, c0=c0, cs=cs: e.copy(out=pflat[:, c0:c0 + cs], in_=bk[:, 0:cs]), r=[bkey], w=['posf'])
                bk, bkey = nbank()
                S.op('pe', lambda e, bk=bk, c0=c0, cs=cs: e.matmul(bk[:, 0:cs], lhsT=onesb[:, :], rhs=cflat[:, c0:c0 + cs], start=True, stop=True),
                     r=['onesb', 'cmpb'], w=[bkey])
                S.op('act', lambda e, bk=bk, c0=c0, cs=cs: e.copy(out=tflat[:, c0:c0 + cs], in_=bk[:, 0:cs]), r=[bkey], w=['tot'])
            S.op('dve', lambda e: e.memset(base[:, 0, :], float(CAPL)), w=['base'])
            S.op('dve', lambda e: e.memset(base[:, NCTX, :], 0.0), w=['base'])
            for t in range(NT):
                if t == NCTX - 1:
                    continue
                S.op('dve', lambda e, t=t: e.tensor_tensor(out=base[:, t + 1, :], in0=base[:, t, :], in1=tot[:, t, :], op=ALU.add),
                     r=['base', 'tot'], w=['base'])
            S.op('dve', lambda e: e.tensor_tensor(out=posf[:, :, :], in0=posf[:, :, :], in1=base[:, 0:NT, :], op=ALU.add), r=['posf', 'base'], w=['posf'])
            S.op('dve', lambda e: e.scalar_tensor_tensor(out=sel[:, NCTX:NT, :], in0=posf[:, NCTX:NT, :], scalar=float(CAPL), in1=cmpb[:, NCTX:NT, :],
                                                         op0=ALU.is_lt, op1=ALU.mult), r=['posf', 'cmpb'], w=['sel'])
            S.op('dve', lambda e: e.scalar_tensor_tensor(out=sel[:, 0:NCTX, :], in0=posf[:, 0:NCTX, :], scalar=float(CAPL + CAPC), in1=cmpb[:, 0:NCTX, :],
                                                         op0=ALU.is_lt, op1=ALU.mult), r=['posf', 'cmpb'], w=['sel'])
            S.op('dve', lambda e: e.scalar_tensor_tensor(out=posf[:, :, :], in0=posf[:, :, :], scalar=-BIG, in1=sel[:, :, :], op0=ALU.add, op1=ALU.mult),
                 r=['posf', 'sel'], w=['posf'])
            S.op('dve', lambda e: e.tensor_scalar(out=posi[:, :, :], in0=posf[:, :, :], scalar1=BIG, scalar2=None, op0=ALU.add), r=['posf'], w=['posi'])

            h2ld = [sb(ph, f"h2ld{i}", [128, RW], BF16) for i in range(3)]
            breg = nc.gpsimd.to_reg(CAPT - 1)
            for n, ti in enumerate(tiles_b):
                hl, hlk = h2ld[n % 3], ('h2ld', n % 3)
                S.dma('sp', lambda q, ti=ti, hl=hl: q.dma_start(out=hl[:, :], in_=h2s[ti * 128:(ti + 1) * 128, :]), r=[('h2s', ti)], w=[hlk])
                for ex_ in range(NE):
                    S.dma('pool', lambda q, ti=ti, ex_=ex_, hl=hl: q.indirect_dma_start(
                        out=Xs[ex_][:, :], out_offset=bass.IndirectOffsetOnAxis(ap=posi[:, ti, ex_:ex_ + 1], axis=0),
                        in_=hl[:, :], in_offset=None, bounds_check=breg, oob_is_err=False), r=[hlk, 'posi'], w=[('Xs', ex_)])
            S.barrier()
        if stop_after == f'R{l}':
            return _finish(nc, S, es)

        NST = (CAPT + 127) // 128
        CAPP = NST * 128
        NSL = 3 if CAPP % 3 == 0 and CAPP // 3 <= 512 else (CAPP + 511) // 512
        SLW = CAPP // NSL
        assert SLW * NSL == CAPP and SLW <= 512
        with ExitStack() as ph:
            wg = [sb(ph, f"wg{i}", [128, 8, D], BF16) for i in range(2)]
            wu = [sb(ph, f"wu{i}", [128, 8, D], BF16) for i in range(2)]
            wd = [sb(ph, f"wd{i}", [128, 8, D], BF16) for i in range(2)]
            xsb = sb(ph, "xsb", [128, NST, RW], BF16)
            XT = sb(ph, "XT", [128, 8, NST * 128], BF16)
            hidT = sb(ph, "hidT", [128, 8, NST * 128], BF16)
            sgt = [sb(ph, f"sgt{i}", [128, 512], F32) for i in range(2)]
            ysb = [sb(ph, f"ysb{i}", [128, D], F32) for i in range(3)]
            gcol = sb(ph, "gcol", [128, NST], F32)
            idxi = sb(ph, "idxi", [128, NST], I32)
            S.op('pool', lambda e: e.memset(xsb[:, NST - 1, :], 0.0), w=['xsb'])
            zt = ysb[0]
            S.op('pool', lambda e: e.memset(zt[:, :], 0.0), w=[('ysb', 0)])
            ztoks = []
            for ti in tiles_b:
                ztoks.append(S.dma('sp', lambda q, ti=ti: q.dma_start(out=macc[ti * 128:(ti + 1) * 128, :], in_=zt[:, :]),
                                   r=[('ysb', 0), ('macc_rd', ti)], w=[('macc_z', ti)]))
            prev_sc = list(ztoks)

            def load_w(ex_):
                sl = ex_ % 2
                for wsb, wdr, nm in ((wg, w_gate, 'wg'), (wu, w_up, 'wu'), (wd, w_down, 'wd')):
                    S.dma('pool', lambda q, wsb=wsb, wdr=wdr: q.dma_start(
                        out=wsb[sl][:, :, :], in_=wdr[l, ex_, :, :].rearrange("(ko ki) n -> ki ko n", ki=128)), w=[(nm, sl)])

            load_w(0)
            ycount = 0
            for ex_ in range(NE):
                sl = ex_ % 2
                if ex_ + 1 < NE:
                    load_w(ex_ + 1)
                nfull = CAPT // 128
                rem = CAPT - nfull * 128
                S.dma('sp', lambda q, ex_=ex_: q.dma_start(out=xsb[:, 0:nfull, :], in_=Xs[ex_][0:nfull * 128, :].rearrange("(s p) w -> p s w", p=128)),
                      r=[('Xs', ex_)], w=['xsb'])
                if rem:
                    S.dma('sp', lambda q, ex_=ex_: q.dma_start(out=xsb[0:rem, nfull, :], in_=Xs[ex_][nfull * 128:CAPT, :]), r=[('Xs', ex_)], w=['xsb'])
                S.op('dve', lambda e, ex_=ex_: e.tensor_tensor(out=gcol[:, 0:nfull], in0=xsb[:, 0:nfull, 1026 + ex_], in1=xsb[:, 0:nfull, 1042 + ex_], op=ALU.add),
                     r=['xsb'], w=['gcol'])
                S.op('dve', lambda e: e.scalar_tensor_tensor(out=idxi[:, 0:nfull], in0=xsb[:, 0:nfull, 1024], scalar=128.0, in1=xsb[:, 0:nfull, 1025],
                                                             op0=ALU.mult, op1=ALU.add), r=['xsb'], w=['idxi'])
                if rem:
                    S.op('dve', lambda e, ex_=ex_: e.tensor_tensor(out=gcol[0:rem, nfull:nfull + 1], in0=xsb[0:rem, nfull, 1026 + ex_:1027 + ex_],
                                                                  in1=xsb[0:rem, nfull, 1042 + ex_:1043 + ex_], op=ALU.add), r=['xsb'], w=['gcol'])
                    S.op('dve', lambda e: e.scalar_tensor_tensor(out=idxi[0:rem, nfull:nfull + 1], in0=xsb[0:rem, nfull, 1024:1025], scalar=128.0,
                                                                 in1=xsb[0:rem, nfull, 1025:1026], op0=ALU.mult, op1=ALU.add), r=['xsb'], w=['idxi'])
                for st in range(NST):
                    rows = 128 if st < nfull else rem
                    transposes_to(XT[:, :, st * 128:(st + 1) * 128], 'XT', lambda k, st=st: xsb[:, st, k * 128:(k + 1) * 128], 8,
                                  src_key='xsb', evac=('act' if st % 2 == 0 else 'dve'))
                for fc in range(8):
                    for sn in range(NSL):
                        n0 = sn * SLW
                        bg, bgk = nbank()
                        for k in range(8):
                            S.op('pe', lambda e, bg=bg, k=k, fc=fc, n0=n0: e.matmul(bg[:, 0:SLW], lhsT=wg[sl][:, k, fc * 128:(fc + 1) * 128],
                                                                                   rhs=XT[:, k, n0:n0 + SLW], start=(k == 0), stop=(k == 7)),
                                 r=[('wg', sl), 'XT'], w=[bgk], signal=(k == 7))
                        bu, buk = nbank()
                        for k in range(8):
                            S.op('pe', lambda e, bu=bu, k=k, fc=fc, n0=n0: e.matmul(bu[:, 0:SLW], lhsT=wu[sl][:, k, fc * 128:(fc + 1) * 128],
                                                                                   rhs=XT[:, k, n0:n0 + SLW], start=(k == 0), stop=(k == 7)),
                                 r=[('wu', sl), 'XT'], w=[buk], signal=(k == 7))
                        sgi = (fc * NSL + sn) % 2
                        S.op('act', lambda e, bg=bg, sgi=sgi: e.activation(out=sgt[sgi][:, 0:SLW], in_=bg[:, 0:SLW], func=AF.Silu), r=[bgk], w=[('sgt', sgi)])
                        S.op('dve', lambda e, bu=bu, sgi=sgi, fc=fc, n0=n0: e.tensor_tensor(out=hidT[:, fc, n0:n0 + SLW], in0=bu[:, 0:SLW], in1=sgt[sgi][:, 0:SLW],
                                                                                           op=ALU.mult), r=[buk, ('sgt', sgi)], w=['hidT'])
                cur_sc = []
                for st in range(NST):
                    rows = 128 if st < nfull else rem
                    yi = ycount % 3
                    ycount += 1
                    for half in range(2):
                        by, byk = nbank()
                        for fc in range(8):
                            S.op('pe', lambda e, by=by, fc=fc, st=st, rows=rows, half=half: e.matmul(
                                by[:, :], lhsT=hidT[:, fc, st * 128:(st + 1) * 128], rhs=wd[sl][:, fc, half * 512:(half + 1) * 512],
                                start=(fc == 0), stop=(fc == 7)), r=['hidT', ('wd', sl)], w=[byk], signal=(fc == 7))
                        if half == 0:
                            S.op('act', lambda e, by=by, yi=yi, st=st, rows=rows: e.activation(out=ysb[yi][0:rows, 0:512], in_=by[0:rows, :], func=AF.Copy,
                                                                                              scale=gcol[0:rows, st:st + 1]), r=[byk, 'gcol'], w=[('ysb', yi)])
                        else:
                            S.op('dve', lambda e, by=by, yi=yi, st=st, rows=rows: e.tensor_scalar(out=ysb[yi][0:rows, 512:1024], in0=by[0:rows, :],
                                                                                                 scalar1=gcol[0:rows, st:st + 1], scalar2=None, op0=ALU.mult),
                                 r=[byk, 'gcol'], w=[('ysb', yi)])
                    cur_sc.append(S.dma('pool', lambda q, yi=yi, st=st, rows=rows: q.indirect_dma_start(
                        out=macc[:, :], out_offset=bass.IndirectOffsetOnAxis(ap=idxi[0:rows, st:st + 1], axis=0),
                        in_=ysb[yi][0:rows, :], in_offset=None, compute_op=ALU.add), r=[('ysb', yi), 'idxi'], w=[('macc_sc', ex_, st)], extra=prev_sc))
                prev_sc = cur_sc
            S.barrier()
        if stop_after == f'M{l}':
            return _finish(nc, S, es)

        with ExitStack() as ph:
            g2 = [sb(ph, f"g2_{s}", [128, D], F32) for s in range(2)]
            ln2g = sb(ph, "ln2g", [128, D], F32)
            ln2b = sb(ph, "ln2b", [128, D], F32)
            xa = [sb(ph, f"xa{i}", [128, D], F32) for i in range(2)]
            xm = [sb(ph, f"xm{i}", [128, D], F32) for i in range(2)]
            xo = [sb(ph, f"xo{i}", [128, D], F32) for i in range(2)]
            for s in range(2):
                S.dma('sp', lambda q, s=s: q.dma_start(out=g2[s][:, :], in_=modbc[l, s, :, 5 * D:6 * D]), r=[('modbc', l, s)], w=[('g2', s)])
            S.dma('sp', lambda q: q.dma_start(out=ln2g[:, :], in_=ln2_g[l:l + 1, :].to_broadcast([128, D])), w=['ln2g'])
            S.dma('sp', lambda q: q.dma_start(out=ln2b[:, :], in_=ln2_b[l:l + 1, :].to_broadcast([128, D])), w=['ln2b'])
            out_toks = []
            for n, ti in enumerate(tiles_b):
                s = 0 if ti < NCTX else 1
                a, ak = xa[n % 2], ('xa', n % 2)
                m, mk = xm[n % 2], ('xm', n % 2)
                o, ok = xo[n % 2], ('xo', n % 2)
                S.dma('sp', lambda q, ti=ti, a=a: q.dma_start(out=a[:, :], in_=x1s[ti * 128:(ti + 1) * 128, :]), r=[('x1s', ti)], w=[ak])
                S.dma('sp', lambda q, ti=ti, m=m: q.dma_start(out=m[:, :], in_=macc[ti * 128:(ti + 1) * 128, :]), w=[mk, ('macc_rd', ti)], extra=prev_sc)
                S.op('dve', lambda e, m=m, s=s: e.tensor_tensor(out=m[:, :], in0=m[:, :], in1=g2[s][:, :], op=ALU.mult), r=[mk, ('g2', s)], w=[mk])
                S.op('dve', lambda e, m=m, a=a: e.scalar_tensor_tensor(out=m[:, :], in0=a[:, :], scalar=ALPHA, in1=m[:, :], op0=ALU.mult, op1=ALU.add),
                     r=[ak, mk], w=[mk])
                ln_stats(m, mk)
                ln_apply(o[:, :], ok, m[:, :], mk)
                S.op('dve', lambda e, o=o: e.tensor_tensor(out=o[:, :], in0=o[:, :], in1=ln2g[:, :], op=ALU.mult), r=[ok, 'ln2g'], w=[ok])
                S.op('pool', lambda e, o=o: e.tensor_tensor(out=o[:, :], in0=o[:, :], in1=ln2b[:, :], op=ALU.add), r=[ok, 'ln2b'], w=[ok])
                if last:
                    i = ti - NCTX
                    out_toks.append(S.dma('sp', lambda q, i=i, o=o: q.dma_start(out=out[i * 128:(i + 1) * 128, :], in_=o[:, :]), r=[ok], w=[('out', i)]))
                else:
                    S.dma('sp', lambda q, ti=ti, o=o: q.dma_start(out=xs_d[ti * 128:(ti + 1) * 128, :], in_=o[:, :]), r=[ok], w=[('xs_d',)])
            S.barrier()
        if stop_after == f'F{l}':
            return _finish(nc, S, es)
    return _finish(nc, S, es)


def _finish(nc, S, es):
    S.barrier()
    es.close()
    return nc


def _consts():
    c = np.zeros((128, 6 * 128), np.float32)
    p = np.arange(128)
    c[:, 0:128] = np.eye(128, dtype=np.float32)
    c[:, 128:256] = (p[:, None] < p[None, :]).astype(np.float32)
    c[:, 256:384] = (p[:, None] >= p[None, :]).astype(np.float32)
    c[:, 384:512] = (p[:, None] <= p[None, :]).astype(np.float32)
    c[:, 512:640] = 1.0
    c[:, 640] = p.astype(np.float32)
    c[:, 641] = (p < 64).astype(np.float32)
    c[:, 642] = (p >= 64).astype(np.float32)
    return c


def _rope_table(NLAT):
    t = np.arange(NLAT * 128, dtype=np.int32)
    row = (t // 64).astype(np.float32)[:, None]
    col = (t % 64).astype(np.float32)[:, None]
    inv = (np.float32(10000.0) ** (-np.arange(16, dtype=np.float32) / np.float32(16))).astype(np.float32)
    ar = (row * inv).astype(np.float32)
    ac = (col * inv).astype(np.float32)
    cr, sr, cc, sc = np.cos(ar), np.sin(ar), np.cos(ac), np.sin(ac)
    tab = np.concatenate([cr, cr, cc, cc, -sr, sr, -sc, sc], axis=1).astype(np.float32)
    return np.ascontiguousarray(tab.reshape(NLAT, 128, 128))


def _nat_entries(NLAT):
    ents = [(2, d) for d in range(5)] if NLAT >= 5 else [(0, 0)] * 5
    ents = [(2, 2 + d - 2) for d in range(5)] if NLAT >= 5 else ents
    ents += [(0, j) for j in range(4)] + [(1, j) for j in range(4)]
    ents += [(NLAT - 2, NLAT - 4 + j) for j in range(4)] + [(NLAT - 1, NLAT - 4 + j) for j in range(4)]
    return ents


def _nat_table(nat_bias, NLAT):
    rows = NLAT * 2
    ents = _nat_entries(NLAT)
    kk = np.arange(128)
    Lh = nat_bias.shape[0]
    out = np.empty((Lh, 128, NCHB, 4, 128), np.float32)
    for n, (i, j) in enumerate(ents):
        kr = 2 * j + kk // 64
        kc = kk % 64
        qr = 2 * i + kk // 64
        qc = kk % 64
        rs = np.clip(qr - 4, 0, rows - 8)
        cs = np.clip(qc - 8, 0, 48)
        valid = ((kr[:, None] >= rs[None, :]) & (kr[:, None] < rs[None, :] + 8) &
                 (kc[:, None] >= cs[None, :]) & (kc[:, None] < cs[None, :] + 16))
        dr = np.clip(kr[:, None] - qr[None, :] + 7, 0, 14)
        dc = np.clip(kc[:, None] - qc[None, :], -15, 15) + 15
        g = nat_bias[:, :, dr, dc]
        g = np.where(valid[None, None], g, np.float32(NEG))
        out[:, :, n, :, :] = np.transpose(g, (0, 2, 1, 3))
    return np.ascontiguousarray(out.reshape(Lh, 128, NCHB * 512))


def make_in_maps(inputs, NLAT, samples, moe=True):
    names = ['w_mod', 'b_mod', 'w_in', 'a_sink', 'conv_w', 'conv_b', 'conv_ln_g', 'conv_ln_b', 'w_out', 'ln1_g', 'ln1_b',
             'w_router', 'ln2_g', 'ln2_b'] + (['w_gate', 'w_up', 'w_down'] if moe else [])
    shared = {k: np.ascontiguousarray(np.asarray(inputs[k], np.float32)) for k in names}
    cw = shared['conv_w']
    shared['conv_w'] = np.ascontiguousarray(cw.reshape(L, 31, 2, 128).transpose(0, 3, 2, 1).reshape(L, 128, 62))
    shared['conv_b'] = np.ascontiguousarray(shared['conv_b'].reshape(L, 2, 128).transpose(0, 2, 1))
    shared['natT'] = _nat_table(np.asarray(inputs['nat_bias'], np.float32), NLAT)
    shared['rope'] = _rope_table(NLAT)
    shared['consts'] = _consts()
    maps = []
    for b in samples:
        m = dict(shared)
        m['xlat'] = np.ascontiguousarray(np.asarray(inputs['x'][b, :NLAT * 128], np.float32))
        m['xctx'] = np.ascontiguousarray(np.asarray(inputs['ctx'][b], np.float32))
        cv = np.stack([np.asarray(inputs['c_ctx'], np.float32), np.asarray(inputs['c'][b], np.float32)])
        m['cvec'] = np.ascontiguousarray(cv.reshape(2, 8, 128).transpose(2, 1, 0).reshape(128, 16))
        maps.append(m)
    return maps


def kernel(**inputs):
    NLAT = 64
    nc = build_program(NLAT)
    maps = make_in_maps(inputs, NLAT, [0, 1, 2, 3, 0, 1, 2, 3])
    res = run_bass_kernel_spmd(nc, maps, core_ids=list(range(8)))
    return np.stack([np.asarray(res.results[b]["out"], np.float32).reshape(NLAT * 128, D) for b in range(4)])
```

```python
import os
import numpy as np
from contextlib import ExitStack
import concourse.bass as bass
import concourse.mybir as mybir
from concourse.bass_utils import run_bass_kernel_spmd

F32 = mybir.dt.float32
BF16 = mybir.dt.bfloat16
I32 = mybir.dt.int32
AF = mybir.ActivationFunctionType
ALU = mybir.AluOpType
AX = mybir.AxisListType

D = 1024
L = 2
NCTX = 2
NE = 16
EPS = 1e-6
ALPHA = float((2 * L) ** 0.25)
SCALE = 0.125
NEG = -30000.0
BIG = 1.0e6
QA, QB, KA, VA, KB, VB, CH, AW = 0, 1024, 1536, 1664, 1794, 2050, 2310, 2566
QW = KA
KVW = CH - KA
RW = 1024 + 2 + 32
NCHB = 21


class Sched:
    EPOCH = 16000

    def __init__(self, nc, es, ndma=16):
        self.nc, self.es = nc, es
        self.eng = dict(pe=nc.tensor, act=nc.scalar, dve=nc.vector, pool=nc.gpsimd, sp=nc.sync)
        self.sems = {k: [] for k in self.eng}
        self.cnt = {k: 0 for k in self.eng}
        self.dpool = {'sp': list(range(0, ndma)), 'pool': list(range(ndma, ndma + 8)), 'act': list(range(ndma + 8, ndma + 12))}
        ntot = ndma + 12
        self.dsem = [es.enter_context(nc.semaphore(f"dq{i}")) for i in range(ntot)]
        self.dval = [0] * ntot
        self.dnext = {'sp': 0, 'pool': 0, 'act': 0}
        self.waited = {k: {} for k in self.eng}
        self.lastw = {}
        self.readers = {}
        self.same_engine_sync = True
        self.nwaits = 0

    def _sem(self, e, seq):
        i = (seq - 1) // self.EPOCH
        while len(self.sems[e]) <= i:
            self.sems[e].append(self.es.enter_context(self.nc.semaphore(f"s_{e}{len(self.sems[e])}")))
        return self.sems[e][i], (seq - 1) % self.EPOCH + 1

    def _wait(self, e, tok):
        kind, src, val = tok
        if kind == 'e':
            if src == e and (e == 'pe' or not self.same_engine_sync):
                return
            assert val <= self.cnt[src], f"dependency on unsignalled op {tok}"
            key = ('e', src)
        else:
            key = ('d', src)
        if self.waited[e].get(key, 0) >= val:
            return
        self.waited[e][key] = val
        if kind == 'e':
            sem, v = self._sem(src, val)
        else:
            sem, v = self.dsem[src], val
        self.eng[e].wait_ge(sem, v)
        self.nwaits += 1

    def _deps(self, r, w):
        deps = []
        for k in r:
            if k in self.lastw:
                deps.append(self.lastw[k])
            if isinstance(k, tuple) and k[0] == 'pb':
                deps.extend(self.readers.get(k, {}).values())
        for k in w:
            if k in self.lastw:
                deps.append(self.lastw[k])
            deps.extend(self.readers.get(k, {}).values())
        return deps

    def _record(self, tok, r, w):
        for k in r:
            d = self.readers.setdefault(k, {})
            key = (tok[0], tok[1])
            if key not in d or d[key][2] < tok[2]:
                d[key] = tok
        for k in w:
            self.lastw[k] = tok
            self.readers[k] = {}

    def op(self, e, fn, r=(), w=(), signal=True, extra=()):
        for d in self._deps(r, w):
            self._wait(e, d)
        for d in extra:
            self._wait(e, d)
        inst = fn(self.eng[e])
        if signal:
            self.cnt[e] += 1
            seq = self.cnt[e]
            sem, _ = self._sem(e, seq)
            inst.then_inc(sem, 1)
        else:
            seq = self.cnt[e] + 1
        tok = ('e', e, seq)
        self._record(tok, r, w)
        return tok

    def dma(self, q, fn, r=(), w=(), extra=()):
        pl = self.dpool[q]
        slot = pl[self.dnext[q] % len(pl)]
        self.dnext[q] += 1
        if self.dval[slot]:
            self._wait(q, ('d', slot, self.dval[slot]))
        for d in self._deps(r, w):
            self._wait(q, d)
        for d in extra:
            self._wait(q, d)
        inst = fn(self.eng[q])
        inst.then_inc(self.dsem[slot], 16)
        self.dval[slot] += 16
        tok = ('d', slot, self.dval[slot])
        self._record(tok, r, w)
        return tok

    def barrier(self):
        toks = [('e', f, self.cnt[f]) for f in self.eng if self.cnt[f] > 0]
        toks += [('d', s, v) for s, v in enumerate(self.dval) if v > 0]
        for e in self.eng:
            for t in toks:
                self._wait(e, t)


def build_program(NLAT=64, dbg=False, nlayers=L, stop_after=None, skip_mods=False):
    NT = NLAT + NCTX
    NTOK = NT * 128
    CAPL = 16 * NLAT
    CAPC = 32

    nc = bass.Bass("TRN2", target_bir_lowering=False)
    dk = "ExternalOutput" if dbg else "Internal"

    def din(name, shape, dt=F32):
        return nc.dram_tensor(name, list(shape), dt, kind="ExternalInput").ap()

    xlat = din("xlat", [NLAT * 128, D])
    xctx = din("xctx", [NCTX * 128, D])
    cvec = din("cvec", [128, 16])
    if not skip_mods:
        w_mod = din("w_mod", [L, D, 6 * D])
        b_mod = din("b_mod", [L, 6 * D])
    w_in = din("w_in", [L, D, 2048])
    a_sink = din("a_sink", [L, 8])
    natT = din("natT", [L, 128, NCHB * 512])
    conv_w = din("conv_w", [L, 128, 62])
    conv_b = din("conv_b", [L, 128, 2])
    conv_ln_g = din("conv_ln_g", [L, 256])
    conv_ln_b = din("conv_ln_b", [L, 256])
    w_out = din("w_out", [L, D, D])
    ln1_g = din("ln1_g", [L, D])
    ln1_b = din("ln1_b", [L, D])
    w_router = din("w_router", [L, D, NE])
    need_moe = stop_after is None or stop_after.startswith(('M', 'F'))
    if need_moe:
        w_gate = din("w_gate", [L, NE, D, D])
        w_up = din("w_up", [L, NE, D, D])
        w_down = din("w_down", [L, NE, D, D])
    ln2_g = din("ln2_g", [L, D])
    ln2_b = din("ln2_b", [L, D])
    rope = din("rope", [NLAT, 128, 128])
    consts = din("consts", [128, 6 * 128])
    out = nc.dram_tensor("out", [NLAT * 128, D], F32, kind="ExternalOutput").ap()

    modbc = nc.dram_tensor("modbc", [L, 2, 128, 6 * D], F32, kind=dk).ap()
    scrA = nc.dram_tensor("scrA", [NT, 128, AW], BF16, kind=dk).ap()
    x1s = nc.dram_tensor("x1s", [NTOK, D], F32, kind=dk).ap()
    h2s = nc.dram_tensor("h2s", [NTOK, RW], BF16, kind=dk).ap()
    xs_d = nc.dram_tensor("xs_d", [NTOK, D], F32, kind=dk).ap()
    macc = nc.dram_tensor("macc", [NTOK, D], F32, kind=dk).ap()
    CAPT0 = CAPL + CAPC
    Xs = [nc.dram_tensor(f"Xs{e}", [CAPT0, RW], BF16, kind=dk).ap() for e in range(NE)]
    otok_d = nc.dram_tensor("otok_d", [NTOK, D], BF16, kind=dk).ap() if dbg else None

    es = ExitStack()
    S = Sched(nc, es)
    es.enter_context(nc.allow_non_contiguous_dma(reason="small strided parameter loads"))
    es.enter_context(nc.allow_low_precision(reason="0/1 mask counts <= 128 are exact in bf16"))

    uid = [0]

    def sb(stack, name, shape, dt):
        uid[0] += 1
        return stack.enter_context(nc.sbuf_tensor(f"{name}_{uid[0]}", list(shape), dt))

    banks = [es.enter_context(nc.psum_tensor(f"pb{i}", [128, 512], F32)) for i in range(8)]
    bank_rr = [0]

    def nbank():
        i = bank_rr[0]
        bank_rr[0] = (i + 1) % 8
        return banks[i], ('pb', i)

    cst = sb(es, "cst", [128, 6 * 128], F32)
    identb = sb(es, "identb", [128, 128], BF16)
    triub = sb(es, "triub", [128, 128], BF16)
    masklr = sb(es, "masklr", [128, 2, 128], BF16)
    onesb = sb(es, "onesb", [128, 128], BF16)
    aff_all = sb(es, "aff_all", [128, NT, NE], F32)
    NLN = 4
    stat_l = [sb(es, "stat", [128, 12], F32) for _ in range(NLN)]
    mv_l = [sb(es, "mv", [128, 2], F32) for _ in range(NLN)]
    sd_l = [sb(es, "sd", [128, 1], F32) for _ in range(NLN)]
    rstd_l = [sb(es, "rstd", [128, 1], F32) for _ in range(NLN)]
    nmr_l = [sb(es, "nmr", [128, 1], F32) for _ in range(NLN)]
    lncur = [0]
    identf = cst[:, 0:128]
    iotap = cst[:, 5 * 128:5 * 128 + 1]
    pm0 = cst[:, 5 * 128 + 1:5 * 128 + 2]
    pm1 = cst[:, 5 * 128 + 2:5 * 128 + 3]

    S.dma('sp', lambda q: q.dma_start(out=cst[:, :], in_=consts), w=['cst'])
    S.op('dve', lambda e: e.tensor_copy(out=identb[:, :], in_=cst[:, 0:128]), r=['cst'], w=['identb'])
    S.op('dve', lambda e: e.tensor_copy(out=triub[:, :], in_=cst[:, 128:256]), r=['cst'], w=['triub'])
    S.op('dve', lambda e: e.tensor_copy(out=masklr[:, :, :].rearrange("p a b -> p (a b)"), in_=cst[:, 256:512]), r=['cst'], w=['masklr'])
    S.op('dve', lambda e: e.tensor_copy(out=onesb[:, :], in_=cst[:, 512:640]), r=['cst'], w=['onesb'])

    def ln_stats(src_ap, src_key, width=1024):
        nch = (width + 511) // 512
        lncur[0] = (lncur[0] + 1) % NLN
        j = lncur[0]
        stat, mv, sd, rstd, nmr = stat_l[j], mv_l[j], sd_l[j], rstd_l[j], nmr_l[j]
        kst_, kmv, ksd, krs, knm = ('stat', j), ('mv', j), ('sd', j), ('rstd', j), ('nmr', j)
        for c in range(nch):
            S.op('dve', lambda e, c=c: e.bn_stats(out=stat[:, 6 * c:6 * c + 6], in_=src_ap[:, c * 512:min(width, (c + 1) * 512)]),
                 r=[src_key], w=[kst_])
        S.op('dve', lambda e: e.bn_aggr(out=mv[:, :], in_=stat[:, 0:6 * nch]), r=[kst_], w=[kmv])
        S.op('dve', lambda e: e.tensor_scalar(out=sd[:, :], in0=mv[:, 1:2], scalar1=EPS, scalar2=None, op0=ALU.add), r=[kmv], w=[ksd])
        S.op('act', lambda e: e.activation(out=sd[:, :], in_=sd[:, :], func=AF.Sqrt), r=[ksd], w=[ksd])
        S.op('dve', lambda e: e.reciprocal(out=rstd[:, :], in_=sd[:, :]), r=[ksd], w=[krs])
        S.op('dve', lambda e: e.tensor_scalar(out=nmr[:, :], in0=mv[:, 0:1], scalar1=rstd[:, 0:1], scalar2=-1.0, op0=ALU.mult, op1=ALU.mult),
             r=[kmv, krs], w=[knm])

    def ln_apply(dst_ap, dst_key, src_ap, src_key):
        j = lncur[0]
        S.op('act', lambda e: e.activation(out=dst_ap, in_=src_ap, func=AF.Identity, scale=rstd_l[j][:, 0:1], bias=nmr_l[j][:, 0:1]),
             r=[src_key, ('rstd', j), ('nmr', j)], w=[dst_key])

    def transposes_to(dst_ap, dst_key, src_fn, n, rows=128, src_key=None, evac='act'):
        bk, bkey = nbank()
        bv = bk[:, :].bitcast(BF16)
        for k in range(n):
            S.op('pe', lambda e, k=k: e.transpose(out=bv[:, k * 128:k * 128 + rows], in_=src_fn(k), identity=identb[:rows, :rows]),
                 r=[src_key, 'identb'], w=[bkey], signal=(k == n - 1))
        if rows == 128:
            src = bv[:, 0:n * 128]
        else:
            src = bv[:, 0:n * 128].rearrange("p (k s) -> p k s", s=128)[:, :, 0:rows]
        if evac == 'act':
            S.op('act', lambda e: e.copy(out=dst_ap, in_=src), r=[bkey], w=[dst_key])
        else:
            S.op('dve', lambda e: e.tensor_copy(out=dst_ap, in_=src), r=[bkey], w=[dst_key])

    with ExitStack() as ph:
        cT = sb(ph, "cT", [128, 8, 2], F32)
        cS = sb(ph, "cS", [128, 8, 2], F32)
        lbc = [sb(ph, f"lbc{s}", [128, 8, 128], BF16) for s in range(2)]
        wm = [sb(ph, f"wm{i}", [128, 8, 512], BF16) for i in range(2)]
        bm = [sb(ph, f"bm{i}", [128, 512], F32) for i in range(2)]
        mo = [sb(ph, f"mo{i}", [128, 512], F32) for i in range(4)]
        S.dma('sp', lambda q: q.dma_start(out=cT[:, :, :], in_=cvec.rearrange("p (k s) -> p k s", s=2)), w=['cT'])
        S.op('act', lambda e: e.activation(out=cS[:, :, :], in_=cT[:, :, :], func=AF.Silu), r=['cT'], w=['cS'])
        for s in range(2):
            S.op('dve', lambda e, s=s: e.tensor_copy(out=lbc[s][:, :, :], in_=cS[:, :, s:s + 1].to_broadcast([128, 8, 128])),
                 r=['cS'], w=[('lbc', s)])
        it = 0
        for l in range(nlayers if not skip_mods else 0):
            for ch in range(12):
                n0 = ch * 512
                slot = it % 2
                S.dma('pool', lambda q, slot=slot, l=l, n0=n0: q.dma_start(
                    out=wm[slot][:, :, :], in_=w_mod[l, :, n0:n0 + 512].rearrange("(ko ki) n -> ki ko n", ki=128)), w=[('wm', slot)])
                S.dma('sp', lambda q, slot=slot, l=l, n0=n0: q.dma_start(
                    out=bm[slot][:, :], in_=b_mod[l:l + 1, n0:n0 + 512].to_broadcast([128, 512])), w=[('bm', slot)])
                addc = 1.0 if (ch // 2) in (1, 4) else 0.0
                for s in range(2):
                    bk, bkey = nbank()
                    for k in range(8):
                        S.op('pe', lambda e, k=k, s=s, slot=slot, bk=bk: e.matmul(bk[:, :], lhsT=lbc[s][:, k, :], rhs=wm[slot][:, k, :],
                                                                               start=(k == 0), stop=(k == 7)),
                             r=[('lbc', s), ('wm', slot)], w=[bkey], signal=(k == 7))
                    ms = (it * 2 + s) % 4
                    S.op('dve', lambda e, bk=bk, ms=ms, slot=slot: e.scalar_tensor_tensor(
                        out=mo[ms][:, :], in0=bk[:, :], scalar=addc, in1=bm[slot][:, :], op0=ALU.add, op1=ALU.add),
                         r=[bkey, ('bm', slot)], w=[('mo', ms)])
                    S.dma('sp', lambda q, ms=ms, l=l, s=s, n0=n0: q.dma_start(out=modbc[l, s, :, n0:n0 + 512], in_=mo[ms][:, :]),
                          r=[('mo', ms)], w=[('modbc', l, s)])
                it += 1
        S.barrier()
    if stop_after == 'mods':
        return _finish(nc, S, es)

    for l in range(nlayers):
        last = (l == L - 1)
        tiles_b = list(range(NT)) if not last else list(range(NCTX, NT))
        CAPT = CAPL + (0 if last else CAPC)

        def xsrc(ti):
            if l == 0:
                return xctx[ti * 128:(ti + 1) * 128, :] if ti < NCTX else xlat[(ti - NCTX) * 128:(ti - NCTX + 1) * 128, :]
            return xs_d[ti * 128:(ti + 1) * 128, :]

        with ExitStack() as ph:
            w_in_sb = sb(ph, "w_in_sb", [128, 8, 2048], BF16)
            modA = [[sb(ph, f"modA{s}{j}", [128, D], F32) for j in range(2)] for s in range(2)]
            xin = [sb(ph, f"xinA{i}", [128, D], F32) for i in range(2)]
            ropet = [sb(ph, f"ropet{i}", [128, 128], F32) for i in range(2)]
            f32a_l = [sb(ph, "f32a", [128, D], F32) for _ in range(2)]
            hb_l = [sb(ph, "hb", [128, D], BF16) for _ in range(2)]
            hT_l = [sb(ph, "hT", [128, 8, 128], BF16) for _ in range(2)]
            ropeA_l = [sb(ph, "ropeA", [128, 512], F32) for _ in range(2)]
            ropeB_l = [sb(ph, "ropeB", [128, 512], F32) for _ in range(2)]
            qperm_l = [sb(ph, "qperm", [128, 512], BF16) for _ in range(2)]
            kst_l = [sb(ph, "kst", [128, 640], BF16) for _ in range(2)]
            sg_l = [sb(ph, "sg", [128, 256], F32) for _ in range(2)]
            chtok_l = [sb(ph, "chtok", [128, 256], BF16) for _ in range(2)]
            aout = [sb(ph, f"aout{i}", [128, AW], BF16) for i in range(2)]
            for i in range(2):
                S.op('pool', lambda e, i=i: e.memset(aout[i][:, VA:VA + 130], 1.0), w=[('aout', i)])
                S.op('pool', lambda e, i=i: e.memset(aout[i][:, VB:VB + 260], 1.0), w=[('aout', i)])
            for hf in range(2):
                S.dma('pool', lambda q, hf=hf: q.dma_start(out=w_in_sb[:, :, hf * 1024:(hf + 1) * 1024],
                                                        in_=w_in[l, :, hf * 1024:(hf + 1) * 1024].rearrange("(ko ki) n -> ki ko n", ki=128)),
                      w=['w_in_sb'])
            for s in range(2):
                for j in range(2):
                    S.dma('sp', lambda q, s=s, j=j: q.dma_start(out=modA[s][j][:, :], in_=modbc[l, s, :, j * D:(j + 1) * D]),
                          r=[('modbc', l, s)], w=[('modA', s, j)])

            for ti in range(NT):
                s = 0 if ti < NCTX else 1
                lat = ti >= NCTX
                xs_, xk = xin[ti % 2], ('xinA', ti % 2)
                f32a = f32a_l[ti % 2]
                k_f32a = ('f32a', ti % 2)
                hb = hb_l[ti % 2]
                k_hb = ('hb', ti % 2)
                hT = hT_l[ti % 2]
                k_hT = ('hT', ti % 2)
                ropeA = ropeA_l[ti % 2]
                k_ropeA = ('ropeA', ti % 2)
                ropeB = ropeB_l[ti % 2]
                k_ropeB = ('ropeB', ti % 2)
                qperm = qperm_l[ti % 2]
                k_qperm = ('qperm', ti % 2)
                kst = kst_l[ti % 2]
                k_kst = ('kst', ti % 2)
                sg = sg_l[ti % 2]
                k_sg = ('sg', ti % 2)
                chtok = chtok_l[ti % 2]
                k_chtok = ('chtok', ti % 2)
                ao, aok = aout[ti % 2], ('aout', ti % 2)
                rt, rk = ropet[ti % 2], ('ropet', ti % 2)
                S.dma('sp', lambda q, ti=ti, xs_=xs_: q.dma_start(out=xs_[:, :], in_=xsrc(ti)), r=[('xs_d',)] if l > 0 else [], w=[xk])
                if lat:
                    S.dma('sp', lambda q, ti=ti, rt=rt: q.dma_start(out=rt[:, :], in_=rope[ti - NCTX, :, :]), w=[rk])
                ln_stats(xs_, xk)
                ln_apply(f32a[:, :], k_f32a, xs_[:, :], xk)
                S.op('dve', lambda e, s=s: e.tensor_tensor(out=f32a[:, :], in0=f32a[:, :], in1=modA[s][1][:, :], op=ALU.mult),
                     r=[k_f32a, ('modA', s, 1)], w=[k_f32a])
                S.op('pool', lambda e, s=s: e.tensor_tensor(out=hb[:, :], in0=f32a[:, :], in1=modA[s][0][:, :], op=ALU.add),
                     r=[k_f32a, ('modA', s, 0)], w=[k_hb])
                transposes_to(hT[:, :, :].rearrange("p k t -> p (k t)"), k_hT, lambda k: hb[:, k * 128:(k + 1) * 128], 8, src_key=k_hb)
                ub = []
                for n in range(4):
                    bk, bkey = nbank()
                    for k in range(8):
                        S.op('pe', lambda e, k=k, n=n, bk=bk: e.matmul(bk[:, :], lhsT=hT[:, k, :], rhs=w_in_sb[:, k, n * 512:(n + 1) * 512],
                                                                     start=(k == 0), stop=(k == 7)),
                             r=[k_hT, 'w_in_sb'], w=[bkey], signal=(k == 7))
                    ub.append((bk, bkey))
                b0, k0 = ub[0]
                qp_v = qperm[:, :].rearrange("p (g r d) -> p r g d", g=4, r=2, d=64)
                if lat:
                    S.op('dve', lambda e, b0=b0, rt=rt: e.tensor_tensor(
                        out=ropeA[:, :].rearrange("p (h d) -> p h d", d=64), in0=b0[:, :].rearrange("p (h d) -> p h d", d=64),
                        in1=rt[:, 0:64].unsqueeze(1).to_broadcast([128, 8, 64]), op=ALU.mult), r=[k0, rk], w=[k_ropeA])
                    for half in range(2):
                        S.op('dve', lambda e, b0=b0, rt=rt, half=half: e.tensor_tensor(
                            out=ropeB[:, :].rearrange("p (h r f d) -> p h r f d", h=8, r=2, f=2, d=16)[:, :, :, half, :],
                            in0=b0[:, :].rearrange("p (h r f d) -> p h r f d", h=8, r=2, f=2, d=16)[:, :, :, 1 - half, :],
                            in1=rt[:, 64:128].rearrange("p (r f d) -> p r f d", r=2, f=2, d=16)[:, :, half, :].unsqueeze(1).to_broadcast([128, 8, 2, 16]),
                            op=ALU.mult), r=[k0, rk], w=[k_ropeB])
                    S.op('pool', lambda e: e.tensor_tensor(out=qp_v, in0=ropeA[:, :].rearrange("p (r g d) -> p r g d", r=2, g=4, d=64),
                                                          in1=ropeB[:, :].rearrange("p (r g d) -> p r g d", r=2, g=4, d=64), op=ALU.add),
                         r=[k_ropeA, k_ropeB], w=[k_qperm])
                else:
                    S.op('act', lambda e, b0=b0: e.copy(out=qp_v, in_=b0[:, :].rearrange("p (r g d) -> p r g d", r=2, g=4, d=64)),
                         r=[k0], w=[k_qperm])
                bk, bkey = nbank()
                bv = bk[:, :].bitcast(BF16)
                for k in range(4):
                    S.op('pe', lambda e, k=k, bv=bv: e.transpose(out=bv[:, k * 128:(k + 1) * 128], in_=qperm[:, k * 128:(k + 1) * 128], identity=identb[:, :]),
                         r=[k_qperm, 'identb'], w=[bkey], signal=(k == 3))
                if os.environ.get('EVAC', 'mask') == 'mask':
                    S.op('act', lambda e, bv=bv, ao=ao: e.activation(out=ao[:, QA:QA + 512], in_=bv[:, 0:512], func=AF.Copy, scale=pm0), r=[bkey, 'cst'], w=[aok])
                    S.op('dve', lambda e, bv=bv, ao=ao: e.tensor_scalar(out=ao[:, QA + 512:QA + 1024], in0=bv[:, 0:512], scalar1=pm1, scalar2=None, op0=ALU.mult),
                         r=[bkey, 'cst'], w=[aok])
                else:
                    S.op('act', lambda e, bv=bv, ao=ao: e.copy(out=ao[:, QA:QA + 512], in_=bv[:, 0:512]), r=[bkey], w=[aok])
                    S.op('dve', lambda e, bv=bv, ao=ao: e.tensor_copy(out=ao[:, QA + 512:QA + 1024], in_=bv[:, 0:512]), r=[bkey], w=[aok])
                b1, k1 = ub[1]
                if lat:
                    S.op('dve', lambda e, b1=b1, rt=rt: e.tensor_tensor(
                        out=ropeA[:, 0:128].rearrange("p (h d) -> p h d", d=64), in0=b1[:, 0:128].rearrange("p (h d) -> p h d", d=64),
                        in1=rt[:, 0:64].unsqueeze(1).to_broadcast([128, 2, 64]), op=ALU.mult), r=[k1, rk], w=[k_ropeA])
                    for half in range(2):
                        S.op('dve', lambda e, b1=b1, rt=rt, half=half: e.tensor_tensor(
                            out=ropeB[:, 0:128].rearrange("p (h r f d) -> p h r f d", h=2, r=2, f=2, d=16)[:, :, :, half, :],
                            in0=b1[:, 0:128].rearrange("p (h r f d) -> p h r f d", h=2, r=2, f=2, d=16)[:, :, :, 1 - half, :],
                            in1=rt[:, 64:128].rearrange("p (r f d) -> p r f d", r=2, f=2, d=16)[:, :, half, :].unsqueeze(1).to_broadcast([128, 2, 2, 16]),
                            op=ALU.mult), r=[k1, rk], w=[k_ropeB])
                    S.op('pool', lambda e: e.tensor_tensor(out=kst[:, 0:128], in0=ropeA[:, 0:128], in1=ropeB[:, 0:128], op=ALU.add),
                         r=[k_ropeA, k_ropeB], w=[k_kst])
                else:
                    S.op('act', lambda e, b1=b1: e.copy(out=kst[:, 0:128], in_=b1[:, 0:128]), r=[k1], w=[k_kst])
                S.op('act', lambda e, b1=b1, ao=ao: e.copy(out=ao[:, VA:VA + 130].rearrange("p (h d) -> p h d", d=65)[:, :, 0:64],
                                                         in_=b1[:, 128:256].rearrange("p (h d) -> p h d", d=64)), r=[k1], w=[aok])
                S.op('act', lambda e, b1=b1: e.copy(out=kst[:, 128:384], in_=b1[:, 256:512]), r=[k1], w=[k_kst])
                b2, k2 = ub[2]
                S.op('dve', lambda e, b2=b2: e.tensor_copy(out=kst[:, 384:640], in_=b2[:, 0:256]), r=[k2], w=[k_kst])
                S.op('act', lambda e, b2=b2, ao=ao: e.copy(out=ao[:, VB:VB + 260].rearrange("p (h d) -> p h d", d=65)[:, :, 0:64],
                                                         in_=b2[:, 256:512].rearrange("p (h d) -> p h d", d=64)), r=[k2], w=[aok])
                order = [1, 2, 0, 3, 4]
                bk, bkey = nbank()
                bv = bk[:, :].bitcast(BF16)
                for j, blk in enumerate(order):
                    S.op('pe', lambda e, j=j, blk=blk, bv=bv: e.transpose(out=bv[:, j * 128:(j + 1) * 128], in_=kst[:, blk * 128:(blk + 1) * 128],
                                                                         identity=identb[:, :]), r=[k_kst, 'identb'], w=[bkey], signal=(j == 4))
                if os.environ.get('EVAC', 'mask') == 'mask':
                    S.op('act', lambda e, bv=bv, ao=ao: e.activation(out=ao[:, QB:QB + 256], in_=bv[:, 0:256], func=AF.Copy, scale=pm0), r=[bkey, 'cst'], w=[aok])
                    S.op('dve', lambda e, bv=bv, ao=ao: e.tensor_scalar(out=ao[:, QB + 256:QB + 512], in0=bv[:, 0:256], scalar1=pm1, scalar2=None, op0=ALU.mult),
                         r=[bkey, 'cst'], w=[aok])
                else:
                    S.op('act', lambda e, bv=bv, ao=ao: e.copy(out=ao[:, QB:QB + 256], in_=bv[:, 0:256]), r=[bkey], w=[aok])
                    S.op('dve', lambda e, bv=bv, ao=ao: e.tensor_copy(out=ao[:, QB + 256:QB + 512], in_=bv[:, 0:256]), r=[bkey], w=[aok])
                S.op('dve', lambda e, bv=bv, ao=ao: e.tensor_copy(out=ao[:, KA:KA + 128], in_=bv[:, 256:384]), r=[bkey], w=[aok])
                S.op('dve', lambda e, bv=bv, ao=ao: e.tensor_copy(out=ao[:, KB:KB + 256], in_=bv[:, 384:640]), r=[bkey], w=[aok])
                b3, k3 = ub[3]
                S.op('act', lambda e, b3=b3: e.activation(out=sg[:, :], in_=b3[:, 256:512], func=AF.Sigmoid), r=[k3], w=[k_sg])
                S.op('dve', lambda e, b3=b3: e.tensor_tensor(out=chtok[:, :], in0=b3[:, 0:256], in1=sg[:, :], op=ALU.mult), r=[k3, k_sg], w=[k_chtok])
                transposes_to(ao[:, CH:CH + 256], aok, lambda k: chtok[:, k * 128:(k + 1) * 128], 2, src_key=k_chtok, evac='dve')
                S.dma('sp', lambda q, ti=ti, ao=ao: q.dma_start(out=scrA[ti, :, 0:QW], in_=ao[:, 0:QW]), r=[aok], w=[('scrAq', ti)])
                S.dma('sp', lambda q, ti=ti, ao=ao: q.dma_start(out=scrA[ti, :, QW:AW], in_=ao[:, QW:AW]), r=[aok], w=[('scrA', ti)])
            S.barrier()
        if stop_after == f'A{l}':
            return _finish(nc, S, es)

        with ExitStack() as ph:
            w_out_sb = sb(ph, "w_out_sb", [128, 8, D], BF16)
            w_r_sb = sb(ph, "w_r_sb", [128, 8, NE], BF16)
            nat_sb = sb(ph, "nat_sb", [128, NCHB, 512], BF16)
            cwT = sb(ph, "cwT", [128, 2, 31], F32)
            cdiag = sb(ph, "cdiag", [128, 2, 31, 128], BF16)
            convb = sb(ph, "convb", [128, 2], F32)
            clng = sb(ph, "clng", [128, 256], F32)
            clnb = sb(ph, "clnb", [128, 256], F32)
            modB = [[sb(ph, f"modB{s}{j}", [128, D], F32) for j in range(3)] for s in range(2)]
            ln1g = sb(ph, "ln1g", [128, D], F32)
            ln1b = sb(ph, "ln1b", [128, D], F32)
            esink = sb(ph, "esink", [128, 8], F32)
            NQR, NKR, NCW = 3, 8, 4
            qring = [sb(ph, f"qring{i}", [128, QW], BF16) for i in range(NQR)]
            kvring = [sb(ph, f"kvring{i}", [128, KVW], BF16) for i in range(NKR)]
            kvctx = [sb(ph, f"kvctx{i}", [128, KVW], BF16) for i in range(NCTX)]
            chwin = [sb(ph, f"chwin{i}", [128, 2, 160], BF16) for i in range(NCW)]
            xin = [sb(ph, f"xinB{i}", [128, D], F32) for i in range(2)]
            pT = sb(ph, "pT", [128, 7, 512], BF16)
            pTA = [sb(ph, "pTA", [128, 5, 512], BF16) for _ in range(2)]
            btmp = [sb(ph, f"btmp{i}", [128, 512], F32) for i in range(2)]
            otok_l = [sb(ph, "otok", [128, D], BF16) for _ in range(2)]
            oT_l = [sb(ph, "oT", [128, 8, 128], BF16) for _ in range(2)]
            cvT_l = [sb(ph, "cvT", [128, 2, 128], F32) for _ in range(2)]
            cvn_l = [sb(ph, "cvn", [128, 256], F32) for _ in range(2)]
            f32b_l = [sb(ph, "f32b", [128, D], F32) for _ in range(2)]
            f32c_l = [sb(ph, "f32c", [128, D], F32) for _ in range(2)]
            h2row = [sb(ph, f"h2row{i}", [128, RW], BF16) for i in range(2)]
            h2T_l = [sb(ph, "h2T", [128, 8, 128], BF16) for _ in range(2)]
            den_l = [sb(ph, "den", [128, 4], F32) for _ in range(2)]
            rden_l = [sb(ph, "rden", [128, 4], F32) for _ in range(2)]
            ex_l = [sb(ph, "ex", [128, NE], F32) for _ in range(2)]
            ssum_l = [sb(ph, "ssum", [128, 1], F32) for _ in range(2)]

            for hf in range(1):
                S.dma('pool', lambda q: q.dma_start(out=w_out_sb[:, :, :], in_=w_out[l, :, :].rearrange("(ko ki) n -> ki ko n", ki=128)), w=['w_out_sb'])
            S.dma('pool', lambda q: q.dma_start(out=w_r_sb[:, :, :], in_=w_router[l, :, :].rearrange("(ko ki) n -> ki ko n", ki=128)), w=['w_r_sb'])
            for c3 in range(0, NCHB, 3):
                c4 = min(NCHB, c3 + 3)
                S.dma('pool', lambda q, c3=c3, c4=c4: q.dma_start(out=nat_sb[:, c3:c4, :], in_=natT[l, :, c3 * 512:c4 * 512].rearrange("p (a b) -> p a b", b=512)),
                      w=['nat_sb'])
            S.dma('sp', lambda q: q.dma_start(out=cwT[:, :, :], in_=conv_w[l, :, :].rearrange("c (cc j) -> c cc j", cc=2)), w=['cwT'])
            S.dma('sp', lambda q: q.dma_start(out=convb[:, :], in_=conv_b[l, :, :]), w=['convb'])
            S.dma('sp', lambda q: q.dma_start(out=clng[:, :], in_=conv_ln_g[l:l + 1, :].to_broadcast([128, 256])), w=['clng'])
            S.dma('sp', lambda q: q.dma_start(out=clnb[:, :], in_=conv_ln_b[l:l + 1, :].to_broadcast([128, 256])), w=['clnb'])
            S.dma('sp', lambda q: q.dma_start(out=ln1g[:, :], in_=ln1_g[l:l + 1, :].to_broadcast([128, D])), w=['ln1g'])
            S.dma('sp', lambda q: q.dma_start(out=ln1b[:, :], in_=ln1_b[l:l + 1, :].to_broadcast([128, D])), w=['ln1b'])
            S.dma('sp', lambda q: q.dma_start(out=esink[:, :], in_=a_sink[l:l + 1, :].to_broadcast([128, 8])), w=['esink'])
            S.op('act', lambda e: e.activation(out=esink[:, :], in_=esink[:, :], func=AF.Exp), r=['esink'], w=['esink'])
            for s in range(2):
                for j, chn in enumerate((2, 4, 3)):
                    S.dma('sp', lambda q, s=s, j=j, chn=chn: q.dma_start(out=modB[s][j][:, :], in_=modbc[l, s, :, chn * D:(chn + 1) * D]),
                          r=[('modbc', l, s)], w=[('modB', s, j)])
            for cc in range(2):
                for j in range(31):
                    S.op('pool', lambda e, cc=cc, j=j: e.tensor_scalar(out=cdiag[:, cc, j, :], in0=cst[:, 0:128], scalar1=cwT[:, cc, j:j + 1],
                                                                      scalar2=None, op0=ALU.mult), r=['cst', 'cwT'], w=['cdiag'])
            for c in range(NCTX):
                S.dma('sp', lambda q, c=c: q.dma_start(out=kvctx[c][:, :], in_=scrA[c, :, KA:CH]), r=[('scrA', c)], w=[('kvctx', c)])

            loaded = set()

            def load_tile(t):
                if t in loaded or t < 0 or t >= NT:
                    return
                loaded.add(t)
                if t >= NCTX:
                    S.dma('sp', lambda q: q.dma_start(out=kvring[t % NKR][:, :], in_=scrA[t, :, KA:CH]), r=[('scrA', t)], w=[('kvring', t % NKR)])

            def load_own(t):
                S.dma('sp', lambda q: q.dma_start(out=qring[t % NQR][:, :], in_=scrA[t, :, 0:QW]), r=[('scrAq', t)], w=[('qring', t % NQR)])
                cw, cwk = chwin[t % NCW], ('chwin', t % NCW)
                first = t in (0, NCTX)
                lastt = t in (NCTX - 1, NT - 1)
                S.dma('sp', lambda q: q.dma_start(out=cw[:, :, 16:144], in_=scrA[t, :, CH:CH + 256].rearrange("p (c n) -> p c n", c=2)),
                      r=[('scrA', t)], w=[cwk])
                if first:
                    S.op('pool', lambda e: e.memset(cw[:, :, 0:16], 0.0), w=[cwk])
                else:
                    S.dma('sp', lambda q: q.dma_start(out=cw[:, :, 0:16], in_=scrA[t - 1, :, CH:CH + 256].rearrange("p (c n) -> p c n", c=2)[:, :, 112:128]),
                          r=[('scrA', t - 1)], w=[cwk])
                if lastt:
                    S.op('pool', lambda e: e.memset(cw[:, :, 144:160], 0.0), w=[cwk])
                else:
                    S.dma('sp', lambda q: q.dma_start(out=cw[:, :, 144:160], in_=scrA[t + 1, :, CH:CH + 256].rearrange("p (c n) -> p c n", c=2)[:, :, 0:16]),
                          r=[('scrA', t + 1)], w=[cwk])
                S.dma('sp', lambda q: q.dma_start(out=xin[t % 2][:, :], in_=xsrc(t)), r=[('xs_d',)] if l > 0 else [], w=[('xinB', t % 2)])

            def kvbuf(t):
                if t < NCTX:
                    return kvctx[t], ('kvctx', t)
                return kvring[t % NKR], ('kvring', t % NKR)

            for ti in tiles_b:
                s = 0 if ti < NCTX else 1
                lat = ti >= NCTX
                i = ti - NCTX
                otok = otok_l[ti % 2]
                k_otok = ('otok', ti % 2)
                oT = oT_l[ti % 2]
                k_oT = ('oT', ti % 2)
                cvT = cvT_l[ti % 2]
                k_cvT = ('cvT', ti % 2)
                cvn = cvn_l[ti % 2]
                k_cvn = ('cvn', ti % 2)
                f32b = f32b_l[ti % 2]
                k_f32b = ('f32b', ti % 2)
                f32c = f32c_l[ti % 2]
                k_f32c = ('f32c', ti % 2)
                h2T = h2T_l[ti % 2]
                k_h2T = ('h2T', ti % 2)
                den = den_l[ti % 2]
                k_den = ('den', ti % 2)
                rden = rden_l[ti % 2]
                k_rden = ('rden', ti % 2)
                ex = ex_l[ti % 2]
                k_ex = ('ex', ti % 2)
                ssum = ssum_l[ti % 2]
                k_ssum = ('ssum', ti % 2)
                for t2 in range(ti - 2, ti + 4):
                    if lat and t2 >= NCTX:
                        load_tile(t2)
                load_own(ti)
                qr, qk = qring[ti % NQR], ('qring', ti % NQR)
                cw, cwk = chwin[ti % NCW], ('chwin', ti % NCW)
                xs_, xk = xin[ti % 2], ('xinB', ti % 2)
                hr, hk = h2row[ti % 2], ('h2row', ti % 2)

                if stop_after == f'B{l}:setup':
                    break
                cbk, cbkey = nbank()
                for cc in range(2):
                    for j in range(31):
                        S.op('pe', lambda e, cbk=cbk, cc=cc, j=j: e.matmul(cbk[:, cc * 128:(cc + 1) * 128], lhsT=cdiag[:, cc, j, :],
                                                                          rhs=cw[:, cc, 1 + j:1 + j + 128], start=(j == 0), stop=(j == 30)),
                             r=['cdiag', cwk], w=[cbkey], signal=(cc == 1 and j == 30))
                for cc in range(2):
                    S.op('act', lambda e, cbk=cbk, cc=cc: e.activation(out=cvT[:, cc, :], in_=cbk[:, cc * 128:(cc + 1) * 128], func=AF.Identity,
                                                                      bias=convb[:, cc:cc + 1], scale=1.0), r=[cbkey, 'convb'], w=[k_cvT])
                if lat:
                    chunksA = []
                    if i - 1 >= 0:
                        chunksA.append((ti - 1, 0))
                    chunksA.append((ti, None))
                    if i + 1 < NLAT:
                        chunksA.append((ti + 1, 1))
                    chunksA += [(0, None), (1, None)]
                else:
                    chunksA = [(0, None), (1, None)]
                for grp in range(2):
                    for ci, (ct, mk) in enumerate(chunksA):
                        kb_, kk = kvbuf(ct)
                        bk, bkey = nbank()
                        S.op('pe', lambda e, bk=bk, kb_=kb_: e.matmul(bk[:, :], lhsT=kb_[:, 0:128], rhs=qr[:, QA + grp * 512:QA + (grp + 1) * 512],
                                                                     start=True, stop=True), r=[kk, qk], w=[bkey])
                        S.op('act', lambda e, bk=bk, ci=ci: e.activation(out=pTA[grp][:, ci, :], in_=bk[:, :], func=AF.Exp, scale=SCALE),
                             r=[bkey], w=[('pTA', grp, ci)])
                        if mk is not None:
                            S.op('pool', lambda e, ci=ci, mk=mk: e.tensor_tensor(
                                out=pTA[grp][:, ci, :].rearrange("p (g t) -> p g t", g=4), in0=pTA[grp][:, ci, :].rearrange("p (g t) -> p g t", g=4),
                                in1=masklr[:, mk, :].unsqueeze(1).to_broadcast([128, 4, 128]), op=ALU.mult), r=[('pTA', grp, ci), 'masklr'], w=[('pTA', grp, ci)])
                if lat:
                    if NLAT >= 5 and 2 <= i <= NLAT - 3:
                        chunksB = [(ti + d - 2, d) for d in range(5)]
                    elif i == 0:
                        chunksB = [(NCTX + j, 5 + j) for j in range(4)]
                    elif i == 1:
                        chunksB = [(NCTX + j, 9 + j) for j in range(4)]
                    elif i == NLAT - 2:
                        chunksB = [(NCTX + NLAT - 4 + j, 13 + j) for j in range(4)]
                    else:
                        chunksB = [(NCTX + NLAT - 4 + j, 17 + j) for j in range(4)]
                    chunksB += [(0, None), (1, None)]
                else:
                    chunksB = [(0, None), (1, None)]
                for ci, (ct, ent) in enumerate(chunksB):
                    kb_, kk = kvbuf(ct)
                    bk, bkey = nbank()
                    for h in range(4):
                        p, m = h // 2, h % 2
                        S.op('pe', lambda e, bk=bk, kb_=kb_, h=h, p=p, m=m: e.matmul(
                            bk[:, h * 128:(h + 1) * 128], lhsT=kb_[:, KB - KA + p * 128:KB - KA + (p + 1) * 128],
                            rhs=qr[:, QB + m * 256 + p * 128:QB + m * 256 + (p + 1) * 128], start=True, stop=True),
                             r=[kk, qk], w=[bkey], signal=(h == 3))
                    if ent is not None:
                        bt, btk = btmp[ci % 2], ('btmp', ci % 2)
                        S.op('dve', lambda e, bk=bk, bt=bt, ent=ent: e.scalar_tensor_tensor(
                            out=bt[:, :], in0=bk[:, :], scalar=SCALE, in1=nat_sb[:, ent, :], op0=ALU.mult, op1=ALU.add),
                             r=[bkey, 'nat_sb'], w=[btk])
                        S.op('act', lambda e, bt=bt, ci=ci: e.activation(out=pT[:, ci, :], in_=bt[:, :], func=AF.Exp), r=[btk], w=[('pT', ci)])
                    else:
                        S.op('act', lambda e, bk=bk, ci=ci: e.activation(out=pT[:, ci, :], in_=bk[:, :], func=AF.Exp, scale=SCALE),
                             r=[bkey], w=[('pT', ci)])
                for grp in range(2):
                    ob, obk = nbank()
                    nchk = len(chunksA)
                    for g in range(4):
                        for ci, (ct, mk) in enumerate(chunksA):
                            kb_, kk = kvbuf(ct)
                            S.op('pe', lambda e, g=g, ci=ci, kb_=kb_, ob=ob: e.matmul(
                                ob[:, g * 65:(g + 1) * 65], lhsT=pTA[grp][:, ci, g * 128:(g + 1) * 128],
                                rhs=kb_[:, VA - KA + grp * 65:VA - KA + (grp + 1) * 65], start=(ci == 0), stop=(ci == nchk - 1)),
                                 r=[('pTA', grp, ci), kk], w=[obk], signal=(g == 3 and ci == nchk - 1))
                    obv = ob[:, 0:260].rearrange("p (g d) -> p g d", d=65)
                    S.op('dve', lambda e, obv=obv: e.tensor_tensor(out=den[:, :], in0=obv[:, :, 64], in1=esink[:, grp * 4:(grp + 1) * 4], op=ALU.add),
                         r=[obk, 'esink'], w=[k_den])
                    S.op('dve', lambda e: e.reciprocal(out=rden[:, :], in_=den[:, :]), r=[k_den], w=[k_rden])
                    S.op('dve', lambda e, obv=obv: e.tensor_tensor(
                        out=otok[:, grp * 256:(grp + 1) * 256].rearrange("p (g d) -> p g d", d=64), in0=obv[:, :, 0:64],
                        in1=rden[:, :].unsqueeze(2).to_broadcast([128, 4, 64]), op=ALU.mult), r=[obk, k_rden], w=[k_otok])
                ob, obk = nbank()
                nchk = len(chunksB)
                for h in range(4):
                    for ci, (ct, ent) in enumerate(chunksB):
                        kb_, kk = kvbuf(ct)
                        S.op('pe', lambda e, h=h, ci=ci, kb_=kb_, ob=ob: e.matmul(
                            ob[:, h * 65:(h + 1) * 65], lhsT=pT[:, ci, h * 128:(h + 1) * 128],
                            rhs=kb_[:, VB - KA + h * 65:VB - KA + (h + 1) * 65], start=(ci == 0), stop=(ci == nchk - 1)),
                             r=[('pT', ci), kk], w=[obk], signal=(h == 3 and ci == nchk - 1))
                obv = ob[:, 0:260].rearrange("p (g d) -> p g d", d=65)
                S.op('dve', lambda e, obv=obv: e.reciprocal(out=rden[:, :], in_=obv[:, :, 64]), r=[obk], w=[k_rden])
                S.op('dve', lambda e, obv=obv: e.tensor_tensor(
                    out=otok[:, 512:768].rearrange("p (g d) -> p g d", d=64), in0=obv[:, :, 0:64],
                    in1=rden[:, :].unsqueeze(2).to_broadcast([128, 4, 64]), op=ALU.mult), r=[obk, k_rden], w=[k_otok])
                bk2, bkey2 = nbank()
                for cc in range(2):
                    S.op('pe', lambda e, bk2=bk2, cc=cc: e.transpose(out=bk2[:, cc * 128:(cc + 1) * 128], in_=cvT[:, cc, :], identity=identf),
                         r=[k_cvT, 'cst'], w=[bkey2], signal=(cc == 1))
                ln_stats(bk2, bkey2, width=256)
                ln_apply(cvn[:, :], k_cvn, bk2[:, 0:256], bkey2)
                S.op('dve', lambda e: e.tensor_tensor(out=cvn[:, :], in0=cvn[:, :], in1=clng[:, :], op=ALU.mult), r=[k_cvn, 'clng'], w=[k_cvn])
                S.op('pool', lambda e: e.tensor_tensor(out=cvn[:, :], in0=cvn[:, :], in1=clnb[:, :], op=ALU.add), r=[k_cvn, 'clnb'], w=[k_cvn])
                S.op('act', lambda e: e.activation(out=otok[:, 768:1024], in_=cvn[:, :], func=AF.Silu), r=[k_cvn], w=[k_otok])

                if dbg:
                    S.dma('sp', lambda q: q.dma_start(out=otok_d[ti * 128:(ti + 1) * 128, :], in_=otok[:, :]), r=[k_otok], w=[('dbg_otok', ti)])

                if stop_after == f'B{l}:conv':
                    break
                transposes_to(oT[:, :, :].rearrange("p k t -> p (k t)"), k_oT, lambda k: otok[:, k * 128:(k + 1) * 128], 8, src_key=k_otok)
                for n in range(2):
                    bk, bkey = nbank()
                    for k in range(8):
                        S.op('pe', lambda e, bk=bk, k=k, n=n: e.matmul(bk[:, :], lhsT=oT[:, k, :], rhs=w_out_sb[:, k, n * 512:(n + 1) * 512],
                                                                     start=(k == 0), stop=(k == 7)), r=[k_oT, 'w_out_sb'], w=[bkey], signal=(k == 7))
                    S.op('dve', lambda e, bk=bk, n=n: e.tensor_tensor(out=f32b[:, n * 512:(n + 1) * 512], in0=bk[:, :],
                                                                     in1=modB[s][0][:, n * 512:(n + 1) * 512], op=ALU.mult),
                         r=[bkey, ('modB', s, 0)], w=[k_f32b])
                S.op('dve', lambda e: e.scalar_tensor_tensor(out=f32b[:, :], in0=xs_[:, :], scalar=ALPHA, in1=f32b[:, :], op0=ALU.mult, op1=ALU.add),
                     r=[xk, k_f32b], w=[k_f32b])
                ln_stats(f32b, k_f32b)
                ln_apply(f32c[:, :], k_f32c, f32b[:, :], k_f32b)
                S.op('dve', lambda e: e.tensor_tensor(out=f32c[:, :], in0=f32c[:, :], in1=ln1g[:, :], op=ALU.mult), r=[k_f32c, 'ln1g'], w=[k_f32c])
                S.op('pool', lambda e: e.tensor_tensor(out=f32c[:, :], in0=f32c[:, :], in1=ln1b[:, :], op=ALU.add), r=[k_f32c, 'ln1b'], w=[k_f32c])
                S.dma('sp', lambda q: q.dma_start(out=x1s[ti * 128:(ti + 1) * 128, :], in_=f32c[:, :]), r=[k_f32c], w=[('x1s', ti)])
                if stop_after == f'B{l}:proj':
                    break
                ln_stats(f32c, k_f32c)
                ln_apply(f32b[:, :], k_f32b, f32c[:, :], k_f32c)
                S.op('dve', lambda e: e.tensor_tensor(out=f32b[:, :], in0=f32b[:, :], in1=modB[s][1][:, :], op=ALU.mult),
                     r=[k_f32b, ('modB', s, 1)], w=[k_f32b])
                S.op('pool', lambda e: e.tensor_tensor(out=hr[:, 0:1024], in0=f32b[:, :], in1=modB[s][2][:, :], op=ALU.add),
                     r=[k_f32b, ('modB', s, 2)], w=[hk])
                transposes_to(h2T[:, :, :].rearrange("p k t -> p (k t)"), k_h2T, lambda k: hr[:, k * 128:(k + 1) * 128], 8, src_key=hk)
                bk, bkey = nbank()
                for k in range(8):
                    S.op('pe', lambda e, bk=bk, k=k: e.matmul(bk[:, 0:NE], lhsT=h2T[:, k, :], rhs=w_r_sb[:, k, :], start=(k == 0), stop=(k == 7)),
                         r=[k_h2T, 'w_r_sb'], w=[bkey], signal=(k == 7))
                S.op('act', lambda e, bk=bk: e.activation(out=ex[:, :], in_=bk[:, 0:NE], func=AF.Exp, accum_out=ssum[:, 0:1]), r=[bkey], w=[k_ex, k_ssum])
                S.op('dve', lambda e: e.reciprocal(out=ssum[:, :], in_=ssum[:, :]), r=[k_ssum], w=[k_ssum])
                S.op('dve', lambda e: e.tensor_scalar(out=aff_all[:, ti, :], in0=ex[:, :], scalar1=ssum[:, 0:1], scalar2=None, op0=ALU.mult),
                     r=[k_ex, k_ssum], w=[('aff', ti)])
                S.op('dve', lambda e: e.tensor_copy(out=hr[:, 1026:1042], in_=aff_all[:, ti, :]), r=[('aff', ti)], w=[hk])
                S.op('dve', lambda e: e.tensor_tensor(out=hr[:, 1042:1058], in0=aff_all[:, ti, :], in1=hr[:, 1026:1042], op=ALU.subtract),
                     r=[('aff', ti), hk], w=[hk])
                S.op('dve', lambda e: e.tensor_scalar(out=hr[:, 1024:1025], in0=iotap, scalar1=0.0, scalar2=float(ti), op0=ALU.mult, op1=ALU.add),
                     r=['cst'], w=[hk])
                S.op('dve', lambda e: e.tensor_copy(out=hr[:, 1025:1026], in_=iotap), r=['cst'], w=[hk])
                S.dma('sp', lambda q: q.dma_start(out=h2s[ti * 128:(ti + 1) * 128, :], in_=hr[:, :]), r=[hk], w=[('h2s', ti)])
            S.barrier()
        if stop_after is not None and stop_after.startswith(f'B{l}'):
            return _finish(nc, S, es)

        NTB = len(tiles_b)
        t0b = tiles_b[0]
        with ExitStack() as ph:
            cmpb = sb(ph, "cmpb", [128, NT, NE], BF16)
            lo = sb(ph, "lo", [128, 32], F32)
            hi = sb(ph, "hi", [128, 32], F32)
            mid = sb(ph, "mid", [128, 32], F32)
            kvec = sb(ph, "kvec", [128, 32], F32)
            cntp = sb(ph, "cntp", [128, 32], BF16)
            ge = sb(ph, "ge", [128, 32], F32)
            gm = sb(ph, "gm", [128, 32], F32)
            posf = sb(ph, "posf", [128, NT, NE], F32)
            tot = sb(ph, "tot", [128, NT, NE], F32)
            base = sb(ph, "base", [128, NT + 1, NE], F32)
            sel = sb(ph, "sel", [128, NT, NE], F32)
            posi = sb(ph, "posi", [128, NT, NE], I32)
            affk = [('aff', t) for t in range(NT)]
            S.op('dve', lambda e: e.memset(lo[:, :], 0.0), w=['lo'])
            S.op('dve', lambda e: e.memset(hi[:, :], 1.0), w=['hi'])
            S.op('dve', lambda e: e.memset(kvec[:, 0:16], float(CAPL)), w=['kvec'])
            S.op('dve', lambda e: e.memset(kvec[:, 16:32], float(CAPC)), w=['kvec'])
            S.op('dve', lambda e: e.memset(cntp[:, :], 0.0), w=['cntp'])
            if last:
                S.op('dve', lambda e: e.memset(cmpb[:, 0:NCTX, :], 0.0), w=['cmpb'])
            lat_aff = aff_all[:, NCTX:NT, :]
            ctx_aff = aff_all[:, 0:NCTX, :]

            def compare(thr):
                S.op('dve', lambda e: e.tensor_tensor(out=cmpb[:, NCTX:NT, :], in0=lat_aff, in1=thr[:, 0:16].unsqueeze(1).to_broadcast([128, NLAT, NE]),
                                                      op=ALU.is_ge), r=affk + ['thr'], w=['cmpb'])
                if not last:
                    S.op('dve', lambda e: e.tensor_tensor(out=cmpb[:, 0:NCTX, :], in0=ctx_aff, in1=thr[:, 16:32].unsqueeze(1).to_broadcast([128, NCTX, NE]),
                                                          op=ALU.is_ge), r=affk + ['thr'], w=['cmpb'])

            for itn in range(30):
                S.op('dve', lambda e: e.tensor_tensor(out=mid[:, :], in0=lo[:, :], in1=hi[:, :], op=ALU.add), r=['lo', 'hi'], w=['thr'])
                S.op('dve', lambda e: e.tensor_scalar(out=mid[:, :], in0=mid[:, :], scalar1=0.5, scalar2=None, op0=ALU.mult), r=['thr'], w=['thr'])
                compare(mid)
                S.op('dve', lambda e: e.tensor_reduce(out=cntp[:, 0:16], in_=cmpb[:, NCTX:NT, :].rearrange("p t e -> p e t"), axis=AX.X, op=ALU.add),
                     r=['cmpb'], w=['cntp'])
                if not last:
                    S.op('dve', lambda e: e.tensor_reduce(out=cntp[:, 16:32], in_=cmpb[:, 0:NCTX, :].rearrange("p t e -> p e t"), axis=AX.X, op=ALU.add),
                         r=['cmpb'], w=['cntp'])
                bk, bkey = nbank()
                S.op('pe', lambda e, bk=bk: e.matmul(bk[:, 0:32], lhsT=onesb[:, :], rhs=cntp[:, :], start=True, stop=True), r=['onesb', 'cntp'], w=[bkey])
                S.op('dve', lambda e, bk=bk: e.tensor_tensor(out=ge[:, :], in0=bk[:, 0:32], in1=kvec[:, :], op=ALU.is_ge), r=[bkey, 'kvec'], w=['ge'])
                S.op('dve', lambda e: e.tensor_tensor(out=gm[:, :], in0=ge[:, :], in1=mid[:, :], op=ALU.mult), r=['ge', 'thr'], w=['gm'])
                S.op('dve', lambda e: e.tensor_tensor(out=lo[:, :], in0=lo[:, :], in1=gm[:, :], op=ALU.max), r=['lo', 'gm'], w=['lo'])
                S.op('dve', lambda e: e.scalar_tensor_tensor(out=gm[:, :], in0=ge[:, :], scalar=2.0, in1=mid[:, :], op0=ALU.mult, op1=ALU.add),
                     r=['ge', 'thr', 'gm'], w=['gm'])
                S.op('dve', lambda e: e.tensor_tensor(out=hi[:, :], in0=hi[:, :], in1=gm[:, :], op=ALU.min), r=['hi', 'gm'], w=['hi'])
            S.op('dve', lambda e: e.tensor_copy(out=mid[:, :], in_=lo[:, :]), r=['lo'], w=['thr'])
            compare(mid)
            cflat = cmpb[:, :, :].rearrange("p t e -> p (t e)")
            pflat = posf[:, :, :].rearrange("p t e -> p (t e)")
            tflat = tot[:, :, :].rearrange("p t e -> p (t e)")
            ncol = NT * NE
            for c0 in range(0, ncol, 512):
                cs = min(512, ncol - c0)
                bk, bkey = nbank()
                S.op('pe', lambda e, bk=bk, c0=c0, cs=cs: e.matmul(bk[:, 0:cs], lhsT=triub[:, :], rhs=cflat[:, c0:c0 + cs], start=True, stop=True),
                     r=['triub', 'cmpb'], w=[bkey])
                S.op('act', lambda e, bk=bk, c0=c0, cs=cs: e.copy(out=pflat[:, c0:c0 + cs], in_=bk[:, 0:cs]), r=[bkey], w=['posf'])
                bk, bkey = nbank()
                S.op('pe', lambda e, bk=bk, c0=c0, cs=cs: e.matmul(bk[:, 0:cs], lhsT=onesb[:, :], rhs=cflat[:, c0:c0 + cs], start=True, stop=True),
                     r=['onesb', 'cmpb'], w=[bkey])
                S.op('act', lambda e, bk=bk, c0=c0, cs=cs: e.copy(out=tflat[:, c0:c0 + cs], in_=bk[:, 0:cs]), r=[bkey], w=['tot'])
            S.op('dve', lambda e: e.memset(base[:, 0, :], float(CAPL)), w=['base'])
            S.op('dve', lambda e: e.memset(base[:, NCTX, :], 0.0), w=['base'])
            for t in range(NT):
                if t == NCTX - 1:
                    continue
                S.op('dve', lambda e, t=t: e.tensor_tensor(out=base[:, t + 1, :], in0=base[:, t, :], in1=tot[:, t, :], op=ALU.add),
                     r=['base', 'tot'], w=['base'])
            S.op('dve', lambda e: e.tensor_tensor(out=posf[:, :, :], in0=posf[:, :, :], in1=base[:, 0:NT, :], op=ALU.add), r=['posf', 'base'], w=['posf'])
            S.op('dve', lambda e: e.scalar_tensor_tensor(out=sel[:, NCTX:NT, :], in0=posf[:, NCTX:NT, :], scalar=float(CAPL), in1=cmpb[:, NCTX:NT, :],
                                                         op0=ALU.is_lt, op1=ALU.mult), r=['posf', 'cmpb'], w=['sel'])
            S.op('dve', lambda e: e.scalar_tensor_tensor(out=sel[:, 0:NCTX, :], in0=posf[:, 0:NCTX, :], scalar=float(CAPL + CAPC), in1=cmpb[:, 0:NCTX, :],
                                                         op0=ALU.is_lt, op1=ALU.mult), r=['posf', 'cmpb'], w=['sel'])
            S.op('dve', lambda e: e.scalar_tensor_tensor(out=posf[:, :, :], in0=posf[:, :, :], scalar=-BIG, in1=sel[:, :, :], op0=ALU.add, op1=ALU.mult),
                 r=['posf', 'sel'], w=['posf'])
            S.op('dve', lambda e: e.tensor_scalar(out=posi[:, :, :], in0=posf[:, :, :], scalar1=BIG, scalar2=None, op0=ALU.add), r=['posf'], w=['posi'])

            h2ld = [sb(ph, f"h2ld{i}", [128, RW], BF16) for i in range(3)]
            breg = nc.gpsimd.to_reg(CAPT - 1)
            for n, ti in enumerate(tiles_b):
                hl, hlk = h2ld[n % 3], ('h2ld', n % 3)
                S.dma('sp', lambda q, ti=ti, hl=hl: q.dma_start(out=hl[:, :], in_=h2s[ti * 128:(ti + 1) * 128, :]), r=[('h2s', ti)], w=[hlk])
                for ex_ in range(NE):
                    S.dma('pool', lambda q, ti=ti, ex_=ex_, hl=hl: q.indirect_dma_start(
                        out=Xs[ex_][:, :], out_offset=bass.IndirectOffsetOnAxis(ap=posi[:, ti, ex_:ex_ + 1], axis=0),
                        in_=hl[:, :], in_offset=None, bounds_check=breg, oob_is_err=False), r=[hlk, 'posi'], w=[('Xs', ex_)])
            S.barrier()
        if stop_after == f'R{l}':
            return _finish(nc, S, es)

        NST = (CAPT + 127) // 128
        CAPP = NST * 128
        NSL = 3 if CAPP % 3 == 0 and CAPP // 3 <= 512 else (CAPP + 511) // 512
        SLW = CAPP // NSL
        assert SLW * NSL == CAPP and SLW <= 512
        with ExitStack() as ph:
            wg = [sb(ph, f"wg{i}", [128, 8, D], BF16) for i in range(2)]
            wu = [sb(ph, f"wu{i}", [128, 8, D], BF16) for i in range(2)]
            wd = [sb(ph, f"wd{i}", [128, 8, D], BF16) for i in range(2)]
            xsb = sb(ph, "xsb", [128, NST, RW], BF16)
            XT = sb(ph, "XT", [128, 8, NST * 128], BF16)
            hidT = sb(ph, "hidT", [128, 8, NST * 128], BF16)
            sgt = [sb(ph, f"sgt{i}", [128, 512], F32) for i in range(2)]
            ysb = [sb(ph, f"ysb{i}", [128, D], F32) for i in range(3)]
            gcol = sb(ph, "gcol", [128, NST], F32)
            idxi = sb(ph, "idxi", [128, NST], I32)
            S.op('pool', lambda e: e.memset(xsb[:, NST - 1, :], 0.0), w=['xsb'])
            zt = ysb[0]
            S.op('pool', lambda e: e.memset(zt[:, :], 0.0), w=[('ysb', 0)])
            ztoks = []
            for ti in tiles_b:
                ztoks.append(S.dma('sp', lambda q, ti=ti: q.dma_start(out=macc[ti * 128:(ti + 1) * 128, :], in_=zt[:, :]),
                                   r=[('ysb', 0), ('macc_rd', ti)], w=[('macc_z', ti)]))
            prev_sc = list(ztoks)

            def load_w(ex_):
                sl = ex_ % 2
                for wsb, wdr, nm in ((wg, w_gate, 'wg'), (wu, w_up, 'wu'), (wd, w_down, 'wd')):
                    S.dma('pool', lambda q, wsb=wsb, wdr=wdr: q.dma_start(
                        out=wsb[sl][:, :, :], in_=wdr[l, ex_, :, :].rearrange("(ko ki) n -> ki ko n", ki=128)), w=[(nm, sl)])

            load_w(0)
            ycount = 0
            for ex_ in range(NE):
                sl = ex_ % 2
                if ex_ + 1 < NE:
                    load_w(ex_ + 1)
                nfull = CAPT // 128
                rem = CAPT - nfull * 128
                S.dma('sp', lambda q, ex_=ex_: q.dma_start(out=xsb[:, 0:nfull, :], in_=Xs[ex_][0:nfull * 128, :].rearrange("(s p) w -> p s w", p=128)),
                      r=[('Xs', ex_)], w=['xsb'])
                if rem:
                    S.dma('sp', lambda q, ex_=ex_: q.dma_start(out=xsb[0:rem, nfull, :], in_=Xs[ex_][nfull * 128:CAPT, :]), r=[('Xs', ex_)], w=['xsb'])
                S.op('dve', lambda e, ex_=ex_: e.tensor_tensor(out=gcol[:, 0:nfull], in0=xsb[:, 0:nfull, 1026 + ex_], in1=xsb[:, 0:nfull, 1042 + ex_], op=ALU.add),
                     r=['xsb'], w=['gcol'])
                S.op('dve', lambda e: e.scalar_tensor_tensor(out=idxi[:, 0:nfull], in0=xsb[:, 0:nfull, 1024], scalar=128.0, in1=xsb[:, 0:nfull, 1025],
                                                             op0=ALU.mult, op1=ALU.add), r=['xsb'], w=['idxi'])
                if rem:
                    S.op('dve', lambda e, ex_=ex_: e.tensor_tensor(out=gcol[0:rem, nfull:nfull + 1], in0=xsb[0:rem, nfull, 1026 + ex_:1027 + ex_],
                                                                  in1=xsb[0:rem, nfull, 1042 + ex_:1043 + ex_], op=ALU.add), r=['xsb'], w=['gcol'])
                    S.op('dve', lambda e: e.scalar_tensor_tensor(out=idxi[0:rem, nfull:nfull + 1], in0=xsb[0:rem, nfull, 1024:1025], scalar=128.0,
                                                                 in1=xsb[0:rem, nfull, 1025:1026], op0=ALU.mult, op1=ALU.add), r=['xsb'], w=['idxi'])
                for st in range(NST):
                    rows = 128 if st < nfull else rem
                    transposes_to(XT[:, :, st * 128:(st + 1) * 128], 'XT', lambda k, st=st: xsb[:, st, k * 128:(k + 1) * 128], 8,
                                  src_key='xsb', evac=('act' if st % 2 == 0 else 'dve'))
                for fc in range(8):
                    for sn in range(NSL):
                        n0 = sn * SLW
                        bg, bgk = nbank()
                        for k in range(8):
                            S.op('pe', lambda e, bg=bg, k=k, fc=fc, n0=n0: e.matmul(bg[:, 0:SLW], lhsT=wg[sl][:, k, fc * 128:(fc + 1) * 128],
                                                                                   rhs=XT[:, k, n0:n0 + SLW], start=(k == 0), stop=(k == 7)),
                                 r=[('wg', sl), 'XT'], w=[bgk], signal=(k == 7))
                        bu, buk = nbank()
                        for k in range(8):
                            S.op('pe', lambda e, bu=bu, k=k, fc=fc, n0=n0: e.matmul(bu[:, 0:SLW], lhsT=wu[sl][:, k, fc * 128:(fc + 1) * 128],
                                                                                   rhs=XT[:, k, n0:n0 + SLW], start=(k == 0), stop=(k == 7)),
                                 r=[('wu', sl), 'XT'], w=[buk], signal=(k == 7))
                        sgi = (fc * NSL + sn) % 2
                        S.op('act', lambda e, bg=bg, sgi=sgi: e.activation(out=sgt[sgi][:, 0:SLW], in_=bg[:, 0:SLW], func=AF.Silu), r=[bgk], w=[('sgt', sgi)])
                        S.op('dve', lambda e, bu=bu, sgi=sgi, fc=fc, n0=n0: e.tensor_tensor(out=hidT[:, fc, n0:n0 + SLW], in0=bu[:, 0:SLW], in1=sgt[sgi][:, 0:SLW],
                                                                                           op=ALU.mult), r=[buk, ('sgt', sgi)], w=['hidT'])
                cur_sc = []
                for st in range(NST):
                    rows = 128 if st < nfull else rem
                    yi = ycount % 3
                    ycount += 1
                    for half in range(2):
                        by, byk = nbank()
                        for fc in range(8):
                            S.op('pe', lambda e, by=by, fc=fc, st=st, rows=rows, half=half: e.matmul(
                                by[:, :], lhsT=hidT[:, fc, st * 128:(st + 1) * 128], rhs=wd[sl][:, fc, half * 512:(half + 1) * 512],
                                start=(fc == 0), stop=(fc == 7)), r=['hidT', ('wd', sl)], w=[byk], signal=(fc == 7))
                        if half == 0:
                            S.op('act', lambda e, by=by, yi=yi, st=st, rows=rows: e.activation(out=ysb[yi][0:rows, 0:512], in_=by[0:rows, :], func=AF.Copy,
                                                                                              scale=gcol[0:rows, st:st + 1]), r=[byk, 'gcol'], w=[('ysb', yi)])
                        else:
                            S.op('dve', lambda e, by=by, yi=yi, st=st, rows=rows: e.tensor_scalar(out=ysb[yi][0:rows, 512:1024], in0=by[0:rows, :],
                                                                                                 scalar1=gcol[0:rows, st:st + 1], scalar2=None, op0=ALU.mult),
                                 r=[byk, 'gcol'], w=[('ysb', yi)])
                    cur_sc.append(S.dma('pool', lambda q, yi=yi, st=st, rows=rows: q.indirect_dma_start(
                        out=macc[:, :], out_offset=bass.IndirectOffsetOnAxis(ap=idxi[0:rows, st:st + 1], axis=0),
                        in_=ysb[yi][0:rows, :], in_offset=None, compute_op=ALU.add), r=[('ysb', yi), 'idxi'], w=[('macc_sc', ex_, st)], extra=prev_sc))
                prev_sc = cur_sc
            S.barrier()
        if stop_after == f'M{l}':
            return _finish(nc, S, es)

        with ExitStack() as ph:
            g2 = [sb(ph, f"g2_{s}", [128, D], F32) for s in range(2)]
            ln2g = sb(ph, "ln2g", [128, D], F32)
            ln2b = sb(ph, "ln2b", [128, D], F32)
            xa = [sb(ph, f"xa{i}", [128, D], F32) for i in range(2)]
            xm = [sb(ph, f"xm{i}", [128, D], F32) for i in range(2)]
            xo = [sb(ph, f"xo{i}", [128, D], F32) for i in range(2)]
            for s in range(2):
                S.dma('sp', lambda q, s=s: q.dma_start(out=g2[s][:, :], in_=modbc[l, s, :, 5 * D:6 * D]), r=[('modbc', l, s)], w=[('g2', s)])
            S.dma('sp', lambda q: q.dma_start(out=ln2g[:, :], in_=ln2_g[l:l + 1, :].to_broadcast([128, D])), w=['ln2g'])
            S.dma('sp', lambda q: q.dma_start(out=ln2b[:, :], in_=ln2_b[l:l + 1, :].to_broadcast([128, D])), w=['ln2b'])
            out_toks = []
            for n, ti in enumerate(tiles_b):
                s = 0 if ti < NCTX else 1
                a, ak = xa[n % 2], ('xa', n % 2)
                m, mk = xm[n % 2], ('xm', n % 2)
                o, ok = xo[n % 2], ('xo', n % 2)
                S.dma('sp', lambda q, ti=ti, a=a: q.dma_start(out=a[:, :], in_=x1s[ti * 128:(ti + 1) * 128, :]), r=[('x1s', ti)], w=[ak])
                S.dma('sp', lambda q, ti=ti, m=m: q.dma_start(out=m[:, :], in_=macc[ti * 128:(ti + 1) * 128, :]), w=[mk, ('macc_rd', ti)], extra=prev_sc)
                S.op('dve', lambda e, m=m, s=s: e.tensor_tensor(out=m[:, :], in0=m[:, :], in1=g2[s][:, :], op=ALU.mult), r=[mk, ('g2', s)], w=[mk])
                S.op('dve', lambda e, m=m, a=a: e.scalar_tensor_tensor(out=m[:, :], in0=a[:, :], scalar=ALPHA, in1=m[:, :], op0=ALU.mult, op1=ALU.add),
                     r=[ak, mk], w=[mk])
                ln_stats(m, mk)
                ln_apply(o[:, :], ok, m[:, :], mk)
                S.op('dve', lambda e, o=o: e.tensor_tensor(out=o[:, :], in0=o[:, :], in1=ln2g[:, :], op=ALU.mult), r=[ok, 'ln2g'], w=[ok])
                S.op('pool', lambda e, o=o: e.tensor_tensor(out=o[:, :], in0=o[:, :], in1=ln2b[:, :], op=ALU.add), r=[ok, 'ln2b'], w=[ok])
                if last:
                    i = ti - NCTX
                    out_toks.append(S.dma('sp', lambda q, i=i, o=o: q.dma_start(out=out[i * 128:(i + 1) * 128, :], in_=o[:, :]), r=[ok], w=[('out', i)]))
                else:
                    S.dma('sp', lambda q, ti=ti, o=o: q.dma_start(out=xs_d[ti * 128:(ti + 1) * 128, :], in_=o[:, :]), r=[ok], w=[('xs_d',)])
            S.barrier()
        if stop_after == f'F{l}':
            return _finish(nc, S, es)
    return _finish(nc, S, es)


def _finish(nc, S, es):
    S.barrier()
    es.close()
    return nc


def _consts():
    c = np.zeros((128, 6 * 128), np.float32)
    p = np.arange(128)
    c[:, 0:128] = np.eye(128, dtype=np.float32)
    c[:, 128:256] = (p[:, None] < p[None, :]).astype(np.float32)
    c[:, 256:384] = (p[:, None] >= p[None, :]).astype(np.float32)
    c[:, 384:512] = (p[:, None] <= p[None, :]).astype(np.float32)
    c[:, 512:640] = 1.0
    c[:, 640] = p.astype(np.float32)
    c[:, 641] = (p < 64).astype(np.float32)
    c[:, 642] = (p >= 64).astype(np.float32)
    return c


def _rope_table(NLAT):
    t = np.arange(NLAT * 128, dtype=np.int32)
    row = (t // 64).astype(np.float32)[:, None]
    col = (t % 64).astype(np.float32)[:, None]
    inv = (np.float32(10000.0) ** (-np.arange(16, dtype=np.float32) / np.float32(16))).astype(np.float32)
    ar = (row * inv).astype(np.float32)
    ac = (col * inv).astype(np.float32)
    cr, sr, cc, sc = np.cos(ar), np.sin(ar), np.cos(ac), np.sin(ac)
    tab = np.concatenate([cr, cr, cc, cc, -sr, sr, -sc, sc], axis=1).astype(np.float32)
    return np.ascontiguousarray(tab.reshape(NLAT, 128, 128))


def _nat_entries(NLAT):
    ents = [(2, d) for d in range(5)] if NLAT >= 5 else [(0, 0)] * 5
    ents = [(2, 2 + d - 2) for d in range(5)] if NLAT >= 5 else ents
    ents += [(0, j) for j in range(4)] + [(1, j) for j in range(4)]
    ents += [(NLAT - 2, NLAT - 4 + j) for j in range(4)] + [(NLAT - 1, NLAT - 4 + j) for j in range(4)]
    return ents


def _nat_table(nat_bias, NLAT):
    rows = NLAT * 2
    ents = _nat_entries(NLAT)
    kk = np.arange(128)
    Lh = nat_bias.shape[0]
    out = np.empty((Lh, 128, NCHB, 4, 128), np.float32)
    for n, (i, j) in enumerate(ents):
        kr = 2 * j + kk // 64
        kc = kk % 64
        qr = 2 * i + kk // 64
        qc = kk % 64
        rs = np.clip(qr - 4, 0, rows - 8)
        cs = np.clip(qc - 8, 0, 48)
        valid = ((kr[:, None] >= rs[None, :]) & (kr[:, None] < rs[None, :] + 8) &
                 (kc[:, None] >= cs[None, :]) & (kc[:, None] < cs[None, :] + 16))
        dr = np.clip(kr[:, None] - qr[None, :] + 7, 0, 14)
        dc = np.clip(kc[:, None] - qc[None, :], -15, 15) + 15
        g = nat_bias[:, :, dr, dc]
        g = np.where(valid[None, None], g, np.float32(NEG))
        out[:, :, n, :, :] = np.transpose(g, (0, 2, 1, 3))
    return np.ascontiguousarray(out.reshape(Lh, 128, NCHB * 512))


def make_in_maps(inputs, NLAT, samples, moe=True):
    names = ['w_mod', 'b_mod', 'w_in', 'a_sink', 'conv_w', 'conv_b', 'conv_ln_g', 'conv_ln_b', 'w_out', 'ln1_g', 'ln1_b',
             'w_router', 'ln2_g', 'ln2_b'] + (['w_gate', 'w_up', 'w_down'] if moe else [])
    shared = {k: np.ascontiguousarray(np.asarray(inputs[k], np.float32)) for k in names}
    cw = shared['conv_w']
    shared['conv_w'] = np.ascontiguousarray(cw.reshape(L, 31, 2, 128).transpose(0, 3, 2, 1).reshape(L, 128, 62))
    shared['conv_b'] = np.ascontiguousarray(shared['conv_b'].reshape(L, 2, 128).transpose(0, 2, 1))
    shared['natT'] = _nat_table(np.asarray(inputs['nat_bias'], np.float32), NLAT)
    shared['rope'] = _rope_table(NLAT)
    shared['consts'] = _consts()
    maps = []
    for b in samples:
        m = dict(shared)
        m['xlat'] = np.ascontiguousarray(np.asarray(inputs['x'][b, :NLAT * 128], np.float32))
        m['xctx'] = np.ascontiguousarray(np.asarray(inputs['ctx'][b], np.float32))
        cv = np.stack([np.asarray(inputs['c_ctx'], np.float32), np.asarray(inputs['c'][b], np.float32)])
        m['cvec'] = np.ascontiguousarray(cv.reshape(2, 8, 128).transpose(2, 1, 0).reshape(128, 16))
        maps.append(m)
    return maps


def kernel(**inputs):
    NLAT = 64
    nc = build_program(NLAT)
    maps = make_in_maps(inputs, NLAT, [0, 1, 2, 3, 0, 1, 2, 3])
    res = run_bass_kernel_spmd(nc, maps, core_ids=list(range(8)))
    return np.stack([np.asarray(res.results[b]["out"], np.float32).reshape(NLAT * 128, D) for b in range(4)])
```

```python
import os
import numpy as np
from contextlib import ExitStack
import concourse.bass as bass
import concourse.mybir as mybir
from concourse.bass_utils import run_bass_kernel_spmd

F32 = mybir.dt.float32
BF16 = mybir.dt.bfloat16
I32 = mybir.dt.int32
AF = mybir.ActivationFunctionType
ALU = mybir.AluOpType
AX = mybir.AxisListType

D = 1024
L = 2
NCTX = 2
NE = 16
EPS = 1e-6
ALPHA = float((2 * L) ** 0.25)
SCALE = 0.125
NEG = -30000.0
BIG = 1.0e6
QA, QB, KA, VA, KB, VB, CH, AW = 0, 1024, 1536, 1664, 1794, 2050, 2310, 2566
QW = KA
KVW = CH - KA
RW = 1024 + 2 + 32
NCHB = 21


class Sched:
    EPOCH = 16000

    def __init__(self, nc, es, ndma=16):
        self.nc, self.es = nc, es
        self.eng = dict(pe=nc.tensor, act=nc.scalar, dve=nc.vector, pool=nc.gpsimd, sp=nc.sync)
        self.sems = {k: [] for k in self.eng}
        self.cnt = {k: 0 for k in self.eng}
        self.dpool = {'sp': list(range(0, ndma)), 'pool': list(range(ndma, ndma + 8)), 'act': list(range(ndma + 8, ndma + 12))}
        ntot = ndma + 12
        self.dsem = [es.enter_context(nc.semaphore(f"dq{i}")) for i in range(ntot)]
        self.dval = [0] * ntot
        self.dnext = {'sp': 0, 'pool': 0, 'act': 0}
        self.waited = {k: {} for k in self.eng}
        self.lastw = {}
        self.readers = {}
        self.same_engine_sync = True
        self.nwaits = 0

    def _sem(self, e, seq):
        i = (seq - 1) // self.EPOCH
        while len(self.sems[e]) <= i:
            self.sems[e].append(self.es.enter_context(self.nc.semaphore(f"s_{e}{len(self.sems[e])}")))
        return self.sems[e][i], (seq - 1) % self.EPOCH + 1

    def _wait(self, e, tok):
        kind, src, val = tok
        if kind == 'e':
            if src == e and (e == 'pe' or not self.same_engine_sync):
                return
            assert val <= self.cnt[src], f"dependency on unsignalled op {tok}"
            key = ('e', src)
        else:
            key = ('d', src)
        if self.waited[e].get(key, 0) >= val:
            return
        self.waited[e][key] = val
        if kind == 'e':
            sem, v = self._sem(src, val)
        else:
            sem, v = self.dsem[src], val
        self.eng[e].wait_ge(sem, v)
        self.nwaits += 1

    def _deps(self, r, w):
        deps = []
        for k in r:
            if k in self.lastw:
                deps.append(self.lastw[k])
            if isinstance(k, tuple) and k[0] == 'pb':
                deps.extend(self.readers.get(k, {}).values())
        for k in w:
            if k in self.lastw:
                deps.append(self.lastw[k])
            deps.extend(self.readers.get(k, {}).values())
        return deps

    def _record(self, tok, r, w):
        for k in r:
            d = self.readers.setdefault(k, {})
            key = (tok[0], tok[1])
            if key not in d or d[key][2] < tok[2]:
                d[key] = tok
        for k in w:
            self.lastw[k] = tok
            self.readers[k] = {}

    def op(self, e, fn, r=(), w=(), signal=True, extra=()):
        for d in self._deps(r, w):
            self._wait(e, d)
        for d in extra:
            self._wait(e, d)
        inst = fn(self.eng[e])
        if signal:
            self.cnt[e] += 1
            seq = self.cnt[e]
            sem, _ = self._sem(e, seq)
            inst.then_inc(sem, 1)
        else:
            seq = self.cnt[e] + 1
        tok = ('e', e, seq)
        self._record(tok, r, w)
        return tok

    def dma(self, q, fn, r=(), w=(), extra=()):
        pl = self.dpool[q]
        slot = pl[self.dnext[q] % len(pl)]
        self.dnext[q] += 1
        if self.dval[slot]:
            self._wait(q, ('d', slot, self.dval[slot]))
        for d in self._deps(r, w):
            self._wait(q, d)
        for d in extra:
            self._wait(q, d)
        inst = fn(self.eng[q])
        inst.then_inc(self.dsem[slot], 16)
        self.dval[slot] += 16
        tok = ('d', slot, self.dval[slot])
        self._record(tok, r, w)
        return tok

    def barrier(self):
        toks = [('e', f, self.cnt[f]) for f in self.eng if self.cnt[f] > 0]
        toks += [('d', s, v) for s, v in enumerate(self.dval) if v > 0]
        for e in self.eng:
            for t in toks:
                self._wait(e, t)


def build_program(NLAT=64, dbg=False, nlayers=L, stop_after=None, skip_mods=False):
    NT = NLAT + NCTX
    NTOK = NT * 128
    CAPL = 16 * NLAT
    CAPC = 32

    nc = bass.Bass("TRN2", target_bir_lowering=False)
    dk = "ExternalOutput" if dbg else "Internal"

    def din(name, shape, dt=F32):
        return nc.dram_tensor(name, list(shape), dt, kind="ExternalInput").ap()

    xlat = din("xlat", [NLAT * 128, D])
    xctx = din("xctx", [NCTX * 128, D])
    cvec = din("cvec", [128, 16])
    if not skip_mods:
        w_mod = din("w_mod", [L, D, 6 * D])
        b_mod = din("b_mod", [L, 6 * D])
    w_in = din("w_in", [L, D, 2048])
    a_sink = din("a_sink", [L, 8])
    natT = din("natT", [L, 128, NCHB * 512])
    conv_w = din("conv_w", [L, 128, 62])
    conv_b = din("conv_b", [L, 128, 2])
    conv_ln_g = din("conv_ln_g", [L, 256])
    conv_ln_b = din("conv_ln_b", [L, 256])
    w_out = din("w_out", [L, D, D])
    ln1_g = din("ln1_g", [L, D])
    ln1_b = din("ln1_b", [L, D])
    w_router = din("w_router", [L, D, NE])
    need_moe = stop_after is None or stop_after.startswith(('M', 'F'))
    if need_moe:
        w_gate = din("w_gate", [L, NE, D, D])
        w_up = din("w_up", [L, NE, D, D])
        w_down = din("w_down", [L, NE, D, D])
    ln2_g = din("ln2_g", [L, D])
    ln2_b = din("ln2_b", [L, D])
    rope = din("rope", [NLAT, 128, 128])
    consts = din("consts", [128, 6 * 128])
    out = nc.dram_tensor("out", [NLAT * 128, D], F32, kind="ExternalOutput").ap()

    modbc = nc.dram_tensor("modbc", [L, 2, 128, 6 * D], F32, kind=dk).ap()
    scrA = nc.dram_tensor("scrA", [NT, 128, AW], BF16, kind=dk).ap()
    x1s = nc.dram_tensor("x1s", [NTOK, D], F32, kind=dk).ap()
    h2s = nc.dram_tensor("h2s", [NTOK, RW], BF16, kind=dk).ap()
    xs_d = nc.dram_tensor("xs_d", [NTOK, D], F32, kind=dk).ap()
    macc = nc.dram_tensor("macc", [NTOK, D], F32, kind=dk).ap()
    CAPT0 = CAPL + CAPC
    Xs = [nc.dram_tensor(f"Xs{e}", [CAPT0, RW], BF16, kind=dk).ap() for e in range(NE)]
    otok_d = nc.dram_tensor("otok_d", [NTOK, D], BF16, kind=dk).ap() if dbg else None

    es = ExitStack()
    S = Sched(nc, es)
    es.enter_context(nc.allow_non_contiguous_dma(reason="small strided parameter loads"))
    es.enter_context(nc.allow_low_precision(reason="0/1 mask counts <= 128 are exact in bf16"))

    uid = [0]

    def sb(stack, name, shape, dt):
        uid[0] += 1
        return stack.enter_context(nc.sbuf_tensor(f"{name}_{uid[0]}", list(shape), dt))

    banks = [es.enter_context(nc.psum_tensor(f"pb{i}", [128, 512], F32)) for i in range(8)]
    bank_rr = [0]

    def nbank():
        i = bank_rr[0]
        bank_rr[0] = (i + 1) % 8
        return banks[i], ('pb', i)

    cst = sb(es, "cst", [128, 6 * 128], F32)
    identb = sb(es, "identb", [128, 128], BF16)
    triub = sb(es, "triub", [128, 128], BF16)
    masklr = sb(es, "masklr", [128, 2, 128], BF16)
    onesb = sb(es, "onesb", [128, 128], BF16)
    aff_all = sb(es, "aff_all", [128, NT, NE], F32)
    NLN = 4
    stat_l = [sb(es, "stat", [128, 12], F32) for _ in range(NLN)]
    mv_l = [sb(es, "mv", [128, 2], F32) for _ in range(NLN)]
    sd_l = [sb(es, "sd", [128, 1], F32) for _ in range(NLN)]
    rstd_l = [sb(es, "rstd", [128, 1], F32) for _ in range(NLN)]
    nmr_l = [sb(es, "nmr", [128, 1], F32) for _ in range(NLN)]
    lncur = [0]
    identf = cst[:, 0:128]
    iotap = cst[:, 5 * 128:5 * 128 + 1]
    pm0 = cst[:, 5 * 128 + 1:5 * 128 + 2]
    pm1 = cst[:, 5 * 128 + 2:5 * 128 + 3]

    S.dma('sp', lambda q: q.dma_start(out=cst[:, :], in_=consts), w=['cst'])
    S.op('dve', lambda e: e.tensor_copy(out=identb[:, :], in_=cst[:, 0:128]), r=['cst'], w=['identb'])
    S.op('dve', lambda e: e.tensor_copy(out=triub[:, :], in_=cst[:, 128:256]), r=['cst'], w=['triub'])
    S.op('dve', lambda e: e.tensor_copy(out=masklr[:, :, :].rearrange("p a b -> p (a b)"), in_=cst[:, 256:512]), r=['cst'], w=['masklr'])
    S.op('dve', lambda e: e.tensor_copy(out=onesb[:, :], in_=cst[:, 512:640]), r=['cst'], w=['onesb'])

    def ln_stats(src_ap, src_key, width=1024):
        nch = (width + 511) // 512
        lncur[0] = (lncur[0] + 1) % NLN
        j = lncur[0]
        stat, mv, sd, rstd, nmr = stat_l[j], mv_l[j], sd_l[j], rstd_l[j], nmr_l[j]
        kst_, kmv, ksd, krs, knm = ('stat', j), ('mv', j), ('sd', j), ('rstd', j), ('nmr', j)
        for c in range(nch):
            S.op('dve', lambda e, c=c: e.bn_stats(out=stat[:, 6 * c:6 * c + 6], in_=src_ap[:, c * 512:min(width, (c + 1) * 512)]),
                 r=[src_key], w=[kst_])
        S.op('dve', lambda e: e.bn_aggr(out=mv[:, :], in_=stat[:, 0:6 * nch]), r=[kst_], w=[kmv])
        S.op('dve', lambda e: e.tensor_scalar(out=sd[:, :], in0=mv[:, 1:2], scalar1=EPS, scalar2=None, op0=ALU.add), r=[kmv], w=[ksd])
        S.op('act', lambda e: e.activation(out=sd[:, :], in_=sd[:, :], func=AF.Sqrt), r=[ksd], w=[ksd])
        S.op('dve', lambda e: e.reciprocal(out=rstd[:, :], in_=sd[:, :]), r=[ksd], w=[krs])
        S.op('dve', lambda e: e.tensor_scalar(out=nmr[:, :], in0=mv[:, 0:1], scalar1=rstd[:, 0:1], scalar2=-1.0, op0=ALU.mult, op1=ALU.mult),
             r=[kmv, krs], w=[knm])

    def ln_apply(dst_ap, dst_key, src_ap, src_key):
        j = lncur[0]
        S.op('act', lambda e: e.activation(out=dst_ap, in_=src_ap, func=AF.Identity, scale=rstd_l[j][:, 0:1], bias=nmr_l[j][:, 0:1]),
             r=[src_key, ('rstd', j), ('nmr', j)], w=[dst_key])

    def transposes_to(dst_ap, dst_key, src_fn, n, rows=128, src_key=None, evac='act'):
        bk, bkey = nbank()
        bv = bk[:, :].bitcast(BF16)
        for k in range(n):
            S.op('pe', lambda e, k=k: e.transpose(out=bv[:, k * 128:k * 128 + rows], in_=src_fn(k), identity=identb[:rows, :rows]),
                 r=[src_key, 'identb'], w=[bkey], signal=(k == n - 1))
        if rows == 128:
            src = bv[:, 0:n * 128]
        else:
            src = bv[:, 0:n * 128].rearrange("p (k s) -> p k s", s=128)[:, :, 0:rows]
        if evac == 'act':
            S.op('act', lambda e: e.copy(out=dst_ap, in_=src), r=[bkey], w=[dst_key])
        else:
            S.op('dve', lambda e: e.tensor_copy(out=dst_ap, in_=src), r=[bkey], w=[dst_key])

    with ExitStack() as ph:
        cT = sb(ph, "cT", [128, 8, 2], F32)
        cS = sb(ph, "cS", [128, 8, 2], F32)
        lbc = [sb(ph, f"lbc{s}", [128, 8, 128], BF16) for s in range(2)]
        wm = [sb(ph, f"wm{i}", [128, 8, 512], BF16) for i in range(2)]
        bm = [sb(ph, f"bm{i}", [128, 512], F32) for i in range(2)]
        mo = [sb(ph, f"mo{i}", [128, 512], F32) for i in range(4)]
        S.dma('sp', lambda q: q.dma_start(out=cT[:, :, :], in_=cvec.rearrange("p (k s) -> p k s", s=2)), w=['cT'])
        S.op('act', lambda e: e.activation(out=cS[:, :, :], in_=cT[:, :, :], func=AF.Silu), r=['cT'], w=['cS'])
        for s in range(2):
            S.op('dve', lambda e, s=s: e.tensor_copy(out=lbc[s][:, :, :], in_=cS[:, :, s:s + 1].to_broadcast([128, 8, 128])),
                 r=['cS'], w=[('lbc', s)])
        it = 0
        for l in range(nlayers if not skip_mods else 0):
            for ch in range(12):
                n0 = ch * 512
                slot = it % 2
                S.dma('pool', lambda q, slot=slot, l=l, n0=n0: q.dma_start(
                    out=wm[slot][:, :, :], in_=w_mod[l, :, n0:n0 + 512].rearrange("(ko ki) n -> ki ko n", ki=128)), w=[('wm', slot)])
                S.dma('sp', lambda q, slot=slot, l=l, n0=n0: q.dma_start(
                    out=bm[slot][:, :], in_=b_mod[l:l + 1, n0:n0 + 512].to_broadcast([128, 512])), w=[('bm', slot)])
                addc = 1.0 if (ch // 2) in (1, 4) else 0.0
                for s in range(2):
                    bk, bkey = nbank()
                    for k in range(8):
                        S.op('pe', lambda e, k=k, s=s, slot=slot, bk=bk: e.matmul(bk[:, :], lhsT=lbc[s][:, k, :], rhs=wm[slot][:, k, :],
                                                                               start=(k == 0), stop=(k == 7)),
                             r=[('lbc', s), ('wm', slot)], w=[bkey], signal=(k == 7))
                    ms = (it * 2 + s) % 4
                    S.op('dve', lambda e, bk=bk, ms=ms, slot=slot: e.scalar_tensor_tensor(
                        out=mo[ms][:, :], in0=bk[:, :], scalar=addc, in1=bm[slot][:, :], op0=ALU.add, op1=ALU.add),
                         r=[bkey, ('bm', slot)], w=[('mo', ms)])
                    S.dma('sp', lambda q, ms=ms, l=l, s=s, n0=n0: q.dma_start(out=modbc[l, s, :, n0:n0 + 512], in_=mo[ms][:, :]),
                          r=[('mo', ms)], w=[('modbc', l, s)])
                it += 1
        S.barrier()
    if stop_after == 'mods':
        return _finish(nc, S, es)

    for l in range(nlayers):
        last = (l == L - 1)
        tiles_b = list(range(NT)) if not last else list(range(NCTX, NT))
        CAPT = CAPL + (0 if last else CAPC)

        def xsrc(ti):
            if l == 0:
                return xctx[ti * 128:(ti + 1) * 128, :] if ti < NCTX else xlat[(ti - NCTX) * 128:(ti - NCTX + 1) * 128, :]
            return xs_d[ti * 128:(ti + 1) * 128, :]

        with ExitStack() as ph:
            w_in_sb = sb(ph, "w_in_sb", [128, 8, 2048], BF16)
            modA = [[sb(ph, f"modA{s}{j}", [128, D], F32) for j in range(2)] for s in range(2)]
            xin = [sb(ph, f"xinA{i}", [128, D], F32) for i in range(2)]
            ropet = [sb(ph, f"ropet{i}", [128, 128], F32) for i in range(2)]
            f32a_l = [sb(ph, "f32a", [128, D], F32) for _ in range(2)]
            hb_l = [sb(ph, "hb", [128, D], BF16) for _ in range(2)]
            hT_l = [sb(ph, "hT", [128, 8, 128], BF16) for _ in range(2)]
            ropeA_l = [sb(ph, "ropeA", [128, 512], F32) for _ in range(2)]
            ropeB_l = [sb(ph, "ropeB", [128, 512], F32) for _ in range(2)]
            qperm_l = [sb(ph, "qperm", [128, 512], BF16) for _ in range(2)]
            kst_l = [sb(ph, "kst", [128, 640], BF16) for _ in range(2)]
            sg_l = [sb(ph, "sg", [128, 256], F32) for _ in range(2)]
            chtok_l = [sb(ph, "chtok", [128, 256], BF16) for _ in range(2)]
            aout = [sb(ph, f"aout{i}", [128, AW], BF16) for i in range(2)]
            for i in range(2):
                S.op('pool', lambda e, i=i: e.memset(aout[i][:, VA:VA + 130], 1.0), w=[('aout', i)])
                S.op('pool', lambda e, i=i: e.memset(aout[i][:, VB:VB + 260], 1.0), w=[('aout', i)])
            for hf in range(2):
                S.dma('pool', lambda q, hf=hf: q.dma_start(out=w_in_sb[:, :, hf * 1024:(hf + 1) * 1024],
                                                        in_=w_in[l, :, hf * 1024:(hf + 1) * 1024].rearrange("(ko ki) n -> ki ko n", ki=128)),
                      w=['w_in_sb'])
            for s in range(2):
                for j in range(2):
                    S.dma('sp', lambda q, s=s, j=j: q.dma_start(out=modA[s][j][:, :], in_=modbc[l, s, :, j * D:(j + 1) * D]),
                          r=[('modbc', l, s)], w=[('modA', s, j)])

            def tileA(ti, part):
                    s = 0 if ti < NCTX else 1
                    lat = ti >= NCTX
                    xs_, xk = xin[ti % 2], ('xinA', ti % 2)
                    f32a = f32a_l[ti % 2]
                    k_f32a = ('f32a', ti % 2)
                    hb = hb_l[ti % 2]
                    k_hb = ('hb', ti % 2)
                    hT = hT_l[ti % 2]
                    k_hT = ('hT', ti % 2)
                    ropeA = ropeA_l[ti % 2]
                    k_ropeA = ('ropeA', ti % 2)
                    ropeB = ropeB_l[ti % 2]
                    k_ropeB = ('ropeB', ti % 2)
                    qperm = qperm_l[ti % 2]
                    k_qperm = ('qperm', ti % 2)
                    kst = kst_l[ti % 2]
                    k_kst = ('kst', ti % 2)
                    sg = sg_l[ti % 2]
                    k_sg = ('sg', ti % 2)
                    chtok = chtok_l[ti % 2]
                    k_chtok = ('chtok', ti % 2)
                    ao, aok = aout[ti % 2], ('aout', ti % 2)
                    rt, rk = ropet[ti % 2], ('ropet', ti % 2)
                    if part == 0:
                        S.dma('sp', lambda q, ti=ti, xs_=xs_: q.dma_start(out=xs_[:, :], in_=xsrc(ti)), r=[('xs_d',)] if l > 0 else [], w=[xk])
                        if lat:
                            S.dma('sp', lambda q, ti=ti, rt=rt: q.dma_start(out=rt[:, :], in_=rope[ti - NCTX, :, :]), w=[rk])
                        ln_stats(xs_, xk)
                        ln_apply(f32a[:, :], k_f32a, xs_[:, :], xk)
                        S.op('dve', lambda e, s=s: e.tensor_tensor(out=f32a[:, :], in0=f32a[:, :], in1=modA[s][1][:, :], op=ALU.mult),
                             r=[k_f32a, ('modA', s, 1)], w=[k_f32a])
                        S.op('pool', lambda e, s=s: e.tensor_tensor(out=hb[:, :], in0=f32a[:, :], in1=modA[s][0][:, :], op=ALU.add),
                             r=[k_f32a, ('modA', s, 0)], w=[k_hb])
                    if part == 1:
                        transposes_to(hT[:, :, :].rearrange("p k t -> p (k t)"), k_hT, lambda k: hb[:, k * 128:(k + 1) * 128], 8, src_key=k_hb)
                        ub = []
                        for n in range(4):
                            bk, bkey = nbank()
                            for k in range(8):
                                S.op('pe', lambda e, k=k, n=n, bk=bk: e.matmul(bk[:, :], lhsT=hT[:, k, :], rhs=w_in_sb[:, k, n * 512:(n + 1) * 512],
                                                                             start=(k == 0), stop=(k == 7)),
                                     r=[k_hT, 'w_in_sb'], w=[bkey], signal=(k == 7))
                            ub.append((bk, bkey))
                        b0, k0 = ub[0]
                        qp_v = qperm[:, :].rearrange("p (g r d) -> p r g d", g=4, r=2, d=64)
                        if lat:
                            S.op('dve', lambda e, b0=b0, rt=rt: e.tensor_tensor(
                                out=ropeA[:, :].rearrange("p (h d) -> p h d", d=64), in0=b0[:, :].rearrange("p (h d) -> p h d", d=64),
                                in1=rt[:, 0:64].unsqueeze(1).to_broadcast([128, 8, 64]), op=ALU.mult), r=[k0, rk], w=[k_ropeA])
                            for half in range(2):
                                S.op('dve', lambda e, b0=b0, rt=rt, half=half: e.tensor_tensor(
                                    out=ropeB[:, :].rearrange("p (h r f d) -> p h r f d", h=8, r=2, f=2, d=16)[:, :, :, half, :],
                                    in0=b0[:, :].rearrange("p (h r f d) -> p h r f d", h=8, r=2, f=2, d=16)[:, :, :, 1 - half, :],
                                    in1=rt[:, 64:128].rearrange("p (r f d) -> p r f d", r=2, f=2, d=16)[:, :, half, :].unsqueeze(1).to_broadcast([128, 8, 2, 16]),
                                    op=ALU.mult), r=[k0, rk], w=[k_ropeB])
                            S.op('pool', lambda e: e.tensor_tensor(out=qp_v, in0=ropeA[:, :].rearrange("p (r g d) -> p r g d", r=2, g=4, d=64),
                                                                  in1=ropeB[:, :].rearrange("p (r g d) -> p r g d", r=2, g=4, d=64), op=ALU.add),
                                 r=[k_ropeA, k_ropeB], w=[k_qperm])
                        else:
                            S.op('act', lambda e, b0=b0: e.copy(out=qp_v, in_=b0[:, :].rearrange("p (r g d) -> p r g d", r=2, g=4, d=64)),
                                 r=[k0], w=[k_qperm])
                        bk, bkey = nbank()
                        bv = bk[:, :].bitcast(BF16)
                        for k in range(4):
                            S.op('pe', lambda e, k=k, bv=bv: e.transpose(out=bv[:, k * 128:(k + 1) * 128], in_=qperm[:, k * 128:(k + 1) * 128], identity=identb[:, :]),
                                 r=[k_qperm, 'identb'], w=[bkey], signal=(k == 3))
                        if os.environ.get('EVAC', 'mask') == 'mask':
                            S.op('act', lambda e, bv=bv, ao=ao: e.activation(out=ao[:, QA:QA + 512], in_=bv[:, 0:512], func=AF.Copy, scale=pm0), r=[bkey, 'cst'], w=[aok])
                            S.op('dve', lambda e, bv=bv, ao=ao: e.tensor_scalar(out=ao[:, QA + 512:QA + 1024], in0=bv[:, 0:512], scalar1=pm1, scalar2=None, op0=ALU.mult),
                                 r=[bkey, 'cst'], w=[aok])
                        else:
                            S.op('act', lambda e, bv=bv, ao=ao: e.copy(out=ao[:, QA:QA + 512], in_=bv[:, 0:512]), r=[bkey], w=[aok])
                            S.op('dve', lambda e, bv=bv, ao=ao: e.tensor_copy(out=ao[:, QA + 512:QA + 1024], in_=bv[:, 0:512]), r=[bkey], w=[aok])
                        b1, k1 = ub[1]
                        if lat:
                            S.op('dve', lambda e, b1=b1, rt=rt: e.tensor_tensor(
                                out=ropeA[:, 0:128].rearrange("p (h d) -> p h d", d=64), in0=b1[:, 0:128].rearrange("p (h d) -> p h d", d=64),
                                in1=rt[:, 0:64].unsqueeze(1).to_broadcast([128, 2, 64]), op=ALU.mult), r=[k1, rk], w=[k_ropeA])
                            for half in range(2):
                                S.op('dve', lambda e, b1=b1, rt=rt, half=half: e.tensor_tensor(
                                    out=ropeB[:, 0:128].rearrange("p (h r f d) -> p h r f d", h=2, r=2, f=2, d=16)[:, :, :, half, :],
                                    in0=b1[:, 0:128].rearrange("p (h r f d) -> p h r f d", h=2, r=2, f=2, d=16)[:, :, :, 1 - half, :],
                                    in1=rt[:, 64:128].rearrange("p (r f d) -> p r f d", r=2, f=2, d=16)[:, :, half, :].unsqueeze(1).to_broadcast([128, 2, 2, 16]),
                                    op=ALU.mult), r=[k1, rk], w=[k_ropeB])
                            S.op('pool', lambda e: e.tensor_tensor(out=kst[:, 0:128], in0=ropeA[:, 0:128], in1=ropeB[:, 0:128], op=ALU.add),
                                 r=[k_ropeA, k_ropeB], w=[k_kst])
                        else:
                            S.op('act', lambda e, b1=b1: e.copy(out=kst[:, 0:128], in_=b1[:, 0:128]), r=[k1], w=[k_kst])
                        S.op('act', lambda e, b1=b1, ao=ao: e.copy(out=ao[:, VA:VA + 130].rearrange("p (h d) -> p h d", d=65)[:, :, 0:64],
                                                                 in_=b1[:, 128:256].rearrange("p (h d) -> p h d", d=64)), r=[k1], w=[aok])
                        S.op('act', lambda e, b1=b1: e.copy(out=kst[:, 128:384], in_=b1[:, 256:512]), r=[k1], w=[k_kst])
                        b2, k2 = ub[2]
                        S.op('dve', lambda e, b2=b2: e.tensor_copy(out=kst[:, 384:640], in_=b2[:, 0:256]), r=[k2], w=[k_kst])
                        S.op('act', lambda e, b2=b2, ao=ao: e.copy(out=ao[:, VB:VB + 260].rearrange("p (h d) -> p h d", d=65)[:, :, 0:64],
                                                                 in_=b2[:, 256:512].rearrange("p (h d) -> p h d", d=64)), r=[k2], w=[aok])
                        order = [1, 2, 0, 3, 4]
                        bk, bkey = nbank()
                        bv = bk[:, :].bitcast(BF16)
                        for j, blk in enumerate(order):
                            S.op('pe', lambda e, j=j, blk=blk, bv=bv: e.transpose(out=bv[:, j * 128:(j + 1) * 128], in_=kst[:, blk * 128:(blk + 1) * 128],
                                                                                 identity=identb[:, :]), r=[k_kst, 'identb'], w=[bkey], signal=(j == 4))
                        if os.environ.get('EVAC', 'mask') == 'mask':
                            S.op('act', lambda e, bv=bv, ao=ao: e.activation(out=ao[:, QB:QB + 256], in_=bv[:, 0:256], func=AF.Copy, scale=pm0), r=[bkey, 'cst'], w=[aok])
                            S.op('dve', lambda e, bv=bv, ao=ao: e.tensor_scalar(out=ao[:, QB + 256:QB + 512], in0=bv[:, 0:256], scalar1=pm1, scalar2=None, op0=ALU.mult),
                                 r=[bkey, 'cst'], w=[aok])
                        else:
                            S.op('act', lambda e, bv=bv, ao=ao: e.copy(out=ao[:, QB:QB + 256], in_=bv[:, 0:256]), r=[bkey], w=[aok])
                            S.op('dve', lambda e, bv=bv, ao=ao: e.tensor_copy(out=ao[:, QB + 256:QB + 512], in_=bv[:, 0:256]), r=[bkey], w=[aok])
                        S.op('dve', lambda e, bv=bv, ao=ao: e.tensor_copy(out=ao[:, KA:KA + 128], in_=bv[:, 256:384]), r=[bkey], w=[aok])
                        S.op('dve', lambda e, bv=bv, ao=ao: e.tensor_copy(out=ao[:, KB:KB + 256], in_=bv[:, 384:640]), r=[bkey], w=[aok])
                        b3, k3 = ub[3]
                        S.op('act', lambda e, b3=b3: e.activation(out=sg[:, :], in_=b3[:, 256:512], func=AF.Sigmoid), r=[k3], w=[k_sg])
                        S.op('dve', lambda e, b3=b3: e.tensor_tensor(out=chtok[:, :], in0=b3[:, 0:256], in1=sg[:, :], op=ALU.mult), r=[k3, k_sg], w=[k_chtok])
                        transposes_to(ao[:, CH:CH + 256], aok, lambda k: chtok[:, k * 128:(k + 1) * 128], 2, src_key=k_chtok, evac='dve')
                        S.dma('sp', lambda q, ti=ti, ao=ao: q.dma_start(out=scrA[ti, :, 0:QW], in_=ao[:, 0:QW]), r=[aok], w=[('scrAq', ti)])
                        S.dma('sp', lambda q, ti=ti, ao=ao: q.dma_start(out=scrA[ti, :, QW:AW], in_=ao[:, QW:AW]), r=[aok], w=[('scrA', ti)])

            for step in range(NT + 1):
                if step < NT:
                    tileA(step, 0)
                if step - 1 >= 0:
                    tileA(step - 1, 1)
            S.barrier()
        if stop_after == f'A{l}':
            return _finish(nc, S, es)

        with ExitStack() as ph:
            w_out_sb = sb(ph, "w_out_sb", [128, 8, D], BF16)
            w_r_sb = sb(ph, "w_r_sb", [128, 8, NE], BF16)
            nat_sb = sb(ph, "nat_sb", [128, NCHB, 512], BF16)
            cwT = sb(ph, "cwT", [128, 2, 31], F32)
            cdiag = sb(ph, "cdiag", [128, 2, 31, 128], BF16)
            convb = sb(ph, "convb", [128, 2], F32)
            clng = sb(ph, "clng", [128, 256], F32)
            clnb = sb(ph, "clnb", [128, 256], F32)
            modB = [[sb(ph, f"modB{s}{j}", [128, D], F32) for j in range(3)] for s in range(2)]
            ln1g = sb(ph, "ln1g", [128, D], F32)
            ln1b = sb(ph, "ln1b", [128, D], F32)
            esink = sb(ph, "esink", [128, 8], F32)
            NQR, NKR, NCW = 3, 8, 4
            qring = [sb(ph, f"qring{i}", [128, QW], BF16) for i in range(NQR)]
            kvring = [sb(ph, f"kvring{i}", [128, KVW], BF16) for i in range(NKR)]
            kvctx = [sb(ph, f"kvctx{i}", [128, KVW], BF16) for i in range(NCTX)]
            chwin = [sb(ph, f"chwin{i}", [128, 2, 160], BF16) for i in range(NCW)]
            xin = [sb(ph, f"xinB{i}", [128, D], F32) for i in range(2)]
            pT = sb(ph, "pT", [128, 7, 512], BF16)
            pTA = [sb(ph, "pTA", [128, 5, 512], BF16) for _ in range(2)]
            btmp = [sb(ph, f"btmp{i}", [128, 512], F32) for i in range(2)]
            otok_l = [sb(ph, "otok", [128, D], BF16) for _ in range(2)]
            oT_l = [sb(ph, "oT", [128, 8, 128], BF16) for _ in range(2)]
            cvT_l = [sb(ph, "cvT", [128, 2, 128], F32) for _ in range(2)]
            cvn_l = [sb(ph, "cvn", [128, 256], F32) for _ in range(2)]
            f32b_l = [sb(ph, "f32b", [128, D], F32) for _ in range(2)]
            f32c_l = [sb(ph, "f32c", [128, D], F32) for _ in range(2)]
            h2row = [sb(ph, f"h2row{i}", [128, RW], BF16) for i in range(2)]
            h2T_l = [sb(ph, "h2T", [128, 8, 128], BF16) for _ in range(2)]
            den_l = [sb(ph, "den", [128, 4], F32) for _ in range(2)]
            rden_l = [sb(ph, "rden", [128, 4], F32) for _ in range(2)]
            ex_l = [sb(ph, "ex", [128, NE], F32) for _ in range(2)]
            ssum_l = [sb(ph, "ssum", [128, 1], F32) for _ in range(2)]

            for hf in range(1):
                S.dma('pool', lambda q: q.dma_start(out=w_out_sb[:, :, :], in_=w_out[l, :, :].rearrange("(ko ki) n -> ki ko n", ki=128)), w=['w_out_sb'])
            S.dma('pool', lambda q: q.dma_start(out=w_r_sb[:, :, :], in_=w_router[l, :, :].rearrange("(ko ki) n -> ki ko n", ki=128)), w=['w_r_sb'])
            for c3 in range(0, NCHB, 3):
                c4 = min(NCHB, c3 + 3)
                S.dma('pool', lambda q, c3=c3, c4=c4: q.dma_start(out=nat_sb[:, c3:c4, :], in_=natT[l, :, c3 * 512:c4 * 512].rearrange("p (a b) -> p a b", b=512)),
                      w=['nat_sb'])
            S.dma('sp', lambda q: q.dma_start(out=cwT[:, :, :], in_=conv_w[l, :, :].rearrange("c (cc j) -> c cc j", cc=2)), w=['cwT'])
            S.dma('sp', lambda q: q.dma_start(out=convb[:, :], in_=conv_b[l, :, :]), w=['convb'])
            S.dma('sp', lambda q: q.dma_start(out=clng[:, :], in_=conv_ln_g[l:l + 1, :].to_broadcast([128, 256])), w=['clng'])
            S.dma('sp', lambda q: q.dma_start(out=clnb[:, :], in_=conv_ln_b[l:l + 1, :].to_broadcast([128, 256])), w=['clnb'])
            S.dma('sp', lambda q: q.dma_start(out=ln1g[:, :], in_=ln1_g[l:l + 1, :].to_broadcast([128, D])), w=['ln1g'])
            S.dma('sp', lambda q: q.dma_start(out=ln1b[:, :], in_=ln1_b[l:l + 1, :].to_broadcast([128, D])), w=['ln1b'])
            S.dma('sp', lambda q: q.dma_start(out=esink[:, :], in_=a_sink[l:l + 1, :].to_broadcast([128, 8])), w=['esink'])
            S.op('act', lambda e: e.activation(out=esink[:, :], in_=esink[:, :], func=AF.Exp), r=['esink'], w=['esink'])
            for s in range(2):
                for j, chn in enumerate((2, 4, 3)):
                    S.dma('sp', lambda q, s=s, j=j, chn=chn: q.dma_start(out=modB[s][j][:, :], in_=modbc[l, s, :, chn * D:(chn + 1) * D]),
                          r=[('modbc', l, s)], w=[('modB', s, j)])
            for cc in range(2):
                for j in range(31):
                    S.op('pool', lambda e, cc=cc, j=j: e.tensor_scalar(out=cdiag[:, cc, j, :], in0=cst[:, 0:128], scalar1=cwT[:, cc, j:j + 1],
                                                                      scalar2=None, op0=ALU.mult), r=['cst', 'cwT'], w=['cdiag'])
            for c in range(NCTX):
                S.dma('sp', lambda q, c=c: q.dma_start(out=kvctx[c][:, :], in_=scrA[c, :, KA:CH]), r=[('scrA', c)], w=[('kvctx', c)])

            loaded = set()

            def load_tile(t):
                if t in loaded or t < 0 or t >= NT:
                    return
                loaded.add(t)
                if t >= NCTX:
                    S.dma('sp', lambda q: q.dma_start(out=kvring[t % NKR][:, :], in_=scrA[t, :, KA:CH]), r=[('scrA', t)], w=[('kvring', t % NKR)])

            def load_own(t):
                S.dma('sp', lambda q: q.dma_start(out=qring[t % NQR][:, :], in_=scrA[t, :, 0:QW]), r=[('scrAq', t)], w=[('qring', t % NQR)])
                cw, cwk = chwin[t % NCW], ('chwin', t % NCW)
                first = t in (0, NCTX)
                lastt = t in (NCTX - 1, NT - 1)
                S.dma('sp', lambda q: q.dma_start(out=cw[:, :, 16:144], in_=scrA[t, :, CH:CH + 256].rearrange("p (c n) -> p c n", c=2)),
                      r=[('scrA', t)], w=[cwk])
                if first:
                    S.op('pool', lambda e: e.memset(cw[:, :, 0:16], 0.0), w=[cwk])
                else:
                    S.dma('sp', lambda q: q.dma_start(out=cw[:, :, 0:16], in_=scrA[t - 1, :, CH:CH + 256].rearrange("p (c n) -> p c n", c=2)[:, :, 112:128]),
                          r=[('scrA', t - 1)], w=[cwk])
                if lastt:
                    S.op('pool', lambda e: e.memset(cw[:, :, 144:160], 0.0), w=[cwk])
                else:
                    S.dma('sp', lambda q: q.dma_start(out=cw[:, :, 144:160], in_=scrA[t + 1, :, CH:CH + 256].rearrange("p (c n) -> p c n", c=2)[:, :, 0:16]),
                          r=[('scrA', t + 1)], w=[cwk])
                S.dma('sp', lambda q: q.dma_start(out=xin[t % 2][:, :], in_=xsrc(t)), r=[('xs_d',)] if l > 0 else [], w=[('xinB', t % 2)])

            def kvbuf(t):
                if t < NCTX:
                    return kvctx[t], ('kvctx', t)
                return kvring[t % NKR], ('kvring', t % NKR)

            def tileB(ti, part):
                    s = 0 if ti < NCTX else 1
                    lat = ti >= NCTX
                    i = ti - NCTX
                    otok = otok_l[ti % 2]
                    k_otok = ('otok', ti % 2)
                    oT = oT_l[ti % 2]
                    k_oT = ('oT', ti % 2)
                    cvT = cvT_l[ti % 2]
                    k_cvT = ('cvT', ti % 2)
                    cvn = cvn_l[ti % 2]
                    k_cvn = ('cvn', ti % 2)
                    f32b = f32b_l[ti % 2]
                    k_f32b = ('f32b', ti % 2)
                    f32c = f32c_l[ti % 2]
                    k_f32c = ('f32c', ti % 2)
                    h2T = h2T_l[ti % 2]
                    k_h2T = ('h2T', ti % 2)
                    den = den_l[ti % 2]
                    k_den = ('den', ti % 2)
                    rden = rden_l[ti % 2]
                    k_rden = ('rden', ti % 2)
                    ex = ex_l[ti % 2]
                    k_ex = ('ex', ti % 2)
                    ssum = ssum_l[ti % 2]
                    k_ssum = ('ssum', ti % 2)
                    if part == 0:
                      for t2 in range(ti - 2, ti + 4):
                        if lat and t2 >= NCTX:
                            load_tile(t2)
                      load_own(ti)
                    qr, qk = qring[ti % NQR], ('qring', ti % NQR)
                    cw, cwk = chwin[ti % NCW], ('chwin', ti % NCW)
                    xs_, xk = xin[ti % 2], ('xinB', ti % 2)
                    hr, hk = h2row[ti % 2], ('h2row', ti % 2)

                    if stop_after == f'B{l}:setup':
                        return
                    if part == 0:
                        cbk, cbkey = nbank()
                        for cc in range(2):
                            for j in range(31):
                                S.op('pe', lambda e, cbk=cbk, cc=cc, j=j: e.matmul(cbk[:, cc * 128:(cc + 1) * 128], lhsT=cdiag[:, cc, j, :],
                                                                                  rhs=cw[:, cc, 1 + j:1 + j + 128], start=(j == 0), stop=(j == 30)),
                                     r=['cdiag', cwk], w=[cbkey], signal=(cc == 1 and j == 30))
                        for cc in range(2):
                            S.op('act', lambda e, cbk=cbk, cc=cc: e.activation(out=cvT[:, cc, :], in_=cbk[:, cc * 128:(cc + 1) * 128], func=AF.Identity,
                                                                              bias=convb[:, cc:cc + 1], scale=1.0), r=[cbkey, 'convb'], w=[k_cvT])
                        if lat:
                            chunksA = []
                            if i - 1 >= 0:
                                chunksA.append((ti - 1, 0))
                            chunksA.append((ti, None))
                            if i + 1 < NLAT:
                                chunksA.append((ti + 1, 1))
                            chunksA += [(0, None), (1, None)]
                        else:
                            chunksA = [(0, None), (1, None)]
                        for grp in range(2):
                            for ci, (ct, mk) in enumerate(chunksA):
                                kb_, kk = kvbuf(ct)
                                bk, bkey = nbank()
                                S.op('pe', lambda e, bk=bk, kb_=kb_: e.matmul(bk[:, :], lhsT=kb_[:, 0:128], rhs=qr[:, QA + grp * 512:QA + (grp + 1) * 512],
                                                                             start=True, stop=True), r=[kk, qk], w=[bkey])
                                S.op('act', lambda e, bk=bk, ci=ci: e.activation(out=pTA[grp][:, ci, :], in_=bk[:, :], func=AF.Exp, scale=SCALE),
                                     r=[bkey], w=[('pTA', grp, ci)])
                                if mk is not None:
                                    S.op('pool', lambda e, ci=ci, mk=mk: e.tensor_tensor(
                                        out=pTA[grp][:, ci, :].rearrange("p (g t) -> p g t", g=4), in0=pTA[grp][:, ci, :].rearrange("p (g t) -> p g t", g=4),
                                        in1=masklr[:, mk, :].unsqueeze(1).to_broadcast([128, 4, 128]), op=ALU.mult), r=[('pTA', grp, ci), 'masklr'], w=[('pTA', grp, ci)])
                        if lat:
                            if NLAT >= 5 and 2 <= i <= NLAT - 3:
                                chunksB = [(ti + d - 2, d) for d in range(5)]
                            elif i == 0:
                                chunksB = [(NCTX + j, 5 + j) for j in range(4)]
                            elif i == 1:
                                chunksB = [(NCTX + j, 9 + j) for j in range(4)]
                            elif i == NLAT - 2:
                                chunksB = [(NCTX + NLAT - 4 + j, 13 + j) for j in range(4)]
                            else:
                                chunksB = [(NCTX + NLAT - 4 + j, 17 + j) for j in range(4)]
                            chunksB += [(0, None), (1, None)]
                        else:
                            chunksB = [(0, None), (1, None)]
                        for ci, (ct, ent) in enumerate(chunksB):
                            kb_, kk = kvbuf(ct)
                            bk, bkey = nbank()
                            for h in range(4):
                                p, m = h // 2, h % 2
                                S.op('pe', lambda e, bk=bk, kb_=kb_, h=h, p=p, m=m: e.matmul(
                                    bk[:, h * 128:(h + 1) * 128], lhsT=kb_[:, KB - KA + p * 128:KB - KA + (p + 1) * 128],
                                    rhs=qr[:, QB + m * 256 + p * 128:QB + m * 256 + (p + 1) * 128], start=True, stop=True),
                                     r=[kk, qk], w=[bkey], signal=(h == 3))
                            if ent is not None:
                                bt, btk = btmp[ci % 2], ('btmp', ci % 2)
                                S.op('dve', lambda e, bk=bk, bt=bt, ent=ent: e.scalar_tensor_tensor(
                                    out=bt[:, :], in0=bk[:, :], scalar=SCALE, in1=nat_sb[:, ent, :], op0=ALU.mult, op1=ALU.add),
                                     r=[bkey, 'nat_sb'], w=[btk])
                                S.op('act', lambda e, bt=bt, ci=ci: e.activation(out=pT[:, ci, :], in_=bt[:, :], func=AF.Exp), r=[btk], w=[('pT', ci)])
                            else:
                                S.op('act', lambda e, bk=bk, ci=ci: e.activation(out=pT[:, ci, :], in_=bk[:, :], func=AF.Exp, scale=SCALE),
                                     r=[bkey], w=[('pT', ci)])
                        for grp in range(2):
                            ob, obk = nbank()
                            nchk = len(chunksA)
                            for g in range(4):
                                for ci, (ct, mk) in enumerate(chunksA):
                                    kb_, kk = kvbuf(ct)
                                    S.op('pe', lambda e, g=g, ci=ci, kb_=kb_, ob=ob: e.matmul(
                                        ob[:, g * 65:(g + 1) * 65], lhsT=pTA[grp][:, ci, g * 128:(g + 1) * 128],
                                        rhs=kb_[:, VA - KA + grp * 65:VA - KA + (grp + 1) * 65], start=(ci == 0), stop=(ci == nchk - 1)),
                                         r=[('pTA', grp, ci), kk], w=[obk], signal=(g == 3 and ci == nchk - 1))
                            obv = ob[:, 0:260].rearrange("p (g d) -> p g d", d=65)
                            S.op('dve', lambda e, obv=obv: e.tensor_tensor(out=den[:, :], in0=obv[:, :, 64], in1=esink[:, grp * 4:(grp + 1) * 4], op=ALU.add),
                                 r=[obk, 'esink'], w=[k_den])
                            S.op('dve', lambda e: e.reciprocal(out=rden[:, :], in_=den[:, :]), r=[k_den], w=[k_rden])
                            S.op('dve', lambda e, obv=obv: e.tensor_tensor(
                                out=otok[:, grp * 256:(grp + 1) * 256].rearrange("p (g d) -> p g d", d=64), in0=obv[:, :, 0:64],
                                in1=rden[:, :].unsqueeze(2).to_broadcast([128, 4, 64]), op=ALU.mult), r=[obk, k_rden], w=[k_otok])
                        ob, obk = nbank()
                        nchk = len(chunksB)
                        for h in range(4):
                            for ci, (ct, ent) in enumerate(chunksB):
                                kb_, kk = kvbuf(ct)
                                S.op('pe', lambda e, h=h, ci=ci, kb_=kb_, ob=ob: e.matmul(
                                    ob[:, h * 65:(h + 1) * 65], lhsT=pT[:, ci, h * 128:(h + 1) * 128],
                                    rhs=kb_[:, VB - KA + h * 65:VB - KA + (h + 1) * 65], start=(ci == 0), stop=(ci == nchk - 1)),
                                     r=[('pT', ci), kk], w=[obk], signal=(h == 3 and ci == nchk - 1))
                        obv = ob[:, 0:260].rearrange("p (g d) -> p g d", d=65)
                        S.op('dve', lambda e, obv=obv: e.reciprocal(out=rden[:, :], in_=obv[:, :, 64]), r=[obk], w=[k_rden])
                        S.op('dve', lambda e, obv=obv: e.tensor_tensor(
                            out=otok[:, 512:768].rearrange("p (g d) -> p g d", d=64), in0=obv[:, :, 0:64],
                            in1=rden[:, :].unsqueeze(2).to_broadcast([128, 4, 64]), op=ALU.mult), r=[obk, k_rden], w=[k_otok])
                        bk2, bkey2 = nbank()
                        for cc in range(2):
                            S.op('pe', lambda e, bk2=bk2, cc=cc: e.transpose(out=bk2[:, cc * 128:(cc + 1) * 128], in_=cvT[:, cc, :], identity=identf),
                                 r=[k_cvT, 'cst'], w=[bkey2], signal=(cc == 1))
                        ln_stats(bk2, bkey2, width=256)
                        ln_apply(cvn[:, :], k_cvn, bk2[:, 0:256], bkey2)
                        S.op('dve', lambda e: e.tensor_tensor(out=cvn[:, :], in0=cvn[:, :], in1=clng[:, :], op=ALU.mult), r=[k_cvn, 'clng'], w=[k_cvn])
                        S.op('pool', lambda e: e.tensor_tensor(out=cvn[:, :], in0=cvn[:, :], in1=clnb[:, :], op=ALU.add), r=[k_cvn, 'clnb'], w=[k_cvn])
                        S.op('act', lambda e: e.activation(out=otok[:, 768:1024], in_=cvn[:, :], func=AF.Silu), r=[k_cvn], w=[k_otok])

                        if dbg:
                            S.dma('sp', lambda q: q.dma_start(out=otok_d[ti * 128:(ti + 1) * 128, :], in_=otok[:, :]), r=[k_otok], w=[('dbg_otok', ti)])

                        if stop_after == f'B{l}:conv':
                            return
                    if part == 1:
                        transposes_to(oT[:, :, :].rearrange("p k t -> p (k t)"), k_oT, lambda k: otok[:, k * 128:(k + 1) * 128], 8, src_key=k_otok)
                        for n in range(2):
                            bk, bkey = nbank()
                            for k in range(8):
                                S.op('pe', lambda e, bk=bk, k=k, n=n: e.matmul(bk[:, :], lhsT=oT[:, k, :], rhs=w_out_sb[:, k, n * 512:(n + 1) * 512],
                                                                             start=(k == 0), stop=(k == 7)), r=[k_oT, 'w_out_sb'], w=[bkey], signal=(k == 7))
                            S.op('dve', lambda e, bk=bk, n=n: e.tensor_tensor(out=f32b[:, n * 512:(n + 1) * 512], in0=bk[:, :],
                                                                             in1=modB[s][0][:, n * 512:(n + 1) * 512], op=ALU.mult),
                                 r=[bkey, ('modB', s, 0)], w=[k_f32b])
                        S.op('dve', lambda e: e.scalar_tensor_tensor(out=f32b[:, :], in0=xs_[:, :], scalar=ALPHA, in1=f32b[:, :], op0=ALU.mult, op1=ALU.add),
                             r=[xk, k_f32b], w=[k_f32b])
                        ln_stats(f32b, k_f32b)
                        ln_apply(f32c[:, :], k_f32c, f32b[:, :], k_f32b)
                        S.op('dve', lambda e: e.tensor_tensor(out=f32c[:, :], in0=f32c[:, :], in1=ln1g[:, :], op=ALU.mult), r=[k_f32c, 'ln1g'], w=[k_f32c])
                        S.op('pool', lambda e: e.tensor_tensor(out=f32c[:, :], in0=f32c[:, :], in1=ln1b[:, :], op=ALU.add), r=[k_f32c, 'ln1b'], w=[k_f32c])
                        S.dma('sp', lambda q: q.dma_start(out=x1s[ti * 128:(ti + 1) * 128, :], in_=f32c[:, :]), r=[k_f32c], w=[('x1s', ti)])
                        if stop_after == f'B{l}:proj':
                            return
                        ln_stats(f32c, k_f32c)
                        ln_apply(f32b[:, :], k_f32b, f32c[:, :], k_f32c)
                        S.op('dve', lambda e: e.tensor_tensor(out=f32b[:, :], in0=f32b[:, :], in1=modB[s][1][:, :], op=ALU.mult),
                             r=[k_f32b, ('modB', s, 1)], w=[k_f32b])
                        S.op('pool', lambda e: e.tensor_tensor(out=hr[:, 0:1024], in0=f32b[:, :], in1=modB[s][2][:, :], op=ALU.add),
                             r=[k_f32b, ('modB', s, 2)], w=[hk])
                    if part == 2:
                        transposes_to(h2T[:, :, :].rearrange("p k t -> p (k t)"), k_h2T, lambda k: hr[:, k * 128:(k + 1) * 128], 8, src_key=hk)
                        bk, bkey = nbank()
                        for k in range(8):
                            S.op('pe', lambda e, bk=bk, k=k: e.matmul(bk[:, 0:NE], lhsT=h2T[:, k, :], rhs=w_r_sb[:, k, :], start=(k == 0), stop=(k == 7)),
                                 r=[k_h2T, 'w_r_sb'], w=[bkey], signal=(k == 7))
                        S.op('act', lambda e, bk=bk: e.activation(out=ex[:, :], in_=bk[:, 0:NE], func=AF.Exp, accum_out=ssum[:, 0:1]), r=[bkey], w=[k_ex, k_ssum])
                        S.op('dve', lambda e: e.reciprocal(out=ssum[:, :], in_=ssum[:, :]), r=[k_ssum], w=[k_ssum])
                        S.op('dve', lambda e: e.tensor_scalar(out=aff_all[:, ti, :], in0=ex[:, :], scalar1=ssum[:, 0:1], scalar2=None, op0=ALU.mult),
                             r=[k_ex, k_ssum], w=[('aff', ti)])
                        S.op('dve', lambda e: e.tensor_copy(out=hr[:, 1026:1042], in_=aff_all[:, ti, :]), r=[('aff', ti)], w=[hk])
                        S.op('dve', lambda e: e.tensor_tensor(out=hr[:, 1042:1058], in0=aff_all[:, ti, :], in1=hr[:, 1026:1042], op=ALU.subtract),
                             r=[('aff', ti), hk], w=[hk])
                        S.op('dve', lambda e: e.tensor_scalar(out=hr[:, 1024:1025], in0=iotap, scalar1=0.0, scalar2=float(ti), op0=ALU.mult, op1=ALU.add),
                             r=['cst'], w=[hk])
                        S.op('dve', lambda e: e.tensor_copy(out=hr[:, 1025:1026], in_=iotap), r=['cst'], w=[hk])
                        S.dma('sp', lambda q: q.dma_start(out=h2s[ti * 128:(ti + 1) * 128, :], in_=hr[:, :]), r=[hk], w=[('h2s', ti)])

            nB = len(tiles_b)
            for step in range(nB + 2):
                if step < nB:
                    tileB(tiles_b[step], 0)
                if 0 <= step - 1 < nB:
                    tileB(tiles_b[step - 1], 1)
                if 0 <= step - 2 < nB:
                    tileB(tiles_b[step - 2], 2)
            S.barrier()
        if stop_after is not None and stop_after.startswith(f'B{l}'):
            return _finish(nc, S, es)

        NTB = len(tiles_b)
        t0b = tiles_b[0]
        with ExitStack() as ph:
            cmpb = sb(ph, "cmpb", [128, NT, NE], BF16)
            lo = sb(ph, "lo", [128, 32], F32)
            hi = sb(ph, "hi", [128, 32], F32)
            mid = sb(ph, "mid", [128, 32], F32)
            kvec = sb(ph, "kvec", [128, 32], F32)
            cntp = sb(ph, "cntp", [128, 32], BF16)
            ge = sb(ph, "ge", [128, 32], F32)
            gm = sb(ph, "gm", [128, 32], F32)
            posf = sb(ph, "posf", [128, NT, NE], F32)
            tot = sb(ph, "tot", [128, NT, NE], F32)
            base = sb(ph, "base", [128, NT + 1, NE], F32)
            sel = sb(ph, "sel", [128, NT, NE], F32)
            posi = sb(ph, "posi", [128, NT, NE], I32)
            affk = [('aff', t) for t in range(NT)]
            S.op('dve', lambda e: e.memset(lo[:, :], 0.0), w=['lo'])
            S.op('dve', lambda e: e.memset(hi[:, :], 1.0), w=['hi'])
            S.op('dve', lambda e: e.memset(kvec[:, 0:16], float(CAPL)), w=['kvec'])
            S.op('dve', lambda e: e.memset(kvec[:, 16:32], float(CAPC)), w=['kvec'])
            S.op('dve', lambda e: e.memset(cntp[:, :], 0.0), w=['cntp'])
            if last:
                S.op('dve', lambda e: e.memset(cmpb[:, 0:NCTX, :], 0.0), w=['cmpb'])
            lat_aff = aff_all[:, NCTX:NT, :]
            ctx_aff = aff_all[:, 0:NCTX, :]

            def compare(thr):
                S.op('dve', lambda e: e.tensor_tensor(out=cmpb[:, NCTX:NT, :], in0=lat_aff, in1=thr[:, 0:16].unsqueeze(1).to_broadcast([128, NLAT, NE]),
                                                      op=ALU.is_ge), r=affk + ['thr'], w=['cmpb'])
                if not last:
                    S.op('dve', lambda e: e.tensor_tensor(out=cmpb[:, 0:NCTX, :], in0=ctx_aff, in1=thr[:, 16:32].unsqueeze(1).to_broadcast([128, NCTX, NE]),
                                                          op=ALU.is_ge), r=affk + ['thr'], w=['cmpb'])

            for itn in range(30):
                S.op('dve', lambda e: e.tensor_tensor(out=mid[:, :], in0=lo[:, :], in1=hi[:, :], op=ALU.add), r=['lo', 'hi'], w=['thr'])
                S.op('dve', lambda e: e.tensor_scalar(out=mid[:, :], in0=mid[:, :], scalar1=0.5, scalar2=None, op0=ALU.mult), r=['thr'], w=['thr'])
                compare(mid)
                S.op('dve', lambda e: e.tensor_reduce(out=cntp[:, 0:16], in_=cmpb[:, NCTX:NT, :].rearrange("p t e -> p e t"), axis=AX.X, op=ALU.add),
                     r=['cmpb'], w=['cntp'])
                if not last:
                    S.op('dve', lambda e: e.tensor_reduce(out=cntp[:, 16:32], in_=cmpb[:, 0:NCTX, :].rearrange("p t e -> p e t"), axis=AX.X, op=ALU.add),
                         r=['cmpb'], w=['cntp'])
                bk, bkey = nbank()
                S.op('pe', lambda e, bk=bk: e.matmul(bk[:, 0:32], lhsT=onesb[:, :], rhs=cntp[:, :], start=True, stop=True), r=['onesb', 'cntp'], w=[bkey])
                S.op('dve', lambda e, bk=bk: e.tensor_tensor(out=ge[:, :], in0=bk[:, 0:32], in1=kvec[:, :], op=ALU.is_ge), r=[bkey, 'kvec'], w=['ge'])
                S.op('dve', lambda e: e.tensor_tensor(out=gm[:, :], in0=ge[:, :], in1=mid[:, :], op=ALU.mult), r=['ge', 'thr'], w=['gm'])
                S.op('dve', lambda e: e.tensor_tensor(out=lo[:, :], in0=lo[:, :], in1=gm[:, :], op=ALU.max), r=['lo', 'gm'], w=['lo'])
                S.op('dve', lambda e: e.scalar_tensor_tensor(out=gm[:, :], in0=ge[:, :], scalar=2.0, in1=mid[:, :], op0=ALU.mult, op1=ALU.add),
                     r=['ge', 'thr', 'gm'], w=['gm'])
                S.op('dve', lambda e: e.tensor_tensor(out=hi[:, :], in0=hi[:, :], in1=gm[:, :], op=ALU.min), r=['hi', 'gm'], w=['hi'])
            S.op('dve', lambda e: e.tensor_copy(out=mid[:, :], in_=lo[:, :]), r=['lo'], w=['thr'])
            compare(mid)
            cflat = cmpb[:, :, :].rearrange("p t e -> p (t e)")
            pflat = posf[:, :, :].rearrange("p t e -> p (t e)")
            tflat = tot[:, :, :].rearrange("p t e -> p (t e)")
            ncol = NT * NE
            for c0 in range(0, ncol, 512):
                cs = min(512, ncol - c0)
                bk, bkey = nbank()
                S.op('pe', lambda e, bk=bk, c0=c0, cs=cs: e.matmul(bk[:, 0:cs], lhsT=triub[:, :], rhs=cflat[:, c0:c0 + cs], start=True, stop=True),
                     r=['triub', 'cmpb'], w=[bkey])
                S.op('act', lambda e, bk=bk, c0=c0, cs=cs: e.copy(out=pflat[:, c0:c0 + cs], in_=bk[:, 0:cs]), r=[bkey], w=['posf'])
                bk, bkey = nbank()
                S.op('pe', lambda e, bk=bk, c0=c0, cs=cs: e.matmul(bk[:, 0:cs], lhsT=onesb[:, :], rhs=cflat[:, c0:c0 + cs], start=True, stop=True),
                     r=['onesb', 'cmpb'], w=[bkey])
                S.op('act', lambda e, bk=bk, c0=c0, cs=cs: e.copy(out=tflat[:, c0:c0 + cs], in_=bk[:, 0:cs]), r=[bkey], w=['tot'])
            S.op('dve', lambda e: e.memset(base[:, 0, :], float(CAPL)), w=['base'])
            S.op('dve', lambda e: e.memset(base[:, NCTX, :], 0.0), w=['base'])
            for t in range(NT):
                if t == NCTX - 1:
                    continue
                S.op('dve', lambda e, t=t: e.tensor_tensor(out=base[:, t + 1, :], in0=base[:, t, :], in1=tot[:, t, :], op=ALU.add),
                     r=['base', 'tot'], w=['base'])
            S.op('dve', lambda e: e.tensor_tensor(out=posf[:, :, :], in0=posf[:, :, :], in1=base[:, 0:NT, :], op=ALU.add), r=['posf', 'base'], w=['posf'])
            S.op('dve', lambda e: e.scalar_tensor_tensor(out=sel[:, NCTX:NT, :], in0=posf[:, NCTX:NT, :], scalar=float(CAPL), in1=cmpb[:, NCTX:NT, :],
                                                         op0=ALU.is_lt, op1=ALU.mult), r=['posf', 'cmpb'], w=['sel'])
            S.op('dve', lambda e: e.scalar_tensor_tensor(out=sel[:, 0:NCTX, :], in0=posf[:, 0:NCTX, :], scalar=float(CAPL + CAPC), in1=cmpb[:, 0:NCTX, :],
                                                         op0=ALU.is_lt, op1=ALU.mult), r=['posf', 'cmpb'], w=['sel'])
            S.op('dve', lambda e: e.scalar_tensor_tensor(out=posf[:, :, :], in0=posf[:, :, :], scalar=-BIG, in1=sel[:, :, :], op0=ALU.add, op1=ALU.mult),
                 r=['posf', 'sel'], w=['posf'])
            S.op('dve', lambda e: e.tensor_scalar(out=posi[:, :, :], in0=posf[:, :, :], scalar1=BIG, scalar2=None, op0=ALU.add), r=['posf'], w=['posi'])

            h2ld = [sb(ph, f"h2ld{i}", [128, RW], BF16) for i in range(3)]
            breg = nc.gpsimd.to_reg(CAPT - 1)
            for n, ti in enumerate(tiles_b):
                hl, hlk = h2ld[n % 3], ('h2ld', n % 3)
                S.dma('sp', lambda q, ti=ti, hl=hl: q.dma_start(out=hl[:, :], in_=h2s[ti * 128:(ti + 1) * 128, :]), r=[('h2s', ti)], w=[hlk])
                for ex_ in range(NE):
                    S.dma('pool', lambda q, ti=ti, ex_=ex_, hl=hl: q.indirect_dma_start(
                        out=Xs[ex_][:, :], out_offset=bass.IndirectOffsetOnAxis(ap=posi[:, ti, ex_:ex_ + 1], axis=0),
                        in_=hl[:, :], in_offset=None, bounds_check=breg, oob_is_err=False), r=[hlk, 'posi'], w=[('Xs', ex_)])
            S.barrier()
        if stop_after == f'R{l}':
            return _finish(nc, S, es)

        NST = (CAPT + 127) // 128
        CAPP = NST * 128
        NSL = 3 if CAPP % 3 == 0 and CAPP // 3 <= 512 else (CAPP + 511) // 512
        SLW = CAPP // NSL
        assert SLW * NSL == CAPP and SLW <= 512
        with ExitStack() as ph:
            wg = [sb(ph, f"wg{i}", [128, 8, D], BF16) for i in range(2)]
            wu = [sb(ph, f"wu{i}", [128, 8, D], BF16) for i in range(2)]
            wd = [sb(ph, f"wd{i}", [128, 8, D], BF16) for i in range(2)]
            xsb = sb(ph, "xsb", [128, NST, RW], BF16)
            XT = sb(ph, "XT", [128, 8, NST * 128], BF16)
            hidT = sb(ph, "hidT", [128, 8, NST * 128], BF16)
            sgt = [sb(ph, f"sgt{i}", [128, 512], F32) for i in range(2)]
            ysb = [sb(ph, f"ysb{i}", [128, D], F32) for i in range(3)]
            gcol = sb(ph, "gcol", [128, NST], F32)
            idxi = sb(ph, "idxi", [128, NST], I32)
            S.op('pool', lambda e: e.memset(xsb[:, NST - 1, :], 0.0), w=['xsb'])
            zt = ysb[0]
            S.op('pool', lambda e: e.memset(zt[:, :], 0.0), w=[('ysb', 0)])
            ztoks = []
            for ti in tiles_b:
                ztoks.append(S.dma('sp', lambda q, ti=ti: q.dma_start(out=macc[ti * 128:(ti + 1) * 128, :], in_=zt[:, :]),
                                   r=[('ysb', 0), ('macc_rd', ti)], w=[('macc_z', ti)]))
            prev_sc = list(ztoks)

            def load_w(ex_):
                sl = ex_ % 2
                for wsb, wdr, nm in ((wg, w_gate, 'wg'), (wu, w_up, 'wu'), (wd, w_down, 'wd')):
                    S.dma('pool', lambda q, wsb=wsb, wdr=wdr: q.dma_start(
                        out=wsb[sl][:, :, :], in_=wdr[l, ex_, :, :].rearrange("(ko ki) n -> ki ko n", ki=128)), w=[(nm, sl)])

            load_w(0)
            ycount = 0
            for ex_ in range(NE):
                sl = ex_ % 2
                if ex_ + 1 < NE:
                    load_w(ex_ + 1)
                nfull = CAPT // 128
                rem = CAPT - nfull * 128
                S.dma('sp', lambda q, ex_=ex_: q.dma_start(out=xsb[:, 0:nfull, :], in_=Xs[ex_][0:nfull * 128, :].rearrange("(s p) w -> p s w", p=128)),
                      r=[('Xs', ex_)], w=['xsb'])
                if rem:
                    S.dma('sp', lambda q, ex_=ex_: q.dma_start(out=xsb[0:rem, nfull, :], in_=Xs[ex_][nfull * 128:CAPT, :]), r=[('Xs', ex_)], w=['xsb'])
                S.op('dve', lambda e, ex_=ex_: e.tensor_tensor(out=gcol[:, 0:nfull], in0=xsb[:, 0:nfull, 1026 + ex_], in1=xsb[:, 0:nfull, 1042 + ex_], op=ALU.add),
                     r=['xsb'], w=['gcol'])
                S.op('dve', lambda e: e.scalar_tensor_tensor(out=idxi[:, 0:nfull], in0=xsb[:, 0:nfull, 1024], scalar=128.0, in1=xsb[:, 0:nfull, 1025],
                                                             op0=ALU.mult, op1=ALU.add), r=['xsb'], w=['idxi'])
                if rem:
                    S.op('dve', lambda e, ex_=ex_: e.tensor_tensor(out=gcol[0:rem, nfull:nfull + 1], in0=xsb[0:rem, nfull, 1026 + ex_:1027 + ex_],
                                                                  in1=xsb[0:rem, nfull, 1042 + ex_:1043 + ex_], op=ALU.add), r=['xsb'], w=['gcol'])
                    S.op('dve', lambda e: e.scalar_tensor_tensor(out=idxi[0:rem, nfull:nfull + 1], in0=xsb[0:rem, nfull, 1024:1025], scalar=128.0,
                                                                 in1=xsb[0:rem, nfull, 1025:1026], op0=ALU.mult, op1=ALU.add), r=['xsb'], w=['idxi'])
                for st in range(NST):
                    rows = 128 if st < nfull else rem
                    transposes_to(XT[:, :, st * 128:(st + 1) * 128], 'XT', lambda k, st=st: xsb[:, st, k * 128:(k + 1) * 128], 8,
                                  src_key='xsb', evac=('act' if st % 2 == 0 else 'dve'))
                for fc in range(8):
                    for sn in range(NSL):
                        n0 = sn * SLW
                        bg, bgk = nbank()
                        for k in range(8):
                            S.op('pe', lambda e, bg=bg, k=k, fc=fc, n0=n0: e.matmul(bg[:, 0:SLW], lhsT=wg[sl][:, k, fc * 128:(fc + 1) * 128],
                                                                                   rhs=XT[:, k, n0:n0 + SLW], start=(k == 0), stop=(k == 7)),
                                 r=[('wg', sl), 'XT'], w=[bgk], signal=(k == 7))
                        bu, buk = nbank()
                        for k in range(8):
                            S.op('pe', lambda e, bu=bu, k=k, fc=fc, n0=n0: e.matmul(bu[:, 0:SLW], lhsT=wu[sl][:, k, fc * 128:(fc + 1) * 128],
                                                                                   rhs=XT[:, k, n0:n0 + SLW], start=(k == 0), stop=(k == 7)),
                                 r=[('wu', sl), 'XT'], w=[buk], signal=(k == 7))
                        sgi = (fc * NSL + sn) % 2
                        S.op('act', lambda e, bg=bg, sgi=sgi: e.activation(out=sgt[sgi][:, 0:SLW], in_=bg[:, 0:SLW], func=AF.Silu), r=[bgk], w=[('sgt', sgi)])
                        S.op('dve', lambda e, bu=bu, sgi=sgi, fc=fc, n0=n0: e.tensor_tensor(out=hidT[:, fc, n0:n0 + SLW], in0=bu[:, 0:SLW], in1=sgt[sgi][:, 0:SLW],
                                                                                           op=ALU.mult), r=[buk, ('sgt', sgi)], w=['hidT'])
                cur_sc = []
                for st in range(NST):
                    rows = 128 if st < nfull else rem
                    yi = ycount % 3
                    ycount += 1
                    for half in range(2):
                        by, byk = nbank()
                        for fc in range(8):
                            S.op('pe', lambda e, by=by, fc=fc, st=st, rows=rows, half=half: e.matmul(
                                by[:, :], lhsT=hidT[:, fc, st * 128:(st + 1) * 128], rhs=wd[sl][:, fc, half * 512:(half + 1) * 512],
                                start=(fc == 0), stop=(fc == 7)), r=['hidT', ('wd', sl)], w=[byk], signal=(fc == 7))
                        if half == 0:
                            S.op('act', lambda e, by=by, yi=yi, st=st, rows=rows: e.activation(out=ysb[yi][0:rows, 0:512], in_=by[0:rows, :], func=AF.Copy,
                                                                                              scale=gcol[0:rows, st:st + 1]), r=[byk, 'gcol'], w=[('ysb', yi)])
                        else:
                            S.op('dve', lambda e, by=by, yi=yi, st=st, rows=rows: e.tensor_scalar(out=ysb[yi][0:rows, 512:1024], in0=by[0:rows, :],
                                                                                                 scalar1=gcol[0:rows, st:st + 1], scalar2=None, op0=ALU.mult),
                                 r=[byk, 'gcol'], w=[('ysb', yi)])
                    cur_sc.append(S.dma('pool', lambda q, yi=yi, st=st, rows=rows: q.indirect_dma_start(
                        out=macc[:, :], out_offset=bass.IndirectOffsetOnAxis(ap=idxi[0:rows, st:st + 1], axis=0),
                        in_=ysb[yi][0:rows, :], in_offset=None, compute_op=ALU.add), r=[('ysb', yi), 'idxi'], w=[('macc_sc', ex_, st)], extra=prev_sc))
                prev_sc = cur_sc
            S.barrier()
        if stop_after == f'M{l}':
            return _finish(nc, S, es)

        with ExitStack() as ph:
            g2 = [sb(ph, f"g2_{s}", [128, D], F32) for s in range(2)]
            ln2g = sb(ph, "ln2g", [128, D], F32)
            ln2b = sb(ph, "ln2b", [128, D], F32)
            xa = [sb(ph, f"xa{i}", [128, D], F32) for i in range(2)]
            xm = [sb(ph, f"xm{i}", [128, D], F32) for i in range(2)]
            xo = [sb(ph, f"xo{i}", [128, D], F32) for i in range(2)]
            for s in range(2):
                S.dma('sp', lambda q, s=s: q.dma_start(out=g2[s][:, :], in_=modbc[l, s, :, 5 * D:6 * D]), r=[('modbc', l, s)], w=[('g2', s)])
            S.dma('sp', lambda q: q.dma_start(out=ln2g[:, :], in_=ln2_g[l:l + 1, :].to_broadcast([128, D])), w=['ln2g'])
            S.dma('sp', lambda q: q.dma_start(out=ln2b[:, :], in_=ln2_b[l:l + 1, :].to_broadcast([128, D])), w=['ln2b'])
            out_toks = []
            for n, ti in enumerate(tiles_b):
                s = 0 if ti < NCTX else 1
                a, ak = xa[n % 2], ('xa', n % 2)
                m, mk = xm[n % 2], ('xm', n % 2)
                o, ok = xo[n % 2], ('xo', n % 2)
                S.dma('sp', lambda q, ti=ti, a=a: q.dma_start(out=a[:, :], in_=x1s[ti * 128:(ti + 1) * 128, :]), r=[('x1s', ti)], w=[ak])
                S.dma('sp', lambda q, ti=ti, m=m: q.dma_start(out=m[:, :], in_=macc[ti * 128:(ti + 1) * 128, :]), w=[mk, ('macc_rd', ti)], extra=prev_sc)
                S.op('dve', lambda e, m=m, s=s: e.tensor_tensor(out=m[:, :], in0=m[:, :], in1=g2[s][:, :], op=ALU.mult), r=[mk, ('g2', s)], w=[mk])
                S.op('dve', lambda e, m=m, a=a: e.scalar_tensor_tensor(out=m[:, :], in0=a[:, :], scalar=ALPHA, in1=m[:, :], op0=ALU.mult, op1=ALU.add),
                     r=[ak, mk], w=[mk])
                ln_stats(m, mk)
                ln_apply(o[:, :], ok, m[:, :], mk)
                S.op('dve', lambda e, o=o: e.tensor_tensor(out=o[:, :], in0=o[:, :], in1=ln2g[:, :], op=ALU.mult), r=[ok, 'ln2g'], w=[ok])
                S.op('pool', lambda e, o=o: e.tensor_tensor(out=o[:, :], in0=o[:, :], in1=ln2b[:, :], op=ALU.add), r=[ok, 'ln2b'], w=[ok])
                if last:
                    i = ti - NCTX
                    out_toks.append(S.dma('sp', lambda q, i=i, o=o: q.dma_start(out=out[i * 128:(i + 1) * 128, :], in_=o[:, :]), r=[ok], w=[('out', i)]))
                else:
                    S.dma('sp', lambda q, ti=ti, o=o: q.dma_start(out=xs_d[ti * 128:(ti + 1) * 128, :], in_=o[:, :]), r=[ok], w=[('xs_d',)])
            S.barrier()
        if stop_after == f'F{l}':
            return _finish(nc, S, es)
    return _finish(nc, S, es)


def _finish(nc, S, es):
    S.barrier()
    es.close()
    return nc


def _consts():
    c = np.zeros((128, 6 * 128), np.float32)
    p = np.arange(128)
    c[:, 0:128] = np.eye(128, dtype=np.float32)
    c[:, 128:256] = (p[:, None] < p[None, :]).astype(np.float32)
    c[:, 256:384] = (p[:, None] >= p[None, :]).astype(np.float32)
    c[:, 384:512] = (p[:, None] <= p[None, :]).astype(np.float32)
    c[:, 512:640] = 1.0
    c[:, 640] = p.astype(np.float32)
    c[:, 641] = (p < 64).astype(np.float32)
    c[:, 642] = (p >= 64).astype(np.float32)
    return c


def _rope_table(NLAT):
    t = np.arange(NLAT * 128, dtype=np.int32)
    row = (t // 64).astype(np.float32)[:, None]
    col = (t % 64).astype(np.float32)[:, None]
    inv = (np.float32(10000.0) ** (-np.arange(16, dtype=np.float32) / np.float32(16))).astype(np.float32)
    ar = (row * inv).astype(np.float32)
    ac = (col * inv).astype(np.float32)
    cr, sr, cc, sc = np.cos(ar), np.sin(ar), np.cos(ac), np.sin(ac)
    tab = np.concatenate([cr, cr, cc, cc, -sr, sr, -sc, sc], axis=1).astype(np.float32)
    return np.ascontiguousarray(tab.reshape(NLAT, 128, 128))


def _nat_entries(NLAT):
    ents = [(2, d) for d in range(5)] if NLAT >= 5 else [(0, 0)] * 5
    ents = [(2, 2 + d - 2) for d in range(5)] if NLAT >= 5 else ents
    ents += [(0, j) for j in range(4)] + [(1, j) for j in range(4)]
    ents += [(NLAT - 2, NLAT - 4 + j) for j in range(4)] + [(NLAT - 1, NLAT - 4 + j) for j in range(4)]
    return ents


def _nat_table(nat_bias, NLAT):
    rows = NLAT * 2
    ents = _nat_entries(NLAT)
    kk = np.arange(128)
    Lh = nat_bias.shape[0]
    out = np.empty((Lh, 128, NCHB, 4, 128), np.float32)
    for n, (i, j) in enumerate(ents):
        kr = 2 * j + kk // 64
        kc = kk % 64
        qr = 2 * i + kk // 64
        qc = kk % 64
        rs = np.clip(qr - 4, 0, rows - 8)
        cs = np.clip(qc - 8, 0, 48)
        valid = ((kr[:, None] >= rs[None, :]) & (kr[:, None] < rs[None, :] + 8) &
                 (kc[:, None] >= cs[None, :]) & (kc[:, None] < cs[None, :] + 16))
        dr = np.clip(kr[:, None] - qr[None, :] + 7, 0, 14)
        dc = np.clip(kc[:, None] - qc[None, :], -15, 15) + 15
        g = nat_bias[:, :, dr, dc]
        g = np.where(valid[None, None], g, np.float32(NEG))
        out[:, :, n, :, :] = np.transpose(g, (0, 2, 1, 3))
    return np.ascontiguousarray(out.reshape(Lh, 128, NCHB * 512))


def make_in_maps(inputs, NLAT, samples, moe=True):
    names = ['w_mod', 'b_mod', 'w_in', 'a_sink', 'conv_w', 'conv_b', 'conv_ln_g', 'conv_ln_b', 'w_out', 'ln1_g', 'ln1_b',
             'w_router', 'ln2_g', 'ln2_b'] + (['w_gate', 'w_up', 'w_down'] if moe else [])
    shared = {k: np.ascontiguousarray(np.asarray(inputs[k], np.float32)) for k in names}
    cw = shared['conv_w']
    shared['conv_w'] = np.ascontiguousarray(cw.reshape(L, 31, 2, 128).transpose(0, 3, 2, 1).reshape(L, 128, 62))
    shared['conv_b'] = np.ascontiguousarray(shared['conv_b'].reshape(L, 2, 128).transpose(0, 2, 1))
    shared['natT'] = _nat_table(np.asarray(inputs['nat_bias'], np.float32), NLAT)
    shared['rope'] = _rope_table(NLAT)
    shared['consts'] = _consts()
    maps = []
    for b in samples:
        m = dict(shared)
        m['xlat'] = np.ascontiguousarray(np.asarray(inputs['x'][b, :NLAT * 128], np.float32))
        m['xctx'] = np.ascontiguousarray(np.asarray(inputs['ctx'][b], np.float32))
        cv = np.stack([np.asarray(inputs['c_ctx'], np.float32), np.asarray(inputs['c'][b], np.float32)])
        m['cvec'] = np.ascontiguousarray(cv.reshape(2, 8, 128).transpose(2, 1, 0).reshape(128, 16))
        maps.append(m)
    return maps


def kernel(**inputs):
    NLAT = 64
    nc = build_program(NLAT)
    maps = make_in_maps(inputs, NLAT, [0, 1, 2, 3, 0, 1, 2, 3])
    res = run_bass_kernel_spmd(nc, maps, core_ids=list(range(8)))
    return np.stack([np.asarray(res.results[b]["out"], np.float32).reshape(NLAT * 128, D) for b in range(4)])
```

```python
import os
import numpy as np
from contextlib import ExitStack
import concourse.bass as bass
import concourse.mybir as mybir
from concourse.bass_utils import run_bass_kernel_spmd

F32 = mybir.dt.float32
BF16 = mybir.dt.bfloat16
I32 = mybir.dt.int32
AF = mybir.ActivationFunctionType
ALU = mybir.AluOpType
AX = mybir.AxisListType

D = 1024
L = 2
NCTX = 2
NE = 16
EPS = 1e-6
ALPHA = float((2 * L) ** 0.25)
SCALE = 0.125
NEG = -30000.0
BIG = 1.0e6
QA, QB, KA, VA, KB, VB, CH, AW = 0, 1024, 1536, 1664, 1794, 2050, 2310, 2566
QW = KA
KVW = CH - KA
RW = 1024 + 2 + 32
NCHB = 21


class Sched:
    EPOCH = 16000

    def __init__(self, nc, es, ndma=16):
        self.nc, self.es = nc, es
        self.eng = dict(pe=nc.tensor, act=nc.scalar, dve=nc.vector, pool=nc.gpsimd, sp=nc.sync)
        self.sems = {k: [] for k in self.eng}
        self.cnt = {k: 0 for k in self.eng}
        self.dpool = {'sp': list(range(0, ndma)), 'pool': list(range(ndma, ndma + 8)), 'act': list(range(ndma + 8, ndma + 12))}
        ntot = ndma + 12
        self.dsem = [es.enter_context(nc.semaphore(f"dq{i}")) for i in range(ntot)]
        self.dval = [0] * ntot
        self.dnext = {'sp': 0, 'pool': 0, 'act': 0}
        self.waited = {k: {} for k in self.eng}
        self.lastw = {}
        self.readers = {}
        self.same_engine_sync = True
        self.nwaits = 0

    def _sem(self, e, seq):
        i = (seq - 1) // self.EPOCH
        while len(self.sems[e]) <= i:
            self.sems[e].append(self.es.enter_context(self.nc.semaphore(f"s_{e}{len(self.sems[e])}")))
        return self.sems[e][i], (seq - 1) % self.EPOCH + 1

    def _wait(self, e, tok):
        kind, src, val = tok
        if kind == 'e':
            if src == e and (e == 'pe' or not self.same_engine_sync):
                return
            assert val <= self.cnt[src], f"dependency on unsignalled op {tok}"
            key = ('e', src)
        else:
            key = ('d', src)
        if self.waited[e].get(key, 0) >= val:
            return
        self.waited[e][key] = val
        if kind == 'e':
            sem, v = self._sem(src, val)
        else:
            sem, v = self.dsem[src], val
        self.eng[e].wait_ge(sem, v)
        self.nwaits += 1

    def _deps(self, r, w):
        deps = []
        for k in r:
            if k in self.lastw:
                deps.append(self.lastw[k])
            if isinstance(k, tuple) and k[0] == 'pb':
                deps.extend(self.readers.get(k, {}).values())
        for k in w:
            if k in self.lastw:
                deps.append(self.lastw[k])
            deps.extend(self.readers.get(k, {}).values())
        return deps

    def _record(self, tok, r, w):
        for k in r:
            d = self.readers.setdefault(k, {})
            key = (tok[0], tok[1])
            if key not in d or d[key][2] < tok[2]:
                d[key] = tok
        for k in w:
            self.lastw[k] = tok
            self.readers[k] = {}

    def begin_record(self):
        self.rec = []

    def end_record(self):
        ops, self.rec = self.rec, None
        return ops

    def emit_interleaved(self, chains):
        idx = [0] * len(chains)
        left = sum(len(c) for c in chains)
        while left:
            for ci, c in enumerate(chains):
                if idx[ci] < len(c):
                    kind, args, kw = c[idx[ci]]
                    idx[ci] += 1
                    left -= 1
                    (self.op if kind == 'op' else self.dma)(*args, **kw)

    def op(self, e, fn, r=(), w=(), signal=True, extra=()):
        if getattr(self, 'rec', None) is not None:
            self.rec.append(('op', (e, fn), dict(r=list(r), w=list(w), signal=signal, extra=list(extra))))
            return None
        for d in self._deps(r, w):
            self._wait(e, d)
        for d in extra:
            self._wait(e, d)
        inst = fn(self.eng[e])
        if signal:
            self.cnt[e] += 1
            seq = self.cnt[e]
            sem, _ = self._sem(e, seq)
            inst.then_inc(sem, 1)
        else:
            seq = self.cnt[e] + 1
        tok = ('e', e, seq)
        self._record(tok, r, w)
        return tok

    def dma(self, q, fn, r=(), w=(), extra=()):
        if getattr(self, 'rec', None) is not None:
            self.rec.append(('dma', (q, fn), dict(r=list(r), w=list(w), extra=list(extra))))
            return None
        pl = self.dpool[q]
        slot = pl[self.dnext[q] % len(pl)]
        self.dnext[q] += 1
        if self.dval[slot]:
            self._wait(q, ('d', slot, self.dval[slot]))
        for d in self._deps(r, w):
            self._wait(q, d)
        for d in extra:
            self._wait(q, d)
        inst = fn(self.eng[q])
        inst.then_inc(self.dsem[slot], 16)
        self.dval[slot] += 16
        tok = ('d', slot, self.dval[slot])
        self._record(tok, r, w)
        return tok

    def barrier(self):
        toks = [('e', f, self.cnt[f]) for f in self.eng if self.cnt[f] > 0]
        toks += [('d', s, v) for s, v in enumerate(self.dval) if v > 0]
        for e in self.eng:
            for t in toks:
                self._wait(e, t)


def build_program(NLAT=64, dbg=False, nlayers=L, stop_after=None, skip_mods=False):
    NT = NLAT + NCTX
    NTOK = NT * 128
    CAPL = 16 * NLAT
    CAPC = 32

    nc = bass.Bass("TRN2", target_bir_lowering=False)
    dk = "ExternalOutput" if dbg else "Internal"

    def din(name, shape, dt=F32):
        return nc.dram_tensor(name, list(shape), dt, kind="ExternalInput").ap()

    xlat = din("xlat", [NLAT * 128, D])
    xctx = din("xctx", [NCTX * 128, D])
    cvec = din("cvec", [128, 16])
    if not skip_mods:
        w_mod = din("w_mod", [L, D, 6 * D])
        b_mod = din("b_mod", [L, 6 * D])
    w_in = din("w_in", [L, D, 2048])
    a_sink = din("a_sink", [L, 8])
    natT = din("natT", [L, 128, NCHB * 512])
    conv_w = din("conv_w", [L, 128, 62])
    conv_b = din("conv_b", [L, 128, 2])
    conv_ln_g = din("conv_ln_g", [L, 256])
    conv_ln_b = din("conv_ln_b", [L, 256])
    w_out = din("w_out", [L, D, D])
    ln1_g = din("ln1_g", [L, D])
    ln1_b = din("ln1_b", [L, D])
    w_router = din("w_router", [L, D, NE])
    need_moe = stop_after is None or stop_after.startswith(('M', 'F'))
    if need_moe:
        w_gate = din("w_gate", [L, NE, D, D])
        w_up = din("w_up", [L, NE, D, D])
        w_down = din("w_down", [L, NE, D, D])
    ln2_g = din("ln2_g", [L, D])
    ln2_b = din("ln2_b", [L, D])
    rope = din("rope", [NLAT, 128, 128])
    consts = din("consts", [128, 6 * 128])
    out = nc.dram_tensor("out", [NLAT * 128, D], F32, kind="ExternalOutput").ap()

    modbc = nc.dram_tensor("modbc", [L, 2, 128, 6 * D], F32, kind=dk).ap()
    scrA = nc.dram_tensor("scrA", [NT, 128, AW], BF16, kind=dk).ap()
    x1s = nc.dram_tensor("x1s", [NTOK, D], F32, kind=dk).ap()
    h2s = nc.dram_tensor("h2s", [NTOK, RW], BF16, kind=dk).ap()
    xs_d = nc.dram_tensor("xs_d", [NTOK, D], F32, kind=dk).ap()
    macc = nc.dram_tensor("macc", [NTOK, D], F32, kind=dk).ap()
    CAPT0 = CAPL + CAPC
    Xs = [nc.dram_tensor(f"Xs{e}", [CAPT0, RW], BF16, kind=dk).ap() for e in range(NE)]
    otok_d = nc.dram_tensor("otok_d", [NTOK, D], BF16, kind=dk).ap() if dbg else None

    es = ExitStack()
    S = Sched(nc, es)
    es.enter_context(nc.allow_non_contiguous_dma(reason="small strided parameter loads"))
    es.enter_context(nc.allow_low_precision(reason="0/1 mask counts <= 128 are exact in bf16"))

    uid = [0]

    def sb(stack, name, shape, dt):
        uid[0] += 1
        return stack.enter_context(nc.sbuf_tensor(f"{name}_{uid[0]}", list(shape), dt))

    banks = [es.enter_context(nc.psum_tensor(f"pb{i}", [128, 512], F32)) for i in range(8)]
    bank_rr = [0]

    def nbank():
        i = bank_rr[0]
        bank_rr[0] = (i + 1) % 8
        return banks[i], ('pb', i)

    cst = sb(es, "cst", [128, 6 * 128], F32)
    identb = sb(es, "identb", [128, 128], BF16)
    triub = sb(es, "triub", [128, 128], BF16)
    masklr = sb(es, "masklr", [128, 2, 128], BF16)
    onesb = sb(es, "onesb", [128, 128], BF16)
    aff_all = sb(es, "aff_all", [128, NT, NE], F32)
    NLN = 4
    stat_l = [sb(es, "stat", [128, 12], F32) for _ in range(NLN)]
    mv_l = [sb(es, "mv", [128, 2], F32) for _ in range(NLN)]
    sd_l = [sb(es, "sd", [128, 1], F32) for _ in range(NLN)]
    rstd_l = [sb(es, "rstd", [128, 1], F32) for _ in range(NLN)]
    nmr_l = [sb(es, "nmr", [128, 1], F32) for _ in range(NLN)]
    lncur = [0]
    identf = cst[:, 0:128]
    iotap = cst[:, 5 * 128:5 * 128 + 1]
    pm0 = cst[:, 5 * 128 + 1:5 * 128 + 2]
    pm1 = cst[:, 5 * 128 + 2:5 * 128 + 3]

    S.dma('sp', lambda q: q.dma_start(out=cst[:, :], in_=consts), w=['cst'])
    S.op('dve', lambda e: e.tensor_copy(out=identb[:, :], in_=cst[:, 0:128]), r=['cst'], w=['identb'])
    S.op('dve', lambda e: e.tensor_copy(out=triub[:, :], in_=cst[:, 128:256]), r=['cst'], w=['triub'])
    S.op('dve', lambda e: e.tensor_copy(out=masklr[:, :, :].rearrange("p a b -> p (a b)"), in_=cst[:, 256:512]), r=['cst'], w=['masklr'])
    S.op('dve', lambda e: e.tensor_copy(out=onesb[:, :], in_=cst[:, 512:640]), r=['cst'], w=['onesb'])

    def ln_stats(src_ap, src_key, width=1024):
        nch = (width + 511) // 512
        lncur[0] = (lncur[0] + 1) % NLN
        j = lncur[0]
        stat, mv, sd, rstd, nmr = stat_l[j], mv_l[j], sd_l[j], rstd_l[j], nmr_l[j]
        kst_, kmv, ksd, krs, knm = ('stat', j), ('mv', j), ('sd', j), ('rstd', j), ('nmr', j)
        for c in range(nch):
            S.op('dve', lambda e, c=c: e.bn_stats(out=stat[:, 6 * c:6 * c + 6], in_=src_ap[:, c * 512:min(width, (c + 1) * 512)]),
                 r=[src_key], w=[kst_])
        S.op('dve', lambda e: e.bn_aggr(out=mv[:, :], in_=stat[:, 0:6 * nch]), r=[kst_], w=[kmv])
        S.op('dve', lambda e: e.tensor_scalar(out=sd[:, :], in0=mv[:, 1:2], scalar1=EPS, scalar2=None, op0=ALU.add), r=[kmv], w=[ksd])
        S.op('act', lambda e: e.activation(out=sd[:, :], in_=sd[:, :], func=AF.Sqrt), r=[ksd], w=[ksd])
        S.op('dve', lambda e: e.reciprocal(out=rstd[:, :], in_=sd[:, :]), r=[ksd], w=[krs])
        S.op('dve', lambda e: e.tensor_scalar(out=nmr[:, :], in0=mv[:, 0:1], scalar1=rstd[:, 0:1], scalar2=-1.0, op0=ALU.mult, op1=ALU.mult),
             r=[kmv, krs], w=[knm])

    def ln_apply(dst_ap, dst_key, src_ap, src_key):
        j = lncur[0]
        S.op('act', lambda e: e.activation(out=dst_ap, in_=src_ap, func=AF.Identity, scale=rstd_l[j][:, 0:1], bias=nmr_l[j][:, 0:1]),
             r=[src_key, ('rstd', j), ('nmr', j)], w=[dst_key])

    def transposes_to(dst_ap, dst_key, src_fn, n, rows=128, src_key=None, evac='act'):
        bk, bkey = nbank()
        bv = bk[:, :].bitcast(BF16)
        for k in range(n):
            S.op('pe', lambda e, k=k: e.transpose(out=bv[:, k * 128:k * 128 + rows], in_=src_fn(k), identity=identb[:rows, :rows]),
                 r=[src_key, 'identb'], w=[bkey], signal=(k == n - 1))
        if rows == 128:
            src = bv[:, 0:n * 128]
        else:
            src = bv[:, 0:n * 128].rearrange("p (k s) -> p k s", s=128)[:, :, 0:rows]
        if evac == 'act':
            S.op('act', lambda e: e.copy(out=dst_ap, in_=src), r=[bkey], w=[dst_key])
        else:
            S.op('dve', lambda e: e.tensor_copy(out=dst_ap, in_=src), r=[bkey], w=[dst_key])

    with ExitStack() as ph:
        cT = sb(ph, "cT", [128, 8, 2], F32)
        cS = sb(ph, "cS", [128, 8, 2], F32)
        lbc = [sb(ph, f"lbc{s}", [128, 8, 128], BF16) for s in range(2)]
        wm = [sb(ph, f"wm{i}", [128, 8, 512], BF16) for i in range(2)]
        bm = [sb(ph, f"bm{i}", [128, 512], F32) for i in range(2)]
        mo = [sb(ph, f"mo{i}", [128, 512], F32) for i in range(4)]
        S.dma('sp', lambda q: q.dma_start(out=cT[:, :, :], in_=cvec.rearrange("p (k s) -> p k s", s=2)), w=['cT'])
        S.op('act', lambda e: e.activation(out=cS[:, :, :], in_=cT[:, :, :], func=AF.Silu), r=['cT'], w=['cS'])
        for s in range(2):
            S.op('dve', lambda e, s=s: e.tensor_copy(out=lbc[s][:, :, :], in_=cS[:, :, s:s + 1].to_broadcast([128, 8, 128])),
                 r=['cS'], w=[('lbc', s)])
        it = 0
        for l in range(nlayers if not skip_mods else 0):
            for ch in range(12):
                n0 = ch * 512
                slot = it % 2
                S.dma('pool', lambda q, slot=slot, l=l, n0=n0: q.dma_start(
                    out=wm[slot][:, :, :], in_=w_mod[l, :, n0:n0 + 512].rearrange("(ko ki) n -> ki ko n", ki=128)), w=[('wm', slot)])
                S.dma('sp', lambda q, slot=slot, l=l, n0=n0: q.dma_start(
                    out=bm[slot][:, :], in_=b_mod[l:l + 1, n0:n0 + 512].to_broadcast([128, 512])), w=[('bm', slot)])
                addc = 1.0 if (ch // 2) in (1, 4) else 0.0
                for s in range(2):
                    bk, bkey = nbank()
                    for k in range(8):
                        S.op('pe', lambda e, k=k, s=s, slot=slot, bk=bk: e.matmul(bk[:, :], lhsT=lbc[s][:, k, :], rhs=wm[slot][:, k, :],
                                                                               start=(k == 0), stop=(k == 7)),
                             r=[('lbc', s), ('wm', slot)], w=[bkey], signal=(k == 7))
                    ms = (it * 2 + s) % 4
                    S.op('dve', lambda e, bk=bk, ms=ms, slot=slot: e.scalar_tensor_tensor(
                        out=mo[ms][:, :], in0=bk[:, :], scalar=addc, in1=bm[slot][:, :], op0=ALU.add, op1=ALU.add),
                         r=[bkey, ('bm', slot)], w=[('mo', ms)])
                    S.dma('sp', lambda q, ms=ms, l=l, s=s, n0=n0: q.dma_start(out=modbc[l, s, :, n0:n0 + 512], in_=mo[ms][:, :]),
                          r=[('mo', ms)], w=[('modbc', l, s)])
                it += 1
        S.barrier()
    if stop_after == 'mods':
        return _finish(nc, S, es)

    for l in range(nlayers):
        last = (l == L - 1)
        tiles_b = list(range(NT)) if not last else list(range(NCTX, NT))
        CAPT = CAPL + (0 if last else CAPC)

        def xsrc(ti):
            if l == 0:
                return xctx[ti * 128:(ti + 1) * 128, :] if ti < NCTX else xlat[(ti - NCTX) * 128:(ti - NCTX + 1) * 128, :]
            return xs_d[ti * 128:(ti + 1) * 128, :]

        with ExitStack() as ph:
            w_in_sb = sb(ph, "w_in_sb", [128, 8, 2048], BF16)
            modA = [[sb(ph, f"modA{s}{j}", [128, D], F32) for j in range(2)] for s in range(2)]
            xin = [sb(ph, f"xinA{i}", [128, D], F32) for i in range(2)]
            ropet = [sb(ph, f"ropet{i}", [128, 128], F32) for i in range(2)]
            f32a_l = [sb(ph, "f32a", [128, D], F32) for _ in range(2)]
            hb_l = [sb(ph, "hb", [128, D], BF16) for _ in range(2)]
            hT_l = [sb(ph, "hT", [128, 8, 128], BF16) for _ in range(2)]
            ropeA_l = [sb(ph, "ropeA", [128, 512], F32) for _ in range(2)]
            ropeB_l = [sb(ph, "ropeB", [128, 512], F32) for _ in range(2)]
            qperm_l = [sb(ph, "qperm", [128, 512], BF16) for _ in range(2)]
            kst_l = [sb(ph, "kst", [128, 640], BF16) for _ in range(2)]
            sg_l = [sb(ph, "sg", [128, 256], F32) for _ in range(2)]
            chtok_l = [sb(ph, "chtok", [128, 256], BF16) for _ in range(2)]
            aout = [sb(ph, f"aout{i}", [128, AW], BF16) for i in range(2)]
            for i in range(2):
                S.op('pool', lambda e, i=i: e.memset(aout[i][:, VA:VA + 130], 1.0), w=[('aout', i)])
                S.op('pool', lambda e, i=i: e.memset(aout[i][:, VB:VB + 260], 1.0), w=[('aout', i)])
            for hf in range(2):
                S.dma('pool', lambda q, hf=hf: q.dma_start(out=w_in_sb[:, :, hf * 1024:(hf + 1) * 1024],
                                                        in_=w_in[l, :, hf * 1024:(hf + 1) * 1024].rearrange("(ko ki) n -> ki ko n", ki=128)),
                      w=['w_in_sb'])
            for s in range(2):
                for j in range(2):
                    S.dma('sp', lambda q, s=s, j=j: q.dma_start(out=modA[s][j][:, :], in_=modbc[l, s, :, j * D:(j + 1) * D]),
                          r=[('modbc', l, s)], w=[('modA', s, j)])

            def tileA(ti, part):
                    s = 0 if ti < NCTX else 1
                    lat = ti >= NCTX
                    xs_, xk = xin[ti % 2], ('xinA', ti % 2)
                    f32a = f32a_l[ti % 2]
                    k_f32a = ('f32a', ti % 2)
                    hb = hb_l[ti % 2]
                    k_hb = ('hb', ti % 2)
                    hT = hT_l[ti % 2]
                    k_hT = ('hT', ti % 2)
                    ropeA = ropeA_l[ti % 2]
                    k_ropeA = ('ropeA', ti % 2)
                    ropeB = ropeB_l[ti % 2]
                    k_ropeB = ('ropeB', ti % 2)
                    qperm = qperm_l[ti % 2]
                    k_qperm = ('qperm', ti % 2)
                    kst = kst_l[ti % 2]
                    k_kst = ('kst', ti % 2)
                    sg = sg_l[ti % 2]
                    k_sg = ('sg', ti % 2)
                    chtok = chtok_l[ti % 2]
                    k_chtok = ('chtok', ti % 2)
                    ao, aok = aout[ti % 2], ('aout', ti % 2)
                    rt, rk = ropet[ti % 2], ('ropet', ti % 2)
                    if part == 0:
                        S.dma('sp', lambda q, ti=ti, xs_=xs_: q.dma_start(out=xs_[:, :], in_=xsrc(ti)), r=[('xs_d',)] if l > 0 else [], w=[xk])
                        if lat:
                            S.dma('sp', lambda q, ti=ti, rt=rt: q.dma_start(out=rt[:, :], in_=rope[ti - NCTX, :, :]), w=[rk])
                        ln_stats(xs_, xk)
                        ln_apply(f32a[:, :], k_f32a, xs_[:, :], xk)
                        S.op('dve', lambda e, s=s: e.tensor_tensor(out=f32a[:, :], in0=f32a[:, :], in1=modA[s][1][:, :], op=ALU.mult),
                             r=[k_f32a, ('modA', s, 1)], w=[k_f32a])
                        S.op('pool', lambda e, s=s: e.tensor_tensor(out=hb[:, :], in0=f32a[:, :], in1=modA[s][0][:, :], op=ALU.add),
                             r=[k_f32a, ('modA', s, 0)], w=[k_hb])
                    if part == 1:
                        transposes_to(hT[:, :, :].rearrange("p k t -> p (k t)"), k_hT, lambda k: hb[:, k * 128:(k + 1) * 128], 8, src_key=k_hb)
                        ub = []
                        for n in range(4):
                            bk, bkey = nbank()
                            for k in range(8):
                                S.op('pe', lambda e, k=k, n=n, bk=bk: e.matmul(bk[:, :], lhsT=hT[:, k, :], rhs=w_in_sb[:, k, n * 512:(n + 1) * 512],
                                                                             start=(k == 0), stop=(k == 7)),
                                     r=[k_hT, 'w_in_sb'], w=[bkey], signal=(k == 7))
                            ub.append((bk, bkey))
                        b0, k0 = ub[0]
                        qp_v = qperm[:, :].rearrange("p (g r d) -> p r g d", g=4, r=2, d=64)
                        if lat:
                            S.op('dve', lambda e, b0=b0, rt=rt: e.tensor_tensor(
                                out=ropeA[:, :].rearrange("p (h d) -> p h d", d=64), in0=b0[:, :].rearrange("p (h d) -> p h d", d=64),
                                in1=rt[:, 0:64].unsqueeze(1).to_broadcast([128, 8, 64]), op=ALU.mult), r=[k0, rk], w=[k_ropeA])
                            for half in range(2):
                                S.op('dve', lambda e, b0=b0, rt=rt, half=half: e.tensor_tensor(
                                    out=ropeB[:, :].rearrange("p (h r f d) -> p h r f d", h=8, r=2, f=2, d=16)[:, :, :, half, :],
                                    in0=b0[:, :].rearrange("p (h r f d) -> p h r f d", h=8, r=2, f=2, d=16)[:, :, :, 1 - half, :],
                                    in1=rt[:, 64:128].rearrange("p (r f d) -> p r f d", r=2, f=2, d=16)[:, :, half, :].unsqueeze(1).to_broadcast([128, 8, 2, 16]),
                                    op=ALU.mult), r=[k0, rk], w=[k_ropeB])
                            S.op('pool', lambda e: e.tensor_tensor(out=qp_v, in0=ropeA[:, :].rearrange("p (r g d) -> p r g d", r=2, g=4, d=64),
                                                                  in1=ropeB[:, :].rearrange("p (r g d) -> p r g d", r=2, g=4, d=64), op=ALU.add),
                                 r=[k_ropeA, k_ropeB], w=[k_qperm])
                        else:
                            S.op('act', lambda e, b0=b0: e.copy(out=qp_v, in_=b0[:, :].rearrange("p (r g d) -> p r g d", r=2, g=4, d=64)),
                                 r=[k0], w=[k_qperm])
                        bk, bkey = nbank()
                        bv = bk[:, :].bitcast(BF16)
                        for k in range(4):
                            S.op('pe', lambda e, k=k, bv=bv: e.transpose(out=bv[:, k * 128:(k + 1) * 128], in_=qperm[:, k * 128:(k + 1) * 128], identity=identb[:, :]),
                                 r=[k_qperm, 'identb'], w=[bkey], signal=(k == 3))
                        if os.environ.get('EVAC', 'mask') == 'mask':
                            S.op('act', lambda e, bv=bv, ao=ao: e.activation(out=ao[:, QA:QA + 512], in_=bv[:, 0:512], func=AF.Copy, scale=pm0), r=[bkey, 'cst'], w=[aok])
                            S.op('dve', lambda e, bv=bv, ao=ao: e.tensor_scalar(out=ao[:, QA + 512:QA + 1024], in0=bv[:, 0:512], scalar1=pm1, scalar2=None, op0=ALU.mult),
                                 r=[bkey, 'cst'], w=[aok])
                        else:
                            S.op('act', lambda e, bv=bv, ao=ao: e.copy(out=ao[:, QA:QA + 512], in_=bv[:, 0:512]), r=[bkey], w=[aok])
                            S.op('dve', lambda e, bv=bv, ao=ao: e.tensor_copy(out=ao[:, QA + 512:QA + 1024], in_=bv[:, 0:512]), r=[bkey], w=[aok])
                        b1, k1 = ub[1]
                        if lat:
                            S.op('dve', lambda e, b1=b1, rt=rt: e.tensor_tensor(
                                out=ropeA[:, 0:128].rearrange("p (h d) -> p h d", d=64), in0=b1[:, 0:128].rearrange("p (h d) -> p h d", d=64),
                                in1=rt[:, 0:64].unsqueeze(1).to_broadcast([128, 2, 64]), op=ALU.mult), r=[k1, rk], w=[k_ropeA])
                            for half in range(2):
                                S.op('dve', lambda e, b1=b1, rt=rt, half=half: e.tensor_tensor(
                                    out=ropeB[:, 0:128].rearrange("p (h r f d) -> p h r f d", h=2, r=2, f=2, d=16)[:, :, :, half, :],
                                    in0=b1[:, 0:128].rearrange("p (h r f d) -> p h r f d", h=2, r=2, f=2, d=16)[:, :, :, 1 - half, :],
                                    in1=rt[:, 64:128].rearrange("p (r f d) -> p r f d", r=2, f=2, d=16)[:, :, half, :].unsqueeze(1).to_broadcast([128, 2, 2, 16]),
                                    op=ALU.mult), r=[k1, rk], w=[k_ropeB])
                            S.op('pool', lambda e: e.tensor_tensor(out=kst[:, 0:128], in0=ropeA[:, 0:128], in1=ropeB[:, 0:128], op=ALU.add),
                                 r=[k_ropeA, k_ropeB], w=[k_kst])
                        else:
                            S.op('act', lambda e, b1=b1: e.copy(out=kst[:, 0:128], in_=b1[:, 0:128]), r=[k1], w=[k_kst])
                        S.op('act', lambda e, b1=b1, ao=ao: e.copy(out=ao[:, VA:VA + 130].rearrange("p (h d) -> p h d", d=65)[:, :, 0:64],
                                                                 in_=b1[:, 128:256].rearrange("p (h d) -> p h d", d=64)), r=[k1], w=[aok])
                        S.op('act', lambda e, b1=b1: e.copy(out=kst[:, 128:384], in_=b1[:, 256:512]), r=[k1], w=[k_kst])
                        b2, k2 = ub[2]
                        S.op('dve', lambda e, b2=b2: e.tensor_copy(out=kst[:, 384:640], in_=b2[:, 0:256]), r=[k2], w=[k_kst])
                        S.op('act', lambda e, b2=b2, ao=ao: e.copy(out=ao[:, VB:VB + 260].rearrange("p (h d) -> p h d", d=65)[:, :, 0:64],
                                                                 in_=b2[:, 256:512].rearrange("p (h d) -> p h d", d=64)), r=[k2], w=[aok])
                        order = [1, 2, 0, 3, 4]
                        bk, bkey = nbank()
                        bv = bk[:, :].bitcast(BF16)
                        for j, blk in enumerate(order):
                            S.op('pe', lambda e, j=j, blk=blk, bv=bv: e.transpose(out=bv[:, j * 128:(j + 1) * 128], in_=kst[:, blk * 128:(blk + 1) * 128],
                                                                                 identity=identb[:, :]), r=[k_kst, 'identb'], w=[bkey], signal=(j == 4))
                        if os.environ.get('EVAC', 'mask') == 'mask':
                            S.op('act', lambda e, bv=bv, ao=ao: e.activation(out=ao[:, QB:QB + 256], in_=bv[:, 0:256], func=AF.Copy, scale=pm0), r=[bkey, 'cst'], w=[aok])
                            S.op('dve', lambda e, bv=bv, ao=ao: e.tensor_scalar(out=ao[:, QB + 256:QB + 512], in0=bv[:, 0:256], scalar1=pm1, scalar2=None, op0=ALU.mult),
                                 r=[bkey, 'cst'], w=[aok])
                        else:
                            S.op('act', lambda e, bv=bv, ao=ao: e.copy(out=ao[:, QB:QB + 256], in_=bv[:, 0:256]), r=[bkey], w=[aok])
                            S.op('dve', lambda e, bv=bv, ao=ao: e.tensor_copy(out=ao[:, QB + 256:QB + 512], in_=bv[:, 0:256]), r=[bkey], w=[aok])
                        S.op('dve', lambda e, bv=bv, ao=ao: e.tensor_copy(out=ao[:, KA:KA + 128], in_=bv[:, 256:384]), r=[bkey], w=[aok])
                        S.op('dve', lambda e, bv=bv, ao=ao: e.tensor_copy(out=ao[:, KB:KB + 256], in_=bv[:, 384:640]), r=[bkey], w=[aok])
                        b3, k3 = ub[3]
                        S.op('act', lambda e, b3=b3: e.activation(out=sg[:, :], in_=b3[:, 256:512], func=AF.Sigmoid), r=[k3], w=[k_sg])
                        S.op('dve', lambda e, b3=b3: e.tensor_tensor(out=chtok[:, :], in0=b3[:, 0:256], in1=sg[:, :], op=ALU.mult), r=[k3, k_sg], w=[k_chtok])
                        transposes_to(ao[:, CH:CH + 256], aok, lambda k: chtok[:, k * 128:(k + 1) * 128], 2, src_key=k_chtok, evac='dve')
                        S.dma('sp', lambda q, ti=ti, ao=ao: q.dma_start(out=scrA[ti, :, 0:QW], in_=ao[:, 0:QW]), r=[aok], w=[('scrAq', ti)])
                        S.dma('sp', lambda q, ti=ti, ao=ao: q.dma_start(out=scrA[ti, :, QW:AW], in_=ao[:, QW:AW]), r=[aok], w=[('scrA', ti)])

            for step in range(NT + 1):
                if step < NT:
                    tileA(step, 0)
                if step - 1 >= 0:
                    tileA(step - 1, 1)
            S.barrier()
        if stop_after == f'A{l}':
            return _finish(nc, S, es)

        with ExitStack() as ph:
            w_out_sb = sb(ph, "w_out_sb", [128, 8, D], BF16)
            w_r_sb = sb(ph, "w_r_sb", [128, 8, NE], BF16)
            nat_sb = sb(ph, "nat_sb", [128, NCHB, 512], BF16)
            cwT = sb(ph, "cwT", [128, 2, 31], F32)
            cdiag = sb(ph, "cdiag", [128, 2, 31, 128], BF16)
            convb = sb(ph, "convb", [128, 2], F32)
            clng = sb(ph, "clng", [128, 256], F32)
            clnb = sb(ph, "clnb", [128, 256], F32)
            modB = [[sb(ph, f"modB{s}{j}", [128, D], F32) for j in range(3)] for s in range(2)]
            ln1g = sb(ph, "ln1g", [128, D], F32)
            ln1b = sb(ph, "ln1b", [128, D], F32)
            esink = sb(ph, "esink", [128, 8], F32)
            NQR, NKR, NCW = 3, 8, 4
            qring = [sb(ph, f"qring{i}", [128, QW], BF16) for i in range(NQR)]
            kvring = [sb(ph, f"kvring{i}", [128, KVW], BF16) for i in range(NKR)]
            kvctx = [sb(ph, f"kvctx{i}", [128, KVW], BF16) for i in range(NCTX)]
            chwin = [sb(ph, f"chwin{i}", [128, 2, 160], BF16) for i in range(NCW)]
            xin = [sb(ph, f"xinB{i}", [128, D], F32) for i in range(2)]
            pT = sb(ph, "pT", [128, 7, 512], BF16)
            pTA = [sb(ph, "pTA", [128, 5, 512], BF16) for _ in range(2)]
            btmp = [sb(ph, f"btmp{i}", [128, 512], F32) for i in range(2)]
            otok_l = [sb(ph, "otok", [128, D], BF16) for _ in range(2)]
            oT_l = [sb(ph, "oT", [128, 8, 128], BF16) for _ in range(2)]
            cvT_l = [sb(ph, "cvT", [128, 2, 128], F32) for _ in range(2)]
            cvn_l = [sb(ph, "cvn", [128, 256], F32) for _ in range(2)]
            f32b_l = [sb(ph, "f32b", [128, D], F32) for _ in range(2)]
            f32c_l = [sb(ph, "f32c", [128, D], F32) for _ in range(2)]
            h2row = [sb(ph, f"h2row{i}", [128, RW], BF16) for i in range(2)]
            h2T_l = [sb(ph, "h2T", [128, 8, 128], BF16) for _ in range(2)]
            den_l = [sb(ph, "den", [128, 4], F32) for _ in range(2)]
            rden_l = [sb(ph, "rden", [128, 4], F32) for _ in range(2)]
            ex_l = [sb(ph, "ex", [128, NE], F32) for _ in range(2)]
            ssum_l = [sb(ph, "ssum", [128, 1], F32) for _ in range(2)]

            for hf in range(1):
                S.dma('pool', lambda q: q.dma_start(out=w_out_sb[:, :, :], in_=w_out[l, :, :].rearrange("(ko ki) n -> ki ko n", ki=128)), w=['w_out_sb'])
            S.dma('pool', lambda q: q.dma_start(out=w_r_sb[:, :, :], in_=w_router[l, :, :].rearrange("(ko ki) n -> ki ko n", ki=128)), w=['w_r_sb'])
            for c3 in range(0, NCHB, 3):
                c4 = min(NCHB, c3 + 3)
                S.dma('pool', lambda q, c3=c3, c4=c4: q.dma_start(out=nat_sb[:, c3:c4, :], in_=natT[l, :, c3 * 512:c4 * 512].rearrange("p (a b) -> p a b", b=512)),
                      w=['nat_sb'])
            S.dma('sp', lambda q: q.dma_start(out=cwT[:, :, :], in_=conv_w[l, :, :].rearrange("c (cc j) -> c cc j", cc=2)), w=['cwT'])
            S.dma('sp', lambda q: q.dma_start(out=convb[:, :], in_=conv_b[l, :, :]), w=['convb'])
            S.dma('sp', lambda q: q.dma_start(out=clng[:, :], in_=conv_ln_g[l:l + 1, :].to_broadcast([128, 256])), w=['clng'])
            S.dma('sp', lambda q: q.dma_start(out=clnb[:, :], in_=conv_ln_b[l:l + 1, :].to_broadcast([128, 256])), w=['clnb'])
            S.dma('sp', lambda q: q.dma_start(out=ln1g[:, :], in_=ln1_g[l:l + 1, :].to_broadcast([128, D])), w=['ln1g'])
            S.dma('sp', lambda q: q.dma_start(out=ln1b[:, :], in_=ln1_b[l:l + 1, :].to_broadcast([128, D])), w=['ln1b'])
            S.dma('sp', lambda q: q.dma_start(out=esink[:, :], in_=a_sink[l:l + 1, :].to_broadcast([128, 8])), w=['esink'])
            S.op('act', lambda e: e.activation(out=esink[:, :], in_=esink[:, :], func=AF.Exp), r=['esink'], w=['esink'])
            for s in range(2):
                for j, chn in enumerate((2, 4, 3)):
                    S.dma('sp', lambda q, s=s, j=j, chn=chn: q.dma_start(out=modB[s][j][:, :], in_=modbc[l, s, :, chn * D:(chn + 1) * D]),
                          r=[('modbc', l, s)], w=[('modB', s, j)])
            for cc in range(2):
                for j in range(31):
                    S.op('pool', lambda e, cc=cc, j=j: e.tensor_scalar(out=cdiag[:, cc, j, :], in0=cst[:, 0:128], scalar1=cwT[:, cc, j:j + 1],
                                                                      scalar2=None, op0=ALU.mult), r=['cst', 'cwT'], w=['cdiag'])
            for c in range(NCTX):
                S.dma('sp', lambda q, c=c: q.dma_start(out=kvctx[c][:, :], in_=scrA[c, :, KA:CH]), r=[('scrA', c)], w=[('kvctx', c)])

            loaded = set()

            def load_tile(t):
                if t in loaded or t < 0 or t >= NT:
                    return
                loaded.add(t)
                if t >= NCTX:
                    S.dma('sp', lambda q: q.dma_start(out=kvring[t % NKR][:, :], in_=scrA[t, :, KA:CH]), r=[('scrA', t)], w=[('kvring', t % NKR)])

            def load_own(t):
                S.dma('sp', lambda q: q.dma_start(out=qring[t % NQR][:, :], in_=scrA[t, :, 0:QW]), r=[('scrAq', t)], w=[('qring', t % NQR)])
                cw, cwk = chwin[t % NCW], ('chwin', t % NCW)
                first = t in (0, NCTX)
                lastt = t in (NCTX - 1, NT - 1)
                S.dma('sp', lambda q: q.dma_start(out=cw[:, :, 16:144], in_=scrA[t, :, CH:CH + 256].rearrange("p (c n) -> p c n", c=2)),
                      r=[('scrA', t)], w=[cwk])
                if first:
                    S.op('pool', lambda e: e.memset(cw[:, :, 0:16], 0.0), w=[cwk])
                else:
                    S.dma('sp', lambda q: q.dma_start(out=cw[:, :, 0:16], in_=scrA[t - 1, :, CH:CH + 256].rearrange("p (c n) -> p c n", c=2)[:, :, 112:128]),
                          r=[('scrA', t - 1)], w=[cwk])
                if lastt:
                    S.op('pool', lambda e: e.memset(cw[:, :, 144:160], 0.0), w=[cwk])
                else:
                    S.dma('sp', lambda q: q.dma_start(out=cw[:, :, 144:160], in_=scrA[t + 1, :, CH:CH + 256].rearrange("p (c n) -> p c n", c=2)[:, :, 0:16]),
                          r=[('scrA', t + 1)], w=[cwk])
                S.dma('sp', lambda q: q.dma_start(out=xin[t % 2][:, :], in_=xsrc(t)), r=[('xs_d',)] if l > 0 else [], w=[('xinB', t % 2)])

            def kvbuf(t):
                if t < NCTX:
                    return kvctx[t], ('kvctx', t)
                return kvring[t % NKR], ('kvring', t % NKR)

            def tileB(ti, part):
                    s = 0 if ti < NCTX else 1
                    lat = ti >= NCTX
                    i = ti - NCTX
                    otok = otok_l[ti % 2]
                    k_otok = ('otok', ti % 2)
                    oT = oT_l[ti % 2]
                    k_oT = ('oT', ti % 2)
                    cvT = cvT_l[ti % 2]
                    k_cvT = ('cvT', ti % 2)
                    cvn = cvn_l[ti % 2]
                    k_cvn = ('cvn', ti % 2)
                    f32b = f32b_l[ti % 2]
                    k_f32b = ('f32b', ti % 2)
                    f32c = f32c_l[ti % 2]
                    k_f32c = ('f32c', ti % 2)
                    h2T = h2T_l[ti % 2]
                    k_h2T = ('h2T', ti % 2)
                    den = den_l[ti % 2]
                    k_den = ('den', ti % 2)
                    rden = rden_l[ti % 2]
                    k_rden = ('rden', ti % 2)
                    ex = ex_l[ti % 2]
                    k_ex = ('ex', ti % 2)
                    ssum = ssum_l[ti % 2]
                    k_ssum = ('ssum', ti % 2)
                    if part == 0:
                      for t2 in range(ti - 2, ti + 4):
                        if lat and t2 >= NCTX:
                            load_tile(t2)
                      load_own(ti)
                    qr, qk = qring[ti % NQR], ('qring', ti % NQR)
                    cw, cwk = chwin[ti % NCW], ('chwin', ti % NCW)
                    xs_, xk = xin[ti % 2], ('xinB', ti % 2)
                    hr, hk = h2row[ti % 2], ('h2row', ti % 2)

                    if stop_after == f'B{l}:setup':
                        return
                    if part == 0:
                        cbk, cbkey = nbank()
                        for cc in range(2):
                            for j in range(31):
                                S.op('pe', lambda e, cbk=cbk, cc=cc, j=j: e.matmul(cbk[:, cc * 128:(cc + 1) * 128], lhsT=cdiag[:, cc, j, :],
                                                                                  rhs=cw[:, cc, 1 + j:1 + j + 128], start=(j == 0), stop=(j == 30)),
                                     r=['cdiag', cwk], w=[cbkey], signal=(cc == 1 and j == 30))
                        for cc in range(2):
                            S.op('act', lambda e, cbk=cbk, cc=cc: e.activation(out=cvT[:, cc, :], in_=cbk[:, cc * 128:(cc + 1) * 128], func=AF.Identity,
                                                                              bias=convb[:, cc:cc + 1], scale=1.0), r=[cbkey, 'convb'], w=[k_cvT])
                        if lat:
                            chunksA = []
                            if i - 1 >= 0:
                                chunksA.append((ti - 1, 0))
                            chunksA.append((ti, None))
                            if i + 1 < NLAT:
                                chunksA.append((ti + 1, 1))
                            chunksA += [(0, None), (1, None)]
                        else:
                            chunksA = [(0, None), (1, None)]
                        for grp in range(2):
                            for ci, (ct, mk) in enumerate(chunksA):
                                kb_, kk = kvbuf(ct)
                                bk, bkey = nbank()
                                S.op('pe', lambda e, bk=bk, kb_=kb_: e.matmul(bk[:, :], lhsT=kb_[:, 0:128], rhs=qr[:, QA + grp * 512:QA + (grp + 1) * 512],
                                                                             start=True, stop=True), r=[kk, qk], w=[bkey])
                                S.op('act', lambda e, bk=bk, ci=ci: e.activation(out=pTA[grp][:, ci, :], in_=bk[:, :], func=AF.Exp, scale=SCALE),
                                     r=[bkey], w=[('pTA', grp, ci)])
                                if mk is not None:
                                    S.op('pool', lambda e, ci=ci, mk=mk: e.tensor_tensor(
                                        out=pTA[grp][:, ci, :].rearrange("p (g t) -> p g t", g=4), in0=pTA[grp][:, ci, :].rearrange("p (g t) -> p g t", g=4),
                                        in1=masklr[:, mk, :].unsqueeze(1).to_broadcast([128, 4, 128]), op=ALU.mult), r=[('pTA', grp, ci), 'masklr'], w=[('pTA', grp, ci)])
                        if lat:
                            if NLAT >= 5 and 2 <= i <= NLAT - 3:
                                chunksB = [(ti + d - 2, d) for d in range(5)]
                            elif i == 0:
                                chunksB = [(NCTX + j, 5 + j) for j in range(4)]
                            elif i == 1:
                                chunksB = [(NCTX + j, 9 + j) for j in range(4)]
                            elif i == NLAT - 2:
                                chunksB = [(NCTX + NLAT - 4 + j, 13 + j) for j in range(4)]
                            else:
                                chunksB = [(NCTX + NLAT - 4 + j, 17 + j) for j in range(4)]
                            chunksB += [(0, None), (1, None)]
                        else:
                            chunksB = [(0, None), (1, None)]
                        for ci, (ct, ent) in enumerate(chunksB):
                            kb_, kk = kvbuf(ct)
                            bk, bkey = nbank()
                            for h in range(4):
                                p, m = h // 2, h % 2
                                S.op('pe', lambda e, bk=bk, kb_=kb_, h=h, p=p, m=m: e.matmul(
                                    bk[:, h * 128:(h + 1) * 128], lhsT=kb_[:, KB - KA + p * 128:KB - KA + (p + 1) * 128],
                                    rhs=qr[:, QB + m * 256 + p * 128:QB + m * 256 + (p + 1) * 128], start=True, stop=True),
                                     r=[kk, qk], w=[bkey], signal=(h == 3))
                            if ent is not None:
                                bt, btk = btmp[ci % 2], ('btmp', ci % 2)
                                S.op('dve', lambda e, bk=bk, bt=bt, ent=ent: e.scalar_tensor_tensor(
                                    out=bt[:, :], in0=bk[:, :], scalar=SCALE, in1=nat_sb[:, ent, :], op0=ALU.mult, op1=ALU.add),
                                     r=[bkey, 'nat_sb'], w=[btk])
                                S.op('act', lambda e, bt=bt, ci=ci: e.activation(out=pT[:, ci, :], in_=bt[:, :], func=AF.Exp), r=[btk], w=[('pT', ci)])
                            else:
                                S.op('act', lambda e, bk=bk, ci=ci: e.activation(out=pT[:, ci, :], in_=bk[:, :], func=AF.Exp, scale=SCALE),
                                     r=[bkey], w=[('pT', ci)])
                        for grp in range(2):
                            ob, obk = nbank()
                            nchk = len(chunksA)
                            for g in range(4):
                                for ci, (ct, mk) in enumerate(chunksA):
                                    kb_, kk = kvbuf(ct)
                                    S.op('pe', lambda e, g=g, ci=ci, kb_=kb_, ob=ob: e.matmul(
                                        ob[:, g * 65:(g + 1) * 65], lhsT=pTA[grp][:, ci, g * 128:(g + 1) * 128],
                                        rhs=kb_[:, VA - KA + grp * 65:VA - KA + (grp + 1) * 65], start=(ci == 0), stop=(ci == nchk - 1)),
                                         r=[('pTA', grp, ci), kk], w=[obk], signal=(g == 3 and ci == nchk - 1))
                            obv = ob[:, 0:260].rearrange("p (g d) -> p g d", d=65)
                            S.op('dve', lambda e, obv=obv: e.tensor_tensor(out=den[:, :], in0=obv[:, :, 64], in1=esink[:, grp * 4:(grp + 1) * 4], op=ALU.add),
                                 r=[obk, 'esink'], w=[k_den])
                            S.op('dve', lambda e: e.reciprocal(out=rden[:, :], in_=den[:, :]), r=[k_den], w=[k_rden])
                            S.op('dve', lambda e, obv=obv: e.tensor_tensor(
                                out=otok[:, grp * 256:(grp + 1) * 256].rearrange("p (g d) -> p g d", d=64), in0=obv[:, :, 0:64],
                                in1=rden[:, :].unsqueeze(2).to_broadcast([128, 4, 64]), op=ALU.mult), r=[obk, k_rden], w=[k_otok])
                        ob, obk = nbank()
                        nchk = len(chunksB)
                        for h in range(4):
                            for ci, (ct, ent) in enumerate(chunksB):
                                kb_, kk = kvbuf(ct)
                                S.op('pe', lambda e, h=h, ci=ci, kb_=kb_, ob=ob: e.matmul(
                                    ob[:, h * 65:(h + 1) * 65], lhsT=pT[:, ci, h * 128:(h + 1) * 128],
                                    rhs=kb_[:, VB - KA + h * 65:VB - KA + (h + 1) * 65], start=(ci == 0), stop=(ci == nchk - 1)),
                                     r=[('pT', ci), kk], w=[obk], signal=(h == 3 and ci == nchk - 1))
                        obv = ob[:, 0:260].rearrange("p (g d) -> p g d", d=65)
                        S.op('dve', lambda e, obv=obv: e.reciprocal(out=rden[:, :], in_=obv[:, :, 64]), r=[obk], w=[k_rden])
                        S.op('dve', lambda e, obv=obv: e.tensor_tensor(
                            out=otok[:, 512:768].rearrange("p (g d) -> p g d", d=64), in0=obv[:, :, 0:64],
                            in1=rden[:, :].unsqueeze(2).to_broadcast([128, 4, 64]), op=ALU.mult), r=[obk, k_rden], w=[k_otok])
                        bk2, bkey2 = nbank()
                        for cc in range(2):
                            S.op('pe', lambda e, bk2=bk2, cc=cc: e.transpose(out=bk2[:, cc * 128:(cc + 1) * 128], in_=cvT[:, cc, :], identity=identf),
                                 r=[k_cvT, 'cst'], w=[bkey2], signal=(cc == 1))
                        ln_stats(bk2, bkey2, width=256)
                        ln_apply(cvn[:, :], k_cvn, bk2[:, 0:256], bkey2)
                        S.op('dve', lambda e: e.tensor_tensor(out=cvn[:, :], in0=cvn[:, :], in1=clng[:, :], op=ALU.mult), r=[k_cvn, 'clng'], w=[k_cvn])
                        S.op('pool', lambda e: e.tensor_tensor(out=cvn[:, :], in0=cvn[:, :], in1=clnb[:, :], op=ALU.add), r=[k_cvn, 'clnb'], w=[k_cvn])
                        S.op('act', lambda e: e.activation(out=otok[:, 768:1024], in_=cvn[:, :], func=AF.Silu), r=[k_cvn], w=[k_otok])

                        if dbg:
                            S.dma('sp', lambda q: q.dma_start(out=otok_d[ti * 128:(ti + 1) * 128, :], in_=otok[:, :]), r=[k_otok], w=[('dbg_otok', ti)])

                        if stop_after == f'B{l}:conv':
                            return
                    if part == 1:
                        transposes_to(oT[:, :, :].rearrange("p k t -> p (k t)"), k_oT, lambda k: otok[:, k * 128:(k + 1) * 128], 8, src_key=k_otok)
                        for n in range(2):
                            bk, bkey = nbank()
                            for k in range(8):
                                S.op('pe', lambda e, bk=bk, k=k, n=n: e.matmul(bk[:, :], lhsT=oT[:, k, :], rhs=w_out_sb[:, k, n * 512:(n + 1) * 512],
                                                                             start=(k == 0), stop=(k == 7)), r=[k_oT, 'w_out_sb'], w=[bkey], signal=(k == 7))
                            S.op('dve', lambda e, bk=bk, n=n: e.tensor_tensor(out=f32b[:, n * 512:(n + 1) * 512], in0=bk[:, :],
                                                                             in1=modB[s][0][:, n * 512:(n + 1) * 512], op=ALU.mult),
                                 r=[bkey, ('modB', s, 0)], w=[k_f32b])
                        S.op('dve', lambda e: e.scalar_tensor_tensor(out=f32b[:, :], in0=xs_[:, :], scalar=ALPHA, in1=f32b[:, :], op0=ALU.mult, op1=ALU.add),
                             r=[xk, k_f32b], w=[k_f32b])
                        ln_stats(f32b, k_f32b)
                        ln_apply(f32c[:, :], k_f32c, f32b[:, :], k_f32b)
                        S.op('dve', lambda e: e.tensor_tensor(out=f32c[:, :], in0=f32c[:, :], in1=ln1g[:, :], op=ALU.mult), r=[k_f32c, 'ln1g'], w=[k_f32c])
                        S.op('pool', lambda e: e.tensor_tensor(out=f32c[:, :], in0=f32c[:, :], in1=ln1b[:, :], op=ALU.add), r=[k_f32c, 'ln1b'], w=[k_f32c])
                        S.dma('sp', lambda q: q.dma_start(out=x1s[ti * 128:(ti + 1) * 128, :], in_=f32c[:, :]), r=[k_f32c], w=[('x1s', ti)])
                        if stop_after == f'B{l}:proj':
                            return
                        ln_stats(f32c, k_f32c)
                        ln_apply(f32b[:, :], k_f32b, f32c[:, :], k_f32c)
                        S.op('dve', lambda e: e.tensor_tensor(out=f32b[:, :], in0=f32b[:, :], in1=modB[s][1][:, :], op=ALU.mult),
                             r=[k_f32b, ('modB', s, 1)], w=[k_f32b])
                        S.op('pool', lambda e: e.tensor_tensor(out=hr[:, 0:1024], in0=f32b[:, :], in1=modB[s][2][:, :], op=ALU.add),
                             r=[k_f32b, ('modB', s, 2)], w=[hk])
                    if part == 2:
                        transposes_to(h2T[:, :, :].rearrange("p k t -> p (k t)"), k_h2T, lambda k: hr[:, k * 128:(k + 1) * 128], 8, src_key=hk)
                        bk, bkey = nbank()
                        for k in range(8):
                            S.op('pe', lambda e, bk=bk, k=k: e.matmul(bk[:, 0:NE], lhsT=h2T[:, k, :], rhs=w_r_sb[:, k, :], start=(k == 0), stop=(k == 7)),
                                 r=[k_h2T, 'w_r_sb'], w=[bkey], signal=(k == 7))
                        S.op('act', lambda e, bk=bk: e.activation(out=ex[:, :], in_=bk[:, 0:NE], func=AF.Exp, accum_out=ssum[:, 0:1]), r=[bkey], w=[k_ex, k_ssum])
                        S.op('dve', lambda e: e.reciprocal(out=ssum[:, :], in_=ssum[:, :]), r=[k_ssum], w=[k_ssum])
                        S.op('dve', lambda e: e.tensor_scalar(out=aff_all[:, ti, :], in0=ex[:, :], scalar1=ssum[:, 0:1], scalar2=None, op0=ALU.mult),
                             r=[k_ex, k_ssum], w=[('aff', ti)])
                        S.op('dve', lambda e: e.tensor_copy(out=hr[:, 1026:1042], in_=aff_all[:, ti, :]), r=[('aff', ti)], w=[hk])
                        S.op('dve', lambda e: e.tensor_tensor(out=hr[:, 1042:1058], in0=aff_all[:, ti, :], in1=hr[:, 1026:1042], op=ALU.subtract),
                             r=[('aff', ti), hk], w=[hk])
                        S.op('dve', lambda e: e.tensor_scalar(out=hr[:, 1024:1025], in0=iotap, scalar1=0.0, scalar2=float(ti), op0=ALU.mult, op1=ALU.add),
                             r=['cst'], w=[hk])
                        S.op('dve', lambda e: e.tensor_copy(out=hr[:, 1025:1026], in_=iotap), r=['cst'], w=[hk])
                        S.dma('sp', lambda q: q.dma_start(out=h2s[ti * 128:(ti + 1) * 128, :], in_=hr[:, :]), r=[hk], w=[('h2s', ti)])

            nB = len(tiles_b)
            for step in range(nB + 2):
                if step < nB:
                    tileB(tiles_b[step], 0)
                if 0 <= step - 1 < nB:
                    tileB(tiles_b[step - 1], 1)
                if 0 <= step - 2 < nB:
                    tileB(tiles_b[step - 2], 2)
            S.barrier()
        if stop_after is not None and stop_after.startswith(f'B{l}'):
            return _finish(nc, S, es)

        NTB = len(tiles_b)
        t0b = tiles_b[0]
        with ExitStack() as ph:
            cmpb = sb(ph, "cmpb", [128, NT, NE], BF16)
            lo = sb(ph, "lo", [128, 32], F32)
            hi = sb(ph, "hi", [128, 32], F32)
            mid = sb(ph, "mid", [128, 32], F32)
            kvec = sb(ph, "kvec", [128, 32], F32)
            cntp = sb(ph, "cntp", [128, 32], BF16)
            ge = sb(ph, "ge", [128, 32], F32)
            gm = sb(ph, "gm", [128, 32], F32)
            posf = sb(ph, "posf", [128, NT, NE], F32)
            tot = sb(ph, "tot", [128, NT, NE], F32)
            base = sb(ph, "base", [128, NT + 1, NE], F32)
            sel = sb(ph, "sel", [128, NT, NE], F32)
            posi = sb(ph, "posi", [128, NT, NE], I32)
            affk = [('aff', t) for t in range(NT)]
            S.op('dve', lambda e: e.memset(lo[:, :], 0.0), w=['lo'])
            S.op('dve', lambda e: e.memset(hi[:, :], 1.0), w=['hi'])
            S.op('dve', lambda e: e.memset(kvec[:, 0:16], float(CAPL)), w=['kvec'])
            S.op('dve', lambda e: e.memset(kvec[:, 16:32], float(CAPC)), w=['kvec'])
            S.op('dve', lambda e: e.memset(cntp[:, :], 0.0), w=['cntp'])
            if last:
                S.op('dve', lambda e: e.memset(cmpb[:, 0:NCTX, :], 0.0), w=['cmpb'])
            lat_aff = aff_all[:, NCTX:NT, :]
            ctx_aff = aff_all[:, 0:NCTX, :]

            def compare(thr):
                S.op('dve', lambda e: e.tensor_tensor(out=cmpb[:, NCTX:NT, :], in0=lat_aff, in1=thr[:, 0:16].unsqueeze(1).to_broadcast([128, NLAT, NE]),
                                                      op=ALU.is_ge), r=affk + ['thr'], w=['cmpb'])
                if not last:
                    S.op('dve', lambda e: e.tensor_tensor(out=cmpb[:, 0:NCTX, :], in0=ctx_aff, in1=thr[:, 16:32].unsqueeze(1).to_broadcast([128, NCTX, NE]),
                                                          op=ALU.is_ge), r=affk + ['thr'], w=['cmpb'])

            for itn in range(30):
                S.op('dve', lambda e: e.tensor_tensor(out=mid[:, :], in0=lo[:, :], in1=hi[:, :], op=ALU.add), r=['lo', 'hi'], w=['thr'])
                S.op('dve', lambda e: e.tensor_scalar(out=mid[:, :], in0=mid[:, :], scalar1=0.5, scalar2=None, op0=ALU.mult), r=['thr'], w=['thr'])
                compare(mid)
                S.op('dve', lambda e: e.tensor_reduce(out=cntp[:, 0:16], in_=cmpb[:, NCTX:NT, :].rearrange("p t e -> p e t"), axis=AX.X, op=ALU.add),
                     r=['cmpb'], w=['cntp'])
                if not last:
                    S.op('dve', lambda e: e.tensor_reduce(out=cntp[:, 16:32], in_=cmpb[:, 0:NCTX, :].rearrange("p t e -> p e t"), axis=AX.X, op=ALU.add),
                         r=['cmpb'], w=['cntp'])
                bk, bkey = nbank()
                S.op('pe', lambda e, bk=bk: e.matmul(bk[:, 0:32], lhsT=onesb[:, :], rhs=cntp[:, :], start=True, stop=True), r=['onesb', 'cntp'], w=[bkey])
                S.op('dve', lambda e, bk=bk: e.tensor_tensor(out=ge[:, :], in0=bk[:, 0:32], in1=kvec[:, :], op=ALU.is_ge), r=[bkey, 'kvec'], w=['ge'])
                S.op('dve', lambda e: e.tensor_tensor(out=gm[:, :], in0=ge[:, :], in1=mid[:, :], op=ALU.mult), r=['ge', 'thr'], w=['gm'])
                S.op('dve', lambda e: e.tensor_tensor(out=lo[:, :], in0=lo[:, :], in1=gm[:, :], op=ALU.max), r=['lo', 'gm'], w=['lo'])
                S.op('dve', lambda e: e.scalar_tensor_tensor(out=gm[:, :], in0=ge[:, :], scalar=2.0, in1=mid[:, :], op0=ALU.mult, op1=ALU.add),
                     r=['ge', 'thr', 'gm'], w=['gm'])
                S.op('dve', lambda e: e.tensor_tensor(out=hi[:, :], in0=hi[:, :], in1=gm[:, :], op=ALU.min), r=['hi', 'gm'], w=['hi'])
            S.op('dve', lambda e: e.tensor_copy(out=mid[:, :], in_=lo[:, :]), r=['lo'], w=['thr'])
            compare(mid)
            cflat = cmpb[:, :, :].rearrange("p t e -> p (t e)")
            pflat = posf[:, :, :].rearrange("p t e -> p (t e)")
            tflat = tot[:, :, :].rearrange("p t e -> p (t e)")
            ncol = NT * NE
            for c0 in range(0, ncol, 512):
                cs = min(512, ncol - c0)
                bk, bkey = nbank()
                S.op('pe', lambda e, bk=bk, c0=c0, cs=cs: e.matmul(bk[:, 0:cs], lhsT=triub[:, :], rhs=cflat[:, c0:c0 + cs], start=True, stop=True),
                     r=['triub', 'cmpb'], w=[bkey])
                S.op('act', lambda e, bk=bk, c0=c0, cs=cs: e.copy(out=pflat[:, c0:c0 + cs], in_=bk[:, 0:cs]), r=[bkey], w=['posf'])
                bk, bkey = nbank()
                S.op('pe', lambda e, bk=bk, c0=c0, cs=cs: e.matmul(bk[:, 0:cs], lhsT=onesb[:, :], rhs=cflat[:, c0:c0 + cs], start=True, stop=True),
                     r=['onesb', 'cmpb'], w=[bkey])
                S.op('act', lambda e, bk=bk, c0=c0, cs=cs: e.copy(out=tflat[:, c0:c0 + cs], in_=bk[:, 0:cs]), r=[bkey], w=['tot'])
            S.op('dve', lambda e: e.memset(base[:, 0, :], float(CAPL)), w=['base'])
            S.op('dve', lambda e: e.memset(base[:, NCTX, :], 0.0), w=['base'])
            for t in range(NT):
                if t == NCTX - 1:
                    continue
                S.op('dve', lambda e, t=t: e.tensor_tensor(out=base[:, t + 1, :], in0=base[:, t, :], in1=tot[:, t, :], op=ALU.add),
                     r=['base', 'tot'], w=['base'])
            S.op('dve', lambda e: e.tensor_tensor(out=posf[:, :, :], in0=posf[:, :, :], in1=base[:, 0:NT, :], op=ALU.add), r=['posf', 'base'], w=['posf'])
            S.op('dve', lambda e: e.scalar_tensor_tensor(out=sel[:, NCTX:NT, :], in0=posf[:, NCTX:NT, :], scalar=float(CAPL), in1=cmpb[:, NCTX:NT, :],
                                                         op0=ALU.is_lt, op1=ALU.mult), r=['posf', 'cmpb'], w=['sel'])
            S.op('dve', lambda e: e.scalar_tensor_tensor(out=sel[:, 0:NCTX, :], in0=posf[:, 0:NCTX, :], scalar=float(CAPL + CAPC), in1=cmpb[:, 0:NCTX, :],
                                                         op0=ALU.is_lt, op1=ALU.mult), r=['posf', 'cmpb'], w=['sel'])
            S.op('dve', lambda e: e.scalar_tensor_tensor(out=posf[:, :, :], in0=posf[:, :, :], scalar=-BIG, in1=sel[:, :, :], op0=ALU.add, op1=ALU.mult),
                 r=['posf', 'sel'], w=['posf'])
            S.op('dve', lambda e: e.tensor_scalar(out=posi[:, :, :], in0=posf[:, :, :], scalar1=BIG, scalar2=None, op0=ALU.add), r=['posf'], w=['posi'])

            h2ld = [sb(ph, f"h2ld{i}", [128, RW], BF16) for i in range(3)]
            breg = nc.gpsimd.to_reg(CAPT - 1)
            for n, ti in enumerate(tiles_b):
                hl, hlk = h2ld[n % 3], ('h2ld', n % 3)
                S.dma('sp', lambda q, ti=ti, hl=hl: q.dma_start(out=hl[:, :], in_=h2s[ti * 128:(ti + 1) * 128, :]), r=[('h2s', ti)], w=[hlk])
                for ex_ in range(NE):
                    S.dma('pool', lambda q, ti=ti, ex_=ex_, hl=hl: q.indirect_dma_start(
                        out=Xs[ex_][:, :], out_offset=bass.IndirectOffsetOnAxis(ap=posi[:, ti, ex_:ex_ + 1], axis=0),
                        in_=hl[:, :], in_offset=None, bounds_check=breg, oob_is_err=False), r=[hlk, 'posi'], w=[('Xs', ex_)])
            S.barrier()
        if stop_after == f'R{l}':
            return _finish(nc, S, es)

        NST = (CAPT + 127) // 128
        CAPP = NST * 128
        NSL = 3 if CAPP % 3 == 0 and CAPP // 3 <= 512 else (CAPP + 511) // 512
        SLW = CAPP // NSL
        assert SLW * NSL == CAPP and SLW <= 512
        with ExitStack() as ph:
            wg = [sb(ph, f"wg{i}", [128, 8, D], BF16) for i in range(2)]
            wu = [sb(ph, f"wu{i}", [128, 8, D], BF16) for i in range(2)]
            wd = [sb(ph, f"wd{i}", [128, 8, D], BF16) for i in range(2)]
            xsb = sb(ph, "xsb", [128, NST, RW], BF16)
            XT = sb(ph, "XT", [128, 8, NST * 128], BF16)
            hidT = sb(ph, "hidT", [128, 8, NST * 128], BF16)
            sgt = [sb(ph, f"sgt{i}", [128, 512], F32) for i in range(2)]
            ysb = [sb(ph, f"ysb{i}", [128, D], F32) for i in range(3)]
            gcol = sb(ph, "gcol", [128, NST], F32)
            idxi = sb(ph, "idxi", [128, NST], I32)
            S.op('pool', lambda e: e.memset(xsb[:, NST - 1, :], 0.0), w=['xsb'])
            zt = ysb[0]
            S.op('pool', lambda e: e.memset(zt[:, :], 0.0), w=[('ysb', 0)])
            ztoks = []
            for ti in tiles_b:
                ztoks.append(S.dma('sp', lambda q, ti=ti: q.dma_start(out=macc[ti * 128:(ti + 1) * 128, :], in_=zt[:, :]),
                                   r=[('ysb', 0), ('macc_rd', ti)], w=[('macc_z', ti)]))
            prev_sc = list(ztoks)

            def load_w(ex_):
                sl = ex_ % 2
                for wsb, wdr, nm in ((wg, w_gate, 'wg'), (wu, w_up, 'wu'), (wd, w_down, 'wd')):
                    S.dma('pool', lambda q, wsb=wsb, wdr=wdr: q.dma_start(
                        out=wsb[sl][:, :, :], in_=wdr[l, ex_, :, :].rearrange("(ko ki) n -> ki ko n", ki=128)), w=[(nm, sl)])

            load_w(0)
            ycount = 0
            for ex_ in range(NE):
                sl = ex_ % 2
                if ex_ + 1 < NE:
                    load_w(ex_ + 1)
                nfull = CAPT // 128
                rem = CAPT - nfull * 128
                S.dma('sp', lambda q, ex_=ex_: q.dma_start(out=xsb[:, 0:nfull, :], in_=Xs[ex_][0:nfull * 128, :].rearrange("(s p) w -> p s w", p=128)),
                      r=[('Xs', ex_)], w=['xsb'])
                if rem:
                    S.dma('sp', lambda q, ex_=ex_: q.dma_start(out=xsb[0:rem, nfull, :], in_=Xs[ex_][nfull * 128:CAPT, :]), r=[('Xs', ex_)], w=['xsb'])
                S.op('dve', lambda e, ex_=ex_: e.tensor_tensor(out=gcol[:, 0:nfull], in0=xsb[:, 0:nfull, 1026 + ex_], in1=xsb[:, 0:nfull, 1042 + ex_], op=ALU.add),
                     r=['xsb'], w=['gcol'])
                S.op('dve', lambda e: e.scalar_tensor_tensor(out=idxi[:, 0:nfull], in0=xsb[:, 0:nfull, 1024], scalar=128.0, in1=xsb[:, 0:nfull, 1025],
                                                             op0=ALU.mult, op1=ALU.add), r=['xsb'], w=['idxi'])
                if rem:
                    S.op('dve', lambda e, ex_=ex_: e.tensor_tensor(out=gcol[0:rem, nfull:nfull + 1], in0=xsb[0:rem, nfull, 1026 + ex_:1027 + ex_],
                                                                  in1=xsb[0:rem, nfull, 1042 + ex_:1043 + ex_], op=ALU.add), r=['xsb'], w=['gcol'])
                    S.op('dve', lambda e: e.scalar_tensor_tensor(out=idxi[0:rem, nfull:nfull + 1], in0=xsb[0:rem, nfull, 1024:1025], scalar=128.0,
                                                                 in1=xsb[0:rem, nfull, 1025:1026], op0=ALU.mult, op1=ALU.add), r=['xsb'], w=['idxi'])
                for st in range(NST):
                    rows = 128 if st < nfull else rem
                    transposes_to(XT[:, :, st * 128:(st + 1) * 128], 'XT', lambda k, st=st: xsb[:, st, k * 128:(k + 1) * 128], 8,
                                  src_key='xsb', evac=('act' if st % 2 == 0 else 'dve'))
                for fc in range(8):
                    for sn in range(NSL):
                        n0 = sn * SLW
                        bg, bgk = nbank()
                        for k in range(8):
                            S.op('pe', lambda e, bg=bg, k=k, fc=fc, n0=n0: e.matmul(bg[:, 0:SLW], lhsT=wg[sl][:, k, fc * 128:(fc + 1) * 128],
                                                                                   rhs=XT[:, k, n0:n0 + SLW], start=(k == 0), stop=(k == 7)),
                                 r=[('wg', sl), 'XT'], w=[bgk], signal=(k == 7))
                        bu, buk = nbank()
                        for k in range(8):
                            S.op('pe', lambda e, bu=bu, k=k, fc=fc, n0=n0: e.matmul(bu[:, 0:SLW], lhsT=wu[sl][:, k, fc * 128:(fc + 1) * 128],
                                                                                   rhs=XT[:, k, n0:n0 + SLW], start=(k == 0), stop=(k == 7)),
                                 r=[('wu', sl), 'XT'], w=[buk], signal=(k == 7))
                        sgi = (fc * NSL + sn) % 2
                        S.op('act', lambda e, bg=bg, sgi=sgi: e.activation(out=sgt[sgi][:, 0:SLW], in_=bg[:, 0:SLW], func=AF.Silu), r=[bgk], w=[('sgt', sgi)])
                        S.op('dve', lambda e, bu=bu, sgi=sgi, fc=fc, n0=n0: e.tensor_tensor(out=hidT[:, fc, n0:n0 + SLW], in0=bu[:, 0:SLW], in1=sgt[sgi][:, 0:SLW],
                                                                                           op=ALU.mult), r=[buk, ('sgt', sgi)], w=['hidT'])
                cur_sc = []
                for st in range(NST):
                    rows = 128 if st < nfull else rem
                    yi = ycount % 3
                    ycount += 1
                    for half in range(2):
                        by, byk = nbank()
                        for fc in range(8):
                            S.op('pe', lambda e, by=by, fc=fc, st=st, rows=rows, half=half: e.matmul(
                                by[:, :], lhsT=hidT[:, fc, st * 128:(st + 1) * 128], rhs=wd[sl][:, fc, half * 512:(half + 1) * 512],
                                start=(fc == 0), stop=(fc == 7)), r=['hidT', ('wd', sl)], w=[byk], signal=(fc == 7))
                        if half == 0:
                            S.op('act', lambda e, by=by, yi=yi, st=st, rows=rows: e.activation(out=ysb[yi][0:rows, 0:512], in_=by[0:rows, :], func=AF.Copy,
                                                                                              scale=gcol[0:rows, st:st + 1]), r=[byk, 'gcol'], w=[('ysb', yi)])
                        else:
                            S.op('dve', lambda e, by=by, yi=yi, st=st, rows=rows: e.tensor_scalar(out=ysb[yi][0:rows, 512:1024], in0=by[0:rows, :],
                                                                                                 scalar1=gcol[0:rows, st:st + 1], scalar2=None, op0=ALU.mult),
                                 r=[byk, 'gcol'], w=[('ysb', yi)])
                    cur_sc.append(S.dma('pool', lambda q, yi=yi, st=st, rows=rows: q.indirect_dma_start(
                        out=macc[:, :], out_offset=bass.IndirectOffsetOnAxis(ap=idxi[0:rows, st:st + 1], axis=0),
                        in_=ysb[yi][0:rows, :], in_offset=None, compute_op=ALU.add), r=[('ysb', yi), 'idxi'], w=[('macc_sc', ex_, st)], extra=prev_sc))
                prev_sc = cur_sc
            S.barrier()
        if stop_after == f'M{l}':
            return _finish(nc, S, es)

        with ExitStack() as ph:
            g2 = [sb(ph, f"g2_{s}", [128, D], F32) for s in range(2)]
            ln2g = sb(ph, "ln2g", [128, D], F32)
            ln2b = sb(ph, "ln2b", [128, D], F32)
            KF = 3
            xa = [sb(ph, f"xa{i}", [128, D], F32) for i in range(KF)]
            xm = [sb(ph, f"xm{i}", [128, D], F32) for i in range(KF)]
            xo = [sb(ph, f"xo{i}", [128, D], F32) for i in range(KF)]
            for s in range(2):
                S.dma('sp', lambda q, s=s: q.dma_start(out=g2[s][:, :], in_=modbc[l, s, :, 5 * D:6 * D]), r=[('modbc', l, s)], w=[('g2', s)])
            S.dma('sp', lambda q: q.dma_start(out=ln2g[:, :], in_=ln2_g[l:l + 1, :].to_broadcast([128, D])), w=['ln2g'])
            S.dma('sp', lambda q: q.dma_start(out=ln2b[:, :], in_=ln2_b[l:l + 1, :].to_broadcast([128, D])), w=['ln2b'])
            out_toks = []
            chains = []
            for n, ti in enumerate(tiles_b):
                S.begin_record()
                s = 0 if ti < NCTX else 1
                a, ak = xa[n % KF], ('xa', n % KF)
                m, mk = xm[n % KF], ('xm', n % KF)
                o, ok = xo[n % KF], ('xo', n % KF)
                S.dma('sp', lambda q, ti=ti, a=a: q.dma_start(out=a[:, :], in_=x1s[ti * 128:(ti + 1) * 128, :]), r=[('x1s', ti)], w=[ak])
                S.dma('sp', lambda q, ti=ti, m=m: q.dma_start(out=m[:, :], in_=macc[ti * 128:(ti + 1) * 128, :]), w=[mk, ('macc_rd', ti)], extra=prev_sc)
                S.op('dve', lambda e, m=m, s=s: e.tensor_tensor(out=m[:, :], in0=m[:, :], in1=g2[s][:, :], op=ALU.mult), r=[mk, ('g2', s)], w=[mk])
                S.op('dve', lambda e, m=m, a=a: e.scalar_tensor_tensor(out=m[:, :], in0=a[:, :], scalar=ALPHA, in1=m[:, :], op0=ALU.mult, op1=ALU.add),
                     r=[ak, mk], w=[mk])
                ln_stats(m, mk)
                ln_apply(o[:, :], ok, m[:, :], mk)
                S.op('dve', lambda e, o=o: e.tensor_tensor(out=o[:, :], in0=o[:, :], in1=ln2g[:, :], op=ALU.mult), r=[ok, 'ln2g'], w=[ok])
                S.op('pool', lambda e, o=o: e.tensor_tensor(out=o[:, :], in0=o[:, :], in1=ln2b[:, :], op=ALU.add), r=[ok, 'ln2b'], w=[ok])
                if last:
                    i = ti - NCTX
                    (S.dma('sp', lambda q, i=i, o=o: q.dma_start(out=out[i * 128:(i + 1) * 128, :], in_=o[:, :]), r=[ok], w=[('out', i)]))
                else:
                    S.dma('sp', lambda q, ti=ti, o=o: q.dma_start(out=xs_d[ti * 128:(ti + 1) * 128, :], in_=o[:, :]), r=[ok], w=[('xs_d',)])
                chains.append(S.end_record())
            for g0 in range(0, len(chains), KF):
                S.emit_interleaved(chains[g0:g0 + KF])
            S.barrier()
        if stop_after == f'F{l}':
            return _finish(nc, S, es)
    return _finish(nc, S, es)


def _finish(nc, S, es):
    S.barrier()
    es.close()
    return nc


def _consts():
    c = np.zeros((128, 6 * 128), np.float32)
    p = np.arange(128)
    c[:, 0:128] = np.eye(128, dtype=np.float32)
    c[:, 128:256] = (p[:, None] < p[None, :]).astype(np.float32)
    c[:, 256:384] = (p[:, None] >= p[None, :]).astype(np.float32)
    c[:, 384:512] = (p[:, None] <= p[None, :]).astype(np.float32)
    c[:, 512:640] = 1.0
    c[:, 640] = p.astype(np.float32)
    c[:, 641] = (p < 64).astype(np.float32)
    c[:, 642] = (p >= 64).astype(np.float32)
    return c


def _rope_table(NLAT):
    t = np.arange(NLAT * 128, dtype=np.int32)
    row = (t // 64).astype(np.float32)[:, None]
    col = (t % 64).astype(np.float32)[:, None]
    inv = (np.float32(10000.0) ** (-np.arange(16, dtype=np.float32) / np.float32(16))).astype(np.float32)
    ar = (row * inv).astype(np.float32)
    ac = (col * inv).astype(np.float32)
    cr, sr, cc, sc = np.cos(ar), np.sin(ar), np.cos(ac), np.sin(ac)
    tab = np.concatenate([cr, cr, cc, cc, -sr, sr, -sc, sc], axis=1).astype(np.float32)
    return np.ascontiguousarray(tab.reshape(NLAT, 128, 128))


def _nat_entries(NLAT):
    ents = [(2, d) for d in range(5)] if NLAT >= 5 else [(0, 0)] * 5
    ents = [(2, 2 + d - 2) for d in range(5)] if NLAT >= 5 else ents
    ents += [(0, j) for j in range(4)] + [(1, j) for j in range(4)]
    ents += [(NLAT - 2, NLAT - 4 + j) for j in range(4)] + [(NLAT - 1, NLAT - 4 + j) for j in range(4)]
    return ents


def _nat_table(nat_bias, NLAT):
    rows = NLAT * 2
    ents = _nat_entries(NLAT)
    kk = np.arange(128)
    Lh = nat_bias.shape[0]
    out = np.empty((Lh, 128, NCHB, 4, 128), np.float32)
    for n, (i, j) in enumerate(ents):
        kr = 2 * j + kk // 64
        kc = kk % 64
        qr = 2 * i + kk // 64
        qc = kk % 64
        rs = np.clip(qr - 4, 0, rows - 8)
        cs = np.clip(qc - 8, 0, 48)
        valid = ((kr[:, None] >= rs[None, :]) & (kr[:, None] < rs[None, :] + 8) &
                 (kc[:, None] >= cs[None, :]) & (kc[:, None] < cs[None, :] + 16))
        dr = np.clip(kr[:, None] - qr[None, :] + 7, 0, 14)
        dc = np.clip(kc[:, None] - qc[None, :], -15, 15) + 15
        g = nat_bias[:, :, dr, dc]
        g = np.where(valid[None, None], g, np.float32(NEG))
        out[:, :, n, :, :] = np.transpose(g, (0, 2, 1, 3))
    return np.ascontiguousarray(out.reshape(Lh, 128, NCHB * 512))


def make_in_maps(inputs, NLAT, samples, moe=True):
    names = ['w_mod', 'b_mod', 'w_in', 'a_sink', 'conv_w', 'conv_b', 'conv_ln_g', 'conv_ln_b', 'w_out', 'ln1_g', 'ln1_b',
             'w_router', 'ln2_g', 'ln2_b'] + (['w_gate', 'w_up', 'w_down'] if moe else [])
    shared = {k: np.ascontiguousarray(np.asarray(inputs[k], np.float32)) for k in names}
    cw = shared['conv_w']
    shared['conv_w'] = np.ascontiguousarray(cw.reshape(L, 31, 2, 128).transpose(0, 3, 2, 1).reshape(L, 128, 62))
    shared['conv_b'] = np.ascontiguousarray(shared['conv_b'].reshape(L, 2, 128).transpose(0, 2, 1))
    shared['natT'] = _nat_table(np.asarray(inputs['nat_bias'], np.float32), NLAT)
    shared['rope'] = _rope_table(NLAT)
    shared['consts'] = _consts()
    maps = []
    for b in samples:
        m = dict(shared)
        m['xlat'] = np.ascontiguousarray(np.asarray(inputs['x'][b, :NLAT * 128], np.float32))
        m['xctx'] = np.ascontiguousarray(np.asarray(inputs['ctx'][b], np.float32))
        cv = np.stack([np.asarray(inputs['c_ctx'], np.float32), np.asarray(inputs['c'][b], np.float32)])
        m['cvec'] = np.ascontiguousarray(cv.reshape(2, 8, 128).transpose(2, 1, 0).reshape(128, 16))
        maps.append(m)
    return maps


def kernel(**inputs):
    NLAT = 64
    nc = build_program(NLAT)
    maps = make_in_maps(inputs, NLAT, [0, 1, 2, 3, 0, 1, 2, 3])
    res = run_bass_kernel_spmd(nc, maps, core_ids=list(range(8)))
    return np.stack([np.asarray(res.results[b]["out"], np.float32).reshape(NLAT * 128, D) for b in range(4)])
```

```python
import os
import types
import numpy as np
from contextlib import ExitStack
import concourse.bass as bass
import concourse.mybir as mybir
from concourse.bass_utils import run_bass_kernel_spmd

F32 = mybir.dt.float32
BF16 = mybir.dt.bfloat16
I32 = mybir.dt.int32
AF = mybir.ActivationFunctionType
ALU = mybir.AluOpType
AX = mybir.AxisListType

D = 1024
L = 2
NCTX = 2
NE = 16
EPS = 1e-6
ALPHA = float((2 * L) ** 0.25)
SCALE = 0.125
NEG = -30000.0
BIG = 1.0e6
QA, QB, KA, VA, KB, VB, CH, AW = 0, 1024, 1536, 1664, 1794, 2050, 2310, 2566
QW = KA
KVW = CH - KA
RW = 1024 + 2 + 32
NCHB = 21


def _freeze(fn):
    if fn.__closure__ is None:
        return fn
    cells = []
    for c in fn.__closure__:
        try:
            cells.append(types.CellType(c.cell_contents))
        except ValueError:
            cells.append(c)
    return types.FunctionType(fn.__code__, fn.__globals__, fn.__name__, fn.__defaults__, tuple(cells))


class Sched:
    EPOCH = 16000

    def __init__(self, nc, es, ndma=16):
        self.nc, self.es = nc, es
        self.eng = dict(pe=nc.tensor, act=nc.scalar, dve=nc.vector, pool=nc.gpsimd, sp=nc.sync)
        self.sems = {k: [] for k in self.eng}
        self.cnt = {k: 0 for k in self.eng}
        self.dpool = {'sp': list(range(0, ndma)), 'pool': list(range(ndma, ndma + 8)), 'act': list(range(ndma + 8, ndma + 12))}
        ntot = ndma + 12
        self.dsem = [es.enter_context(nc.semaphore(f"dq{i}")) for i in range(ntot)]
        self.dval = [0] * ntot
        self.dnext = {'sp': 0, 'pool': 0, 'act': 0}
        self.waited = {k: {} for k in self.eng}
        self.lastw = {}
        self.readers = {}
        self.same_engine_sync = True
        self.nwaits = 0

    def _sem(self, e, seq):
        i = (seq - 1) // self.EPOCH
        while len(self.sems[e]) <= i:
            self.sems[e].append(self.es.enter_context(self.nc.semaphore(f"s_{e}{len(self.sems[e])}")))
        return self.sems[e][i], (seq - 1) % self.EPOCH + 1

    def _wait(self, e, tok):
        kind, src, val = tok
        if kind == 'e':
            if src == e and (e == 'pe' or not self.same_engine_sync):
                return
            assert val <= self.cnt[src], f"dependency on unsignalled op {tok}"
            key = ('e', src)
        else:
            key = ('d', src)
        if self.waited[e].get(key, 0) >= val:
            return
        self.waited[e][key] = val
        if kind == 'e':
            sem, v = self._sem(src, val)
        else:
            sem, v = self.dsem[src], val
        self.eng[e].wait_ge(sem, v)
        self.nwaits += 1

    def _deps(self, r, w):
        deps = []
        for k in r:
            if k in self.lastw:
                deps.append(self.lastw[k])
            if isinstance(k, tuple) and k[0] == 'pb':
                deps.extend(self.readers.get(k, {}).values())
        for k in w:
            if k in self.lastw:
                deps.append(self.lastw[k])
            deps.extend(self.readers.get(k, {}).values())
        return deps

    def _record(self, tok, r, w):
        for k in r:
            d = self.readers.setdefault(k, {})
            key = (tok[0], tok[1])
            if key not in d or d[key][2] < tok[2]:
                d[key] = tok
        for k in w:
            self.lastw[k] = tok
            self.readers[k] = {}

    def begin_record(self):
        self.rec = []

    def end_record(self):
        ops, self.rec = self.rec, None
        return ops

    def emit_interleaved(self, chains):
        idx = [0] * len(chains)
        left = sum(len(c) for c in chains)
        while left:
            for ci, c in enumerate(chains):
                if idx[ci] < len(c):
                    kind, args, kw = c[idx[ci]]
                    idx[ci] += 1
                    left -= 1
                    (self.op if kind == 'op' else self.dma)(*args, **kw)

    def op(self, e, fn, r=(), w=(), signal=True, extra=()):
        if getattr(self, 'rec', None) is not None:
            self.rec.append(('op', (e, _freeze(fn)), dict(r=list(r), w=list(w), signal=signal, extra=list(extra))))
            return None
        for d in self._deps(r, w):
            self._wait(e, d)
        for d in extra:
            self._wait(e, d)
        inst = fn(self.eng[e])
        if signal:
            self.cnt[e] += 1
            seq = self.cnt[e]
            sem, _ = self._sem(e, seq)
            inst.then_inc(sem, 1)
        else:
            seq = self.cnt[e] + 1
        tok = ('e', e, seq)
        self._record(tok, r, w)
        return tok

    def dma(self, q, fn, r=(), w=(), extra=()):
        if getattr(self, 'rec', None) is not None:
            self.rec.append(('dma', (q, _freeze(fn)), dict(r=list(r), w=list(w), extra=list(extra))))
            return None
        pl = self.dpool[q]
        slot = pl[self.dnext[q] % len(pl)]
        self.dnext[q] += 1
        if self.dval[slot]:
            self._wait(q, ('d', slot, self.dval[slot]))
        for d in self._deps(r, w):
            self._wait(q, d)
        for d in extra:
            self._wait(q, d)
        inst = fn(self.eng[q])
        inst.then_inc(self.dsem[slot], 16)
        self.dval[slot] += 16
        tok = ('d', slot, self.dval[slot])
        self._record(tok, r, w)
        return tok

    def barrier(self):
        toks = [('e', f, self.cnt[f]) for f in self.eng if self.cnt[f] > 0]
        toks += [('d', s, v) for s, v in enumerate(self.dval) if v > 0]
        for e in self.eng:
            for t in toks:
                self._wait(e, t)


def build_program(NLAT=64, dbg=False, nlayers=L, stop_after=None, skip_mods=False):
    NT = NLAT + NCTX
    NTOK = NT * 128
    CAPL = 16 * NLAT
    CAPC = 32

    nc = bass.Bass("TRN2", target_bir_lowering=False)
    dk = "ExternalOutput" if dbg else "Internal"

    def din(name, shape, dt=F32):
        return nc.dram_tensor(name, list(shape), dt, kind="ExternalInput").ap()

    xlat = din("xlat", [NLAT * 128, D])
    xctx = din("xctx", [NCTX * 128, D])
    cvec = din("cvec", [128, 16])
    if not skip_mods:
        w_mod = din("w_mod", [L, D, 6 * D])
        b_mod = din("b_mod", [L, 6 * D])
    w_in = din("w_in", [L, D, 2048])
    a_sink = din("a_sink", [L, 8])
    natT = din("natT", [L, 128, NCHB * 512])
    conv_w = din("conv_w", [L, 128, 62])
    conv_b = din("conv_b", [L, 128, 2])
    conv_ln_g = din("conv_ln_g", [L, 256])
    conv_ln_b = din("conv_ln_b", [L, 256])
    w_out = din("w_out", [L, D, D])
    ln1_g = din("ln1_g", [L, D])
    ln1_b = din("ln1_b", [L, D])
    w_router = din("w_router", [L, D, NE])
    need_moe = stop_after is None or stop_after.startswith(('M', 'F'))
    if need_moe:
        w_gate = din("w_gate", [L, NE, D, D])
        w_up = din("w_up", [L, NE, D, D])
        w_down = din("w_down", [L, NE, D, D])
    ln2_g = din("ln2_g", [L, D])
    ln2_b = din("ln2_b", [L, D])
    rope = din("rope", [NLAT, 128, 128])
    consts = din("consts", [128, 6 * 128])
    out = nc.dram_tensor("out", [NLAT * 128, D], F32, kind="ExternalOutput").ap()

    modbc = nc.dram_tensor("modbc", [L, 2, 128, 6 * D], F32, kind=dk).ap()
    scrA = nc.dram_tensor("scrA", [NT, 128, AW], BF16, kind=dk).ap()
    x1s = nc.dram_tensor("x1s", [NTOK, D], F32, kind=dk).ap()
    h2s = nc.dram_tensor("h2s", [NTOK, RW], BF16, kind=dk).ap()
    xs_d = nc.dram_tensor("xs_d", [NTOK, D], F32, kind=dk).ap()
    macc = nc.dram_tensor("macc", [NTOK, D], F32, kind=dk).ap()
    CAPT0 = CAPL + CAPC
    Xs = [nc.dram_tensor(f"Xs{e}", [CAPT0, RW], BF16, kind=dk).ap() for e in range(NE)]
    otok_d = nc.dram_tensor("otok_d", [NTOK, D], BF16, kind=dk).ap() if dbg else None

    es = ExitStack()
    S = Sched(nc, es)
    es.enter_context(nc.allow_non_contiguous_dma(reason="small strided parameter loads"))
    es.enter_context(nc.allow_low_precision(reason="0/1 mask counts <= 128 are exact in bf16"))

    uid = [0]

    def sb(stack, name, shape, dt):
        uid[0] += 1
        return stack.enter_context(nc.sbuf_tensor(f"{name}_{uid[0]}", list(shape), dt))

    banks = [es.enter_context(nc.psum_tensor(f"pb{i}", [128, 512], F32)) for i in range(8)]
    bank_pools = {'all': list(range(8)), 'b0': [0, 1, 2, 3, 4], 'b1': [5, 6], 'b2': [7]}
    bank_rr = {k: 0 for k in bank_pools}
    bank_pool = ['all']

    def nbank():
        pl = bank_pools[bank_pool[0]]
        i = pl[bank_rr[bank_pool[0]] % len(pl)]
        bank_rr[bank_pool[0]] += 1
        return banks[i], ('pb', i)

    cst = sb(es, "cst", [128, 6 * 128], F32)
    identb = sb(es, "identb", [128, 128], BF16)
    triub = sb(es, "triub", [128, 128], BF16)
    masklr = sb(es, "masklr", [128, 2, 128], BF16)
    onesb = sb(es, "onesb", [128, 128], BF16)
    aff_all = sb(es, "aff_all", [128, NT, NE], F32)
    NLN = 4
    stat_l = [sb(es, "stat", [128, 12], F32) for _ in range(NLN)]
    mv_l = [sb(es, "mv", [128, 2], F32) for _ in range(NLN)]
    sd_l = [sb(es, "sd", [128, 1], F32) for _ in range(NLN)]
    rstd_l = [sb(es, "rstd", [128, 1], F32) for _ in range(NLN)]
    nmr_l = [sb(es, "nmr", [128, 1], F32) for _ in range(NLN)]
    lncur = [0]
    identf = cst[:, 0:128]
    iotap = cst[:, 5 * 128:5 * 128 + 1]
    pm0 = cst[:, 5 * 128 + 1:5 * 128 + 2]
    pm1 = cst[:, 5 * 128 + 2:5 * 128 + 3]

    S.dma('sp', lambda q: q.dma_start(out=cst[:, :], in_=consts), w=['cst'])
    S.op('dve', lambda e: e.tensor_copy(out=identb[:, :], in_=cst[:, 0:128]), r=['cst'], w=['identb'])
    S.op('dve', lambda e: e.tensor_copy(out=triub[:, :], in_=cst[:, 128:256]), r=['cst'], w=['triub'])
    S.op('dve', lambda e: e.tensor_copy(out=masklr[:, :, :].rearrange("p a b -> p (a b)"), in_=cst[:, 256:512]), r=['cst'], w=['masklr'])
    S.op('dve', lambda e: e.tensor_copy(out=onesb[:, :], in_=cst[:, 512:640]), r=['cst'], w=['onesb'])

    def ln_stats(src_ap, src_key, width=1024):
        nch = (width + 511) // 512
        lncur[0] = (lncur[0] + 1) % NLN
        j = lncur[0]
        stat, mv, sd, rstd, nmr = stat_l[j], mv_l[j], sd_l[j], rstd_l[j], nmr_l[j]
        kst_, kmv, ksd, krs, knm = ('stat', j), ('mv', j), ('sd', j), ('rstd', j), ('nmr', j)
        for c in range(nch):
            S.op('dve', lambda e, c=c: e.bn_stats(out=stat[:, 6 * c:6 * c + 6], in_=src_ap[:, c * 512:min(width, (c + 1) * 512)]),
                 r=[src_key], w=[kst_])
        S.op('dve', lambda e: e.bn_aggr(out=mv[:, :], in_=stat[:, 0:6 * nch]), r=[kst_], w=[kmv])
        S.op('dve', lambda e: e.tensor_scalar(out=sd[:, :], in0=mv[:, 1:2], scalar1=EPS, scalar2=None, op0=ALU.add), r=[kmv], w=[ksd])
        S.op('act', lambda e: e.activation(out=sd[:, :], in_=sd[:, :], func=AF.Sqrt), r=[ksd], w=[ksd])
        S.op('dve', lambda e: e.reciprocal(out=rstd[:, :], in_=sd[:, :]), r=[ksd], w=[krs])
        S.op('dve', lambda e: e.tensor_scalar(out=nmr[:, :], in0=mv[:, 0:1], scalar1=rstd[:, 0:1], scalar2=-1.0, op0=ALU.mult, op1=ALU.mult),
             r=[kmv, krs], w=[knm])

    def ln_apply(dst_ap, dst_key, src_ap, src_key):
        j = lncur[0]
        S.op('act', lambda e: e.activation(out=dst_ap, in_=src_ap, func=AF.Identity, scale=rstd_l[j][:, 0:1], bias=nmr_l[j][:, 0:1]),
             r=[src_key, ('rstd', j), ('nmr', j)], w=[dst_key])

    def transposes_to(dst_ap, dst_key, src_fn, n, rows=128, src_key=None, evac='act'):
        bk, bkey = nbank()
        bv = bk[:, :].bitcast(BF16)
        for k in range(n):
            S.op('pe', lambda e, k=k: e.transpose(out=bv[:, k * 128:k * 128 + rows], in_=src_fn(k), identity=identb[:rows, :rows]),
                 r=[src_key, 'identb'], w=[bkey], signal=(k == n - 1))
        if rows == 128:
            src = bv[:, 0:n * 128]
        else:
            src = bv[:, 0:n * 128].rearrange("p (k s) -> p k s", s=128)[:, :, 0:rows]
        if evac == 'act':
            S.op('act', lambda e: e.copy(out=dst_ap, in_=src), r=[bkey], w=[dst_key])
        else:
            S.op('dve', lambda e: e.tensor_copy(out=dst_ap, in_=src), r=[bkey], w=[dst_key])

    with ExitStack() as ph:
        cT = sb(ph, "cT", [128, 8, 2], F32)
        cS = sb(ph, "cS", [128, 8, 2], F32)
        lbc = [sb(ph, f"lbc{s}", [128, 8, 128], BF16) for s in range(2)]
        wm = [sb(ph, f"wm{i}", [128, 8, 512], BF16) for i in range(2)]
        bm = [sb(ph, f"bm{i}", [128, 512], F32) for i in range(2)]
        mo = [sb(ph, f"mo{i}", [128, 512], F32) for i in range(4)]
        S.dma('sp', lambda q: q.dma_start(out=cT[:, :, :], in_=cvec.rearrange("p (k s) -> p k s", s=2)), w=['cT'])
        S.op('act', lambda e: e.activation(out=cS[:, :, :], in_=cT[:, :, :], func=AF.Silu), r=['cT'], w=['cS'])
        for s in range(2):
            S.op('dve', lambda e, s=s: e.tensor_copy(out=lbc[s][:, :, :], in_=cS[:, :, s:s + 1].to_broadcast([128, 8, 128])),
                 r=['cS'], w=[('lbc', s)])
        it = 0
        for l in range(nlayers if not skip_mods else 0):
            for ch in range(12):
                n0 = ch * 512
                slot = it % 2
                S.dma('pool', lambda q, slot=slot, l=l, n0=n0: q.dma_start(
                    out=wm[slot][:, :, :], in_=w_mod[l, :, n0:n0 + 512].rearrange("(ko ki) n -> ki ko n", ki=128)), w=[('wm', slot)])
                S.dma('sp', lambda q, slot=slot, l=l, n0=n0: q.dma_start(
                    out=bm[slot][:, :], in_=b_mod[l:l + 1, n0:n0 + 512].to_broadcast([128, 512])), w=[('bm', slot)])
                addc = 1.0 if (ch // 2) in (1, 4) else 0.0
                for s in range(2):
                    bk, bkey = nbank()
                    for k in range(8):
                        S.op('pe', lambda e, k=k, s=s, slot=slot, bk=bk: e.matmul(bk[:, :], lhsT=lbc[s][:, k, :], rhs=wm[slot][:, k, :],
                                                                               start=(k == 0), stop=(k == 7)),
                             r=[('lbc', s), ('wm', slot)], w=[bkey], signal=(k == 7))
                    ms = (it * 2 + s) % 4
                    S.op('dve', lambda e, bk=bk, ms=ms, slot=slot: e.scalar_tensor_tensor(
                        out=mo[ms][:, :], in0=bk[:, :], scalar=addc, in1=bm[slot][:, :], op0=ALU.add, op1=ALU.add),
                         r=[bkey, ('bm', slot)], w=[('mo', ms)])
                    S.dma('sp', lambda q, ms=ms, l=l, s=s, n0=n0: q.dma_start(out=modbc[l, s, :, n0:n0 + 512], in_=mo[ms][:, :]),
                          r=[('mo', ms)], w=[('modbc', l, s)])
                it += 1
        S.barrier()
    if stop_after == 'mods':
        return _finish(nc, S, es)

    for l in range(nlayers):
        last = (l == L - 1)
        tiles_b = list(range(NT)) if not last else list(range(NCTX, NT))
        CAPT = CAPL + (0 if last else CAPC)

        def xsrc(ti):
            if l == 0:
                return xctx[ti * 128:(ti + 1) * 128, :] if ti < NCTX else xlat[(ti - NCTX) * 128:(ti - NCTX + 1) * 128, :]
            return xs_d[ti * 128:(ti + 1) * 128, :]

        with ExitStack() as ph:
            w_in_sb = sb(ph, "w_in_sb", [128, 8, 2048], BF16)
            modA = [[sb(ph, f"modA{s}{j}", [128, D], F32) for j in range(2)] for s in range(2)]
            xin = [sb(ph, f"xinA{i}", [128, D], F32) for i in range(2)]
            ropet = [sb(ph, f"ropet{i}", [128, 128], F32) for i in range(2)]
            f32a_l = [sb(ph, "f32a", [128, D], F32) for _ in range(2)]
            hb_l = [sb(ph, "hb", [128, D], BF16) for _ in range(2)]
            hT_l = [sb(ph, "hT", [128, 8, 128], BF16) for _ in range(2)]
            ropeA_l = [sb(ph, "ropeA", [128, 512], F32) for _ in range(2)]
            ropeB_l = [sb(ph, "ropeB", [128, 512], F32) for _ in range(2)]
            qperm_l = [sb(ph, "qperm", [128, 512], BF16) for _ in range(2)]
            kst_l = [sb(ph, "kst", [128, 640], BF16) for _ in range(2)]
            sg_l = [sb(ph, "sg", [128, 256], F32) for _ in range(2)]
            chtok_l = [sb(ph, "chtok", [128, 256], BF16) for _ in range(2)]
            aout = [sb(ph, f"aout{i}", [128, AW], BF16) for i in range(2)]
            for i in range(2):
                S.op('pool', lambda e, i=i: e.memset(aout[i][:, VA:VA + 130], 1.0), w=[('aout', i)])
                S.op('pool', lambda e, i=i: e.memset(aout[i][:, VB:VB + 260], 1.0), w=[('aout', i)])
            for hf in range(2):
                S.dma('pool', lambda q, hf=hf: q.dma_start(out=w_in_sb[:, :, hf * 1024:(hf + 1) * 1024],
                                                        in_=w_in[l, :, hf * 1024:(hf + 1) * 1024].rearrange("(ko ki) n -> ki ko n", ki=128)),
                      w=['w_in_sb'])
            for s in range(2):
                for j in range(2):
                    S.dma('sp', lambda q, s=s, j=j: q.dma_start(out=modA[s][j][:, :], in_=modbc[l, s, :, j * D:(j + 1) * D]),
                          r=[('modbc', l, s)], w=[('modA', s, j)])

            def tileA(ti, part):
                    s = 0 if ti < NCTX else 1
                    lat = ti >= NCTX
                    xs_, xk = xin[ti % 2], ('xinA', ti % 2)
                    f32a = f32a_l[ti % 2]
                    k_f32a = ('f32a', ti % 2)
                    hb = hb_l[ti % 2]
                    k_hb = ('hb', ti % 2)
                    hT = hT_l[ti % 2]
                    k_hT = ('hT', ti % 2)
                    ropeA = ropeA_l[ti % 2]
                    k_ropeA = ('ropeA', ti % 2)
                    ropeB = ropeB_l[ti % 2]
                    k_ropeB = ('ropeB', ti % 2)
                    qperm = qperm_l[ti % 2]
                    k_qperm = ('qperm', ti % 2)
                    kst = kst_l[ti % 2]
                    k_kst = ('kst', ti % 2)
                    sg = sg_l[ti % 2]
                    k_sg = ('sg', ti % 2)
                    chtok = chtok_l[ti % 2]
                    k_chtok = ('chtok', ti % 2)
                    ao, aok = aout[ti % 2], ('aout', ti % 2)
                    rt, rk = ropet[ti % 2], ('ropet', ti % 2)
                    if part == 0:
                        S.dma('sp', lambda q, ti=ti, xs_=xs_: q.dma_start(out=xs_[:, :], in_=xsrc(ti)), r=[('xs_d',)] if l > 0 else [], w=[xk])
                        if lat:
                            S.dma('sp', lambda q, ti=ti, rt=rt: q.dma_start(out=rt[:, :], in_=rope[ti - NCTX, :, :]), w=[rk])
                        ln_stats(xs_, xk)
                        ln_apply(f32a[:, :], k_f32a, xs_[:, :], xk)
                        S.op('dve', lambda e, s=s: e.tensor_tensor(out=f32a[:, :], in0=f32a[:, :], in1=modA[s][1][:, :], op=ALU.mult),
                             r=[k_f32a, ('modA', s, 1)], w=[k_f32a])
                        S.op('pool', lambda e, s=s: e.tensor_tensor(out=hb[:, :], in0=f32a[:, :], in1=modA[s][0][:, :], op=ALU.add),
                             r=[k_f32a, ('modA', s, 0)], w=[k_hb])
                    if part == 1:
                        transposes_to(hT[:, :, :].rearrange("p k t -> p (k t)"), k_hT, lambda k: hb[:, k * 128:(k + 1) * 128], 8, src_key=k_hb)
                        ub = []
                        for n in range(4):
                            bk, bkey = nbank()
                            for k in range(8):
                                S.op('pe', lambda e, k=k, n=n, bk=bk: e.matmul(bk[:, :], lhsT=hT[:, k, :], rhs=w_in_sb[:, k, n * 512:(n + 1) * 512],
                                                                             start=(k == 0), stop=(k == 7)),
                                     r=[k_hT, 'w_in_sb'], w=[bkey], signal=(k == 7))
                            ub.append((bk, bkey))
                        b0, k0 = ub[0]
                        qp_v = qperm[:, :].rearrange("p (g r d) -> p r g d", g=4, r=2, d=64)
                        if lat:
                            S.op('dve', lambda e, b0=b0, rt=rt: e.tensor_tensor(
                                out=ropeA[:, :].rearrange("p (h d) -> p h d", d=64), in0=b0[:, :].rearrange("p (h d) -> p h d", d=64),
                                in1=rt[:, 0:64].unsqueeze(1).to_broadcast([128, 8, 64]), op=ALU.mult), r=[k0, rk], w=[k_ropeA])
                            for half in range(2):
                                S.op('dve', lambda e, b0=b0, rt=rt, half=half: e.tensor_tensor(
                                    out=ropeB[:, :].rearrange("p (h r f d) -> p h r f d", h=8, r=2, f=2, d=16)[:, :, :, half, :],
                                    in0=b0[:, :].rearrange("p (h r f d) -> p h r f d", h=8, r=2, f=2, d=16)[:, :, :, 1 - half, :],
                                    in1=rt[:, 64:128].rearrange("p (r f d) -> p r f d", r=2, f=2, d=16)[:, :, half, :].unsqueeze(1).to_broadcast([128, 8, 2, 16]),
                                    op=ALU.mult), r=[k0, rk], w=[k_ropeB])
                            S.op('pool', lambda e: e.tensor_tensor(out=qp_v, in0=ropeA[:, :].rearrange("p (r g d) -> p r g d", r=2, g=4, d=64),
                                                                  in1=ropeB[:, :].rearrange("p (r g d) -> p r g d", r=2, g=4, d=64), op=ALU.add),
                                 r=[k_ropeA, k_ropeB], w=[k_qperm])
                        else:
                            S.op('act', lambda e, b0=b0: e.copy(out=qp_v, in_=b0[:, :].rearrange("p (r g d) -> p r g d", r=2, g=4, d=64)),
                                 r=[k0], w=[k_qperm])
                        bk, bkey = nbank()
                        bv = bk[:, :].bitcast(BF16)
                        for k in range(4):
                            S.op('pe', lambda e, k=k, bv=bv: e.transpose(out=bv[:, k * 128:(k + 1) * 128], in_=qperm[:, k * 128:(k + 1) * 128], identity=identb[:, :]),
                                 r=[k_qperm, 'identb'], w=[bkey], signal=(k == 3))
                        if os.environ.get('EVAC', 'mask') == 'mask':
                            S.op('act', lambda e, bv=bv, ao=ao: e.activation(out=ao[:, QA:QA + 512], in_=bv[:, 0:512], func=AF.Copy, scale=pm0), r=[bkey, 'cst'], w=[aok])
                            S.op('dve', lambda e, bv=bv, ao=ao: e.tensor_scalar(out=ao[:, QA + 512:QA + 1024], in0=bv[:, 0:512], scalar1=pm1, scalar2=None, op0=ALU.mult),
                                 r=[bkey, 'cst'], w=[aok])
                        else:
                            S.op('act', lambda e, bv=bv, ao=ao: e.copy(out=ao[:, QA:QA + 512], in_=bv[:, 0:512]), r=[bkey], w=[aok])
                            S.op('dve', lambda e, bv=bv, ao=ao: e.tensor_copy(out=ao[:, QA + 512:QA + 1024], in_=bv[:, 0:512]), r=[bkey], w=[aok])
                        b1, k1 = ub[1]
                        if lat:
                            S.op('dve', lambda e, b1=b1, rt=rt: e.tensor_tensor(
                                out=ropeA[:, 0:128].rearrange("p (h d) -> p h d", d=64), in0=b1[:, 0:128].rearrange("p (h d) -> p h d", d=64),
                                in1=rt[:, 0:64].unsqueeze(1).to_broadcast([128, 2, 64]), op=ALU.mult), r=[k1, rk], w=[k_ropeA])
                            for half in range(2):
                                S.op('dve', lambda e, b1=b1, rt=rt, half=half: e.tensor_tensor(
                                    out=ropeB[:, 0:128].rearrange("p (h r f d) -> p h r f d", h=2, r=2, f=2, d=16)[:, :, :, half, :],
                                    in0=b1[:, 0:128].rearrange("p (h r f d) -> p h r f d", h=2, r=2, f=2, d=16)[:, :, :, 1 - half, :],
                                    in1=rt[:, 64:128].rearrange("p (r f d) -> p r f d", r=2, f=2, d=16)[:, :, half, :].unsqueeze(1).to_broadcast([128, 2, 2, 16]),
                                    op=ALU.mult), r=[k1, rk], w=[k_ropeB])
                            S.op('pool', lambda e: e.tensor_tensor(out=kst[:, 0:128], in0=ropeA[:, 0:128], in1=ropeB[:, 0:128], op=ALU.add),
                                 r=[k_ropeA, k_ropeB], w=[k_kst])
                        else:
                            S.op('act', lambda e, b1=b1: e.copy(out=kst[:, 0:128], in_=b1[:, 0:128]), r=[k1], w=[k_kst])
                        S.op('act', lambda e, b1=b1, ao=ao: e.copy(out=ao[:, VA:VA + 130].rearrange("p (h d) -> p h d", d=65)[:, :, 0:64],
                                                                 in_=b1[:, 128:256].rearrange("p (h d) -> p h d", d=64)), r=[k1], w=[aok])
                        S.op('act', lambda e, b1=b1: e.copy(out=kst[:, 128:384], in_=b1[:, 256:512]), r=[k1], w=[k_kst])
                        b2, k2 = ub[2]
                        S.op('dve', lambda e, b2=b2: e.tensor_copy(out=kst[:, 384:640], in_=b2[:, 0:256]), r=[k2], w=[k_kst])
                        S.op('act', lambda e, b2=b2, ao=ao: e.copy(out=ao[:, VB:VB + 260].rearrange("p (h d) -> p h d", d=65)[:, :, 0:64],
                                                                 in_=b2[:, 256:512].rearrange("p (h d) -> p h d", d=64)), r=[k2], w=[aok])
                        order = [1, 2, 0, 3, 4]
                        bk, bkey = nbank()
                        bv = bk[:, :].bitcast(BF16)
                        for j, blk in enumerate(order):
                            S.op('pe', lambda e, j=j, blk=blk, bv=bv: e.transpose(out=bv[:, j * 128:(j + 1) * 128], in_=kst[:, blk * 128:(blk + 1) * 128],
                                                                                 identity=identb[:, :]), r=[k_kst, 'identb'], w=[bkey], signal=(j == 4))
                        if os.environ.get('EVAC', 'mask') == 'mask':
                            S.op('act', lambda e, bv=bv, ao=ao: e.activation(out=ao[:, QB:QB + 256], in_=bv[:, 0:256], func=AF.Copy, scale=pm0), r=[bkey, 'cst'], w=[aok])
                            S.op('dve', lambda e, bv=bv, ao=ao: e.tensor_scalar(out=ao[:, QB + 256:QB + 512], in0=bv[:, 0:256], scalar1=pm1, scalar2=None, op0=ALU.mult),
                                 r=[bkey, 'cst'], w=[aok])
                        else:
                            S.op('act', lambda e, bv=bv, ao=ao: e.copy(out=ao[:, QB:QB + 256], in_=bv[:, 0:256]), r=[bkey], w=[aok])
                            S.op('dve', lambda e, bv=bv, ao=ao: e.tensor_copy(out=ao[:, QB + 256:QB + 512], in_=bv[:, 0:256]), r=[bkey], w=[aok])
                        S.op('dve', lambda e, bv=bv, ao=ao: e.tensor_copy(out=ao[:, KA:KA + 128], in_=bv[:, 256:384]), r=[bkey], w=[aok])
                        S.op('dve', lambda e, bv=bv, ao=ao: e.tensor_copy(out=ao[:, KB:KB + 256], in_=bv[:, 384:640]), r=[bkey], w=[aok])
                        b3, k3 = ub[3]
                        S.op('act', lambda e, b3=b3: e.activation(out=sg[:, :], in_=b3[:, 256:512], func=AF.Sigmoid), r=[k3], w=[k_sg])
                        S.op('dve', lambda e, b3=b3: e.tensor_tensor(out=chtok[:, :], in0=b3[:, 0:256], in1=sg[:, :], op=ALU.mult), r=[k3, k_sg], w=[k_chtok])
                        transposes_to(ao[:, CH:CH + 256], aok, lambda k: chtok[:, k * 128:(k + 1) * 128], 2, src_key=k_chtok, evac='dve')
                        S.dma('sp', lambda q, ti=ti, ao=ao: q.dma_start(out=scrA[ti, :, 0:QW], in_=ao[:, 0:QW]), r=[aok], w=[('scrAq', ti)])
                        S.dma('sp', lambda q, ti=ti, ao=ao: q.dma_start(out=scrA[ti, :, QW:AW], in_=ao[:, QW:AW]), r=[aok], w=[('scrA', ti)])

            for step in range(NT + 1):
                chains = []
                if step < NT:
                    S.begin_record()
                    tileA(step, 0)
                    chains.append(S.end_record())
                if step - 1 >= 0:
                    S.begin_record()
                    tileA(step - 1, 1)
                    chains.append(S.end_record())
                S.emit_interleaved(chains)
            S.barrier()
        if stop_after == f'A{l}':
            return _finish(nc, S, es)

        with ExitStack() as ph:
            w_out_sb = sb(ph, "w_out_sb", [128, 8, D], BF16)
            w_r_sb = sb(ph, "w_r_sb", [128, 8, NE], BF16)
            nat_sb = sb(ph, "nat_sb", [128, NCHB, 512], BF16)
            cwT = sb(ph, "cwT", [128, 2, 31], F32)
            cdiag = sb(ph, "cdiag", [128, 2, 31, 128], BF16)
            convb = sb(ph, "convb", [128, 2], F32)
            clng = sb(ph, "clng", [128, 256], F32)
            clnb = sb(ph, "clnb", [128, 256], F32)
            modB = [[sb(ph, f"modB{s}{j}", [128, D], F32) for j in range(3)] for s in range(2)]
            ln1g = sb(ph, "ln1g", [128, D], F32)
            ln1b = sb(ph, "ln1b", [128, D], F32)
            esink = sb(ph, "esink", [128, 8], F32)
            NQR, NKR, NCW = 3, 8, 4
            qring = [sb(ph, f"qring{i}", [128, QW], BF16) for i in range(NQR)]
            kvring = [sb(ph, f"kvring{i}", [128, KVW], BF16) for i in range(NKR)]
            kvctx = [sb(ph, f"kvctx{i}", [128, KVW], BF16) for i in range(NCTX)]
            chwin = [sb(ph, f"chwin{i}", [128, 2, 160], BF16) for i in range(NCW)]
            xin = [sb(ph, f"xinB{i}", [128, D], F32) for i in range(2)]
            pT = sb(ph, "pT", [128, 7, 512], BF16)
            pTA = [sb(ph, "pTA", [128, 5, 512], BF16) for _ in range(2)]
            btmp = [sb(ph, f"btmp{i}", [128, 512], F32) for i in range(2)]
            otok_l = [sb(ph, "otok", [128, D], BF16) for _ in range(2)]
            oT_l = [sb(ph, "oT", [128, 8, 128], BF16) for _ in range(2)]
            cvT_l = [sb(ph, "cvT", [128, 2, 128], F32) for _ in range(2)]
            cvn_l = [sb(ph, "cvn", [128, 256], F32) for _ in range(2)]
            f32b_l = [sb(ph, "f32b", [128, D], F32) for _ in range(2)]
            f32c_l = [sb(ph, "f32c", [128, D], F32) for _ in range(2)]
            h2row = [sb(ph, f"h2row{i}", [128, RW], BF16) for i in range(2)]
            h2T_l = [sb(ph, "h2T", [128, 8, 128], BF16) for _ in range(2)]
            den_l = [sb(ph, "den", [128, 4], F32) for _ in range(2)]
            rden_l = [sb(ph, "rden", [128, 4], F32) for _ in range(2)]
            ex_l = [sb(ph, "ex", [128, NE], F32) for _ in range(2)]
            ssum_l = [sb(ph, "ssum", [128, 1], F32) for _ in range(2)]

            for hf in range(1):
                S.dma('pool', lambda q: q.dma_start(out=w_out_sb[:, :, :], in_=w_out[l, :, :].rearrange("(ko ki) n -> ki ko n", ki=128)), w=['w_out_sb'])
            S.dma('pool', lambda q: q.dma_start(out=w_r_sb[:, :, :], in_=w_router[l, :, :].rearrange("(ko ki) n -> ki ko n", ki=128)), w=['w_r_sb'])
            for c3 in range(0, NCHB, 3):
                c4 = min(NCHB, c3 + 3)
                S.dma('pool', lambda q, c3=c3, c4=c4: q.dma_start(out=nat_sb[:, c3:c4, :], in_=natT[l, :, c3 * 512:c4 * 512].rearrange("p (a b) -> p a b", b=512)),
                      w=['nat_sb'])
            S.dma('sp', lambda q: q.dma_start(out=cwT[:, :, :], in_=conv_w[l, :, :].rearrange("c (cc j) -> c cc j", cc=2)), w=['cwT'])
            S.dma('sp', lambda q: q.dma_start(out=convb[:, :], in_=conv_b[l, :, :]), w=['convb'])
            S.dma('sp', lambda q: q.dma_start(out=clng[:, :], in_=conv_ln_g[l:l + 1, :].to_broadcast([128, 256])), w=['clng'])
            S.dma('sp', lambda q: q.dma_start(out=clnb[:, :], in_=conv_ln_b[l:l + 1, :].to_broadcast([128, 256])), w=['clnb'])
            S.dma('sp', lambda q: q.dma_start(out=ln1g[:, :], in_=ln1_g[l:l + 1, :].to_broadcast([128, D])), w=['ln1g'])
            S.dma('sp', lambda q: q.dma_start(out=ln1b[:, :], in_=ln1_b[l:l + 1, :].to_broadcast([128, D])), w=['ln1b'])
            S.dma('sp', lambda q: q.dma_start(out=esink[:, :], in_=a_sink[l:l + 1, :].to_broadcast([128, 8])), w=['esink'])
            S.op('act', lambda e: e.activation(out=esink[:, :], in_=esink[:, :], func=AF.Exp), r=['esink'], w=['esink'])
            for s in range(2):
                for j, chn in enumerate((2, 4, 3)):
                    S.dma('sp', lambda q, s=s, j=j, chn=chn: q.dma_start(out=modB[s][j][:, :], in_=modbc[l, s, :, chn * D:(chn + 1) * D]),
                          r=[('modbc', l, s)], w=[('modB', s, j)])
            for cc in range(2):
                for j in range(31):
                    S.op('pool', lambda e, cc=cc, j=j: e.tensor_scalar(out=cdiag[:, cc, j, :], in0=cst[:, 0:128], scalar1=cwT[:, cc, j:j + 1],
                                                                      scalar2=None, op0=ALU.mult), r=['cst', 'cwT'], w=['cdiag'])
            for c in range(NCTX):
                S.dma('sp', lambda q, c=c: q.dma_start(out=kvctx[c][:, :], in_=scrA[c, :, KA:CH]), r=[('scrA', c)], w=[('kvctx', c)])

            loaded = set()

            def load_tile(t):
                if t in loaded or t < 0 or t >= NT:
                    return
                loaded.add(t)
                if t >= NCTX:
                    S.dma('sp', lambda q: q.dma_start(out=kvring[t % NKR][:, :], in_=scrA[t, :, KA:CH]), r=[('scrA', t)], w=[('kvring', t % NKR)])

            def load_own(t):
                S.dma('sp', lambda q: q.dma_start(out=qring[t % NQR][:, :], in_=scrA[t, :, 0:QW]), r=[('scrAq', t)], w=[('qring', t % NQR)])
                cw, cwk = chwin[t % NCW], ('chwin', t % NCW)
                first = t in (0, NCTX)
                lastt = t in (NCTX - 1, NT - 1)
                S.dma('sp', lambda q: q.dma_start(out=cw[:, :, 16:144], in_=scrA[t, :, CH:CH + 256].rearrange("p (c n) -> p c n", c=2)),
                      r=[('scrA', t)], w=[cwk])
                if first:
                    S.op('pool', lambda e: e.memset(cw[:, :, 0:16], 0.0), w=[cwk])
                else:
                    S.dma('sp', lambda q: q.dma_start(out=cw[:, :, 0:16], in_=scrA[t - 1, :, CH:CH + 256].rearrange("p (c n) -> p c n", c=2)[:, :, 112:128]),
                          r=[('scrA', t - 1)], w=[cwk])
                if lastt:
                    S.op('pool', lambda e: e.memset(cw[:, :, 144:160], 0.0), w=[cwk])
                else:
                    S.dma('sp', lambda q: q.dma_start(out=cw[:, :, 144:160], in_=scrA[t + 1, :, CH:CH + 256].rearrange("p (c n) -> p c n", c=2)[:, :, 0:16]),
                          r=[('scrA', t + 1)], w=[cwk])
                S.dma('sp', lambda q: q.dma_start(out=xin[t % 2][:, :], in_=xsrc(t)), r=[('xs_d',)] if l > 0 else [], w=[('xinB', t % 2)])

            def kvbuf(t):
                if t < NCTX:
                    return kvctx[t], ('kvctx', t)
                return kvring[t % NKR], ('kvring', t % NKR)

            def tileB(ti, part):
                    s = 0 if ti < NCTX else 1
                    lat = ti >= NCTX
                    i = ti - NCTX
                    otok = otok_l[ti % 2]
                    k_otok = ('otok', ti % 2)
                    oT = oT_l[ti % 2]
                    k_oT = ('oT', ti % 2)
                    cvT = cvT_l[ti % 2]
                    k_cvT = ('cvT', ti % 2)
                    cvn = cvn_l[ti % 2]
                    k_cvn = ('cvn', ti % 2)
                    f32b = f32b_l[ti % 2]
                    k_f32b = ('f32b', ti % 2)
                    f32c = f32c_l[ti % 2]
                    k_f32c = ('f32c', ti % 2)
                    h2T = h2T_l[ti % 2]
                    k_h2T = ('h2T', ti % 2)
                    den = den_l[ti % 2]
                    k_den = ('den', ti % 2)
                    rden = rden_l[ti % 2]
                    k_rden = ('rden', ti % 2)
                    ex = ex_l[ti % 2]
                    k_ex = ('ex', ti % 2)
                    ssum = ssum_l[ti % 2]
                    k_ssum = ('ssum', ti % 2)
                    if part == 0:
                      for t2 in range(ti - 2, ti + 4):
                        if lat and t2 >= NCTX:
                            load_tile(t2)
                      load_own(ti)
                    qr, qk = qring[ti % NQR], ('qring', ti % NQR)
                    cw, cwk = chwin[ti % NCW], ('chwin', ti % NCW)
                    xs_, xk = xin[ti % 2], ('xinB', ti % 2)
                    hr, hk = h2row[ti % 2], ('h2row', ti % 2)

                    if stop_after == f'B{l}:setup':
                        return
                    if part == 0:
                        cbk, cbkey = nbank()
                        for cc in range(2):
                            for j in range(31):
                                S.op('pe', lambda e, cbk=cbk, cc=cc, j=j: e.matmul(cbk[:, cc * 128:(cc + 1) * 128], lhsT=cdiag[:, cc, j, :],
                                                                                  rhs=cw[:, cc, 1 + j:1 + j + 128], start=(j == 0), stop=(j == 30)),
                                     r=['cdiag', cwk], w=[cbkey], signal=(cc == 1 and j == 30))
                        for cc in range(2):
                            S.op('act', lambda e, cbk=cbk, cc=cc: e.activation(out=cvT[:, cc, :], in_=cbk[:, cc * 128:(cc + 1) * 128], func=AF.Identity,
                                                                              bias=convb[:, cc:cc + 1], scale=1.0), r=[cbkey, 'convb'], w=[k_cvT])
                        if lat:
                            chunksA = []
                            if i - 1 >= 0:
                                chunksA.append((ti - 1, 0))
                            chunksA.append((ti, None))
                            if i + 1 < NLAT:
                                chunksA.append((ti + 1, 1))
                            chunksA += [(0, None), (1, None)]
                        else:
                            chunksA = [(0, None), (1, None)]
                        for grp in range(2):
                            for ci, (ct, mk) in enumerate(chunksA):
                                kb_, kk = kvbuf(ct)
                                bk, bkey = nbank()
                                S.op('pe', lambda e, bk=bk, kb_=kb_: e.matmul(bk[:, :], lhsT=kb_[:, 0:128], rhs=qr[:, QA + grp * 512:QA + (grp + 1) * 512],
                                                                             start=True, stop=True), r=[kk, qk], w=[bkey])
                                S.op('act', lambda e, bk=bk, ci=ci: e.activation(out=pTA[grp][:, ci, :], in_=bk[:, :], func=AF.Exp, scale=SCALE),
                                     r=[bkey], w=[('pTA', grp, ci)])
                                if mk is not None:
                                    S.op('pool', lambda e, ci=ci, mk=mk: e.tensor_tensor(
                                        out=pTA[grp][:, ci, :].rearrange("p (g t) -> p g t", g=4), in0=pTA[grp][:, ci, :].rearrange("p (g t) -> p g t", g=4),
                                        in1=masklr[:, mk, :].unsqueeze(1).to_broadcast([128, 4, 128]), op=ALU.mult), r=[('pTA', grp, ci), 'masklr'], w=[('pTA', grp, ci)])
                        if lat:
                            if NLAT >= 5 and 2 <= i <= NLAT - 3:
                                chunksB = [(ti + d - 2, d) for d in range(5)]
                            elif i == 0:
                                chunksB = [(NCTX + j, 5 + j) for j in range(4)]
                            elif i == 1:
                                chunksB = [(NCTX + j, 9 + j) for j in range(4)]
                            elif i == NLAT - 2:
                                chunksB = [(NCTX + NLAT - 4 + j, 13 + j) for j in range(4)]
                            else:
                                chunksB = [(NCTX + NLAT - 4 + j, 17 + j) for j in range(4)]
                            chunksB += [(0, None), (1, None)]
                        else:
                            chunksB = [(0, None), (1, None)]
                        for ci, (ct, ent) in enumerate(chunksB):
                            kb_, kk = kvbuf(ct)
                            bk, bkey = nbank()
                            for h in range(4):
                                p, m = h // 2, h % 2
                                S.op('pe', lambda e, bk=bk, kb_=kb_, h=h, p=p, m=m: e.matmul(
                                    bk[:, h * 128:(h + 1) * 128], lhsT=kb_[:, KB - KA + p * 128:KB - KA + (p + 1) * 128],
                                    rhs=qr[:, QB + m * 256 + p * 128:QB + m * 256 + (p + 1) * 128], start=True, stop=True),
                                     r=[kk, qk], w=[bkey], signal=(h == 3))
                            if ent is not None:
                                bt, btk = btmp[ci % 2], ('btmp', ci % 2)
                                S.op('dve', lambda e, bk=bk, bt=bt, ent=ent: e.scalar_tensor_tensor(
                                    out=bt[:, :], in0=bk[:, :], scalar=SCALE, in1=nat_sb[:, ent, :], op0=ALU.mult, op1=ALU.add),
                                     r=[bkey, 'nat_sb'], w=[btk])
                                S.op('act', lambda e, bt=bt, ci=ci: e.activation(out=pT[:, ci, :], in_=bt[:, :], func=AF.Exp), r=[btk], w=[('pT', ci)])
                            else:
                                S.op('act', lambda e, bk=bk, ci=ci: e.activation(out=pT[:, ci, :], in_=bk[:, :], func=AF.Exp, scale=SCALE),
                                     r=[bkey], w=[('pT', ci)])
                        for grp in range(2):
                            ob, obk = nbank()
                            nchk = len(chunksA)
                            for g in range(4):
                                for ci, (ct, mk) in enumerate(chunksA):
                                    kb_, kk = kvbuf(ct)
                                    S.op('pe', lambda e, g=g, ci=ci, kb_=kb_, ob=ob: e.matmul(
                                        ob[:, g * 65:(g + 1) * 65], lhsT=pTA[grp][:, ci, g * 128:(g + 1) * 128],
                                        rhs=kb_[:, VA - KA + grp * 65:VA - KA + (grp + 1) * 65], start=(ci == 0), stop=(ci == nchk - 1)),
                                         r=[('pTA', grp, ci), kk], w=[obk], signal=(g == 3 and ci == nchk - 1))
                            obv = ob[:, 0:260].rearrange("p (g d) -> p g d", d=65)
                            S.op('dve', lambda e, obv=obv: e.tensor_tensor(out=den[:, :], in0=obv[:, :, 64], in1=esink[:, grp * 4:(grp + 1) * 4], op=ALU.add),
                                 r=[obk, 'esink'], w=[k_den])
                            S.op('dve', lambda e: e.reciprocal(out=rden[:, :], in_=den[:, :]), r=[k_den], w=[k_rden])
                            S.op('dve', lambda e, obv=obv: e.tensor_tensor(
                                out=otok[:, grp * 256:(grp + 1) * 256].rearrange("p (g d) -> p g d", d=64), in0=obv[:, :, 0:64],
                                in1=rden[:, :].unsqueeze(2).to_broadcast([128, 4, 64]), op=ALU.mult), r=[obk, k_rden], w=[k_otok])
                        ob, obk = nbank()
                        nchk = len(chunksB)
                        for h in range(4):
                            for ci, (ct, ent) in enumerate(chunksB):
                                kb_, kk = kvbuf(ct)
                                S.op('pe', lambda e, h=h, ci=ci, kb_=kb_, ob=ob: e.matmul(
                                    ob[:, h * 65:(h + 1) * 65], lhsT=pT[:, ci, h * 128:(h + 1) * 128],
                                    rhs=kb_[:, VB - KA + h * 65:VB - KA + (h + 1) * 65], start=(ci == 0), stop=(ci == nchk - 1)),
                                     r=[('pT', ci), kk], w=[obk], signal=(h == 3 and ci == nchk - 1))
                        obv = ob[:, 0:260].rearrange("p (g d) -> p g d", d=65)
                        S.op('dve', lambda e, obv=obv: e.reciprocal(out=rden[:, :], in_=obv[:, :, 64]), r=[obk], w=[k_rden])
                        S.op('dve', lambda e, obv=obv: e.tensor_tensor(
                            out=otok[:, 512:768].rearrange("p (g d) -> p g d", d=64), in0=obv[:, :, 0:64],
                            in1=rden[:, :].unsqueeze(2).to_broadcast([128, 4, 64]), op=ALU.mult), r=[obk, k_rden], w=[k_otok])
                        bk2, bkey2 = nbank()
                        for cc in range(2):
                            S.op('pe', lambda e, bk2=bk2, cc=cc: e.transpose(out=bk2[:, cc * 128:(cc + 1) * 128], in_=cvT[:, cc, :], identity=identf),
                                 r=[k_cvT, 'cst'], w=[bkey2], signal=(cc == 1))
                        ln_stats(bk2, bkey2, width=256)
                        ln_apply(cvn[:, :], k_cvn, bk2[:, 0:256], bkey2)
                        S.op('dve', lambda e: e.tensor_tensor(out=cvn[:, :], in0=cvn[:, :], in1=clng[:, :], op=ALU.mult), r=[k_cvn, 'clng'], w=[k_cvn])
                        S.op('pool', lambda e: e.tensor_tensor(out=cvn[:, :], in0=cvn[:, :], in1=clnb[:, :], op=ALU.add), r=[k_cvn, 'clnb'], w=[k_cvn])
                        S.op('act', lambda e: e.activation(out=otok[:, 768:1024], in_=cvn[:, :], func=AF.Silu), r=[k_cvn], w=[k_otok])

                        if dbg:
                            S.dma('sp', lambda q: q.dma_start(out=otok_d[ti * 128:(ti + 1) * 128, :], in_=otok[:, :]), r=[k_otok], w=[('dbg_otok', ti)])

                        if stop_after == f'B{l}:conv':
                            return
                    if part == 1:
                        transposes_to(oT[:, :, :].rearrange("p k t -> p (k t)"), k_oT, lambda k: otok[:, k * 128:(k + 1) * 128], 8, src_key=k_otok)
                        for n in range(2):
                            bk, bkey = nbank()
                            for k in range(8):
                                S.op('pe', lambda e, bk=bk, k=k, n=n: e.matmul(bk[:, :], lhsT=oT[:, k, :], rhs=w_out_sb[:, k, n * 512:(n + 1) * 512],
                                                                             start=(k == 0), stop=(k == 7)), r=[k_oT, 'w_out_sb'], w=[bkey], signal=(k == 7))
                            S.op('dve', lambda e, bk=bk, n=n: e.tensor_tensor(out=f32b[:, n * 512:(n + 1) * 512], in0=bk[:, :],
                                                                             in1=modB[s][0][:, n * 512:(n + 1) * 512], op=ALU.mult),
                                 r=[bkey, ('modB', s, 0)], w=[k_f32b])
                        S.op('dve', lambda e: e.scalar_tensor_tensor(out=f32b[:, :], in0=xs_[:, :], scalar=ALPHA, in1=f32b[:, :], op0=ALU.mult, op1=ALU.add),
                             r=[xk, k_f32b], w=[k_f32b])
                        ln_stats(f32b, k_f32b)
                        ln_apply(f32c[:, :], k_f32c, f32b[:, :], k_f32b)
                        S.op('dve', lambda e: e.tensor_tensor(out=f32c[:, :], in0=f32c[:, :], in1=ln1g[:, :], op=ALU.mult), r=[k_f32c, 'ln1g'], w=[k_f32c])
                        S.op('pool', lambda e: e.tensor_tensor(out=f32c[:, :], in0=f32c[:, :], in1=ln1b[:, :], op=ALU.add), r=[k_f32c, 'ln1b'], w=[k_f32c])
                        S.dma('sp', lambda q: q.dma_start(out=x1s[ti * 128:(ti + 1) * 128, :], in_=f32c[:, :]), r=[k_f32c], w=[('x1s', ti)])
                        if stop_after == f'B{l}:proj':
                            return
                        ln_stats(f32c, k_f32c)
                        ln_apply(f32b[:, :], k_f32b, f32c[:, :], k_f32c)
                        S.op('dve', lambda e: e.tensor_tensor(out=f32b[:, :], in0=f32b[:, :], in1=modB[s][1][:, :], op=ALU.mult),
                             r=[k_f32b, ('modB', s, 1)], w=[k_f32b])
                        S.op('pool', lambda e: e.tensor_tensor(out=hr[:, 0:1024], in0=f32b[:, :], in1=modB[s][2][:, :], op=ALU.add),
                             r=[k_f32b, ('modB', s, 2)], w=[hk])
                    if part == 2:
                        transposes_to(h2T[:, :, :].rearrange("p k t -> p (k t)"), k_h2T, lambda k: hr[:, k * 128:(k + 1) * 128], 8, src_key=hk)
                        bk, bkey = nbank()
                        for k in range(8):
                            S.op('pe', lambda e, bk=bk, k=k: e.matmul(bk[:, 0:NE], lhsT=h2T[:, k, :], rhs=w_r_sb[:, k, :], start=(k == 0), stop=(k == 7)),
                                 r=[k_h2T, 'w_r_sb'], w=[bkey], signal=(k == 7))
                        S.op('act', lambda e, bk=bk: e.activation(out=ex[:, :], in_=bk[:, 0:NE], func=AF.Exp, accum_out=ssum[:, 0:1]), r=[bkey], w=[k_ex, k_ssum])
                        S.op('dve', lambda e: e.reciprocal(out=ssum[:, :], in_=ssum[:, :]), r=[k_ssum], w=[k_ssum])
                        S.op('dve', lambda e: e.tensor_scalar(out=aff_all[:, ti, :], in0=ex[:, :], scalar1=ssum[:, 0:1], scalar2=None, op0=ALU.mult),
                             r=[k_ex, k_ssum], w=[('aff', ti)])
                        S.op('dve', lambda e: e.tensor_copy(out=hr[:, 1026:1042], in_=aff_all[:, ti, :]), r=[('aff', ti)], w=[hk])
                        S.op('dve', lambda e: e.tensor_tensor(out=hr[:, 1042:1058], in0=aff_all[:, ti, :], in1=hr[:, 1026:1042], op=ALU.subtract),
                             r=[('aff', ti), hk], w=[hk])
                        S.op('dve', lambda e: e.tensor_scalar(out=hr[:, 1024:1025], in0=iotap, scalar1=0.0, scalar2=float(ti), op0=ALU.mult, op1=ALU.add),
                             r=['cst'], w=[hk])
                        S.op('dve', lambda e: e.tensor_copy(out=hr[:, 1025:1026], in_=iotap), r=['cst'], w=[hk])
                        S.dma('sp', lambda q: q.dma_start(out=h2s[ti * 128:(ti + 1) * 128, :], in_=hr[:, :]), r=[hk], w=[('h2s', ti)])

            nB = len(tiles_b)
            for step in range(nB + 2):
                chains = []
                for part in range(3):
                    if 0 <= step - part < nB:
                        bank_pool[0] = 'b%d' % part
                        S.begin_record()
                        tileB(tiles_b[step - part], part)
                        chains.append(S.end_record())
                bank_pool[0] = 'all'
                S.emit_interleaved(chains)
            S.barrier()
        if stop_after is not None and stop_after.startswith(f'B{l}'):
            return _finish(nc, S, es)

        NTB = len(tiles_b)
        t0b = tiles_b[0]
        with ExitStack() as ph:
            cmpb = sb(ph, "cmpb", [128, NT, NE], BF16)
            lo = sb(ph, "lo", [128, 32], F32)
            hi = sb(ph, "hi", [128, 32], F32)
            mid = sb(ph, "mid", [128, 32], F32)
            kvec = sb(ph, "kvec", [128, 32], F32)
            cntp = sb(ph, "cntp", [128, 32], BF16)
            ge = sb(ph, "ge", [128, 32], F32)
            gm = sb(ph, "gm", [128, 32], F32)
            posf = sb(ph, "posf", [128, NT, NE], F32)
            tot = sb(ph, "tot", [128, NT, NE], F32)
            base = sb(ph, "base", [128, NT + 1, NE], F32)
            sel = sb(ph, "sel", [128, NT, NE], F32)
            posi = sb(ph, "posi", [128, NT, NE], I32)
            affk = [('aff', t) for t in range(NT)]
            S.op('dve', lambda e: e.memset(lo[:, :], 0.0), w=['lo'])
            S.op('dve', lambda e: e.memset(hi[:, :], 1.0), w=['hi'])
            S.op('dve', lambda e: e.memset(kvec[:, 0:16], float(CAPL)), w=['kvec'])
            S.op('dve', lambda e: e.memset(kvec[:, 16:32], float(CAPC)), w=['kvec'])
            S.op('dve', lambda e: e.memset(cntp[:, :], 0.0), w=['cntp'])
            if last:
                S.op('dve', lambda e: e.memset(cmpb[:, 0:NCTX, :], 0.0), w=['cmpb'])
            lat_aff = aff_all[:, NCTX:NT, :]
            ctx_aff = aff_all[:, 0:NCTX, :]

            def compare(thr):
                S.op('dve', lambda e: e.tensor_tensor(out=cmpb[:, NCTX:NT, :], in0=lat_aff, in1=thr[:, 0:16].unsqueeze(1).to_broadcast([128, NLAT, NE]),
                                                      op=ALU.is_ge), r=affk + ['thr'], w=['cmpb'])
                if not last:
                    S.op('dve', lambda e: e.tensor_tensor(out=cmpb[:, 0:NCTX, :], in0=ctx_aff, in1=thr[:, 16:32].unsqueeze(1).to_broadcast([128, NCTX, NE]),
                                                          op=ALU.is_ge), r=affk + ['thr'], w=['cmpb'])

            for itn in range(30):
                S.op('dve', lambda e: e.tensor_tensor(out=mid[:, :], in0=lo[:, :], in1=hi[:, :], op=ALU.add), r=['lo', 'hi'], w=['thr'])
                S.op('dve', lambda e: e.tensor_scalar(out=mid[:, :], in0=mid[:, :], scalar1=0.5, scalar2=None, op0=ALU.mult), r=['thr'], w=['thr'])
                compare(mid)
                S.op('dve', lambda e: e.tensor_reduce(out=cntp[:, 0:16], in_=cmpb[:, NCTX:NT, :].rearrange("p t e -> p e t"), axis=AX.X, op=ALU.add),
                     r=['cmpb'], w=['cntp'])
                if not last:
                    S.op('dve', lambda e: e.tensor_reduce(out=cntp[:, 16:32], in_=cmpb[:, 0:NCTX, :].rearrange("p t e -> p e t"), axis=AX.X, op=ALU.add),
                         r=['cmpb'], w=['cntp'])
                bk, bkey = nbank()
                S.op('pe', lambda e, bk=bk: e.matmul(bk[:, 0:32], lhsT=onesb[:, :], rhs=cntp[:, :], start=True, stop=True), r=['onesb', 'cntp'], w=[bkey])
                S.op('dve', lambda e, bk=bk: e.tensor_tensor(out=ge[:, :], in0=bk[:, 0:32], in1=kvec[:, :], op=ALU.is_ge), r=[bkey, 'kvec'], w=['ge'])
                S.op('dve', lambda e: e.tensor_tensor(out=gm[:, :], in0=ge[:, :], in1=mid[:, :], op=ALU.mult), r=['ge', 'thr'], w=['gm'])
                S.op('dve', lambda e: e.tensor_tensor(out=lo[:, :], in0=lo[:, :], in1=gm[:, :], op=ALU.max), r=['lo', 'gm'], w=['lo'])
                S.op('dve', lambda e: e.scalar_tensor_tensor(out=gm[:, :], in0=ge[:, :], scalar=2.0, in1=mid[:, :], op0=ALU.mult, op1=ALU.add),
                     r=['ge', 'thr', 'gm'], w=['gm'])
                S.op('dve', lambda e: e.tensor_tensor(out=hi[:, :], in0=hi[:, :], in1=gm[:, :], op=ALU.min), r=['hi', 'gm'], w=['hi'])
            S.op('dve', lambda e: e.tensor_copy(out=mid[:, :], in_=lo[:, :]), r=['lo'], w=['thr'])
            compare(mid)
            cflat = cmpb[:, :, :].rearrange("p t e -> p (t e)")
            pflat = posf[:, :, :].rearrange("p t e -> p (t e)")
            tflat = tot[:, :, :].rearrange("p t e -> p (t e)")
            ncol = NT * NE
            for c0 in range(0, ncol, 512):
                cs = min(512, ncol - c0)
                bk, bkey = nbank()
                S.op('pe', lambda e, bk=bk, c0=c0, cs=cs: e.matmul(bk[:, 0:cs], lhsT=triub[:, :], rhs=cflat[:, c0:c0 + cs], start=True, stop=True),
                     r=['triub', 'cmpb'], w=[bkey])
                S.op('act', lambda e, bk=bk, c0=c0, cs=cs: e.copy(out=pflat[:, c0:c0 + cs], in_=bk[:, 0:cs]), r=[bkey], w=['posf'])
                bk, bkey = nbank()
                S.op('pe', lambda e, bk=bk, c0=c0, cs=cs: e.matmul(bk[:, 0:cs], lhsT=onesb[:, :], rhs=cflat[:, c0:c0 + cs], start=True, stop=True),
                     r=['onesb', 'cmpb'], w=[bkey])
                S.op('act', lambda e, bk=bk, c0=c0, cs=cs: e.copy(out=tflat[:, c0:c0 + cs], in_=bk[:, 0:cs]), r=[bkey], w=['tot'])
            S.op('dve', lambda e: e.memset(base[:, 0, :], float(CAPL)), w=['base'])
            S.op('dve', lambda e: e.memset(base[:, NCTX, :], 0.0), w=['base'])
            for t in range(NT):
                if t == NCTX - 1:
                    continue
                S.op('dve', lambda e, t=t: e.tensor_tensor(out=base[:, t + 1, :], in0=base[:, t, :], in1=tot[:, t, :], op=ALU.add),
                     r=['base', 'tot'], w=['base'])
            S.op('dve', lambda e: e.tensor_tensor(out=posf[:, :, :], in0=posf[:, :, :], in1=base[:, 0:NT, :], op=ALU.add), r=['posf', 'base'], w=['posf'])
            S.op('dve', lambda e: e.scalar_tensor_tensor(out=sel[:, NCTX:NT, :], in0=posf[:, NCTX:NT, :], scalar=float(CAPL), in1=cmpb[:, NCTX:NT, :],
                                                         op0=ALU.is_lt, op1=ALU.mult), r=['posf', 'cmpb'], w=['sel'])
            S.op('dve', lambda e: e.scalar_tensor_tensor(out=sel[:, 0:NCTX, :], in0=posf[:, 0:NCTX, :], scalar=float(CAPL + CAPC), in1=cmpb[:, 0:NCTX, :],
                                                         op0=ALU.is_lt, op1=ALU.mult), r=['posf', 'cmpb'], w=['sel'])
            S.op('dve', lambda e: e.scalar_tensor_tensor(out=posf[:, :, :], in0=posf[:, :, :], scalar=-BIG, in1=sel[:, :, :], op0=ALU.add, op1=ALU.mult),
                 r=['posf', 'sel'], w=['posf'])
            S.op('dve', lambda e: e.tensor_scalar(out=posi[:, :, :], in0=posf[:, :, :], scalar1=BIG, scalar2=None, op0=ALU.add), r=['posf'], w=['posi'])

            h2ld = [sb(ph, f"h2ld{i}", [128, RW], BF16) for i in range(3)]
            breg = nc.gpsimd.to_reg(CAPT - 1)
            for n, ti in enumerate(tiles_b):
                hl, hlk = h2ld[n % 3], ('h2ld', n % 3)
                S.dma('sp', lambda q, ti=ti, hl=hl: q.dma_start(out=hl[:, :], in_=h2s[ti * 128:(ti + 1) * 128, :]), r=[('h2s', ti)], w=[hlk])
                for ex_ in range(NE):
                    S.dma('pool', lambda q, ti=ti, ex_=ex_, hl=hl: q.indirect_dma_start(
                        out=Xs[ex_][:, :], out_offset=bass.IndirectOffsetOnAxis(ap=posi[:, ti, ex_:ex_ + 1], axis=0),
                        in_=hl[:, :], in_offset=None, bounds_check=breg, oob_is_err=False), r=[hlk, 'posi'], w=[('Xs', ex_)])
            S.barrier()
        if stop_after == f'R{l}':
            return _finish(nc, S, es)

        NST = (CAPT + 127) // 128
        CAPP = NST * 128
        NSL = 3 if CAPP % 3 == 0 and CAPP // 3 <= 512 else (CAPP + 511) // 512
        SLW = CAPP // NSL
        assert SLW * NSL == CAPP and SLW <= 512
        with ExitStack() as ph:
            wg = [sb(ph, f"wg{i}", [128, 8, D], BF16) for i in range(2)]
            wu = [sb(ph, f"wu{i}", [128, 8, D], BF16) for i in range(2)]
            wd = [sb(ph, f"wd{i}", [128, 8, D], BF16) for i in range(2)]
            xsb = sb(ph, "xsb", [128, NST, RW], BF16)
            XT = sb(ph, "XT", [128, 8, NST * 128], BF16)
            hidT = sb(ph, "hidT", [128, 8, NST * 128], BF16)
            sgt = [sb(ph, f"sgt{i}", [128, 512], F32) for i in range(2)]
            ysb = [sb(ph, f"ysb{i}", [128, D], F32) for i in range(3)]
            gcol = sb(ph, "gcol", [128, NST], F32)
            idxi = sb(ph, "idxi", [128, NST], I32)
            S.op('pool', lambda e: e.memset(xsb[:, NST - 1, :], 0.0), w=['xsb'])
            zt = ysb[0]
            S.op('pool', lambda e: e.memset(zt[:, :], 0.0), w=[('ysb', 0)])
            ztoks = []
            for ti in tiles_b:
                ztoks.append(S.dma('sp', lambda q, ti=ti: q.dma_start(out=macc[ti * 128:(ti + 1) * 128, :], in_=zt[:, :]),
                                   r=[('ysb', 0), ('macc_rd', ti)], w=[('macc_z', ti)]))
            prev_sc = list(ztoks)

            def load_w(ex_):
                sl = ex_ % 2
                for wsb, wdr, nm in ((wg, w_gate, 'wg'), (wu, w_up, 'wu'), (wd, w_down, 'wd')):
                    S.dma('pool', lambda q, wsb=wsb, wdr=wdr: q.dma_start(
                        out=wsb[sl][:, :, :], in_=wdr[l, ex_, :, :].rearrange("(ko ki) n -> ki ko n", ki=128)), w=[(nm, sl)])

            load_w(0)
            ycount = 0
            for ex_ in range(NE):
                sl = ex_ % 2
                if ex_ + 1 < NE:
                    load_w(ex_ + 1)
                nfull = CAPT // 128
                rem = CAPT - nfull * 128
                S.dma('sp', lambda q, ex_=ex_: q.dma_start(out=xsb[:, 0:nfull, :], in_=Xs[ex_][0:nfull * 128, :].rearrange("(s p) w -> p s w", p=128)),
                      r=[('Xs', ex_)], w=['xsb'])
                if rem:
                    S.dma('sp', lambda q, ex_=ex_: q.dma_start(out=xsb[0:rem, nfull, :], in_=Xs[ex_][nfull * 128:CAPT, :]), r=[('Xs', ex_)], w=['xsb'])
                S.op('dve', lambda e, ex_=ex_: e.tensor_tensor(out=gcol[:, 0:nfull], in0=xsb[:, 0:nfull, 1026 + ex_], in1=xsb[:, 0:nfull, 1042 + ex_], op=ALU.add),
                     r=['xsb'], w=['gcol'])
                S.op('dve', lambda e: e.scalar_tensor_tensor(out=idxi[:, 0:nfull], in0=xsb[:, 0:nfull, 1024], scalar=128.0, in1=xsb[:, 0:nfull, 1025],
                                                             op0=ALU.mult, op1=ALU.add), r=['xsb'], w=['idxi'])
                if rem:
                    S.op('dve', lambda e, ex_=ex_: e.tensor_tensor(out=gcol[0:rem, nfull:nfull + 1], in0=xsb[0:rem, nfull, 1026 + ex_:1027 + ex_],
                                                                  in1=xsb[0:rem, nfull, 1042 + ex_:1043 + ex_], op=ALU.add), r=['xsb'], w=['gcol'])
                    S.op('dve', lambda e: e.scalar_tensor_tensor(out=idxi[0:rem, nfull:nfull + 1], in0=xsb[0:rem, nfull, 1024:1025], scalar=128.0,
                                                                 in1=xsb[0:rem, nfull, 1025:1026], op0=ALU.mult, op1=ALU.add), r=['xsb'], w=['idxi'])
                for st in range(NST):
                    rows = 128 if st < nfull else rem
                    transposes_to(XT[:, :, st * 128:(st + 1) * 128], 'XT', lambda k, st=st: xsb[:, st, k * 128:(k + 1) * 128], 8,
                                  src_key='xsb', evac=('act' if st % 2 == 0 else 'dve'))
                for fc in range(8):
                    for sn in range(NSL):
                        n0 = sn * SLW
                        bg, bgk = nbank()
                        for k in range(8):
                            S.op('pe', lambda e, bg=bg, k=k, fc=fc, n0=n0: e.matmul(bg[:, 0:SLW], lhsT=wg[sl][:, k, fc * 128:(fc + 1) * 128],
                                                                                   rhs=XT[:, k, n0:n0 + SLW], start=(k == 0), stop=(k == 7)),
                                 r=[('wg', sl), 'XT'], w=[bgk], signal=(k == 7))
                        bu, buk = nbank()
                        for k in range(8):
                            S.op('pe', lambda e, bu=bu, k=k, fc=fc, n0=n0: e.matmul(bu[:, 0:SLW], lhsT=wu[sl][:, k, fc * 128:(fc + 1) * 128],
                                                                                   rhs=XT[:, k, n0:n0 + SLW], start=(k == 0), stop=(k == 7)),
                                 r=[('wu', sl), 'XT'], w=[buk], signal=(k == 7))
                        sgi = (fc * NSL + sn) % 2
                        S.op('act', lambda e, bg=bg, sgi=sgi: e.activation(out=sgt[sgi][:, 0:SLW], in_=bg[:, 0:SLW], func=AF.Silu), r=[bgk], w=[('sgt', sgi)])
                        S.op('dve', lambda e, bu=bu, sgi=sgi, fc=fc, n0=n0: e.tensor_tensor(out=hidT[:, fc, n0:n0 + SLW], in0=bu[:, 0:SLW], in1=sgt[sgi][:, 0:SLW],
                                                                                           op=ALU.mult), r=[buk, ('sgt', sgi)], w=['hidT'])
                cur_sc = []
                for st in range(NST):
                    rows = 128 if st < nfull else rem
                    yi = ycount % 3
                    ycount += 1
                    for half in range(2):
                        by, byk = nbank()
                        for fc in range(8):
                            S.op('pe', lambda e, by=by, fc=fc, st=st, rows=rows, half=half: e.matmul(
                                by[:, :], lhsT=hidT[:, fc, st * 128:(st + 1) * 128], rhs=wd[sl][:, fc, half * 512:(half + 1) * 512],
                                start=(fc == 0), stop=(fc == 7)), r=['hidT', ('wd', sl)], w=[byk], signal=(fc == 7))
                        if half == 0:
                            S.op('act', lambda e, by=by, yi=yi, st=st, rows=rows: e.activation(out=ysb[yi][0:rows, 0:512], in_=by[0:rows, :], func=AF.Copy,
                                                                                              scale=gcol[0:rows, st:st + 1]), r=[byk, 'gcol'], w=[('ysb', yi)])
                        else:
                            S.op('dve', lambda e, by=by, yi=yi, st=st, rows=rows: e.tensor_scalar(out=ysb[yi][0:rows, 512:1024], in0=by[0:rows, :],
                                                                                                 scalar1=gcol[0:rows, st:st + 1], scalar2=None, op0=ALU.mult),
                                 r=[byk, 'gcol'], w=[('ysb', yi)])
                    cur_sc.append(S.dma('pool', lambda q, yi=yi, st=st, rows=rows: q.indirect_dma_start(
                        out=macc[:, :], out_offset=bass.IndirectOffsetOnAxis(ap=idxi[0:rows, st:st + 1], axis=0),
                        in_=ysb[yi][0:rows, :], in_offset=None, compute_op=ALU.add), r=[('ysb', yi), 'idxi'], w=[('macc_sc', ex_, st)], extra=prev_sc))
                prev_sc = cur_sc
            S.barrier()
        if stop_after == f'M{l}':
            return _finish(nc, S, es)

        with ExitStack() as ph:
            g2 = [sb(ph, f"g2_{s}", [128, D], F32) for s in range(2)]
            ln2g = sb(ph, "ln2g", [128, D], F32)
            ln2b = sb(ph, "ln2b", [128, D], F32)
            KF = 3
            xa = [sb(ph, f"xa{i}", [128, D], F32) for i in range(KF)]
            xm = [sb(ph, f"xm{i}", [128, D], F32) for i in range(KF)]
            xo = [sb(ph, f"xo{i}", [128, D], F32) for i in range(KF)]
            for s in range(2):
                S.dma('sp', lambda q, s=s: q.dma_start(out=g2[s][:, :], in_=modbc[l, s, :, 5 * D:6 * D]), r=[('modbc', l, s)], w=[('g2', s)])
            S.dma('sp', lambda q: q.dma_start(out=ln2g[:, :], in_=ln2_g[l:l + 1, :].to_broadcast([128, D])), w=['ln2g'])
            S.dma('sp', lambda q: q.dma_start(out=ln2b[:, :], in_=ln2_b[l:l + 1, :].to_broadcast([128, D])), w=['ln2b'])
            out_toks = []
            chains = []
            for n, ti in enumerate(tiles_b):
                S.begin_record()
                s = 0 if ti < NCTX else 1
                a, ak = xa[n % KF], ('xa', n % KF)
                m, mk = xm[n % KF], ('xm', n % KF)
                o, ok = xo[n % KF], ('xo', n % KF)
                S.dma('sp', lambda q, ti=ti, a=a: q.dma_start(out=a[:, :], in_=x1s[ti * 128:(ti + 1) * 128, :]), r=[('x1s', ti)], w=[ak])
                S.dma('sp', lambda q, ti=ti, m=m: q.dma_start(out=m[:, :], in_=macc[ti * 128:(ti + 1) * 128, :]), w=[mk, ('macc_rd', ti)], extra=prev_sc)
                S.op('dve', lambda e, m=m, s=s: e.tensor_tensor(out=m[:, :], in0=m[:, :], in1=g2[s][:, :], op=ALU.mult), r=[mk, ('g2', s)], w=[mk])
                S.op('dve', lambda e, m=m, a=a: e.scalar_tensor_tensor(out=m[:, :], in0=a[:, :], scalar=ALPHA, in1=m[:, :], op0=ALU.mult, op1=ALU.add),
                     r=[ak, mk], w=[mk])
                ln_stats(m, mk)
                ln_apply(o[:, :], ok, m[:, :], mk)
                S.op('dve', lambda e, o=o: e.tensor_tensor(out=o[:, :], in0=o[:, :], in1=ln2g[:, :], op=ALU.mult), r=[ok, 'ln2g'], w=[ok])
                S.op('pool', lambda e, o=o: e.tensor_tensor(out=o[:, :], in0=o[:, :], in1=ln2b[:, :], op=ALU.add), r=[ok, 'ln2b'], w=[ok])
                if last:
                    i = ti - NCTX
                    (S.dma('sp', lambda q, i=i, o=o: q.dma_start(out=out[i * 128:(i + 1) * 128, :], in_=o[:, :]), r=[ok], w=[('out', i)]))
                else:
                    S.dma('sp', lambda q, ti=ti, o=o: q.dma_start(out=xs_d[ti * 128:(ti + 1) * 128, :], in_=o[:, :]), r=[ok], w=[('xs_d',)])
                chains.append(S.end_record())
            for g0 in range(0, len(chains), KF):
                S.emit_interleaved(chains[g0:g0 + KF])
            S.barrier()
        if stop_after == f'F{l}':
            return _finish(nc, S, es)
    return _finish(nc, S, es)


def _finish(nc, S, es):
    S.barrier()
    es.close()
    return nc


def _consts():
    c = np.zeros((128, 6 * 128), np.float32)
    p = np.arange(128)
    c[:, 0:128] = np.eye(128, dtype=np.float32)
    c[:, 128:256] = (p[:, None] < p[None, :]).astype(np.float32)
    c[:, 256:384] = (p[:, None] >= p[None, :]).astype(np.float32)
    c[:, 384:512] = (p[:, None] <= p[None, :]).astype(np.float32)
    c[:, 512:640] = 1.0
    c[:, 640] = p.astype(np.float32)
    c[:, 641] = (p < 64).astype(np.float32)
    c[:, 642] = (p >= 64).astype(np.float32)
    return c


def _rope_table(NLAT):
    t = np.arange(NLAT * 128, dtype=np.int32)
    row = (t // 64).astype(np.float32)[:, None]
    col = (t % 64).astype(np.float32)[:, None]
    inv = (np.float32(10000.0) ** (-np.arange(16, dtype=np.float32) / np.float32(16))).astype(np.float32)
    ar = (row * inv).astype(np.float32)
    ac = (col * inv).astype(np.float32)
    cr, sr, cc, sc = np.cos(ar), np.sin(ar), np.cos(ac), np.sin(ac)
    tab = np.concatenate([cr, cr, cc, cc, -sr, sr, -sc, sc], axis=1).astype(np.float32)
    return np.ascontiguousarray(tab.reshape(NLAT, 128, 128))


def _nat_entries(NLAT):
    ents = [(2, d) for d in range(5)] if NLAT >= 5 else [(0, 0)] * 5
    ents = [(2, 2 + d - 2) for d in range(5)] if NLAT >= 5 else ents
    ents += [(0, j) for j in range(4)] + [(1, j) for j in range(4)]
    ents += [(NLAT - 2, NLAT - 4 + j) for j in range(4)] + [(NLAT - 1, NLAT - 4 + j) for j in range(4)]
    return ents


def _nat_table(nat_bias, NLAT):
    rows = NLAT * 2
    ents = _nat_entries(NLAT)
    kk = np.arange(128)
    Lh = nat_bias.shape[0]
    out = np.empty((Lh, 128, NCHB, 4, 128), np.float32)
    for n, (i, j) in enumerate(ents):
        kr = 2 * j + kk // 64
        kc = kk % 64
        qr = 2 * i + kk // 64
        qc = kk % 64
        rs = np.clip(qr - 4, 0, rows - 8)
        cs = np.clip(qc - 8, 0, 48)
        valid = ((kr[:, None] >= rs[None, :]) & (kr[:, None] < rs[None, :] + 8) &
                 (kc[:, None] >= cs[None, :]) & (kc[:, None] < cs[None, :] + 16))
        dr = np.clip(kr[:, None] - qr[None, :] + 7, 0, 14)
        dc = np.clip(kc[:, None] - qc[None, :], -15, 15) + 15
        g = nat_bias[:, :, dr, dc]
        g = np.where(valid[None, None], g, np.float32(NEG))
        out[:, :, n, :, :] = np.transpose(g, (0, 2, 1, 3))
    return np.ascontiguousarray(out.reshape(Lh, 128, NCHB * 512))


def make_in_maps(inputs, NLAT, samples, moe=True):
    names = ['w_mod', 'b_mod', 'w_in', 'a_sink', 'conv_w', 'conv_b', 'conv_ln_g', 'conv_ln_b', 'w_out', 'ln1_g', 'ln1_b',
             'w_router', 'ln2_g', 'ln2_b'] + (['w_gate', 'w_up', 'w_down'] if moe else [])
    shared = {k: np.ascontiguousarray(np.asarray(inputs[k], np.float32)) for k in names}
    cw = shared['conv_w']
    shared['conv_w'] = np.ascontiguousarray(cw.reshape(L, 31, 2, 128).transpose(0, 3, 2, 1).reshape(L, 128, 62))
    shared['conv_b'] = np.ascontiguousarray(shared['conv_b'].reshape(L, 2, 128).transpose(0, 2, 1))
    shared['natT'] = _nat_table(np.asarray(inputs['nat_bias'], np.float32), NLAT)
    shared['rope'] = _rope_table(NLAT)
    shared['consts'] = _consts()
    maps = []
    for b in samples:
        m = dict(shared)
        m['xlat'] = np.ascontiguousarray(np.asarray(inputs['x'][b, :NLAT * 128], np.float32))
        m['xctx'] = np.ascontiguousarray(np.asarray(inputs['ctx'][b], np.float32))
        cv = np.stack([np.asarray(inputs['c_ctx'], np.float32), np.asarray(inputs['c'][b], np.float32)])
        m['cvec'] = np.ascontiguousarray(cv.reshape(2, 8, 128).transpose(2, 1, 0).reshape(128, 16))
        maps.append(m)
    return maps


def kernel(**inputs):
    NLAT = 64
    nc = build_program(NLAT)
    maps = make_in_maps(inputs, NLAT, [0, 1, 2, 3, 0, 1, 2, 3])
    res = run_bass_kernel_spmd(nc, maps, core_ids=list(range(8)))
    return np.stack([np.asarray(res.results[b]["out"], np.float32).reshape(NLAT * 128, D) for b in range(4)])
```
